# Optimizing a Trainium2 kernel written in Bass

```python
import jax
import jax.numpy as jnp
from jax import lax
import numpy as np

D_MODEL = 1024
BATCH = 4
SEQ = 8192
DEPTH = 2

GRID_W = 64
CTX_LEN = 256
HEAD_DIM = 64
A_Q_HEADS = 6
A_KV_HEADS = 2
A_GROUP = A_Q_HEADS // A_KV_HEADS
A_WINDOW = 128
A_BLOCK = 128
B_WIDTH = 256
B_CONV = 3
C_HEADS = 6
NA_ROWS = 8
NA_COLS = 16
A_WIDTH = A_Q_HEADS * HEAD_DIM
A_KV_WIDTH = A_KV_HEADS * HEAD_DIM
C_WIDTH = C_HEADS * HEAD_DIM
MIX_WIDTH = A_WIDTH + B_WIDTH + C_WIDTH
IN_SPLITS = (A_WIDTH, A_KV_WIDTH, A_KV_WIDTH, B_WIDTH, B_WIDTH, B_WIDTH, C_WIDTH, C_WIDTH, C_WIDTH)
IN_WIDTH = sum(IN_SPLITS)
N_EXPERTS = 16
EXPERT_FF = 512
CAPACITY_FACTOR = 2
ROPE_THETA = 10000.0
ROPE_AXIS_DIM = HEAD_DIM // 2
LN_EPS = 1e-6
N_MOD = 6
DEEPNORM_ALPHA = (2 * DEPTH) ** 0.25
DEEPNORM_BETA = (8 * DEPTH) ** -0.25
NEG_INF = -1e30

kernel_name = "hybrid_parallel_group_dit_block"


def _split_last(a, sizes):
    out, start = [], 0
    for n in sizes:
        out.append(a[..., start:start + n])
        start += n
    return out


def _heads(a, n_heads):
    return a.reshape(a.shape[0], a.shape[1], n_heads, HEAD_DIM)


def _ln_stats(x):
    xf = x.astype(jnp.float32)
    mu = jnp.mean(xf, axis=-1, keepdims=True)
    var = jnp.mean(jnp.square(xf - mu), axis=-1, keepdims=True)
    return (xf - mu) * lax.rsqrt(var + LN_EPS)


def ln_plain(x):
    return _ln_stats(x).astype(x.dtype)


def ln_affine(x, g, b):
    return (_ln_stats(x) * g.astype(jnp.float32) + b.astype(jnp.float32)).astype(x.dtype)


def modulate(h, shift, scale):
    return h * (1 + scale) + shift


def joint_softmax(parts):
    probs = jax.nn.softmax(jnp.concatenate([p.astype(jnp.float32) for p in parts], axis=-1), axis=-1)
    return _split_last(probs, [p.shape[-1] for p in parts])


def axial_rope(x, row_pos, col_pos):
    inv = ROPE_THETA ** (-jnp.arange(0, ROPE_AXIS_DIM, 2, dtype=jnp.float32) / ROPE_AXIS_DIM)

    def rot(xa, pos):
        ang = pos.astype(jnp.float32)[:, None] * inv[None, :]
        cos = jnp.cos(ang)[None, :, None, :].astype(x.dtype)
        sin = jnp.sin(ang)[None, :, None, :].astype(x.dtype)
        x1, x2 = jnp.split(xa, 2, axis=-1)
        return jnp.concatenate([x1 * cos - x2 * sin, x2 * cos + x1 * sin], axis=-1)

    return jnp.concatenate([rot(x[..., :ROPE_AXIS_DIM], row_pos), rot(x[..., ROPE_AXIS_DIM:], col_pos)], axis=-1)


def windowed_gqa_latent(q, k, v, k_ctx, v_ctx, sink):
    bsz, seq = q.shape[:2]
    nb = seq // A_BLOCK
    scale = HEAD_DIM ** -0.5
    qb = q.reshape(bsz, nb, A_BLOCK, A_KV_HEADS, A_GROUP, HEAD_DIM)
    pad = ((0, 0), (A_BLOCK, A_BLOCK), (0, 0), (0, 0))
    kp = jnp.pad(k, pad).reshape(bsz, nb + 2, A_BLOCK, A_KV_HEADS, HEAD_DIM)
    vp = jnp.pad(v, pad).reshape(bsz, nb + 2, A_BLOCK, A_KV_HEADS, HEAD_DIM)
    kw = jnp.concatenate([kp[:, :-2], kp[:, 1:-1], kp[:, 2:]], axis=2)
    vw = jnp.concatenate([vp[:, :-2], vp[:, 1:-1], vp[:, 2:]], axis=2)
    s_win = jnp.einsum('bnqkgd,bnjkd->bnkgqj', qb, kw).astype(jnp.float32) * scale
    blk = jnp.arange(nb)[:, None] * A_BLOCK
    qpos = blk + jnp.arange(A_BLOCK)[None, :]
    kpos = blk - A_BLOCK + jnp.arange(3 * A_BLOCK)[None, :]
    valid = ((jnp.abs(qpos[:, :, None] - kpos[:, None, :]) <= A_WINDOW)
             & (kpos[:, None, :] >= 0) & (kpos[:, None, :] < seq))
    s_win = jnp.where(valid[None, :, None, None], s_win, NEG_INF)
    s_ctx = jnp.einsum('bnqkgd,bjkd->bnkgqj', qb, k_ctx).astype(jnp.float32) * scale
    s_sink = jnp.broadcast_to(sink.reshape(A_KV_HEADS, A_GROUP)[None, None, :, :, None, None].astype(jnp.float32),
                              s_win.shape[:-1] + (1,))
    p_win, p_ctx, _ = joint_softmax([s_win, s_ctx, s_sink])
    o = (jnp.einsum('bnkgqj,bnjkd->bnqkgd', p_win.astype(v.dtype), vw)
         + jnp.einsum('bnkgqj,bjkd->bnqkgd', p_ctx.astype(v.dtype), v_ctx))
    return o.reshape(bsz, seq, A_WIDTH)


def context_gqa(q, k, v, sink):
    bsz, ln = q.shape[:2]
    qg = q.reshape(bsz, ln, A_KV_HEADS, A_GROUP, HEAD_DIM)
    s = jnp.einsum('blkgd,bjkd->bkglj', qg, k).astype(jnp.float32) * HEAD_DIM ** -0.5
    s_sink = jnp.broadcast_to(sink.reshape(A_KV_HEADS, A_GROUP)[None, :, :, None, None].astype(jnp.float32),
                              s.shape[:-1] + (1,))
    p, _ = joint_softmax([s, s_sink])
    o = jnp.einsum('bkglj,bjkd->blkgd', p.astype(v.dtype), v)
    return o.reshape(bsz, ln, A_WIDTH)


def gated_short_conv(xin, gate_b, gate_c, w):
    u = gate_c * xin
    tlen = u.shape[1]
    up = jnp.pad(u, ((0, 0), (1, 1), (0, 0)))
    y = up[:, :tlen] * w[0] + up[:, 1:tlen + 1] * w[1] + up[:, 2:] * w[2]
    return gate_b * y


def neighbourhood_attention_latent(q, k, v, k_ctx, v_ctx, rpb):
    bsz, seq = q.shape[:2]
    rows = seq // GRID_W
    kh = min(NA_ROWS, rows)
    scale = HEAD_DIM ** -0.5
    qg = q.reshape(bsz, rows, GRID_W, C_HEADS, HEAD_DIM)
    kg = k.reshape(bsz, rows, GRID_W, C_HEADS, HEAD_DIM)
    vg = v.reshape(bsz, rows, GRID_W, C_HEADS, HEAD_DIM)
    r = jnp.arange(rows)
    row_start = jnp.clip(r - kh // 2, 0, rows - kh)
    key_rows = row_start[:, None] + jnp.arange(kh)[None, :]
    kn = kg[:, key_rows]
    vn = vg[:, key_rows]
    s_nb = jnp.einsum('brqhd,brachd->brhqac', qg, kn).astype(jnp.float32) * scale
    cq = jnp.arange(GRID_W)
    col_start = jnp.clip(cq - NA_COLS // 2, 0, GRID_W - NA_COLS)
    col_ok = (cq[None, :] >= col_start[:, None]) & (cq[None, :] < col_start[:, None] + NA_COLS)
    roff = key_rows - r[:, None] + (NA_ROWS - 1)
    coff = jnp.clip(cq[None, :] - cq[:, None], -(NA_COLS - 1), NA_COLS - 1) + (NA_COLS - 1)
    bias = rpb[:, roff[:, None, :, None], coff[None, :, None, :]]
    s_nb = s_nb + jnp.transpose(bias, (1, 0, 2, 3, 4))[None].astype(jnp.float32)
    s_nb = jnp.where(col_ok[None, None, None, :, None, :], s_nb, NEG_INF)
    s_nb = s_nb.reshape(bsz, rows, C_HEADS, GRID_W, kh * GRID_W)
    s_ctx = jnp.einsum('brqhd,bjhd->brhqj', qg, k_ctx).astype(jnp.float32) * scale
    p_nb, p_ctx = joint_softmax([s_nb, s_ctx])
    p_nb = p_nb.reshape(bsz, rows, C_HEADS, GRID_W, kh, GRID_W).astype(v.dtype)
    o = (jnp.einsum('brhqac,brachd->brqhd', p_nb, vn)
         + jnp.einsum('brhqj,bjhd->brqhd', p_ctx.astype(v.dtype), v_ctx))
    return o.reshape(bsz, seq, C_WIDTH)


def context_mha(q, k, v):
    bsz, ln = q.shape[:2]
    s = jnp.einsum('blhd,bjhd->bhlj', q, k).astype(jnp.float32) * HEAD_DIM ** -0.5
    p = jax.nn.softmax(s, axis=-1).astype(v.dtype)
    return jnp.einsum('bhlj,bjhd->blhd', p, v).reshape(bsz, ln, C_WIDTH)


def expert_choice_ffn(h, w_router, w_gate, w_up, w_down):
    bsz, tlen, dm = h.shape
    cap = CAPACITY_FACTOR * tlen // N_EXPERTS
    aff = jax.nn.softmax(jnp.einsum('btd,de->bte', h, w_router).astype(jnp.float32), axis=-1)
    gate, idx = lax.top_k(jnp.transpose(aff, (0, 2, 1)), cap)
    xe = jax.vmap(lambda hb, ib: hb[ib])(h, idx)
    a = jnp.einsum('becd,edf->becf', xe, w_gate)
    u = jnp.einsum('becd,edf->becf', xe, w_up)
    y = jnp.einsum('becf,efd->becd', jax.nn.silu(a) * u, w_down) * gate[..., None].astype(h.dtype)
    return jax.vmap(lambda yb, ib: jnp.zeros((tlen, dm), yb.dtype).at[ib.reshape(-1)].add(yb.reshape(-1, dm)))(y, idx)


def setup_inputs(seed: int = 0) -> dict:
    key = jax.random.key(seed)
    ks = jax.random.split(key, 20)
    f32 = jnp.float32
    nrm = lambda k, shape, s: jax.random.normal(k, shape, f32) * s
    return {
        "x": nrm(ks[0], (BATCH, SEQ, D_MODEL), 1.0),
        "c": nrm(ks[1], (BATCH, D_MODEL), 1.0),
        "ctx": nrm(ks[2], (BATCH, CTX_LEN, D_MODEL), 1.0),
        "c_ctx": nrm(ks[3], (D_MODEL,), 1.0),
        "w_mod": nrm(ks[4], (DEPTH, D_MODEL, N_MOD * D_MODEL), D_MODEL ** -0.5),
        "b_mod": nrm(ks[5], (DEPTH, N_MOD * D_MODEL), 0.02),
        "w_in": nrm(ks[6], (DEPTH, D_MODEL, IN_WIDTH), D_MODEL ** -0.5),
        "conv_w": nrm(ks[7], (DEPTH, B_CONV, B_WIDTH), B_CONV ** -0.5),
        "attn_sink": nrm(ks[8], (DEPTH, A_Q_HEADS), 0.5),
        "na_rpb": nrm(ks[9], (DEPTH, C_HEADS, 2 * NA_ROWS - 1, 2 * NA_COLS - 1), 0.1),
        "w_out": nrm(ks[10], (DEPTH, MIX_WIDTH, D_MODEL), MIX_WIDTH ** -0.5 * DEEPNORM_BETA),
        "ln1_g": 1.0 + nrm(ks[11], (DEPTH, D_MODEL), 0.02),
        "ln1_b": nrm(ks[12], (DEPTH, D_MODEL), 0.02),
        "w_router": nrm(ks[13], (DEPTH, D_MODEL, N_EXPERTS), D_MODEL ** -0.5),
        "w_gate": nrm(ks[14], (DEPTH, N_EXPERTS, D_MODEL, EXPERT_FF), D_MODEL ** -0.5),
        "w_up": nrm(ks[15], (DEPTH, N_EXPERTS, D_MODEL, EXPERT_FF), D_MODEL ** -0.5),
        "w_down": nrm(ks[16], (DEPTH, N_EXPERTS, EXPERT_FF, D_MODEL), EXPERT_FF ** -0.5 * DEEPNORM_BETA),
        "ln2_g": 1.0 + nrm(ks[17], (DEPTH, D_MODEL), 0.02),
        "ln2_b": nrm(ks[18], (DEPTH, D_MODEL), 0.02),
    }


def reference(x, c, ctx, c_ctx, w_mod, b_mod, w_in, conv_w, attn_sink, na_rpb, w_out,
              ln1_g, ln1_b, w_router, w_gate, w_up, w_down, ln2_g, ln2_b):
    seq = x.shape[1]
    t = jnp.arange(seq)
    row_pos = t // GRID_W
    col_pos = t % GRID_W
    for l in range(DEPTH):
        last = l == DEPTH - 1
        mod = jax.nn.silu(c) @ w_mod[l] + b_mod[l]
        mod_c = jax.nn.silu(c_ctx) @ w_mod[l] + b_mod[l]
        sh1, sc1, g1, sh2, sc2, g2 = jnp.split(mod[:, None, :], N_MOD, axis=-1)
        csh1, csc1, cg1, csh2, csc2, cg2 = jnp.split(mod_c, N_MOD, axis=-1)

        h = modulate(ln_plain(x), sh1, sc1)
        hc = modulate(ln_plain(ctx), csh1, csc1)
        qa, ka, va, bx, bb, bc, qn, kn, vn = _split_last(h @ w_in[l], IN_SPLITS)
        qa_c, ka_c, va_c, bx_c, bb_c, bc_c, qn_c, kn_c, vn_c = _split_last(hc @ w_in[l], IN_SPLITS)
        ka_c = _heads(ka_c, A_KV_HEADS)
        va_c = _heads(va_c, A_KV_HEADS)
        kn_c = _heads(kn_c, C_HEADS)
        vn_c = _heads(vn_c, C_HEADS)

        o_a = windowed_gqa_latent(axial_rope(_heads(qa, A_Q_HEADS), row_pos, col_pos),
                                  axial_rope(_heads(ka, A_KV_HEADS), row_pos, col_pos),
                                  _heads(va, A_KV_HEADS), ka_c, va_c, attn_sink[l])
        o_b = gated_short_conv(bx, bb, bc, conv_w[l])
        o_c = neighbourhood_attention_latent(_heads(qn, C_HEADS), _heads(kn, C_HEADS), _heads(vn, C_HEADS),
                                             kn_c, vn_c, na_rpb[l])
        mix = jnp.concatenate([o_a, o_b, o_c], axis=-1) @ w_out[l]
        x_mid = ln_affine(DEEPNORM_ALPHA * x + g1 * mix, ln1_g[l], ln1_b[l])

        if not last:
            o_a_c = context_gqa(_heads(qa_c, A_Q_HEADS), ka_c, va_c, attn_sink[l])
            o_b_c = gated_short_conv(bx_c, bb_c, bc_c, conv_w[l])
            o_c_c = context_mha(_heads(qn_c, C_HEADS), kn_c, vn_c)
            mix_c = jnp.concatenate([o_a_c, o_b_c, o_c_c], axis=-1) @ w_out[l]
            ctx_mid = ln_affine(DEEPNORM_ALPHA * ctx + cg1 * mix_c, ln1_g[l], ln1_b[l])
            h2c = modulate(ln_plain(ctx_mid), csh2, csc2)
            ffn_c = expert_choice_ffn(h2c, w_router[l], w_gate[l], w_up[l], w_down[l])
            ctx = ln_affine(DEEPNORM_ALPHA * ctx_mid + cg2 * ffn_c, ln2_g[l], ln2_b[l])

        h2 = modulate(ln_plain(x_mid), sh2, sc2)
        ffn = expert_choice_ffn(h2, w_router[l], w_gate[l], w_up[l], w_down[l])
        x = ln_affine(DEEPNORM_ALPHA * x_mid + g2 * ffn, ln2_g[l], ln2_b[l])
    return x
```

```python
import numpy as np
import concourse.bass as bass
import concourse.mybir as mybir
from concourse.bass_utils import run_bass_kernel_spmd

F32 = mybir.dt.float32
BF16 = mybir.dt.bfloat16
I32 = mybir.dt.int32
AF = mybir.ActivationFunctionType
ALU = mybir.AluOpType
AX = mybir.AxisListType

D = 1024
NT = 66
TOK = NT * 128
DEPTH = 2
ALPHA = float((2 * DEPTH) ** 0.25)
NE = 16
RW = 1024 + 2 + 32
COMPUTE = ("pe", "dve", "act", "pool")
N_CORES = 4


class Res:
    __slots__ = ("name", "w", "r", "dsem", "wg")

    def __init__(self, name="r"):
        self.name = name
        self.w = None
        self.r = []
        self.dsem = {}
        self.wg = None


class Sched:
    def __init__(self, nc):
        self.nc = nc
        self.ins = {e: [] for e in ("pe", "dve", "act", "pool", "sp")}
        self.dram = {}
        self.ndsem = 0
        self.owners = []
        self.free = {"sp": [], "pool": []}
        self.phase = 0
        self.phase_ev = {0: []}
        self.last_dma = {}
        self.pool_dmas = []
        self.last_pew = {}

    def dres(self, *key):
        r = self.dram.get(key)
        if r is None:
            r = self.dram[key] = Res(str(key))
        return r

    def _deps(self, eng, reads, writes, pe_acc, group=None):
        deps = []
        for r in reads:
            if r.w is not None:
                if isinstance(r.w, list):
                    deps.extend(r.w)
                else:
                    deps.append(r.w)
        for r in writes:
            if r.w is not None:
                if isinstance(r.w, list):
                    if not (group is not None and r.wg == group):
                        deps.extend(r.w)
                elif not (pe_acc and r.w[0] == "E" and r.w[1] == "pe" and eng == "pe"):
                    deps.append(r.w)
            deps.extend(r.r)
        return deps

    def _post(self, ev, reads, writes, group=None):
        for r in reads:
            r.r.append(ev)
        for r in writes:
            if group is not None and r.wg == group and isinstance(r.w, list):
                r.w.append(ev)
            else:
                r.w = [ev] if group is not None else ev
                r.wg = group
                r.r = []

    def op(self, eng, fn, reads=(), writes=(), pe_acc=False, cost=0.5):
        deps = self._deps(eng, reads, writes, pe_acc)
        idx = len(self.ins[eng])
        order = []
        if eng == "pe":
            for r in writes:
                p = self.last_pew.get(id(r))
                if p is not None:
                    order.append(p)
                self.last_pew[id(r)] = idx
        ev = ("E", eng, idx)
        self.ins[eng].append([fn, deps, None, self.phase, cost, order, cost])
        self._post(ev, reads, writes)
        return ev

    def dma(self, q, fn, reads=(), writes=(), owner=None, group=None, cost=0.1, lat=3.0):
        deps = self._deps(q, reads, writes, False, group)
        sc = owner.dsem.get(q)
        if sc is None:
            if self.free[q]:
                sc = list(self.free[q].pop())
            else:
                sc = [self.ndsem, 0]
                self.ndsem += 1
            owner.dsem[q] = sc
            self.owners.append((owner, q))
        sc[1] += 16
        ev = ("D", sc[0], sc[1])
        if q == "pool":
            if len(self.pool_dmas) >= 24:
                deps.append(self.pool_dmas[-24])
            self.pool_dmas.append(ev)
        idx = len(self.ins[q])
        order = []
        p = self.last_dma.get(sc[0])
        if p is not None:
            order.append(p)
        self.last_dma[sc[0]] = idx
        self.ins[q].append([fn, deps, sc[0], self.phase, cost, order, cost + lat, ev])
        self._post(ev, reads, writes, group)
        return ev

    def barrier(self):
        evs = []
        for e in COMPUTE:
            for i in range(len(self.ins[e]) - 1, -1, -1):
                if self.ins[e][i][2] is None:
                    evs.append(("E", e, i))
                    break
        for (o, q) in self.owners:
            sc = o.dsem.pop(q)
            evs.append(("D", sc[0], sc[1]))
            self.free[q].append((sc[0], sc[1]))
        self.owners = []
        self.phase += 1
        self.phase_ev[self.phase] = evs

    def _schedule(self, only=None):
        import heapq
        engs = list(self.ins.keys())
        dprod = {}
        for q in ("sp", "pool"):
            for i, rec in enumerate(self.ins[q]):
                if rec[2] is not None:
                    dprod[(rec[7][1], rec[7][2])] = (q, i)
        fin = {}
        issue = {}
        order = {e: [] for e in engs}
        ptr0 = {e: 0 for e in engs}
        tnow = 0.0
        for ph in range(self.phase + 1):
            nodes = []
            for e in engs:
                lst = self.ins[e]
                i = ptr0[e]
                while i < len(lst) and lst[i][3] == ph:
                    nodes.append((e, i))
                    i += 1
                ptr0[e] = i
            if not nodes:
                continue
            if only is not None and ph not in only:
                for (e, i) in nodes:
                    order[e].append(i)
                continue
            inph = set(nodes)
            ndep = {}
            users = {}
            for (e, i) in nodes:
                rec = self.ins[e][i]
                preds = set()
                for d in rec[1]:
                    p = (d[1], d[2]) if d[0] == "E" else dprod.get((d[1], d[2]))
                    if p is not None and p in inph and p != (e, i):
                        preds.add((p, 0))
                for j in rec[5]:
                    if (e, j) in inph:
                        preds.add(((e, j), 1))
                ndep[(e, i)] = len(preds)
                for pk in preds:
                    users.setdefault(pk[0], []).append(((e, i), pk[1]))
            ready = {}
            heaps = {e: [] for e in engs}
            avail = {e: [] for e in engs}
            efree = {e: tnow for e in engs}
            for n in nodes:
                ready[n] = tnow
                if ndep[n] == 0:
                    heapq.heappush(heaps[n[0]], (tnow, n[1]))
            left = len(nodes)
            tmax = tnow
            while left:
                best = None
                for e in engs:
                    if avail[e]:
                        st_ = efree[e]
                    elif heaps[e]:
                        st_ = max(heaps[e][0][0], efree[e])
                    else:
                        continue
                    if best is None or st_ < best[0]:
                        best = (st_, e)
                st_, e = best
                while heaps[e] and heaps[e][0][0] <= st_:
                    heapq.heappush(avail[e], heapq.heappop(heaps[e])[1])
                i = heapq.heappop(avail[e])
                rec = self.ins[e][i]
                issue[(e, i)] = st_
                efree[e] = st_ + rec[4]
                f_ = st_ + rec[6]
                fin[(e, i)] = f_
                tmax = max(tmax, f_)
                order[e].append(i)
                left -= 1
                for (u, kind) in users.get((e, i), ()):
                    t_ = f_ if kind == 0 else st_
                    if t_ > ready[u]:
                        ready[u] = t_
                    ndep[u] -= 1
                    if ndep[u] == 0:
                        heapq.heappush(heaps[u[0]], (ready[u], u[1]))
            tnow = tmax
        self.sim_time = tnow
        return order

    def _check(self, order, val):
        sems = {}
        pos = {e: 0 for e in self.ins}
        curph = {e: 0 for e in self.ins}
        progress = True
        total = sum(len(v) for v in self.ins.values())
        done = 0
        while progress:
            progress = False
            for e in self.ins:
                while pos[e] < len(order[e]):
                    i = order[e][pos[e]]
                    rec = self.ins[e][i]
                    deps = list(rec[1])
                    if rec[3] != curph[e]:
                        for p in range(curph[e] + 1, rec[3] + 1):
                            deps.extend(self.phase_ev.get(p, ()))
                    ok = True
                    for d in deps:
                        if d[0] == "E":
                            if sems.get(("E", d[1]), 0) < val[d[1]][d[2]]:
                                ok = False
                                break
                        elif sems.get(("D", d[1]), 0) < d[2]:
                            ok = False
                            break
                    if not ok:
                        break
                    curph[e] = rec[3]
                    if rec[2] is not None:
                        sems[("D", rec[2])] = sems.get(("D", rec[2]), 0) + 16
                    elif i in val.get(e, {}):
                        sems[("E", e)] = val[e][i]
                    pos[e] += 1
                    done += 1
                    progress = True
        if done != total:
            msg = []
            for e in self.ins:
                if pos[e] < len(order[e]):
                    i = order[e][pos[e]]
                    msg.append((e, pos[e], i, self.ins[e][i][3], self.ins[e][i][1][:6]))
            raise RuntimeError("schedule deadlock: %s" % msg)

    def emit(self, final_waits=(), reorder=True, only=None):
        import contextlib
        nc = self.nc
        if reorder:
            order = self._schedule(only)
        else:
            order = {e: list(range(len(l))) for e, l in self.ins.items()}
        for e in self.ins:
            assert sorted(order[e]) == list(range(len(self.ins[e]))), e
        lastc = {}
        for e in COMPUTE:
            cur = None
            per = {}
            for i in order[e]:
                if self.ins[e][i][2] is None:
                    per[self.ins[e][i][3]] = i
            lastc[e] = per
        for p in list(self.phase_ev.keys()):
            evs = [d for d in self.phase_ev[p] if d[0] == "D"]
            for e in COMPUTE:
                qs = [q for q in lastc[e] if q < p]
                if qs:
                    evs.append(("E", e, lastc[e][max(qs)]))
            self.phase_ev[p] = evs
        need = {e: set() for e in COMPUTE}
        for e, lst in self.ins.items():
            for rec in lst:
                for d in rec[1]:
                    if d[0] == "E":
                        need[d[1]].add(d[2])
        for evs in self.phase_ev.values():
            for d in evs:
                if d[0] == "E":
                    need[d[1]].add(d[2])
        val = {}
        for e in COMPUTE:
            c = 0
            v = {}
            for i in order[e]:
                if i in need[e]:
                    c += 1
                    v[i] = c
            val[e] = v
        self._check(order, val)
        self.nwaits = {}
        with contextlib.ExitStack() as st:
            esem = {e: st.enter_context(nc.semaphore("s_" + e)) for e in COMPUTE}
            dsem = [st.enter_context(nc.semaphore("d%d" % i)) for i in range(self.ndsem)]
            block = st.enter_context(nc.Block())

            def run(ename, eng):
                waited = {}
                lst = self.ins[ename]
                cur_ph = 0
                for i in order[ename]:
                    rec = lst[i]
                    deps = rec[1]
                    if rec[3] != cur_ph:
                        deps = list(deps)
                        for p in range(cur_ph + 1, rec[3] + 1):
                            deps.extend(self.phase_ev.get(p, ()))
                        cur_ph = rec[3]
                    tg = {}
                    for d in deps:
                        if d[0] == "E":
                            key = ("E", d[1]); v = val[d[1]][d[2]]; sem = esem[d[1]]
                        else:
                            key = ("D", d[1]); v = d[2]; sem = dsem[d[1]]
                        if tg.get(key, (None, 0))[1] < v:
                            tg[key] = (sem, v)
                    for key, (sem, v) in tg.items():
                        if waited.get(key, 0) >= v:
                            continue
                        eng.wait_ge(sem, v)
                        waited[key] = v
                        self.nwaits[ename] = self.nwaits.get(ename, 0) + 1
                    ins = rec[0](eng)
                    if rec[2] is not None:
                        ins.then_inc(dsem[rec[2]], 16)
                    elif i in need[ename]:
                        ins.then_inc(esem[ename], 1)
                if ename == "sp":
                    for d in final_waits:
                        eng.wait_ge(dsem[d[1]], d[2])

            block.tensor(lambda e: run("pe", e))
            block.vector(lambda e: run("dve", e))
            block.scalar(lambda e: run("act", e))
            block.gpsimd(lambda e: run("pool", e))
            block.sync(lambda e: run("sp", e))


def interleave(gens):
    gens = [g for g in gens if g is not None]
    while gens:
        nxt = []
        for g in gens:
            try:
                next(g)
                nxt.append(g)
            except StopIteration:
                pass
            yield
        gens = nxt


class Tl:
    __slots__ = ("t", "r")

    def __init__(self, t, name):
        self.t = t
        self.r = Res(name)

    def __getitem__(self, k):
        return self.t[k]


def build_nc(dbg=False, depth_run=DEPTH, reorder=True, only=None):
    nc = bass.Bass("TRN2", target_bir_lowering=False)
    S = Sched(nc)

    def din(name, shape, dt=F32):
        return nc.dram_tensor(name, list(shape), dt, kind="ExternalInput").ap()

    def dscr(name, shape, dt):
        return nc.dram_tensor(name, list(shape), dt, kind="ExternalOutput" if dbg else "Internal").ap()

    XIN = din("xin", [TOK, D])
    CVEC = din("cvec", [2, D])
    WMOD = din("w_mod", [DEPTH, D, 6 * D])
    BMOD = din("b_mod", [DEPTH, 6 * D])
    WIN = din("w_in", [DEPTH, D, 3072])
    ROPE = din("rope", [4, 128, TOK])
    CONVW = din("convw", [DEPTH, 128, 2, 3])
    SINK = din("sink", [DEPTH, 6])
    NAB = din("nab", [DEPTH, 5, 128, 6, 640])
    AMASK = din("amask", [128, 2, 384])
    WOUT = din("w_out", [DEPTH, D, D])
    LN1G = din("ln1_g", [DEPTH, D]); LN1B = din("ln1_b", [DEPTH, D])
    LN2G = din("ln2_g", [DEPTH, D]); LN2B = din("ln2_b", [DEPTH, D])
    WR = din("w_router", [DEPTH, D, NE])
    WG = din("w_gate", [DEPTH, NE, D, 512]); WU = din("w_up", [DEPTH, NE, D, 512])
    WD = din("w_down", [DEPTH, NE, 512, D])
    Y = nc.dram_tensor("y", [8192, D], F32, kind="ExternalOutput").ap()

    MODROW = dscr("modrow", [2, 6 * D], F32)
    FM = dscr("fm", [128, 14, TOK], BF16)
    VV = dscr("vv", [TOK, 8, 65], BF16)
    XMID = dscr("xmid", [TOK, D], F32)
    H2T = dscr("h2t", [128, 8, TOK], BF16)
    XCUR = dscr("xcur", [TOK, D], F32)
    XH2 = dscr("xh2", [TOK, RW], BF16)
    XE = [dscr("xe%d" % e, [1024, RW], BF16) for e in range(NE)]
    FFN = dscr("ffn", [TOK, D], F32)

    SB_LO = 16512
    SB_HI = 229344
    st = {"pers": SB_LO, "ph": None}

    def _alloc(name, shape, dt, key):
        nb = int(np.prod(shape[1:])) * (2 if dt == BF16 else 4)
        nb = (nb + 31) // 32 * 32
        off = st[key]
        assert off + nb <= SB_HI, (name, off, nb)
        st[key] = off + nb
        return Tl(nc.alloc_sbuf_tensor_at(name, list(shape), dt, offset=off), name)

    def pers(name, shape, dt=F32):
        return _alloc(name, shape, dt, "pers")

    cnt = [0]

    def ph(name, shape, dt=F32):
        cnt[0] += 1
        return _alloc("%s_%d" % (name, cnt[0]), shape, dt, "ph")

    def new_phase():
        S.barrier()
        st["ph"] = st["pers_end"]

    pbig = Tl(nc.alloc_psum_tensor("pbig", [128, 6, 512], F32), "pbig")
    ptr = [Tl(nc.alloc_psum_tensor("ptr%d" % i, [128, 8, 128], BF16), "ptr%d" % i) for i in range(2)]
    pbr = [Res("pb%d" % i) for i in range(6)]

    ident = pers("ident", [128, 128], BF16)
    identf = pers("identf", [128, 128], F32)
    onesf = pers("onesf", [128, 128], F32)
    aff = pers("aff", [128, NT, NE], F32)
    wsel = pers("wsel", [128, NT, NE], F32)
    modT = pers("modT", [128, 48], F32)
    modcT = pers("modcT", [128, 48], F32)
    esink = pers("esink", [128, 6], F32)
    convw = pers("convw", [128, 2, 3], F32)
    amask = pers("amask", [128, 2, 384], BF16)
    eps_t = pers("eps", [128, 1], F32)
    ltri = pers("ltri", [128, 128], F32)
    idxT = pers("idxT", [128, NE, 64], I32)
    st["pers_end"] = st["pers"]
    st["ph"] = st["pers_end"]

    _breg = {}

    def breg(e):
        if "r" not in _breg:
            _breg["r"] = e.to_reg(1023)
        return _breg["r"]

    def rs(lst):
        return [x.r if isinstance(x, Tl) else x for x in lst]

    def fsz(ap):
        try:
            return float(ap.free_size())
        except Exception:
            return 512.0

    def mm(out, lhsT, rhs, start, stop, reads, writes, tr=False):
        if tr:
            S.op("pe", lambda e: e.matmul(out, lhsT=lhsT, rhs=rhs, is_transpose=True),
                 reads=rs(reads), writes=rs(writes), pe_acc=True, cost=0.08)
        else:
            c = max(fsz(rhs), 64.0) / 2000.0 * (4.0 if lhsT.dtype == F32 else 1.0) + 0.03
            S.op("pe", lambda e: e.matmul(out, lhsT=lhsT, rhs=rhs, start=start, stop=stop),
                 reads=rs(reads), writes=rs(writes), pe_acc=True, cost=c)

    def act(out, in_, func, reads, writes, bias=0.0, scale=1.0, accum=None):
        if accum is None:
            S.op("act", lambda e: e.activation(out=out, in_=in_, func=func, bias=bias, scale=scale),
                 reads=rs(reads), writes=rs(writes), cost=0.2 + fsz(out) / 1300.0)
        else:
            S.op("act", lambda e: e.activation(out=out, in_=in_, func=func, bias=bias, scale=scale,
                                               accum_out=accum), reads=rs(reads), writes=rs(writes))

    def vcost(eng, ap):
        return (0.1 + fsz(ap) / 900.0) if eng == "dve" else (0.2 + fsz(ap) / 450.0)

    def tt(out, in0, in1, op, reads, writes, eng="dve"):
        S.op(eng, lambda e: e.tensor_tensor(out=out, in0=in0, in1=in1, op=op), reads=rs(reads), writes=rs(writes),
             cost=vcost(eng, out))

    def ts(out, in0, s1, s2, op0, op1, reads, writes, eng="dve"):
        if op1 is None:
            S.op(eng, lambda e: e.tensor_scalar(out=out, in0=in0, scalar1=s1, scalar2=None, op0=op0),
                 reads=rs(reads), writes=rs(writes), cost=vcost(eng, out))
        else:
            S.op(eng, lambda e: e.tensor_scalar(out=out, in0=in0, scalar1=s1, scalar2=s2, op0=op0, op1=op1),
                 reads=rs(reads), writes=rs(writes), cost=vcost(eng, out))

    def stt(out, in0, scalar, in1, op0, op1, reads, writes):
        S.op("dve", lambda e: e.scalar_tensor_tensor(out=out, in0=in0, scalar=scalar, in1=in1, op0=op0, op1=op1),
             reads=rs(reads), writes=rs(writes), cost=vcost("dve", out))

    def cp(out, in_, reads, writes, eng="dve"):
        S.op(eng, lambda e: e.tensor_copy(out=out, in_=in_), reads=rs(reads), writes=rs(writes), cost=vcost(eng, out))

    def recip(out, in_, reads, writes):
        S.op("dve", lambda e: e.reciprocal(out=out, in_=in_), reads=rs(reads), writes=rs(writes))

    def reduce(out, in_, op, reads, writes, negate=False):
        S.op("dve", lambda e: e.tensor_reduce(out=out, in_=in_, axis=AX.X, op=op, negate=negate),
             reads=rs(reads), writes=rs(writes), cost=vcost("dve", in_))

    def memset(eng, ap, val, writes):
        S.op(eng, lambda e: e.memset(ap, val), writes=rs(writes), cost=vcost(eng, ap))

    def single(out, in_, scalar, op, reads, writes):
        S.op("dve", lambda e: e.tensor_single_scalar(out=out, in_=in_, scalar=scalar, op=op),
             reads=rs(reads), writes=rs(writes))

    def scan(out, d0, d1, reads, writes):
        S.op("dve", lambda e: e.tensor_tensor_scan(out=out, data0=d0, data1=d1, initial=0.0, op0=ALU.add, op1=ALU.add),
             reads=rs(reads), writes=rs(writes))

    def iota_tail(ap, base, writes):
        S.op("pool", lambda e: e.iota(ap, pattern=[[0, 1]], base=base, channel_multiplier=1), writes=rs(writes))

    def dma(q, out, in_, reads, writes, owner, slow=False, group=None):
        try:
            nbytes = float(out.nbytes())
        except Exception:
            nbytes = 1.0e5
        lat = 2.5 + nbytes / 1.5e5
        cost = 0.1 if q == "sp" else 1.0
        ow = owner.r if isinstance(owner, Tl) else owner
        if group is not None:
            return S.dma(q, lambda e: e.dma_start(out=out, in_=in_), reads=rs(reads), writes=rs(writes),
                         owner=ow, group=group, cost=cost, lat=lat)
        if slow:
            return S.dma(q, lambda e: e.dma_start(out=out, in_=in_, allow_slow_non_contiguous=True),
                         reads=rs(reads), writes=rs(writes), owner=ow, cost=cost, lat=lat + 3.0)
        return S.dma(q, lambda e: e.dma_start(out=out, in_=in_),
                     reads=rs(reads), writes=rs(writes), owner=ow, cost=cost, lat=lat)

    def ln_stats(src_ap, src_res, stats, mv, rstd, nmr=None):
        for h in range(2):
            S.op("dve", lambda e, h=h: e.bn_stats(out=stats[:, h, :], in_=src_ap[:, h * 512:(h + 1) * 512]),
                 reads=rs([src_res]), writes=rs([stats]))
        S.op("dve", lambda e: e.bn_aggr(out=mv[:], in_=stats[:].rearrange("p a b -> p (a b)")),
             reads=rs([stats]), writes=rs([mv]))
        act(rstd[:], mv[:, 1:2], AF.Sqrt, [mv, eps_t], [rstd], bias=eps_t[:, 0:1])
        S.op("dve", lambda e: e.reciprocal(out=rstd[:], in_=rstd[:]), reads=rs([rstd]), writes=rs([rstd]))
        if nmr is not None:
            stt(nmr[:], mv[:, 0:1], -1.0, rstd[:], ALU.mult, ALU.mult, [mv, rstd], [nmr])

    S.op("pool", lambda e: e.iota(identf[:], pattern=[[1, 128]], base=0, channel_multiplier=-1,
                                  allow_small_or_imprecise_dtypes=True), writes=rs([identf]))
    S.op("dve", lambda e: e.tensor_single_scalar(out=ident[:], in_=identf[:], scalar=0.0, op=ALU.is_equal),
         reads=rs([identf]), writes=rs([ident]))
    S.op("dve", lambda e: e.memset(onesf[:], 1.0), writes=rs([onesf]))
    S.op("dve", lambda e: e.tensor_single_scalar(out=ltri[:], in_=identf[:], scalar=0.0, op=ALU.is_gt),
         reads=rs([identf]), writes=rs([ltri]))
    S.op("dve", lambda e: e.memset(eps_t[:], 1e-6), writes=rs([eps_t]))
    dma("pool", amask[:], AMASK, [S.dres("amask")], [amask], amask)

    fm_res = [S.dres("fm", t) for t in range(NT)]
    vv_res = [S.dres("vv", t) for t in range(NT)]
    xmid_res = [S.dres("xmid", t) for t in range(NT)]
    h2t_res = [S.dres("h2t", t) for t in range(NT)]
    xcur_res = [S.dres("xcur", t) for t in range(NT)]
    xh2_res = [S.dres("xh2", t) for t in range(NT)]
    y_res = [S.dres("y", t) for t in range(64)]
    final = []

    for l in range(depth_run):
        last = l == DEPTH - 1
        T0 = 2 if last else 0
        new_phase()
        cT = ph("cT", [128, 8, 2])
        bm = ph("bm", [2, 6 * D])
        mrow = ph("mrow", [2, 6 * D])
        wm = [ph("wm%d" % i, [128, 8, 512]) for i in range(2)]
        for m_ in range(2):
            dma("sp", cT[:, :, m_], CVEC[m_].rearrange("(k p) -> p k", p=128), [S.dres("cvec")], [cT], cT, slow=True)
        dma("sp", bm[:], BMOD[l].partition_broadcast(2), [S.dres("bmod")], [bm], bm)
        dma("sp", esink[:], SINK[l].partition_broadcast(128), [S.dres("sink")], [esink], esink)
        dma("sp", convw[:], CONVW[l], [S.dres("convw")], [convw], convw)
        act(cT[:], cT[:], AF.Silu, [cT], [cT])
        act(esink[:], esink[:], AF.Exp, [esink], [esink])
        wmv = WMOD[l].rearrange("(k p) n -> p k n", p=128)
        for cc in range(12):
            w_ = wm[cc % 2]
            dma("sp", w_[:], wmv[:, :, cc * 512:(cc + 1) * 512], [S.dres("wmod")], [w_], w_)
            pb = pbig[0:2, cc % 2, :]
            for k in range(8):
                mm(pb, cT[:, k, :], w_[:, k, :], k == 0, k == 7, [cT, w_], [pbr[cc % 2]])
            tt(mrow[:, cc * 512:(cc + 1) * 512], pb, bm[:, cc * 512:(cc + 1) * 512], ALU.add,
               [pbr[cc % 2], bm], [mrow])
        dma("sp", MODROW, mrow[:], [mrow], [S.dres("modrow")], mrow)
        dma("sp", modT[:], MODROW[0].rearrange("(j p) -> p j", p=128), [S.dres("modrow")], [modT], modT, slow=True)
        dma("sp", modcT[:], MODROW[1].rearrange("(j p) -> p j", p=128), [S.dres("modrow")], [modcT], modcT, slow=True)
        for m_ in (modT, modcT):
            ts(m_[:, 8:16], m_[:, 8:16], 1.0, None, ALU.add, None, [m_], [m_])
            ts(m_[:, 32:40], m_[:, 32:40], 1.0, None, ALU.add, None, [m_], [m_])

        new_phase()
        win = ph("win", [128, 8, 3072], BF16)
        wiv = WIN[l].rearrange("(k p) n -> p k n", p=128)
        for k in range(8):
            dma("pool", win[:, k, :], wiv[:, k, :], [S.dres("win")], [win], win)
        xt = [ph("xt%d" % i, [128, D]) for i in range(2)]
        xh = [ph("xh%d" % i, [128, D], BF16) for i in range(2)]
        hT = [ph("hT%d" % i, [128, 8, 512], BF16) for i in range(2)]
        fmo = [ph("fmo%d" % i, [128, 14, 512], BF16) for i in range(2)]
        vo = [ph("vo%d" % i, [128, 4, 8, 65], BF16) for i in range(2)]
        rp = [ph("rp%d" % i, [128, 4, 512]) for i in range(2)]
        tmp = [ph("tmp%d" % i, [128, 512]) for i in range(3)]
        stats = ph("stats", [128, 2, 6]); mv = ph("mv", [128, 2]); rstd = ph("rstd", [128, 1])
        for v_ in vo:
            S.op("pool", lambda e, v_=v_: e.memset(v_[:], 1.0), writes=rs([v_]))
        src = XIN if l == 0 else XCUR
        src_res = (lambda t: S.dres("xin", t)) if l == 0 else (lambda t: xcur_res[t])
        groups = [(0, 2)] + [(2 + 4 * i, 4) for i in range(16)]
        bank = [0]

        def nextbank():
            b = bank[0]
            bank[0] = (b + 1) % 6
            return b

        def gen_L(gi, t0, nt):
            N = nt * 128
            sl = gi % 2
            mT = modcT if gi == 0 else modT
            rpt = rp[sl]
            dma("sp", rpt[:, :, :N], ROPE[:, :, t0 * 128:t0 * 128 + N].rearrange("a p n -> p a n"),
                [S.dres("rope")], [rpt], rpt)
            for s in range(nt):
                tl = t0 + s
                x_ = xt[tl % 2]; xh_ = xh[tl % 2]; pt_ = ptr[tl % 2]
                dma("sp", x_[:], src[tl * 128:(tl + 1) * 128, :], [src_res(tl)], [x_], x_)
                ln_stats(x_, x_, stats, mv, rstd)
                yield
                ts(xh_[:], x_[:], mv[:, 0:1], rstd[:, 0:1], ALU.subtract, ALU.mult, [x_, mv, rstd], [xh_])
                for k in range(8):
                    mm(pt_[:, k, :], xh_[:, k * 128:(k + 1) * 128], ident[:], True, True, [xh_, ident], [pt_], tr=True)
                yield
                for k in range(8):
                    act(hT[sl][:, k, s * 128:(s + 1) * 128], pt_[:, k, :], AF.Identity, [pt_, mT], [hT[sl]],
                        bias=mT[:, k:k + 1], scale=mT[:, 8 + k:9 + k])
                    if k % 4 == 3:
                        yield

        def gen_P(gi, t0, nt):
            N = nt * 128
            sl = gi % 2
            rpt = rp[sl]
            h_ = hT[sl]; fo = fmo[sl]

            def proj(ch):
                b = nextbank()
                for k in range(8):
                    mm(pbig[:, b, :N], win[:, k, ch * 128:(ch + 1) * 128], h_[:, k, :N], k == 0, k == 7,
                       [win, h_], [pbr[b]])
                return b

            def rope(chq, chs, tq, tsn, dst):
                b1 = proj(chq); b2 = proj(chs)
                tt(tmp[0][:, :N], pbig[:, b1, :N], rpt[:, tq, :N], ALU.mult, [pbr[b1], rpt], [tmp[0]])
                tt(tmp[1][:, :N], pbig[:, b2, :N], rpt[:, tsn, :N], ALU.mult, [pbr[b2], rpt], [tmp[1]])
                tt(fo[:, dst, :N], tmp[0][:, :N], tmp[1][:, :N], ALU.add, [tmp[0], tmp[1]], [fo], eng="pool")

            for c_ in range(3):
                rope(c_, 3 + c_, 0, 1, c_)
                yield
            rope(6, 7, 2, 3, 6)
            yield
            for c_ in range(2):
                b1 = proj(8 + c_)
                act(tmp[2][:, :N], pbig[:, b1, :N], AF.Identity, [pbr[b1]], [tmp[2]])
                b2 = proj(12 + c_)
                tt(fo[:, 10 + c_, :N], pbig[:, b2, :N], tmp[2][:, :N], ALU.mult, [pbr[b2], tmp[2]], [fo])
                yield
                b3 = proj(10 + c_)
                act(fo[:, 12 + c_, :N], pbig[:, b3, :N], AF.Identity, [pbr[b3]], [fo])
                yield
            for c_ in range(3):
                b1 = proj(14 + c_)
                act(fo[:, 3 + c_, :N], pbig[:, b1, :N], AF.Identity, [pbr[b1]], [fo], scale=0.125)
                yield
                b2 = proj(17 + c_)
                cp(fo[:, 7 + c_, :N], pbig[:, b2, :N], [pbr[b2]], [fo])
                yield
            for s in range(nt):
                b = nextbank()
                for k in range(8):
                    mm(pbig[:, b, :], h_[:, k, s * 128:(s + 1) * 128], win[:, k, 2560:3072], k == 0, k == 7,
                       [win, h_], [pbr[b]])
                act(vo[sl][:, s, :, 0:64], pbig[:, b, :].rearrange("p (h d) -> p h d", h=8), AF.Identity,
                    [pbr[b]], [vo[sl]])
                yield
            dma("sp", FM[:, :, t0 * 128:t0 * 128 + N], fo[:, :, :N], [fo], fm_res[t0:t0 + nt], fo)
            dma("sp", VV[t0 * 128:t0 * 128 + N].rearrange("(s p) h d -> p s h d", p=128), vo[sl][:, :nt],
                [vo[sl]], vv_res[t0:t0 + nt], vo[sl])
            yield

        prevP = None
        for gi, (t0, nt) in enumerate(groups):
            for _ in interleave([gen_L(gi, t0, nt), prevP]):
                pass
            prevP = gen_P(gi, t0, nt)
        for _ in prevP:
            pass

        new_phase()
        wout = ph("wout", [128, 8, D], BF16)
        wov = WOUT[l].rearrange("(k p) n -> p k n", p=128)
        for k in range(0, 8, 2):
            dma("pool", wout[:, k:k + 2, :], wov[:, k:k + 2, :], [S.dres("wout")], [wout], wout)
        wr = ph("wr", [128, 8, NE], BF16)
        dma("pool", wr[:], WR[l].rearrange("(k p) n -> p k n", p=128), [S.dres("wr")], [wr], wr)
        nabi = ph("nabi", [128, 6, 640])
        nabe = ph("nabe", [128, 6, 640])
        dma("sp", nabi[:], NAB[l, 0], [S.dres("nab")], [nabi], nabi)
        kctx = ph("kctx", [128, 4, 256], BF16)
        vctx = ph("vctx", [128, 2, 8, 65], BF16)
        dma("sp", kctx[:], FM[:, 6:10, 0:256], fm_res[0:2], [kctx], kctx)
        dma("sp", vctx[:], VV[0:256].rearrange("(s p) h d -> p s h d", p=128), vv_res[0:2], [vctx], vctx)
        g1b = ph("g1b", [128, D]); cg1b = ph("cg1b", [128, D])
        l1g = ph("l1g", [128, D]); l1b = ph("l1b", [128, D])
        dma("sp", g1b[:], MODROW[0, 2048:3072].partition_broadcast(128), [S.dres("modrow")], [g1b], g1b)
        dma("sp", cg1b[:], MODROW[1, 2048:3072].partition_broadcast(128), [S.dres("modrow")], [cg1b], cg1b)
        dma("sp", l1g[:], LN1G[l].partition_broadcast(128), [S.dres("ln1g")], [l1g], l1g)
        dma("sp", l1b[:], LN1B[l].partition_broadcast(128), [S.dres("ln1b")], [l1b], l1b)
        qw = [ph("qw%d" % i, [128, 6, 128], BF16) for i in range(2)]
        kw = [ph("kw%d" % i, [128, 4, 640], BF16) for i in range(2)]
        vw = [ph("vw%d" % i, [128, 5, 8, 65], BF16) for i in range(2)]
        uw = [ph("uw%d" % i, [128, 2, 130], BF16) for i in range(2)]
        bbw = [ph("bbw%d" % i, [128, 2, 128], BF16) for i in range(2)]
        xa = [ph("xa%d" % i, [128, D]) for i in range(2)]
        pta = [ph("pta%d" % i, [128, 5, 384], BF16) for i in range(2)]
        ptn = [ph("ptn%d" % i, [128, 896], BF16) for i in range(2)]
        sfn = [ph("sfn%d" % i, [128, 640]) for i in range(2)]
        mixc = ph("mixc", [128, 768], BF16)
        mixT = ph("mixT", [128, 8, 128], BF16)
        ctmp = ph("ctmp", [128, 128])
        den = ph("den", [128, 6]);
        t1 = ph("t1", [128, D]); y1 = ph("y1", [128, D]); xm = [ph("xm%d" % i, [128, D]) for i in range(2)]
        xh2 = ph("xh2", [128, RW], BF16)
        h2o = [ph("h2o%d" % i, [128, 8, 128], BF16) for i in range(2)]
        stats = ph("stats", [128, 2, 6]); mv = ph("mv", [128, 2]); rstd = ph("rstd", [128, 1]); nmr = ph("nmr", [128, 1])
        rmx = ph("rmx", [128, 1]); rsum = ph("rsum", [128, 1]); rexp = ph("rexp", [128, NE])

        mixT2 = [mixT, ph("mixTb", [128, 8, 128], BF16)]

        def gen_att(T):
            sl = T % 2
            is_ctx = T < 2
            i = T - 2
            q_ = qw[sl]; k_ = kw[sl]; v_ = vw[sl]; u_ = uw[sl]; bb_ = bbw[sl]
            mT_ = mixT2[sl]
            dma("sp", q_[:], FM[:, 0:6, T * 128:(T + 1) * 128], [fm_res[T]], [q_], q_)
            nb = nabi
            base = 0
            if not is_ctx:
                base = min(max(i - 2, 0), 59) + 2
                dma("sp", k_[:], FM[:, 6:10, base * 128:(base + 5) * 128], fm_res[base:base + 5], [k_], k_)
                dma("sp", v_[:], VV[base * 128:(base + 5) * 128].rearrange("(s p) h d -> p s h d", p=128),
                    vv_res[base:base + 5], [v_], v_)
                var = 0 if 2 <= i <= 61 else (1 + i if i < 2 else i - 59)
                if var != 0:
                    dma("sp", nabe[:], NAB[l, var], [S.dres("nab")], [nabe], nabe)
                    nb = nabe
            lo_pad = T in (0, 2); hi_pad = T in (1, NT - 1)
            if lo_pad or hi_pad:
                memset("pool", u_[:], 0.0, [u_])
            c0 = T * 128 - (0 if lo_pad else 1); c1 = (T + 1) * 128 + (0 if hi_pad else 1)
            o0 = 1 if lo_pad else 0
            fr = fm_res[max(T - 1, 0):min(T + 2, NT)]
            dma("sp", u_[:, :, o0:o0 + (c1 - c0)], FM[:, 10:12, c0:c1], fr, [u_], u_)
            dma("sp", bb_[:], FM[:, 12:14, T * 128:(T + 1) * 128], [fm_res[T]], [bb_], bb_)
            yield
            if is_ctx:
                akeys = [(("c", 0), None), (("c", 1), None)]
                nkeys = [("c", 0), ("c", 1)]
            else:
                akeys = []
                if i > 0:
                    akeys.append((("w", T - 1 - base), 0))
                akeys.append((("w", T - base), None))
                if i < 63:
                    akeys.append((("w", T + 1 - base), 1))
                akeys += [(("c", 0), None), (("c", 1), None)]
                nkeys = [("w", j) for j in range(5)] + [("c", 0), ("c", 1)]

            def kap(kt, ch, p0):
                if kt[0] == "c":
                    return kctx[p0:p0 + 64, ch, kt[1] * 128:(kt[1] + 1) * 128], kctx
                return k_[p0:p0 + 64, ch, kt[1] * 128:(kt[1] + 1) * 128], k_

            def vap(kt, head):
                if kt[0] == "c":
                    return vctx[:, kt[1], head, :], vctx
                return v_[:, kt[1], head, :], v_

            def gen_A():
                for g in range(2):
                    p0 = g * 64
                    pa_ = pta[g]
                    for ki, (kt, mk) in enumerate(akeys):
                        ka_, kr = kap(kt, 0, p0)
                        mm(pbig[:, 0, 0:384], ka_, q_[p0:p0 + 64, 0:3, :].rearrange("p a b -> p (a b)"), True, True,
                           [kr, q_], [pbr[0]])
                        act(pa_[:, ki, :], pbig[:, 0, 0:384], AF.Exp, [pbr[0]], [pa_])
                        if mk is not None:
                            tt(pa_[:, ki, :], pa_[:, ki, :], amask[:, mk, :], ALU.mult, [pa_, amask], [pa_])
                        yield
                    po = pbig[:, 2, 0:195].rearrange("p (c d) -> p c d", c=3)
                    for c_ in range(3):
                        for ki, (kt, mk) in enumerate(akeys):
                            va_, vr = vap(kt, g)
                            mm(po[:, c_, :], pa_[:, ki, c_ * 128:(c_ + 1) * 128], va_, ki == 0, ki == len(akeys) - 1,
                               [pa_, vr], [pbr[2]])
                        yield
                    tt(den[:, 0:3], po[:, :, 64], esink[:, 3 * g:3 * g + 3], ALU.add, [pbr[2], esink], [den])
                    recip(den[:, 0:3], den[:, 0:3], [den], [den])
                    tt(mixc[:, g * 192:(g + 1) * 192].rearrange("p (c d) -> p c d", c=3), po[:, :, 0:64],
                       den[:, 0:3].unsqueeze(2).to_broadcast([128, 3, 64]), ALU.mult, [pbr[2], den], [mixcA])
                    yield

            def gen_N():
                po2 = pbig[:, 3, 0:390].rearrange("p (c d) -> p c d", c=6)
                nk = len(nkeys)
                for h in range(6):
                    ch = h // 2; p0 = (h % 2) * 64
                    pn_ = ptn[h % 2]; sf_ = sfn[h % 2]
                    ps2 = pbig[:, 4:6, :].rearrange("p a b -> p (a b)")
                    for j, kt in enumerate(nkeys):
                        ka_, kr = kap(kt, 1 + ch, p0)
                        mm(ps2[:, j * 128:(j + 1) * 128], ka_, q_[p0:p0 + 64, 3 + ch, :], True, True,
                           [kr, q_], [pbr[4], pbr[5]])
                    if is_ctx:
                        act(pn_[:, 0:256], ps2[:, 0:256], AF.Exp, [pbr[4], pbr[5]], [pn_])
                    else:
                        tt(sf_[:], ps2[:, 0:640], nb[:, h, :], ALU.add, [pbr[4], pbr[5], nb], [sf_])
                        act(pn_[:, 640:896], ps2[:, 640:896], AF.Exp, [pbr[4], pbr[5]], [pn_])
                        act(pn_[:, 0:640], sf_[:], AF.Exp, [sf_], [pn_])
                    yield
                    for j, kt in enumerate(nkeys):
                        va_, vr = vap(kt, 2 + h)
                        mm(po2[:, h, :], pn_[:, j * 128:(j + 1) * 128], va_, j == 0, j == nk - 1, [pn_, vr], [pbr[3]])
                    yield
                recip(den2[:], po2[:, :, 64], [pbr[3]], [den2])
                tt(mixc[:, 384:768].rearrange("p (c d) -> p c d", c=6), po2[:, :, 0:64],
                   den2[:].unsqueeze(2).to_broadcast([128, 6, 64]), ALU.mult, [pbr[3], den2], [mixcN])
                yield

            def gen_B():
                for c_ in range(2):
                    ts(ctmp[:], u_[:, c_, 0:128], convw[:, c_, 0:1], None, ALU.mult, None, [u_, convw], [ctmp])
                    stt(ctmp[:], u_[:, c_, 1:129], convw[:, c_, 1:2], ctmp[:], ALU.mult, ALU.add, [u_, convw, ctmp], [ctmp])
                    stt(ctmp[:], u_[:, c_, 2:130], convw[:, c_, 2:3], ctmp[:], ALU.mult, ALU.add, [u_, convw, ctmp], [ctmp])
                    tt(mT_[:, 3 + c_, :], ctmp[:], bb_[:, c_, :], ALU.mult, [ctmp, bb_], [mT_])
                    yield

            for _ in interleave([gen_A(), gen_N(), gen_B()]):
                yield
            pt_ = ptr[0]
            for c_ in range(6):
                mm(pt_[:, c_, :], mixc[:, c_ * 128:(c_ + 1) * 128], ident[:], True, True, [mixcA, mixcN, ident], [pt_], tr=True)
            cp(mT_[:, 0:3, :], pt_[:, 0:3, :], [pt_], [mT_])
            act(mT_[:, 5:8, :], pt_[:, 3:6, :], AF.Identity, [pt_], [mT_])
            yield

        def gen_epi(T):
            sl = T % 2
            is_ctx = T < 2
            x_ = xa[sl]; mT_ = mixT2[sl]
            dma("sp", x_[:], src[T * 128:(T + 1) * 128, :], [src_res(T)], [x_], x_)
            gb = cg1b if is_ctx else g1b
            for hf in range(2):
                for k in range(8):
                    mm(pbig[:, 1, :], mT_[:, k, :], wout[:, k, hf * 512:(hf + 1) * 512], k == 0, k == 7,
                       [mT_, wout], [pbr[1]])
                tt(t1[:, hf * 512:(hf + 1) * 512], pbig[:, 1, :], gb[:, hf * 512:(hf + 1) * 512], ALU.mult,
                   [pbr[1], gb], [t1])
                yield
            stt(y1[:], x_[:], ALPHA, t1[:], ALU.mult, ALU.add, [x_, t1], [y1])
            yield
            ln_stats(y1, y1, stats, mv, rstd, nmr)
            yield
            xm_ = xm[sl]
            act(t1[:], y1[:], AF.Identity, [y1, rstd, nmr], [t1], bias=nmr[:, 0:1], scale=rstd[:, 0:1])
            yield
            tt(t1[:], t1[:], l1g[:], ALU.mult, [t1, l1g], [t1], eng="pool")
            yield
            tt(xm_[:], t1[:], l1b[:], ALU.add, [t1, l1b], [xm_], eng="pool")
            dma("sp", XMID[T * 128:(T + 1) * 128, :], xm_[:], [xm_], [xmid_res[T]], xm_)
            yield
            ln_stats(xm_, xm_, stats, mv, rstd)
            yield
            ts(xh2[:, 0:D], xm_[:], mv[:, 0:1], rstd[:, 0:1], ALU.subtract, ALU.mult, [xm_, mv, rstd], [xh2])
            yield
            pt2 = ptr[1]
            for k in range(8):
                mm(pt2[:, k, :], xh2[:, k * 128:(k + 1) * 128], ident[:], True, True, [xh2, ident], [pt2], tr=True)
            mT = modcT if is_ctx else modT
            h2_ = h2o[sl]
            for k in range(8):
                act(h2_[:, k, :], pt2[:, k, :], AF.Identity, [pt2, mT], [h2_],
                    bias=mT[:, 24 + k:25 + k], scale=mT[:, 32 + k:33 + k])
                if k % 4 == 3:
                    yield
            if is_ctx:
                dma("sp", H2T[:, :, T * 128:(T + 1) * 128], h2_[:], [h2_], [h2t_res[T]], h2_)
            pr = pbig[:, 1, 0:NE]
            for k in range(8):
                mm(pr, h2_[:, k, :], wr[:, k, :], k == 0, k == 7, [h2_, wr], [pbr[1]])
            reduce(rmx[:], pr, ALU.max, [pbr[1]], [rmx], negate=True)
            act(rexp[:], pr, AF.Exp, [pbr[1], rmx], [rexp], bias=rmx[:, 0:1])
            yield
            reduce(rsum[:], rexp[:], ALU.add, [rexp], [rsum])
            recip(rsum[:], rsum[:], [rsum], [rsum])
            ts(aff[:, T, :], rexp[:], rsum[:, 0:1], None, ALU.mult, None, [rexp, rsum], [aff])
            if not is_ctx:
                ts(xh2[:, 1026:RW].bitcast(F32), rexp[:], rsum[:, 0:1], None, ALU.mult, None, [rexp, rsum], [xh2])
                iota_tail(xh2[:, 1024:1026].bitcast(I32), T * 128, [xh2])
                dma("sp", XH2[T * 128:(T + 1) * 128, :], xh2[:], [xh2], [xh2_res[T]], xh2)
            yield

        mixcA = Res("mixcA"); mixcN = Res("mixcN")
        den2 = ph("den2", [128, 6])
        prev = None
        for T in range(T0, NT):
            for _ in interleave([gen_att(T), prev]):
                pass
            prev = gen_epi(T)
        for _ in prev:
            pass

        new_phase()
        lo_t = ph("lo", [128, NE]); mid_t = ph("mid", [128, NE]); cntp = ph("cntp", [128, NE]); sel = ph("sel", [128, NE])
        cmp = ph("cmp", [128, NE, 64])
        incl = ph("incl", [128, NE, 64])
        zt = ph("zt", [128, 64])
        offs = ph("offs", [128, NE])
        zbig = ph("zbig", [128, 4096])
        memset("pool", zbig[:], 0.0, [zbig])
        memset("pool", zt[:], 0.0, [zt])
        for j in range(16):
            dma("sp", FFN[256 + j * 512:256 + (j + 1) * 512, :].rearrange("(p a) d -> p (a d)", p=128), zbig[:],
                [zbig], [S.dres("ffn")], zbig, group=("ffnz", l))
        sets = [(2, 64, 1024.0)] if last else [(0, 2, 32.0), (2, 64, 1024.0)]
        for (ta, tn, cap) in sets:
            av = aff[:, ta:ta + tn, :].rearrange("p t e -> p e t")
            memset("dve", lo_t[:], 0.0, [lo_t])
            for it in range(30):
                w_ = 0.5 ** (it + 1)
                ts(mid_t[:], lo_t[:], w_, None, ALU.add, None, [lo_t], [mid_t])
                tt(cmp[:, :, :tn], av, mid_t[:].unsqueeze(2).to_broadcast([128, NE, tn]), ALU.is_ge, [aff, mid_t], [cmp])
                reduce(cntp[:], cmp[:, :, :tn], ALU.add, [cmp], [cntp])
                mm(pbig[:, 0, 0:NE], onesf[:], cntp[:], True, True, [onesf, cntp], [pbr[0]])
                single(sel[:], pbig[:, 0, 0:NE], cap - 0.5, ALU.is_ge, [pbr[0]], [sel])
                stt(lo_t[:], sel[:], w_, lo_t[:], ALU.mult, ALU.add, [sel, lo_t], [lo_t])
            tt(cmp[:, :, :tn], av, lo_t[:].unsqueeze(2).to_broadcast([128, NE, tn]), ALU.is_ge, [aff, lo_t], [cmp])
            if dbg:
                LDBG = nc.dram_tensor("ldbg%d_%d" % (l, tn), [128, NE], F32, kind="ExternalOutput").ap()
                dma("sp", LDBG, lo_t[:], [lo_t], [S.dres("ldbg", tn)], lo_t)
                ADBG = nc.dram_tensor("adbg%d_%d" % (l, tn), [128, NT * NE], F32, kind="ExternalOutput").ap()
                dma("sp", ADBG, aff[:].rearrange("p a b -> p (a b)"), [aff], [S.dres("adbg", tn)], aff)
            if tn == 2:
                wv = wsel[:, ta:ta + tn, :].rearrange("p t e -> p e t")
                tt(wv, av, cmp[:, :, :tn], ALU.mult, [aff, cmp], [wsel])
                continue
            for ex in range(NE):
                scan(incl[:, ex, :], cmp[:, ex, :], zt[:], [cmp, zt], [incl])
            cp(cntp[:], incl[:, :, 63], [incl], [cntp])
            mm(pbig[:, 0, 0:NE], ltri[:], cntp[:], True, True, [ltri, cntp], [pbr[0]])
            cp(offs[:], pbig[:, 0, 0:NE], [pbr[0]], [offs])
            tt(incl[:], incl[:], cmp[:], ALU.subtract, [incl, cmp], [incl])
            tt(incl[:], incl[:], offs[:].unsqueeze(2).to_broadcast([128, NE, 64]), ALU.add, [incl, offs], [incl])
            stt(incl[:].rearrange("p a b -> p (a b)"), incl[:].rearrange("p a b -> p (a b)"), -1.0e6,
                cmp[:].rearrange("p a b -> p (a b)"), ALU.add, ALU.mult, [incl, cmp], [incl])
            ts(idxT[:], incl[:], 1.0e6, None, ALU.add, None, [incl], [idxT])
            if dbg:
                IDBG = nc.dram_tensor("idbg%d" % l, [128, NE * 64], I32, kind="ExternalOutput").ap()
                dma("sp", IDBG, idxT[:].rearrange("p a b -> p (a b)"), [idxT], [S.dres("idbg")], idxT)
                ODBG = nc.dram_tensor("odbg%d" % l, [128, NE], F32, kind="ExternalOutput").ap()
                dma("sp", ODBG, offs[:], [offs], [S.dres("odbg")], offs)
                CDBG = nc.dram_tensor("cdbg%d" % l, [128, NE * 64], F32, kind="ExternalOutput").ap()
                dma("sp", CDBG, cmp[:].rearrange("p a b -> p (a b)"), [cmp], [S.dres("cdbg")], cmp)

        new_phase()
        wgt = [ph("wg%d" % i, [128, 8, 512], BF16) for i in range(2)]
        wut = [ph("wu%d" % i, [128, 8, 512], BF16) for i in range(2)]
        wdt = [ph("wd%d" % i, [128, 4, D], BF16) for i in range(2)]
        tokc = [ph("tokc%d" % i, [128, 8, RW], BF16) for i in range(2)]
        xet = [ph("xet%d" % i, [128, RW], BF16) for i in range(2)]
        h2e = [ph("h2e%d" % i, [128, 8, 512], BF16) for i in range(2)]
        gT = [ph("gT%d" % i, [128, 4, 512], BF16) for i in range(2)]
        sa = [ph("sa%d" % i, [128, 512], BF16) for i in range(2)]
        yo = [ph("yo%d" % i, [128, D]) for i in range(2)]
        idxe = [ph("idxe%d" % i, [128, 1], I32) for i in range(8)]
        gate = ph("gate", [128, 8])
        rmx = ph("rmx", [128, 1]); rsum = ph("rsum", [128, 1]); rexp = ph("rexp", [128, NE])
        wr = ph("wr", [128, 8, NE], BF16)
        dma("pool", wr[:], WR[l].rearrange("(k p) n -> p k n", p=128), [S.dres("wr")], [wr], wr)
        h2g = ph("h2g", [128, 8, 256], BF16)
        acc = ph("acc", [128, 2, D])
        accr = [Res("acc%d" % i) for i in range(2)]
        if not last:
            dma("sp", h2g[:], H2T[:, :, 0:256], h2t_res[0:2], [h2g], h2g)
        xe_res = [S.dres("xe", e) for e in range(NE)]
        tcnt = [0]

        def load_w(ex):
            sl = ex % 2
            dma("pool", wgt[sl][:], WG[l, ex].rearrange("(k p) f -> p k f", p=128), [S.dres("wg")], [wgt[sl]], wgt[sl])
            dma("pool", wut[sl][:], WU[l, ex].rearrange("(k p) f -> p k f", p=128), [S.dres("wu")], [wut[sl]], wut[sl])
            dma("pool", wdt[sl][:], WD[l, ex].rearrange("(k p) f -> p k f", p=128), [S.dres("wd")], [wdt[sl]], wdt[sl])

        def dispatch(exs):
            def load(cg):
                tk = tokc[cg % 2]
                dma("pool", tk[:], XH2[(2 + cg * 8) * 128:(2 + cg * 8 + 8) * 128, :].rearrange("(s p) d -> p s d", p=128),
                    xh2_res[2 + cg * 8:2 + cg * 8 + 8], [tk], tk)
            load(0)
            for cg in range(8):
                if cg + 1 < 8:
                    load(cg + 1)
                tk = tokc[cg % 2]
                for s in range(8):
                    T = 2 + cg * 8 + s
                    for ex in exs:
                        S.dma("pool", lambda e, tk=tk, s=s, ex=ex, T=T: e.indirect_dma_start(
                            out=XE[ex][:, :], out_offset=bass.IndirectOffsetOnAxis(
                                ap=idxT[:].rearrange("p a b -> p (a b)")[:, ex * 64 + T - 2:ex * 64 + T - 1], axis=0),
                            in_=tk[:, s, :], in_offset=None, bounds_check=breg(e), oob_is_err=False),
                            reads=rs([tk, idxT]), writes=[xe_res[ex]], owner=tk.r, group=("xe", l, ex), cost=1.2, lat=4.0)

        egroups = [[0, 1], [2, 3, 4, 5], [6, 7, 8, 9], [10, 11, 12, 13], [14, 15]]
        gstart = {g[0]: gi for gi, g in enumerate(egroups)}
        gater = [Res("gate%d" % i) for i in range(8)]
        dispatch(egroups[0])
        load_w(0)
        load_w(1)

        def ctx_dense(ex, wg_, wu_, wd_):
                def ffn_chunk(h_src, N, g_):
                    for fc in range(4):
                        ba = 2 * (fc % 2); bu = ba + 1
                        for k in range(8):
                            mm(pbig[:, ba, :N], wg_[:, k, fc * 128:(fc + 1) * 128], h_src[0][:, k, :N], k == 0, k == 7,
                               [wg_, h_src[1]], [pbr[ba]])
                        for k in range(8):
                            mm(pbig[:, bu, :N], wu_[:, k, fc * 128:(fc + 1) * 128], h_src[0][:, k, :N], k == 0, k == 7,
                               [wu_, h_src[1]], [pbr[bu]])
                        s_ = sa[fc % 2]
                        act(s_[:, :N], pbig[:, ba, :N], AF.Silu, [pbr[ba]], [s_])
                        tt(g_[:, fc, :N], pbig[:, bu, :N], s_[:, :N], ALU.mult, [pbr[bu], s_], [g_])

                def down(g_, s):
                    for hf in range(2):
                        for fc in range(4):
                            mm(pbig[:, 4 + hf, :], g_[:, fc, s * 128:(s + 1) * 128], wd_[:, fc, hf * 512:(hf + 1) * 512],
                               fc == 0, fc == 3, [g_, wd_], [pbr[4 + hf]])
                    return pbig[:, 4:6, :]

                if not last:
                    g_ = gT[0]
                    ffn_chunk((h2g, h2g), 256, g_)
                    for s in range(2):
                        py = down(g_, s)
                        av_ = acc[:, s, :].rearrange("p (a b) -> p a b", a=2)
                        if ex == 0:
                            ts(av_, py, wsel[:, s, ex:ex + 1], None, ALU.mult, None, [pbr[4], pbr[5], wsel], [accr[s]])
                        else:
                            stt(av_, py, wsel[:, s, ex:ex + 1], av_, ALU.mult, ALU.add,
                                [pbr[4], pbr[5], wsel, accr[s]], [accr[s]])

        def gen_prep(ex, ci):
            h_ = h2e[ci]
            for s in range(4):
                st_ = ci * 4 + s
                x_ = xet[st_ % 2]
                dma("sp", x_[:], XE[ex][st_ * 128:(st_ + 1) * 128, :], [xe_res[ex]], [x_], x_)
                cp(idxe[st_][:], x_[:, 1024:1026].bitcast(I32), [x_], [idxe[st_]])
                cp(gate[:, st_:st_ + 1], x_[:, 1026:RW].bitcast(F32)[:, ex:ex + 1], [x_], [gater[st_]])
                pt_ = ptr[st_ % 2]
                for k in range(8):
                    mm(pt_[:, k, :], x_[:, k * 128:(k + 1) * 128], ident[:], True, True, [x_, ident], [pt_], tr=True)
                yield
                for k in range(8):
                    act(h_[:, k, s * 128:(s + 1) * 128], pt_[:, k, :], AF.Identity, [pt_, modT], [h_],
                        bias=modT[:, 24 + k:25 + k], scale=modT[:, 32 + k:33 + k])
                    if k % 4 == 3:
                        yield

        def gen_ffn(ex, ci, wg_, wu_, wd_):
            h_ = h2e[ci]
            g_ = gT[ci]
            for fc in range(4):
                ba = 2 * (fc % 2); bu = ba + 1
                for k in range(8):
                    mm(pbig[:, ba, :], wg_[:, k, fc * 128:(fc + 1) * 128], h_[:, k, :], k == 0, k == 7, [wg_, h_], [pbr[ba]])
                for k in range(8):
                    mm(pbig[:, bu, :], wu_[:, k, fc * 128:(fc + 1) * 128], h_[:, k, :], k == 0, k == 7, [wu_, h_], [pbr[bu]])
                s_ = sa[fc % 2]
                act(s_[:], pbig[:, ba, :], AF.Silu, [pbr[ba]], [s_])
                tt(g_[:, fc, :], pbig[:, bu, :], s_[:], ALU.mult, [pbr[bu], s_], [g_])
                yield
            for s in range(4):
                st_ = ci * 4 + s
                for hf in range(2):
                    for fc in range(4):
                        mm(pbig[:, 4 + hf, :], g_[:, fc, s * 128:(s + 1) * 128], wd_[:, fc, hf * 512:(hf + 1) * 512],
                           fc == 0, fc == 3, [g_, wd_], [pbr[4 + hf]])
                y_ = yo[st_ % 2]
                act(y_[:].rearrange("p (a b) -> p a b", a=2), pbig[:, 4:6, :], AF.Identity, [pbr[4], pbr[5], gater[st_]], [y_],
                    scale=gate[:, st_:st_ + 1])
                S.dma("pool", lambda e, y_=y_, ie_=idxe[st_]: e.indirect_dma_start(
                    out=FFN[:, :], out_offset=bass.IndirectOffsetOnAxis(ap=ie_[:, :], axis=0),
                    in_=y_[:, :], in_offset=None, compute_op=ALU.add),
                    reads=rs([y_, idxe[st_]]), writes=[S.dres("ffn")], owner=y_.r, group=("ffn", l, ex), cost=1.2, lat=8.0)
                yield

        prev = None
        for ex in range(NE):
            sl = ex % 2
            if ex in gstart and gstart[ex] + 1 < len(egroups):
                dispatch(egroups[gstart[ex] + 1])
            ctx_dense(ex, wgt[sl], wut[sl], wdt[sl])
            for ci in range(2):
                for _ in interleave([gen_prep(ex, ci), prev]):
                    pass
                if ci == 0 and ex >= 1 and ex + 1 < NE:
                    load_w(ex + 1)
                prev = gen_ffn(ex, ci, wgt[sl], wut[sl], wdt[sl])
        for _ in prev:
            pass

        g2b = ph("g2b", [128, D]); cg2b = ph("cg2b", [128, D])
        l2g = ph("l2g", [128, D]); l2b = ph("l2b", [128, D])
        dma("sp", g2b[:], MODROW[0, 5120:6144].partition_broadcast(128), [S.dres("modrow")], [g2b], g2b)
        dma("sp", cg2b[:], MODROW[1, 5120:6144].partition_broadcast(128), [S.dres("modrow")], [cg2b], cg2b)
        dma("sp", l2g[:], LN2G[l].partition_broadcast(128), [S.dres("ln2g")], [l2g], l2g)
        dma("sp", l2b[:], LN2B[l].partition_broadcast(128), [S.dres("ln2b")], [l2b], l2b)
        xmt = [ph("xmt%d" % i, [128, D]) for i in range(2)]
        fft = [ph("fft%d" % i, [128, D]) for i in range(2)]
        ot = [ph("ot%d" % i, [128, D]) for i in range(2)]
        y2 = [ph("y2%d" % i, [128, D]) for i in range(2)]
        st2 = [(ph("stats", [128, 2, 6]), ph("mv", [128, 2]), ph("rstd", [128, 1]), ph("nmr", [128, 1])) for _ in range(2)]

        def gen_ln2(T):
            xm_ = xmt[T % 2]; o_ = ot[T % 2]; y2_ = y2[T % 2]
            stats, mv, rstd, nmr = st2[T % 2]
            dma("sp", xm_[:], XMID[T * 128:(T + 1) * 128, :], [xmid_res[T]], [xm_], xm_)
            if T < 2:
                tt(y2_[:], acc[:, T, :], cg2b[:], ALU.mult, [accr[T], cg2b], [y2_])
            else:
                f_ = fft[T % 2]
                dma("sp", f_[:], FFN[T * 128:(T + 1) * 128, :], [S.dres("ffn")], [f_], f_)
                tt(y2_[:], f_[:], g2b[:], ALU.mult, [f_, g2b], [y2_])
            yield
            stt(y2_[:], xm_[:], ALPHA, y2_[:], ALU.mult, ALU.add, [xm_, y2_], [y2_])
            yield
            ln_stats(y2_, y2_, stats, mv, rstd, nmr)
            yield
            act(y2_[:], y2_[:], AF.Identity, [y2_, rstd, nmr], [y2_], bias=nmr[:, 0:1], scale=rstd[:, 0:1])
            yield
            tt(y2_[:], y2_[:], l2g[:], ALU.mult, [y2_, l2g], [y2_], eng="pool")
            yield
            tt(o_[:], y2_[:], l2b[:], ALU.add, [y2_, l2b], [o_], eng="pool")
            if last:
                ev = dma("sp", Y[(T - 2) * 128:(T - 1) * 128, :], o_[:], [o_], [y_res[T - 2]], o_)
                final.append(ev)
            else:
                dma("sp", XCUR[T * 128:(T + 1) * 128, :], o_[:], [o_], [xcur_res[T]], o_)
            yield

        tl_ = list(range(T0, NT))
        for j in range(0, len(tl_), 2):
            for _ in interleave([gen_ln2(T) for T in tl_[j:j + 2]]):
                pass

    S.emit(final_waits=final, reorder=reorder, only=only)
    if dbg:
        print("instr counts", {e: len(v) for e, v in S.ins.items()}, "waits", S.nwaits, flush=True)
    return nc


def _rope_tables():
    t = np.arange(8192)
    row = (t // 64).astype(np.float32); col = (t % 64).astype(np.float32)
    inv = (10000.0 ** (-np.arange(0, 32, 2, dtype=np.float32) / 32)).astype(np.float32)
    cs = np.ones((64, TOK), np.float32); sn = np.zeros((64, TOK), np.float32)
    for a, pos in enumerate((row, col)):
        ang = (pos[:, None] * inv[None, :]).astype(np.float32)
        c = np.cos(ang).T; s = np.sin(ang).T
        cs[a * 32:a * 32 + 16, 256:] = c; cs[a * 32 + 16:a * 32 + 32, 256:] = c
        sn[a * 32:a * 32 + 16, 256:] = -s; sn[a * 32 + 16:a * 32 + 32, 256:] = s
    cs = np.concatenate([cs, cs], 0); sn = np.concatenate([sn, sn], 0)
    return np.stack([cs * 0.125, sn * 0.125, cs, sn]).astype(np.float32)


def _win_ext(w_in):
    qa = w_in[:, :, 0:384]; ka = w_in[:, :, 384:512]; va = w_in[:, :, 512:640]
    bx = w_in[:, :, 640:896]; bb = w_in[:, :, 896:1152]; bc = w_in[:, :, 1152:1408]
    qn = w_in[:, :, 1408:1792]; kn = w_in[:, :, 1792:2176]; vn = w_in[:, :, 2176:2560]
    sw = np.concatenate([np.arange(16, 32), np.arange(0, 16), np.arange(48, 64), np.arange(32, 48)])

    def heads_sw(w, nh):
        idx = np.concatenate([h * 64 + sw for h in range(nh)])
        return w[:, :, idx]

    def qperm(w):
        idx = np.concatenate([np.concatenate([np.arange(c * 64, c * 64 + 64), np.arange((3 + c) * 64, (3 + c) * 64 + 64)])
                              for c in range(3)])
        return w[:, :, idx]

    return np.ascontiguousarray(np.concatenate(
        [qperm(qa), qperm(heads_sw(qa, 6)), ka, heads_sw(ka, 2), bx, bb, bc, qn, kn, va, vn], axis=2))


def _na_bias(rpb):
    NEG = -30000.0
    out = np.full((DEPTH, 5, 6, 5, 128, 128), NEG, np.float32)
    cq = np.arange(64)
    col_start = np.clip(cq - 8, 0, 48)
    col_ok = (cq[None, :] >= col_start[:, None]) & (cq[None, :] < col_start[:, None] + 16)
    coff = np.clip(cq[None, :] - cq[:, None], -15, 15) + 15
    variants = [10, 0, 1, 62, 63]
    for vi, P in enumerate(variants):
        base = min(max(P - 2, 0), 59)
        for rho in range(2):
            r = 2 * P + rho
            rs_ = min(max(r - 4, 0), 120)
            for j in range(5):
                for kap in range(2):
                    kr = 2 * (base + j) + kap
                    if not (rs_ <= kr < rs_ + 8):
                        continue
                    roff = kr - r + 7
                    b = rpb[:, :, roff, :][:, :, coff]
                    b = np.where(col_ok[None, None], b, NEG)
                    out[:, vi, :, j, kap * 64:(kap + 1) * 64, rho * 64:(rho + 1) * 64] = np.transpose(b, (0, 1, 3, 2))
    out = np.transpose(out, (0, 1, 4, 2, 3, 5)).reshape(DEPTH, 5, 128, 6, 640)
    return np.ascontiguousarray(out)


def _amask():
    k = np.arange(128)[:, None]; q = np.arange(128)[None, :]
    mp = (k >= q).astype(np.float32); mn = (k <= q).astype(np.float32)
    return np.ascontiguousarray(np.stack([np.tile(mp, (1, 3)), np.tile(mn, (1, 3))], axis=1))


def make_in_maps(x, c, ctx, c_ctx, w_mod, b_mod, w_in, conv_w, attn_sink, na_rpb, w_out,
                 ln1_g, ln1_b, w_router, w_gate, w_up, w_down, ln2_g, ln2_b):
    f = lambda a: np.ascontiguousarray(np.asarray(a, dtype=np.float32))
    shared = dict(
        w_mod=f(w_mod), b_mod=f(b_mod), w_in=_win_ext(f(w_in)), rope=_rope_tables(),
        convw=np.ascontiguousarray(np.transpose(f(conv_w).reshape(DEPTH, 3, 2, 128), (0, 3, 2, 1))),
        sink=f(attn_sink), nab=_na_bias(f(na_rpb)), amask=_amask(), w_out=f(w_out),
        ln1_g=f(ln1_g), ln1_b=f(ln1_b), ln2_g=f(ln2_g), ln2_b=f(ln2_b), w_router=f(w_router),
        w_gate=f(w_gate), w_up=f(w_up), w_down=f(w_down))
    x = f(x); ctx = f(ctx); c = f(c); c_ctx = f(c_ctx)
    maps = []
    for b in range(N_CORES):
        m = dict(shared)
        m["xin"] = np.ascontiguousarray(np.concatenate([ctx[b], x[b]], axis=0))
        m["cvec"] = np.ascontiguousarray(np.stack([c[b], c_ctx], axis=0))
        maps.append(m)
    return maps


_NC = {}


def kernel(**inputs):
    if "nc" not in _NC:
        _NC["nc"] = build_nc(reorder=False)
    maps = make_in_maps(**inputs)
    res = run_bass_kernel_spmd(_NC["nc"], maps, core_ids=list(range(N_CORES)))
    return np.stack([np.asarray(r["y"], dtype=np.float32) for r in res.results], axis=0)
```

```python
import numpy as np
import concourse.bass as bass
import concourse.mybir as mybir
from concourse.bass_utils import run_bass_kernel_spmd

F32 = mybir.dt.float32
BF16 = mybir.dt.bfloat16
I32 = mybir.dt.int32
AF = mybir.ActivationFunctionType
ALU = mybir.AluOpType
AX = mybir.AxisListType

D = 1024
NT = 66
TOK = NT * 128
DEPTH = 2
ALPHA = float((2 * DEPTH) ** 0.25)
NE = 16
RW = 1024 + 2 + 32
COMPUTE = ("pe", "dve", "act", "pool")
N_CORES = 4


class Res:
    __slots__ = ("name", "w", "r", "dsem", "wg")

    def __init__(self, name="r"):
        self.name = name
        self.w = None
        self.r = []
        self.dsem = {}
        self.wg = None


class Sched:
    def __init__(self, nc):
        self.nc = nc
        self.ins = {e: [] for e in ("pe", "dve", "act", "pool", "sp")}
        self.dram = {}
        self.ndsem = 0
        self.owners = []
        self.free = {"sp": [], "pool": []}
        self.phase = 0
        self.phase_ev = {0: []}
        self.last_dma = {}
        self.pool_dmas = []
        self.last_pew = {}

    def dres(self, *key):
        r = self.dram.get(key)
        if r is None:
            r = self.dram[key] = Res(str(key))
        return r

    def _deps(self, eng, reads, writes, pe_acc, group=None):
        deps = []
        for r in reads:
            if r.w is not None:
                if isinstance(r.w, list):
                    deps.extend(r.w)
                else:
                    deps.append(r.w)
        for r in writes:
            if r.w is not None:
                if isinstance(r.w, list):
                    if not (group is not None and r.wg == group):
                        deps.extend(r.w)
                elif not (pe_acc and r.w[0] == "E" and r.w[1] == "pe" and eng == "pe"):
                    deps.append(r.w)
            deps.extend(r.r)
        return deps

    def _post(self, ev, reads, writes, group=None):
        for r in reads:
            r.r.append(ev)
        for r in writes:
            if group is not None and r.wg == group and isinstance(r.w, list):
                r.w.append(ev)
            else:
                r.w = [ev] if group is not None else ev
                r.wg = group
                r.r = []

    def op(self, eng, fn, reads=(), writes=(), pe_acc=False, cost=0.5):
        deps = self._deps(eng, reads, writes, pe_acc)
        idx = len(self.ins[eng])
        order = []
        if eng == "pe":
            for r in writes:
                p = self.last_pew.get(id(r))
                if p is not None:
                    order.append(p)
                self.last_pew[id(r)] = idx
        ev = ("E", eng, idx)
        self.ins[eng].append([fn, deps, None, self.phase, cost, order, cost])
        self._post(ev, reads, writes)
        return ev

    def dma(self, q, fn, reads=(), writes=(), owner=None, group=None, cost=0.1, lat=3.0):
        deps = self._deps(q, reads, writes, False, group)
        sc = owner.dsem.get(q)
        if sc is None:
            if self.free[q]:
                sc = list(self.free[q].pop())
            else:
                sc = [self.ndsem, 0]
                self.ndsem += 1
            owner.dsem[q] = sc
            self.owners.append((owner, q))
        sc[1] += 16
        ev = ("D", sc[0], sc[1])
        if q == "pool":
            if len(self.pool_dmas) >= 24:
                deps.append(self.pool_dmas[-24])
            self.pool_dmas.append(ev)
        idx = len(self.ins[q])
        order = []
        p = self.last_dma.get(sc[0])
        if p is not None:
            order.append(p)
        self.last_dma[sc[0]] = idx
        self.ins[q].append([fn, deps, sc[0], self.phase, cost, order, cost + lat, ev])
        self._post(ev, reads, writes, group)
        return ev

    def barrier(self):
        evs = []
        for e in COMPUTE:
            for i in range(len(self.ins[e]) - 1, -1, -1):
                if self.ins[e][i][2] is None:
                    evs.append(("E", e, i))
                    break
        for (o, q) in self.owners:
            sc = o.dsem.pop(q)
            evs.append(("D", sc[0], sc[1]))
            self.free[q].append((sc[0], sc[1]))
        self.owners = []
        self.phase += 1
        self.phase_ev[self.phase] = evs

    def _schedule(self, only=None):
        import heapq
        engs = list(self.ins.keys())
        dprod = {}
        for q in ("sp", "pool"):
            for i, rec in enumerate(self.ins[q]):
                if rec[2] is not None:
                    dprod[(rec[7][1], rec[7][2])] = (q, i)
        fin = {}
        issue = {}
        order = {e: [] for e in engs}
        ptr0 = {e: 0 for e in engs}
        tnow = 0.0
        for ph in range(self.phase + 1):
            nodes = []
            for e in engs:
                lst = self.ins[e]
                i = ptr0[e]
                while i < len(lst) and lst[i][3] == ph:
                    nodes.append((e, i))
                    i += 1
                ptr0[e] = i
            if not nodes:
                continue
            if only is not None and ph not in only:
                for (e, i) in nodes:
                    order[e].append(i)
                continue
            inph = set(nodes)
            ndep = {}
            users = {}
            for (e, i) in nodes:
                rec = self.ins[e][i]
                preds = set()
                for d in rec[1]:
                    p = (d[1], d[2]) if d[0] == "E" else dprod.get((d[1], d[2]))
                    if p is not None and p in inph and p != (e, i):
                        preds.add((p, 0))
                for j in rec[5]:
                    if (e, j) in inph:
                        preds.add(((e, j), 1))
                ndep[(e, i)] = len(preds)
                for pk in preds:
                    users.setdefault(pk[0], []).append(((e, i), pk[1]))
            ready = {}
            heaps = {e: [] for e in engs}
            avail = {e: [] for e in engs}
            efree = {e: tnow for e in engs}
            for n in nodes:
                ready[n] = tnow
                if ndep[n] == 0:
                    heapq.heappush(heaps[n[0]], (tnow, n[1]))
            left = len(nodes)
            tmax = tnow
            while left:
                best = None
                for e in engs:
                    if avail[e]:
                        st_ = efree[e]
                    elif heaps[e]:
                        st_ = max(heaps[e][0][0], efree[e])
                    else:
                        continue
                    if best is None or st_ < best[0]:
                        best = (st_, e)
                st_, e = best
                while heaps[e] and heaps[e][0][0] <= st_:
                    heapq.heappush(avail[e], heapq.heappop(heaps[e])[1])
                i = heapq.heappop(avail[e])
                rec = self.ins[e][i]
                issue[(e, i)] = st_
                efree[e] = st_ + rec[4]
                f_ = st_ + rec[6]
                fin[(e, i)] = f_
                tmax = max(tmax, f_)
                order[e].append(i)
                left -= 1
                for (u, kind) in users.get((e, i), ()):
                    t_ = f_ if kind == 0 else st_
                    if t_ > ready[u]:
                        ready[u] = t_
                    ndep[u] -= 1
                    if ndep[u] == 0:
                        heapq.heappush(heaps[u[0]], (ready[u], u[1]))
            tnow = tmax
        self.sim_time = tnow
        return order

    def _check(self, order, val):
        sems = {}
        pos = {e: 0 for e in self.ins}
        curph = {e: 0 for e in self.ins}
        progress = True
        total = sum(len(v) for v in self.ins.values())
        done = 0
        while progress:
            progress = False
            for e in self.ins:
                while pos[e] < len(order[e]):
                    i = order[e][pos[e]]
                    rec = self.ins[e][i]
                    deps = list(rec[1])
                    if rec[3] != curph[e]:
                        for p in range(curph[e] + 1, rec[3] + 1):
                            deps.extend(self.phase_ev.get(p, ()))
                    ok = True
                    for d in deps:
                        if d[0] == "E":
                            if sems.get(("E", d[1]), 0) < val[d[1]][d[2]]:
                                ok = False
                                break
                        elif sems.get(("D", d[1]), 0) < d[2]:
                            ok = False
                            break
                    if not ok:
                        break
                    curph[e] = rec[3]
                    if rec[2] is not None:
                        sems[("D", rec[2])] = sems.get(("D", rec[2]), 0) + 16
                    elif i in val.get(e, {}):
                        sems[("E", e)] = val[e][i]
                    pos[e] += 1
                    done += 1
                    progress = True
        if done != total:
            msg = []
            for e in self.ins:
                if pos[e] < len(order[e]):
                    i = order[e][pos[e]]
                    msg.append((e, pos[e], i, self.ins[e][i][3], self.ins[e][i][1][:6]))
            raise RuntimeError("schedule deadlock: %s" % msg)

    def emit(self, final_waits=(), reorder=True, only=None):
        import contextlib
        nc = self.nc
        if reorder:
            order = self._schedule(only)
        else:
            order = {e: list(range(len(l))) for e, l in self.ins.items()}
        for e in self.ins:
            assert sorted(order[e]) == list(range(len(self.ins[e]))), e
        lastc = {}
        for e in COMPUTE:
            cur = None
            per = {}
            for i in order[e]:
                if self.ins[e][i][2] is None:
                    per[self.ins[e][i][3]] = i
            lastc[e] = per
        for p in list(self.phase_ev.keys()):
            evs = [d for d in self.phase_ev[p] if d[0] == "D"]
            for e in COMPUTE:
                qs = [q for q in lastc[e] if q < p]
                if qs:
                    evs.append(("E", e, lastc[e][max(qs)]))
            self.phase_ev[p] = evs
        need = {e: set() for e in COMPUTE}
        for e, lst in self.ins.items():
            for rec in lst:
                for d in rec[1]:
                    if d[0] == "E":
                        need[d[1]].add(d[2])
        for evs in self.phase_ev.values():
            for d in evs:
                if d[0] == "E":
                    need[d[1]].add(d[2])
        val = {}
        for e in COMPUTE:
            c = 0
            v = {}
            for i in order[e]:
                if i in need[e]:
                    c += 1
                    v[i] = c
            val[e] = v
        self._check(order, val)
        self.nwaits = {}
        with contextlib.ExitStack() as st:
            esem = {e: st.enter_context(nc.semaphore("s_" + e)) for e in COMPUTE}
            dsem = [st.enter_context(nc.semaphore("d%d" % i)) for i in range(self.ndsem)]
            block = st.enter_context(nc.Block())

            def run(ename, eng):
                waited = {}
                lst = self.ins[ename]
                cur_ph = 0
                for i in order[ename]:
                    rec = lst[i]
                    deps = rec[1]
                    if rec[3] != cur_ph:
                        deps = list(deps)
                        for p in range(cur_ph + 1, rec[3] + 1):
                            deps.extend(self.phase_ev.get(p, ()))
                        cur_ph = rec[3]
                    tg = {}
                    for d in deps:
                        if d[0] == "E":
                            key = ("E", d[1]); v = val[d[1]][d[2]]; sem = esem[d[1]]
                        else:
                            key = ("D", d[1]); v = d[2]; sem = dsem[d[1]]
                        if tg.get(key, (None, 0))[1] < v:
                            tg[key] = (sem, v)
                    for key, (sem, v) in tg.items():
                        if waited.get(key, 0) >= v:
                            continue
                        eng.wait_ge(sem, v)
                        waited[key] = v
                        self.nwaits[ename] = self.nwaits.get(ename, 0) + 1
                    ins = rec[0](eng)
                    if rec[2] is not None:
                        ins.then_inc(dsem[rec[2]], 16)
                    elif i in need[ename]:
                        ins.then_inc(esem[ename], 1)
                if ename == "sp":
                    for d in final_waits:
                        eng.wait_ge(dsem[d[1]], d[2])

            block.tensor(lambda e: run("pe", e))
            block.vector(lambda e: run("dve", e))
            block.scalar(lambda e: run("act", e))
            block.gpsimd(lambda e: run("pool", e))
            block.sync(lambda e: run("sp", e))


def interleave(gens):
    gens = [g for g in gens if g is not None]
    while gens:
        nxt = []
        for g in gens:
            try:
                next(g)
                nxt.append(g)
            except StopIteration:
                pass
            yield
        gens = nxt


class Tl:
    __slots__ = ("t", "r")

    def __init__(self, t, name):
        self.t = t
        self.r = Res(name)

    def __getitem__(self, k):
        return self.t[k]


def build_nc(dbg=False, depth_run=DEPTH, reorder=True, only=None):
    nc = bass.Bass("TRN2", target_bir_lowering=False)
    S = Sched(nc)

    def din(name, shape, dt=F32):
        return nc.dram_tensor(name, list(shape), dt, kind="ExternalInput").ap()

    def dscr(name, shape, dt):
        return nc.dram_tensor(name, list(shape), dt, kind="ExternalOutput" if dbg else "Internal").ap()

    XIN = din("xin", [TOK, D])
    CVEC = din("cvec", [2, D])
    WMOD = din("w_mod", [DEPTH, D, 6 * D])
    BMOD = din("b_mod", [DEPTH, 6 * D])
    WIN = din("w_in", [DEPTH, D, 3072])
    ROPE = din("rope", [4, 128, TOK])
    CONVW = din("convw", [DEPTH, 128, 2, 3])
    SINK = din("sink", [DEPTH, 6])
    NAB = din("nab", [DEPTH, 5, 128, 6, 640])
    AMASK = din("amask", [128, 2, 384])
    WOUT = din("w_out", [DEPTH, D, D])
    LN1G = din("ln1_g", [DEPTH, D]); LN1B = din("ln1_b", [DEPTH, D])
    LN2G = din("ln2_g", [DEPTH, D]); LN2B = din("ln2_b", [DEPTH, D])
    WR = din("w_router", [DEPTH, D, NE])
    WG = din("w_gate", [DEPTH, NE, D, 512]); WU = din("w_up", [DEPTH, NE, D, 512])
    WD = din("w_down", [DEPTH, NE, 512, D])
    Y = nc.dram_tensor("y", [8192, D], F32, kind="ExternalOutput").ap()

    MODROW = dscr("modrow", [2, 6 * D], F32)
    FM = dscr("fm", [128, 14, TOK], BF16)
    VV = dscr("vv", [TOK, 8, 65], BF16)
    XMID = dscr("xmid", [TOK, D], F32)
    H2T = dscr("h2t", [128, 8, TOK], BF16)
    XCUR = dscr("xcur", [TOK, D], F32)
    XH2 = dscr("xh2", [TOK, RW], BF16)
    XE = [dscr("xe%d" % e, [1024, RW], BF16) for e in range(NE)]
    FFN = dscr("ffn", [TOK, D], F32)

    SB_LO = 16512
    SB_HI = 229344
    st = {"pers": SB_LO, "ph": None}

    def _alloc(name, shape, dt, key):
        nb = int(np.prod(shape[1:])) * (2 if dt == BF16 else 4)
        nb = (nb + 31) // 32 * 32
        off = st[key]
        assert off + nb <= SB_HI, (name, off, nb)
        st[key] = off + nb
        return Tl(nc.alloc_sbuf_tensor_at(name, list(shape), dt, offset=off), name)

    def pers(name, shape, dt=F32):
        return _alloc(name, shape, dt, "pers")

    cnt = [0]

    def ph(name, shape, dt=F32):
        cnt[0] += 1
        return _alloc("%s_%d" % (name, cnt[0]), shape, dt, "ph")

    def new_phase():
        S.barrier()
        st["ph"] = st["pers_end"]

    pbig = Tl(nc.alloc_psum_tensor("pbig", [128, 6, 512], F32), "pbig")
    ptr = [Tl(nc.alloc_psum_tensor("ptr%d" % i, [128, 8, 128], BF16), "ptr%d" % i) for i in range(2)]
    pbr = [Res("pb%d" % i) for i in range(6)]

    ident = pers("ident", [128, 128], BF16)
    identf = pers("identf", [128, 128], F32)
    onesf = pers("onesf", [128, 128], F32)
    aff = pers("aff", [128, NT, NE], F32)
    wsel = pers("wsel", [128, NT, NE], F32)
    modT = pers("modT", [128, 48], F32)
    modcT = pers("modcT", [128, 48], F32)
    esink = pers("esink", [128, 6], F32)
    convw = pers("convw", [128, 2, 3], F32)
    amask = pers("amask", [128, 2, 384], BF16)
    eps_t = pers("eps", [128, 1], F32)
    ltri = pers("ltri", [128, 128], F32)
    idxT = pers("idxT", [128, NE, 64], I32)
    st["pers_end"] = st["pers"]
    st["ph"] = st["pers_end"]

    _breg = {}

    def breg(e):
        if "r" not in _breg:
            _breg["r"] = e.to_reg(1023)
        return _breg["r"]

    def rs(lst):
        return [x.r if isinstance(x, Tl) else x for x in lst]

    def fsz(ap):
        try:
            return float(ap.free_size())
        except Exception:
            return 512.0

    def mm(out, lhsT, rhs, start, stop, reads, writes, tr=False):
        if tr:
            S.op("pe", lambda e: e.matmul(out, lhsT=lhsT, rhs=rhs, is_transpose=True),
                 reads=rs(reads), writes=rs(writes), pe_acc=True, cost=0.08)
        else:
            c = max(fsz(rhs), 64.0) / 2000.0 * (4.0 if lhsT.dtype == F32 else 1.0) + 0.03
            S.op("pe", lambda e: e.matmul(out, lhsT=lhsT, rhs=rhs, start=start, stop=stop),
                 reads=rs(reads), writes=rs(writes), pe_acc=True, cost=c)

    def act(out, in_, func, reads, writes, bias=0.0, scale=1.0, accum=None):
        if accum is None:
            S.op("act", lambda e: e.activation(out=out, in_=in_, func=func, bias=bias, scale=scale),
                 reads=rs(reads), writes=rs(writes), cost=0.2 + fsz(out) / 1300.0)
        else:
            S.op("act", lambda e: e.activation(out=out, in_=in_, func=func, bias=bias, scale=scale,
                                               accum_out=accum), reads=rs(reads), writes=rs(writes))

    def vcost(eng, ap):
        return (0.1 + fsz(ap) / 900.0) if eng == "dve" else (0.2 + fsz(ap) / 450.0)

    def tt(out, in0, in1, op, reads, writes, eng="dve"):
        S.op(eng, lambda e: e.tensor_tensor(out=out, in0=in0, in1=in1, op=op), reads=rs(reads), writes=rs(writes),
             cost=vcost(eng, out))

    def ts(out, in0, s1, s2, op0, op1, reads, writes, eng="dve"):
        if op1 is None:
            S.op(eng, lambda e: e.tensor_scalar(out=out, in0=in0, scalar1=s1, scalar2=None, op0=op0),
                 reads=rs(reads), writes=rs(writes), cost=vcost(eng, out))
        else:
            S.op(eng, lambda e: e.tensor_scalar(out=out, in0=in0, scalar1=s1, scalar2=s2, op0=op0, op1=op1),
                 reads=rs(reads), writes=rs(writes), cost=vcost(eng, out))

    def stt(out, in0, scalar, in1, op0, op1, reads, writes):
        S.op("dve", lambda e: e.scalar_tensor_tensor(out=out, in0=in0, scalar=scalar, in1=in1, op0=op0, op1=op1),
             reads=rs(reads), writes=rs(writes), cost=vcost("dve", out))

    def cp(out, in_, reads, writes, eng="dve"):
        S.op(eng, lambda e: e.tensor_copy(out=out, in_=in_), reads=rs(reads), writes=rs(writes), cost=vcost(eng, out))

    def recip(out, in_, reads, writes):
        S.op("dve", lambda e: e.reciprocal(out=out, in_=in_), reads=rs(reads), writes=rs(writes))

    def reduce(out, in_, op, reads, writes, negate=False):
        S.op("dve", lambda e: e.tensor_reduce(out=out, in_=in_, axis=AX.X, op=op, negate=negate),
             reads=rs(reads), writes=rs(writes), cost=vcost("dve", in_))

    def memset(eng, ap, val, writes):
        S.op(eng, lambda e: e.memset(ap, val), writes=rs(writes), cost=vcost(eng, ap))

    def single(out, in_, scalar, op, reads, writes):
        S.op("dve", lambda e: e.tensor_single_scalar(out=out, in_=in_, scalar=scalar, op=op),
             reads=rs(reads), writes=rs(writes))

    def scan(out, d0, d1, reads, writes):
        S.op("dve", lambda e: e.tensor_tensor_scan(out=out, data0=d0, data1=d1, initial=0.0, op0=ALU.add, op1=ALU.add),
             reads=rs(reads), writes=rs(writes))

    def iota_tail(ap, base, writes):
        S.op("pool", lambda e: e.iota(ap, pattern=[[0, 1]], base=base, channel_multiplier=1), writes=rs(writes))

    def dma(q, out, in_, reads, writes, owner, slow=False, group=None):
        try:
            nbytes = float(out.nbytes())
        except Exception:
            nbytes = 1.0e5
        lat = 2.5 + nbytes / 1.5e5
        cost = 0.1 if q == "sp" else 1.0
        ow = owner.r if isinstance(owner, Tl) else owner
        if group is not None:
            return S.dma(q, lambda e: e.dma_start(out=out, in_=in_), reads=rs(reads), writes=rs(writes),
                         owner=ow, group=group, cost=cost, lat=lat)
        if slow:
            return S.dma(q, lambda e: e.dma_start(out=out, in_=in_, allow_slow_non_contiguous=True),
                         reads=rs(reads), writes=rs(writes), owner=ow, cost=cost, lat=lat + 3.0)
        return S.dma(q, lambda e: e.dma_start(out=out, in_=in_),
                     reads=rs(reads), writes=rs(writes), owner=ow, cost=cost, lat=lat)

    def ln_stats(src_ap, src_res, stats, mv, rstd, nmr=None):
        for h in range(2):
            S.op("dve", lambda e, h=h: e.bn_stats(out=stats[:, h, :], in_=src_ap[:, h * 512:(h + 1) * 512]),
                 reads=rs([src_res]), writes=rs([stats]))
        S.op("dve", lambda e: e.bn_aggr(out=mv[:], in_=stats[:].rearrange("p a b -> p (a b)")),
             reads=rs([stats]), writes=rs([mv]))
        act(rstd[:], mv[:, 1:2], AF.Sqrt, [mv, eps_t], [rstd], bias=eps_t[:, 0:1])
        S.op("dve", lambda e: e.reciprocal(out=rstd[:], in_=rstd[:]), reads=rs([rstd]), writes=rs([rstd]))
        if nmr is not None:
            stt(nmr[:], mv[:, 0:1], -1.0, rstd[:], ALU.mult, ALU.mult, [mv, rstd], [nmr])

    S.op("pool", lambda e: e.iota(identf[:], pattern=[[1, 128]], base=0, channel_multiplier=-1,
                                  allow_small_or_imprecise_dtypes=True), writes=rs([identf]))
    S.op("dve", lambda e: e.tensor_single_scalar(out=ident[:], in_=identf[:], scalar=0.0, op=ALU.is_equal),
         reads=rs([identf]), writes=rs([ident]))
    S.op("dve", lambda e: e.memset(onesf[:], 1.0), writes=rs([onesf]))
    S.op("dve", lambda e: e.tensor_single_scalar(out=ltri[:], in_=identf[:], scalar=0.0, op=ALU.is_gt),
         reads=rs([identf]), writes=rs([ltri]))
    S.op("dve", lambda e: e.memset(eps_t[:], 1e-6), writes=rs([eps_t]))
    dma("pool", amask[:], AMASK, [S.dres("amask")], [amask], amask)

    fm_res = [S.dres("fm", t) for t in range(NT)]
    vv_res = [S.dres("vv", t) for t in range(NT)]
    xmid_res = [S.dres("xmid", t) for t in range(NT)]
    h2t_res = [S.dres("h2t", t) for t in range(NT)]
    xcur_res = [S.dres("xcur", t) for t in range(NT)]
    xh2_res = [S.dres("xh2", t) for t in range(NT)]
    y_res = [S.dres("y", t) for t in range(64)]
    final = []

    for l in range(depth_run):
        last = l == DEPTH - 1
        T0 = 2 if last else 0
        new_phase()
        cT = ph("cT", [128, 8, 2])
        bm = ph("bm", [2, 6 * D])
        mrow = ph("mrow", [2, 6 * D])
        wm = [ph("wm%d" % i, [128, 8, 512]) for i in range(2)]
        for m_ in range(2):
            dma("sp", cT[:, :, m_], CVEC[m_].rearrange("(k p) -> p k", p=128), [S.dres("cvec")], [cT], cT, slow=True)
        dma("sp", bm[:], BMOD[l].partition_broadcast(2), [S.dres("bmod")], [bm], bm)
        dma("sp", esink[:], SINK[l].partition_broadcast(128), [S.dres("sink")], [esink], esink)
        dma("sp", convw[:], CONVW[l], [S.dres("convw")], [convw], convw)
        act(cT[:], cT[:], AF.Silu, [cT], [cT])
        act(esink[:], esink[:], AF.Exp, [esink], [esink])
        wmv = WMOD[l].rearrange("(k p) n -> p k n", p=128)
        for cc in range(12):
            w_ = wm[cc % 2]
            dma("sp", w_[:], wmv[:, :, cc * 512:(cc + 1) * 512], [S.dres("wmod")], [w_], w_)
            pb = pbig[0:2, cc % 2, :]
            for k in range(8):
                mm(pb, cT[:, k, :], w_[:, k, :], k == 0, k == 7, [cT, w_], [pbr[cc % 2]])
            tt(mrow[:, cc * 512:(cc + 1) * 512], pb, bm[:, cc * 512:(cc + 1) * 512], ALU.add,
               [pbr[cc % 2], bm], [mrow])
        dma("sp", MODROW, mrow[:], [mrow], [S.dres("modrow")], mrow)
        dma("sp", modT[:], MODROW[0].rearrange("(j p) -> p j", p=128), [S.dres("modrow")], [modT], modT, slow=True)
        dma("sp", modcT[:], MODROW[1].rearrange("(j p) -> p j", p=128), [S.dres("modrow")], [modcT], modcT, slow=True)
        for m_ in (modT, modcT):
            ts(m_[:, 8:16], m_[:, 8:16], 1.0, None, ALU.add, None, [m_], [m_])
            ts(m_[:, 32:40], m_[:, 32:40], 1.0, None, ALU.add, None, [m_], [m_])

        new_phase()
        win = ph("win", [128, 8, 3072], BF16)
        wiv = WIN[l].rearrange("(k p) n -> p k n", p=128)
        for k in range(8):
            dma("pool", win[:, k, :], wiv[:, k, :], [S.dres("win")], [win], win)
        xt = [ph("xt%d" % i, [128, D]) for i in range(2)]
        xh = [ph("xh%d" % i, [128, D], BF16) for i in range(2)]
        hT = [ph("hT%d" % i, [128, 8, 512], BF16) for i in range(2)]
        fmo = [ph("fmo%d" % i, [128, 14, 512], BF16) for i in range(2)]
        vo = [ph("vo%d" % i, [128, 4, 8, 65], BF16) for i in range(2)]
        rp = [ph("rp%d" % i, [128, 4, 512]) for i in range(2)]
        tmp = [ph("tmp%d" % i, [128, 512]) for i in range(3)]
        stats = ph("stats", [128, 2, 6]); mv = ph("mv", [128, 2]); rstd = ph("rstd", [128, 1])
        for v_ in vo:
            S.op("pool", lambda e, v_=v_: e.memset(v_[:], 1.0), writes=rs([v_]))
        src = XIN if l == 0 else XCUR
        src_res = (lambda t: S.dres("xin", t)) if l == 0 else (lambda t: xcur_res[t])
        groups = [(0, 2)] + [(2 + 4 * i, 4) for i in range(16)]
        bank = [0]

        def nextbank():
            b = bank[0]
            bank[0] = (b + 1) % 6
            return b

        def gen_L(gi, t0, nt):
            N = nt * 128
            sl = gi % 2
            mT = modcT if gi == 0 else modT
            rpt = rp[sl]
            dma("sp", rpt[:, :, :N], ROPE[:, :, t0 * 128:t0 * 128 + N].rearrange("a p n -> p a n"),
                [S.dres("rope")], [rpt], rpt)
            for s in range(nt):
                tl = t0 + s
                x_ = xt[tl % 2]; xh_ = xh[tl % 2]; pt_ = ptr[tl % 2]
                dma("sp", x_[:], src[tl * 128:(tl + 1) * 128, :], [src_res(tl)], [x_], x_)
                ln_stats(x_, x_, stats, mv, rstd)
                yield
                ts(xh_[:], x_[:], mv[:, 0:1], rstd[:, 0:1], ALU.subtract, ALU.mult, [x_, mv, rstd], [xh_])
                for k in range(8):
                    mm(pt_[:, k, :], xh_[:, k * 128:(k + 1) * 128], ident[:], True, True, [xh_, ident], [pt_], tr=True)
                yield
                for k in range(8):
                    act(hT[sl][:, k, s * 128:(s + 1) * 128], pt_[:, k, :], AF.Identity, [pt_, mT], [hT[sl]],
                        bias=mT[:, k:k + 1], scale=mT[:, 8 + k:9 + k])
                    if k % 4 == 3:
                        yield

        def gen_P(gi, t0, nt):
            N = nt * 128
            sl = gi % 2
            rpt = rp[sl]
            h_ = hT[sl]; fo = fmo[sl]

            def proj(ch):
                b = nextbank()
                for k in range(8):
                    mm(pbig[:, b, :N], win[:, k, ch * 128:(ch + 1) * 128], h_[:, k, :N], k == 0, k == 7,
                       [win, h_], [pbr[b]])
                return b

            def rope(chq, chs, tq, tsn, dst):
                b1 = proj(chq); b2 = proj(chs)
                tt(tmp[0][:, :N], pbig[:, b1, :N], rpt[:, tq, :N], ALU.mult, [pbr[b1], rpt], [tmp[0]])
                tt(tmp[1][:, :N], pbig[:, b2, :N], rpt[:, tsn, :N], ALU.mult, [pbr[b2], rpt], [tmp[1]])
                tt(fo[:, dst, :N], tmp[0][:, :N], tmp[1][:, :N], ALU.add, [tmp[0], tmp[1]], [fo], eng="pool")

            for c_ in range(3):
                rope(c_, 3 + c_, 0, 1, c_)
                yield
            rope(6, 7, 2, 3, 6)
            yield
            for c_ in range(2):
                b1 = proj(8 + c_)
                act(tmp[2][:, :N], pbig[:, b1, :N], AF.Identity, [pbr[b1]], [tmp[2]])
                b2 = proj(12 + c_)
                tt(fo[:, 10 + c_, :N], pbig[:, b2, :N], tmp[2][:, :N], ALU.mult, [pbr[b2], tmp[2]], [fo])
                yield
                b3 = proj(10 + c_)
                act(fo[:, 12 + c_, :N], pbig[:, b3, :N], AF.Identity, [pbr[b3]], [fo])
                yield
            for c_ in range(3):
                b1 = proj(14 + c_)
                act(fo[:, 3 + c_, :N], pbig[:, b1, :N], AF.Identity, [pbr[b1]], [fo], scale=0.125)
                yield
                b2 = proj(17 + c_)
                cp(fo[:, 7 + c_, :N], pbig[:, b2, :N], [pbr[b2]], [fo])
                yield
            for s in range(nt):
                b = nextbank()
                for k in range(8):
                    mm(pbig[:, b, :], h_[:, k, s * 128:(s + 1) * 128], win[:, k, 2560:3072], k == 0, k == 7,
                       [win, h_], [pbr[b]])
                act(vo[sl][:, s, :, 0:64], pbig[:, b, :].rearrange("p (h d) -> p h d", h=8), AF.Identity,
                    [pbr[b]], [vo[sl]])
                yield
            dma("sp", FM[:, :, t0 * 128:t0 * 128 + N], fo[:, :, :N], [fo], fm_res[t0:t0 + nt], fo)
            dma("sp", VV[t0 * 128:t0 * 128 + N].rearrange("(s p) h d -> p s h d", p=128), vo[sl][:, :nt],
                [vo[sl]], vv_res[t0:t0 + nt], vo[sl])
            yield

        prevP = None
        for gi, (t0, nt) in enumerate(groups):
            for _ in interleave([gen_L(gi, t0, nt), prevP]):
                pass
            prevP = gen_P(gi, t0, nt)
        for _ in prevP:
            pass

        new_phase()
        wout = ph("wout", [128, 8, D], BF16)
        wov = WOUT[l].rearrange("(k p) n -> p k n", p=128)
        for k in range(0, 8, 2):
            dma("pool", wout[:, k:k + 2, :], wov[:, k:k + 2, :], [S.dres("wout")], [wout], wout)
        wr = ph("wr", [128, 8, NE], BF16)
        dma("pool", wr[:], WR[l].rearrange("(k p) n -> p k n", p=128), [S.dres("wr")], [wr], wr)
        nabi = ph("nabi", [128, 6, 640])
        nabe = ph("nabe", [128, 6, 640])
        dma("sp", nabi[:], NAB[l, 0], [S.dres("nab")], [nabi], nabi)
        kctx = ph("kctx", [128, 4, 256], BF16)
        vctx = ph("vctx", [128, 2, 8, 65], BF16)
        dma("sp", kctx[:], FM[:, 6:10, 0:256], fm_res[0:2], [kctx], kctx)
        dma("sp", vctx[:], VV[0:256].rearrange("(s p) h d -> p s h d", p=128), vv_res[0:2], [vctx], vctx)
        g1b = ph("g1b", [128, D]); cg1b = ph("cg1b", [128, D])
        l1g = ph("l1g", [128, D]); l1b = ph("l1b", [128, D])
        dma("sp", g1b[:], MODROW[0, 2048:3072].partition_broadcast(128), [S.dres("modrow")], [g1b], g1b)
        dma("sp", cg1b[:], MODROW[1, 2048:3072].partition_broadcast(128), [S.dres("modrow")], [cg1b], cg1b)
        dma("sp", l1g[:], LN1G[l].partition_broadcast(128), [S.dres("ln1g")], [l1g], l1g)
        dma("sp", l1b[:], LN1B[l].partition_broadcast(128), [S.dres("ln1b")], [l1b], l1b)
        qw = [ph("qw%d" % i, [128, 6, 128], BF16) for i in range(2)]
        kw = [ph("kw%d" % i, [128, 4, 640], BF16) for i in range(2)]
        vw = [ph("vw%d" % i, [128, 5, 8, 65], BF16) for i in range(2)]
        uw = [ph("uw%d" % i, [128, 2, 130], BF16) for i in range(2)]
        bbw = [ph("bbw%d" % i, [128, 2, 128], BF16) for i in range(2)]
        xa = [ph("xa%d" % i, [128, D]) for i in range(2)]
        pta = [ph("pta%d" % i, [128, 5, 384], BF16) for i in range(2)]
        ptn = [ph("ptn%d" % i, [128, 896], BF16) for i in range(2)]
        sfn = [ph("sfn%d" % i, [128, 640]) for i in range(2)]
        mixc = ph("mixc", [128, 768], BF16)
        mixT = ph("mixT", [128, 8, 128], BF16)
        ctmp = ph("ctmp", [128, 128])
        den = ph("den", [128, 6]);
        t1 = ph("t1", [128, D]); y1 = ph("y1", [128, D]); xm = [ph("xm%d" % i, [128, D]) for i in range(2)]
        xh2 = ph("xh2", [128, RW], BF16)
        h2o = [ph("h2o%d" % i, [128, 8, 128], BF16) for i in range(2)]
        stats = ph("stats", [128, 2, 6]); mv = ph("mv", [128, 2]); rstd = ph("rstd", [128, 1]); nmr = ph("nmr", [128, 1])
        rmx = ph("rmx", [128, 1]); rsum = ph("rsum", [128, 1]); rexp = ph("rexp", [128, NE])

        mixT2 = [mixT, ph("mixTb", [128, 8, 128], BF16)]

        def gen_att(T):
            sl = T % 2
            is_ctx = T < 2
            i = T - 2
            q_ = qw[sl]; k_ = kw[sl]; v_ = vw[sl]; u_ = uw[sl]; bb_ = bbw[sl]
            mT_ = mixT2[sl]
            dma("sp", q_[:], FM[:, 0:6, T * 128:(T + 1) * 128], [fm_res[T]], [q_], q_)
            nb = nabi
            base = 0
            if not is_ctx:
                base = min(max(i - 2, 0), 59) + 2
                dma("sp", k_[:], FM[:, 6:10, base * 128:(base + 5) * 128], fm_res[base:base + 5], [k_], k_)
                dma("sp", v_[:], VV[base * 128:(base + 5) * 128].rearrange("(s p) h d -> p s h d", p=128),
                    vv_res[base:base + 5], [v_], v_)
                var = 0 if 2 <= i <= 61 else (1 + i if i < 2 else i - 59)
                if var != 0:
                    dma("sp", nabe[:], NAB[l, var], [S.dres("nab")], [nabe], nabe)
                    nb = nabe
            lo_pad = T in (0, 2); hi_pad = T in (1, NT - 1)
            if lo_pad or hi_pad:
                memset("pool", u_[:], 0.0, [u_])
            c0 = T * 128 - (0 if lo_pad else 1); c1 = (T + 1) * 128 + (0 if hi_pad else 1)
            o0 = 1 if lo_pad else 0
            fr = fm_res[max(T - 1, 0):min(T + 2, NT)]
            dma("sp", u_[:, :, o0:o0 + (c1 - c0)], FM[:, 10:12, c0:c1], fr, [u_], u_)
            dma("sp", bb_[:], FM[:, 12:14, T * 128:(T + 1) * 128], [fm_res[T]], [bb_], bb_)
            yield
            if is_ctx:
                akeys = [(("c", 0), None), (("c", 1), None)]
                nkeys = [("c", 0), ("c", 1)]
            else:
                akeys = []
                if i > 0:
                    akeys.append((("w", T - 1 - base), 0))
                akeys.append((("w", T - base), None))
                if i < 63:
                    akeys.append((("w", T + 1 - base), 1))
                akeys += [(("c", 0), None), (("c", 1), None)]
                nkeys = [("w", j) for j in range(5)] + [("c", 0), ("c", 1)]

            def kap(kt, ch, p0):
                if kt[0] == "c":
                    return kctx[p0:p0 + 64, ch, kt[1] * 128:(kt[1] + 1) * 128], kctx
                return k_[p0:p0 + 64, ch, kt[1] * 128:(kt[1] + 1) * 128], k_

            def vap(kt, head):
                if kt[0] == "c":
                    return vctx[:, kt[1], head, :], vctx
                return v_[:, kt[1], head, :], v_

            def gen_A():
                for g in range(2):
                    p0 = g * 64
                    pa_ = pta[g]
                    for ki, (kt, mk) in enumerate(akeys):
                        ka_, kr = kap(kt, 0, p0)
                        mm(pbig[:, 0, 0:384], ka_, q_[p0:p0 + 64, 0:3, :].rearrange("p a b -> p (a b)"), True, True,
                           [kr, q_], [pbr[0]])
                        act(pa_[:, ki, :], pbig[:, 0, 0:384], AF.Exp, [pbr[0]], [pa_])
                        if mk is not None:
                            tt(pa_[:, ki, :], pa_[:, ki, :], amask[:, mk, :], ALU.mult, [pa_, amask], [pa_])
                        yield
                    po = pbig[:, 2, 0:195].rearrange("p (c d) -> p c d", c=3)
                    for c_ in range(3):
                        for ki, (kt, mk) in enumerate(akeys):
                            va_, vr = vap(kt, g)
                            mm(po[:, c_, :], pa_[:, ki, c_ * 128:(c_ + 1) * 128], va_, ki == 0, ki == len(akeys) - 1,
                               [pa_, vr], [pbr[2]])
                        yield
                    tt(den[:, 0:3], po[:, :, 64], esink[:, 3 * g:3 * g + 3], ALU.add, [pbr[2], esink], [den])
                    recip(den[:, 0:3], den[:, 0:3], [den], [den])
                    tt(mixc[:, g * 192:(g + 1) * 192].rearrange("p (c d) -> p c d", c=3), po[:, :, 0:64],
                       den[:, 0:3].unsqueeze(2).to_broadcast([128, 3, 64]), ALU.mult, [pbr[2], den], [mixcA])
                    yield

            def gen_N():
                po2 = pbig[:, 3, 0:390].rearrange("p (c d) -> p c d", c=6)
                nk = len(nkeys)
                for h in range(6):
                    ch = h // 2; p0 = (h % 2) * 64
                    pn_ = ptn[h % 2]; sf_ = sfn[h % 2]
                    ps2 = pbig[:, 4:6, :].rearrange("p a b -> p (a b)")
                    for j, kt in enumerate(nkeys):
                        ka_, kr = kap(kt, 1 + ch, p0)
                        mm(ps2[:, j * 128:(j + 1) * 128], ka_, q_[p0:p0 + 64, 3 + ch, :], True, True,
                           [kr, q_], [pbr[4], pbr[5]])
                    if is_ctx:
                        act(pn_[:, 0:256], ps2[:, 0:256], AF.Exp, [pbr[4], pbr[5]], [pn_])
                    else:
                        tt(sf_[:], ps2[:, 0:640], nb[:, h, :], ALU.add, [pbr[4], pbr[5], nb], [sf_])
                        act(pn_[:, 640:896], ps2[:, 640:896], AF.Exp, [pbr[4], pbr[5]], [pn_])
                        act(pn_[:, 0:640], sf_[:], AF.Exp, [sf_], [pn_])
                    yield
                    for j, kt in enumerate(nkeys):
                        va_, vr = vap(kt, 2 + h)
                        mm(po2[:, h, :], pn_[:, j * 128:(j + 1) * 128], va_, j == 0, j == nk - 1, [pn_, vr], [pbr[3]])
                    yield
                recip(den2[:], po2[:, :, 64], [pbr[3]], [den2])
                tt(mixc[:, 384:768].rearrange("p (c d) -> p c d", c=6), po2[:, :, 0:64],
                   den2[:].unsqueeze(2).to_broadcast([128, 6, 64]), ALU.mult, [pbr[3], den2], [mixcN])
                yield

            def gen_B():
                for c_ in range(2):
                    ts(ctmp[:], u_[:, c_, 0:128], convw[:, c_, 0:1], None, ALU.mult, None, [u_, convw], [ctmp])
                    stt(ctmp[:], u_[:, c_, 1:129], convw[:, c_, 1:2], ctmp[:], ALU.mult, ALU.add, [u_, convw, ctmp], [ctmp])
                    stt(ctmp[:], u_[:, c_, 2:130], convw[:, c_, 2:3], ctmp[:], ALU.mult, ALU.add, [u_, convw, ctmp], [ctmp])
                    tt(mT_[:, 3 + c_, :], ctmp[:], bb_[:, c_, :], ALU.mult, [ctmp, bb_], [mT_])
                    yield

            for _ in interleave([gen_A(), gen_N(), gen_B()]):
                yield
            pt_ = ptr[0]
            for c_ in range(6):
                mm(pt_[:, c_, :], mixc[:, c_ * 128:(c_ + 1) * 128], ident[:], True, True, [mixcA, mixcN, ident], [pt_], tr=True)
            cp(mT_[:, 0:3, :], pt_[:, 0:3, :], [pt_], [mT_])
            act(mT_[:, 5:8, :], pt_[:, 3:6, :], AF.Identity, [pt_], [mT_])
            yield

        def gen_epi(T):
            sl = T % 2
            is_ctx = T < 2
            x_ = xa[sl]; mT_ = mixT2[sl]
            dma("sp", x_[:], src[T * 128:(T + 1) * 128, :], [src_res(T)], [x_], x_)
            gb = cg1b if is_ctx else g1b
            for hf in range(2):
                for k in range(8):
                    mm(pbig[:, 1, :], mT_[:, k, :], wout[:, k, hf * 512:(hf + 1) * 512], k == 0, k == 7,
                       [mT_, wout], [pbr[1]])
                tt(t1[:, hf * 512:(hf + 1) * 512], pbig[:, 1, :], gb[:, hf * 512:(hf + 1) * 512], ALU.mult,
                   [pbr[1], gb], [t1])
                yield
            stt(y1[:], x_[:], ALPHA, t1[:], ALU.mult, ALU.add, [x_, t1], [y1])
            yield
            ln_stats(y1, y1, stats, mv, rstd, nmr)
            yield
            xm_ = xm[sl]
            act(t1[:], y1[:], AF.Identity, [y1, rstd, nmr], [t1], bias=nmr[:, 0:1], scale=rstd[:, 0:1])
            yield
            tt(t1[:], t1[:], l1g[:], ALU.mult, [t1, l1g], [t1], eng="pool")
            yield
            tt(xm_[:], t1[:], l1b[:], ALU.add, [t1, l1b], [xm_], eng="pool")
            dma("sp", XMID[T * 128:(T + 1) * 128, :], xm_[:], [xm_], [xmid_res[T]], xm_)
            yield
            ln_stats(xm_, xm_, stats, mv, rstd)
            yield
            ts(xh2[:, 0:D], xm_[:], mv[:, 0:1], rstd[:, 0:1], ALU.subtract, ALU.mult, [xm_, mv, rstd], [xh2])
            yield
            pt2 = ptr[1]
            for k in range(8):
                mm(pt2[:, k, :], xh2[:, k * 128:(k + 1) * 128], ident[:], True, True, [xh2, ident], [pt2], tr=True)
            mT = modcT if is_ctx else modT
            h2_ = h2o[sl]
            for k in range(8):
                act(h2_[:, k, :], pt2[:, k, :], AF.Identity, [pt2, mT], [h2_],
                    bias=mT[:, 24 + k:25 + k], scale=mT[:, 32 + k:33 + k])
                if k % 4 == 3:
                    yield
            if is_ctx:
                dma("sp", H2T[:, :, T * 128:(T + 1) * 128], h2_[:], [h2_], [h2t_res[T]], h2_)
            pr = pbig[:, 1, 0:NE]
            for k in range(8):
                mm(pr, h2_[:, k, :], wr[:, k, :], k == 0, k == 7, [h2_, wr], [pbr[1]])
            reduce(rmx[:], pr, ALU.max, [pbr[1]], [rmx], negate=True)
            act(rexp[:], pr, AF.Exp, [pbr[1], rmx], [rexp], bias=rmx[:, 0:1])
            yield
            reduce(rsum[:], rexp[:], ALU.add, [rexp], [rsum])
            recip(rsum[:], rsum[:], [rsum], [rsum])
            ts(aff[:, T, :], rexp[:], rsum[:, 0:1], None, ALU.mult, None, [rexp, rsum], [aff])
            if not is_ctx:
                ts(xh2[:, 1026:1042], rexp[:], rsum[:, 0:1], None, ALU.mult, None, [rexp, rsum], [xh2])
                stt(xh2[:, 1042:1058], rexp[:], rsum[:, 0:1], xh2[:, 1026:1042], ALU.mult, ALU.subtract,
                    [rexp, rsum, xh2], [xh2])
                iota_tail(xh2[:, 1024:1026].bitcast(I32), T * 128, [xh2])
                dma("sp", XH2[T * 128:(T + 1) * 128, :], xh2[:], [xh2], [xh2_res[T]], xh2)
            yield

        mixcA = Res("mixcA"); mixcN = Res("mixcN")
        den2 = ph("den2", [128, 6])
        prev = None
        for T in range(T0, NT):
            for _ in interleave([gen_att(T), prev]):
                pass
            prev = gen_epi(T)
        for _ in prev:
            pass

        new_phase()
        lo_t = ph("lo", [128, NE]); mid_t = ph("mid", [128, NE]); cntp = ph("cntp", [128, NE]); sel = ph("sel", [128, NE])
        cmp = ph("cmp", [128, NE, 64])
        incl = ph("incl", [128, NE, 64])
        zt = ph("zt", [128, 64])
        offs = ph("offs", [128, NE])
        zbig = ph("zbig", [128, 4096])
        memset("pool", zbig[:], 0.0, [zbig])
        memset("pool", zt[:], 0.0, [zt])
        for j in range(16):
            dma("sp", FFN[256 + j * 512:256 + (j + 1) * 512, :].rearrange("(p a) d -> p (a d)", p=128), zbig[:],
                [zbig], [S.dres("ffn")], zbig, group=("ffnz", l))
        sets = [(2, 64, 1024.0)] if last else [(0, 2, 32.0), (2, 64, 1024.0)]
        for (ta, tn, cap) in sets:
            av = aff[:, ta:ta + tn, :].rearrange("p t e -> p e t")
            memset("dve", lo_t[:], 0.0, [lo_t])
            for it in range(30):
                w_ = 0.5 ** (it + 1)
                ts(mid_t[:], lo_t[:], w_, None, ALU.add, None, [lo_t], [mid_t])
                tt(cmp[:, :, :tn], av, mid_t[:].unsqueeze(2).to_broadcast([128, NE, tn]), ALU.is_ge, [aff, mid_t], [cmp])
                reduce(cntp[:], cmp[:, :, :tn], ALU.add, [cmp], [cntp])
                mm(pbig[:, 0, 0:NE], onesf[:], cntp[:], True, True, [onesf, cntp], [pbr[0]])
                single(sel[:], pbig[:, 0, 0:NE], cap - 0.5, ALU.is_ge, [pbr[0]], [sel])
                stt(lo_t[:], sel[:], w_, lo_t[:], ALU.mult, ALU.add, [sel, lo_t], [lo_t])
            tt(cmp[:, :, :tn], av, lo_t[:].unsqueeze(2).to_broadcast([128, NE, tn]), ALU.is_ge, [aff, lo_t], [cmp])
            if dbg:
                LDBG = nc.dram_tensor("ldbg%d_%d" % (l, tn), [128, NE], F32, kind="ExternalOutput").ap()
                dma("sp", LDBG, lo_t[:], [lo_t], [S.dres("ldbg", tn)], lo_t)
                ADBG = nc.dram_tensor("adbg%d_%d" % (l, tn), [128, NT * NE], F32, kind="ExternalOutput").ap()
                dma("sp", ADBG, aff[:].rearrange("p a b -> p (a b)"), [aff], [S.dres("adbg", tn)], aff)
            if tn == 2:
                wv = wsel[:, ta:ta + tn, :].rearrange("p t e -> p e t")
                tt(wv, av, cmp[:, :, :tn], ALU.mult, [aff, cmp], [wsel])
                continue
            for ex in range(NE):
                scan(incl[:, ex, :], cmp[:, ex, :], zt[:], [cmp, zt], [incl])
            cp(cntp[:], incl[:, :, 63], [incl], [cntp])
            mm(pbig[:, 0, 0:NE], ltri[:], cntp[:], True, True, [ltri, cntp], [pbr[0]])
            cp(offs[:], pbig[:, 0, 0:NE], [pbr[0]], [offs])
            tt(incl[:], incl[:], cmp[:], ALU.subtract, [incl, cmp], [incl])
            tt(incl[:], incl[:], offs[:].unsqueeze(2).to_broadcast([128, NE, 64]), ALU.add, [incl, offs], [incl])
            stt(incl[:].rearrange("p a b -> p (a b)"), incl[:].rearrange("p a b -> p (a b)"), -1.0e6,
                cmp[:].rearrange("p a b -> p (a b)"), ALU.add, ALU.mult, [incl, cmp], [incl])
            ts(idxT[:], incl[:], 1.0e6, None, ALU.add, None, [incl], [idxT])
            if dbg:
                IDBG = nc.dram_tensor("idbg%d" % l, [128, NE * 64], I32, kind="ExternalOutput").ap()
                dma("sp", IDBG, idxT[:].rearrange("p a b -> p (a b)"), [idxT], [S.dres("idbg")], idxT)
                ODBG = nc.dram_tensor("odbg%d" % l, [128, NE], F32, kind="ExternalOutput").ap()
                dma("sp", ODBG, offs[:], [offs], [S.dres("odbg")], offs)
                CDBG = nc.dram_tensor("cdbg%d" % l, [128, NE * 64], F32, kind="ExternalOutput").ap()
                dma("sp", CDBG, cmp[:].rearrange("p a b -> p (a b)"), [cmp], [S.dres("cdbg")], cmp)

        new_phase()
        wgt = [ph("wg%d" % i, [128, 8, 512], BF16) for i in range(2)]
        wut = [ph("wu%d" % i, [128, 8, 512], BF16) for i in range(2)]
        wdt = [ph("wd%d" % i, [128, 4, D], BF16) for i in range(2)]
        tokc = [ph("tokc%d" % i, [128, 8, RW], BF16) for i in range(2)]
        xet = [ph("xet%d" % i, [128, RW], BF16) for i in range(2)]
        h2e = [ph("h2e%d" % i, [128, 8, 512], BF16) for i in range(2)]
        gT = [ph("gT%d" % i, [128, 4, 512], BF16) for i in range(2)]
        sa = [ph("sa%d" % i, [128, 512], BF16) for i in range(2)]
        yo = [ph("yo%d" % i, [128, D]) for i in range(2)]
        idxe = [ph("idxe%d" % i, [128, 1], I32) for i in range(8)]
        gate = ph("gate", [128, 8])
        rmx = ph("rmx", [128, 1]); rsum = ph("rsum", [128, 1]); rexp = ph("rexp", [128, NE])
        wr = ph("wr", [128, 8, NE], BF16)
        dma("pool", wr[:], WR[l].rearrange("(k p) n -> p k n", p=128), [S.dres("wr")], [wr], wr)
        h2g = ph("h2g", [128, 8, 256], BF16)
        acc = ph("acc", [128, 2, D])
        accr = [Res("acc%d" % i) for i in range(2)]
        if not last:
            dma("sp", h2g[:], H2T[:, :, 0:256], h2t_res[0:2], [h2g], h2g)
        xe_res = [S.dres("xe", e) for e in range(NE)]
        tcnt = [0]

        def load_w(ex):
            sl = ex % 2
            dma("pool", wgt[sl][:], WG[l, ex].rearrange("(k p) f -> p k f", p=128), [S.dres("wg")], [wgt[sl]], wgt[sl])
            dma("pool", wut[sl][:], WU[l, ex].rearrange("(k p) f -> p k f", p=128), [S.dres("wu")], [wut[sl]], wut[sl])
            dma("pool", wdt[sl][:], WD[l, ex].rearrange("(k p) f -> p k f", p=128), [S.dres("wd")], [wdt[sl]], wdt[sl])

        def dispatch(exs):
            for cg in range(8):
                tk = tokc[tcnt[0] % 2]
                tcnt[0] += 1
                dma("sp", tk[:], XH2[(2 + cg * 8) * 128:(2 + cg * 8 + 8) * 128, :].rearrange("(s p) d -> p s d", p=128),
                    xh2_res[2 + cg * 8:2 + cg * 8 + 8], [tk], tk)
                for s in range(8):
                    T = 2 + cg * 8 + s
                    for ex in exs:
                        S.dma("pool", lambda e, tk=tk, s=s, ex=ex, T=T: e.indirect_dma_start(
                            out=XE[ex][:, :], out_offset=bass.IndirectOffsetOnAxis(
                                ap=idxT[:].rearrange("p a b -> p (a b)")[:, ex * 64 + T - 2:ex * 64 + T - 1], axis=0),
                            in_=tk[:, s, :], in_offset=None, bounds_check=breg(e), oob_is_err=False),
                            reads=rs([tk, idxT]), writes=[xe_res[ex]], owner=tk.r, group=("xe", l, ex), cost=1.2, lat=4.0)

        egroups = [[0, 1], [2, 3, 4, 5], [6, 7, 8, 9], [10, 11, 12, 13], [14, 15]]
        gstart = {g[0]: gi for gi, g in enumerate(egroups)}
        gater = [Res("gate%d" % i) for i in range(8)]
        dispatch(egroups[0])
        load_w(0)
        load_w(1)

        def ctx_dense(ex, wg_, wu_, wd_):
                def ffn_chunk(h_src, N, g_):
                    for fc in range(4):
                        ba = 2 * (fc % 2); bu = ba + 1
                        for k in range(8):
                            mm(pbig[:, ba, :N], wg_[:, k, fc * 128:(fc + 1) * 128], h_src[0][:, k, :N], k == 0, k == 7,
                               [wg_, h_src[1]], [pbr[ba]])
                        for k in range(8):
                            mm(pbig[:, bu, :N], wu_[:, k, fc * 128:(fc + 1) * 128], h_src[0][:, k, :N], k == 0, k == 7,
                               [wu_, h_src[1]], [pbr[bu]])
                        s_ = sa[fc % 2]
                        act(s_[:, :N], pbig[:, ba, :N], AF.Silu, [pbr[ba]], [s_])
                        tt(g_[:, fc, :N], pbig[:, bu, :N], s_[:, :N], ALU.mult, [pbr[bu], s_], [g_])

                def down(g_, s):
                    for hf in range(2):
                        for fc in range(4):
                            mm(pbig[:, 4 + hf, :], g_[:, fc, s * 128:(s + 1) * 128], wd_[:, fc, hf * 512:(hf + 1) * 512],
                               fc == 0, fc == 3, [g_, wd_], [pbr[4 + hf]])
                    return pbig[:, 4:6, :]

                if not last:
                    g_ = gT[0]
                    ffn_chunk((h2g, h2g), 256, g_)
                    for s in range(2):
                        py = down(g_, s)
                        av_ = acc[:, s, :].rearrange("p (a b) -> p a b", a=2)
                        if ex == 0:
                            ts(av_, py, wsel[:, s, ex:ex + 1], None, ALU.mult, None, [pbr[4], pbr[5], wsel], [accr[s]])
                        else:
                            stt(av_, py, wsel[:, s, ex:ex + 1], av_, ALU.mult, ALU.add,
                                [pbr[4], pbr[5], wsel, accr[s]], [accr[s]])

        def gen_prep(ex, ci):
            h_ = h2e[ci]
            for s in range(4):
                st_ = ci * 4 + s
                x_ = xet[st_ % 2]
                dma("sp", x_[:], XE[ex][st_ * 128:(st_ + 1) * 128, :], [xe_res[ex]], [x_], x_)
                cp(idxe[st_][:], x_[:, 1024:1026].bitcast(I32), [x_], [idxe[st_]])
                tt(gate[:, st_:st_ + 1], x_[:, 1026 + ex:1027 + ex], x_[:, 1042 + ex:1043 + ex], ALU.add, [x_], [gater[st_]])
                pt_ = ptr[st_ % 2]
                for k in range(8):
                    mm(pt_[:, k, :], x_[:, k * 128:(k + 1) * 128], ident[:], True, True, [x_, ident], [pt_], tr=True)
                yield
                for k in range(8):
                    act(h_[:, k, s * 128:(s + 1) * 128], pt_[:, k, :], AF.Identity, [pt_, modT], [h_],
                        bias=modT[:, 24 + k:25 + k], scale=modT[:, 32 + k:33 + k])
                    if k % 4 == 3:
                        yield

        def gen_ffn(ex, ci, wg_, wu_, wd_):
            h_ = h2e[ci]
            g_ = gT[ci]
            for fc in range(4):
                ba = 2 * (fc % 2); bu = ba + 1
                for k in range(8):
                    mm(pbig[:, ba, :], wg_[:, k, fc * 128:(fc + 1) * 128], h_[:, k, :], k == 0, k == 7, [wg_, h_], [pbr[ba]])
                for k in range(8):
                    mm(pbig[:, bu, :], wu_[:, k, fc * 128:(fc + 1) * 128], h_[:, k, :], k == 0, k == 7, [wu_, h_], [pbr[bu]])
                s_ = sa[fc % 2]
                act(s_[:], pbig[:, ba, :], AF.Silu, [pbr[ba]], [s_])
                tt(g_[:, fc, :], pbig[:, bu, :], s_[:], ALU.mult, [pbr[bu], s_], [g_])
                yield
            for s in range(4):
                st_ = ci * 4 + s
                for hf in range(2):
                    for fc in range(4):
                        mm(pbig[:, 4 + hf, :], g_[:, fc, s * 128:(s + 1) * 128], wd_[:, fc, hf * 512:(hf + 1) * 512],
                           fc == 0, fc == 3, [g_, wd_], [pbr[4 + hf]])
                y_ = yo[st_ % 2]
                act(y_[:].rearrange("p (a b) -> p a b", a=2), pbig[:, 4:6, :], AF.Identity, [pbr[4], pbr[5], gater[st_]], [y_],
                    scale=gate[:, st_:st_ + 1])
                S.dma("pool", lambda e, y_=y_, ie_=idxe[st_]: e.indirect_dma_start(
                    out=FFN[:, :], out_offset=bass.IndirectOffsetOnAxis(ap=ie_[:, :], axis=0),
                    in_=y_[:, :], in_offset=None, compute_op=ALU.add),
                    reads=rs([y_, idxe[st_]]), writes=[S.dres("ffn")], owner=y_.r, group=("ffn", l, ex), cost=1.2, lat=8.0)
                yield

        prev = None
        for ex in range(NE):
            sl = ex % 2
            if ex in gstart and gstart[ex] + 1 < len(egroups):
                dispatch(egroups[gstart[ex] + 1])
            ctx_dense(ex, wgt[sl], wut[sl], wdt[sl])
            for ci in range(2):
                for _ in interleave([gen_prep(ex, ci), prev]):
                    pass
                if ci == 0 and ex >= 1 and ex + 1 < NE:
                    load_w(ex + 1)
                prev = gen_ffn(ex, ci, wgt[sl], wut[sl], wdt[sl])
        for _ in prev:
            pass

        g2b = ph("g2b", [128, D]); cg2b = ph("cg2b", [128, D])
        l2g = ph("l2g", [128, D]); l2b = ph("l2b", [128, D])
        dma("sp", g2b[:], MODROW[0, 5120:6144].partition_broadcast(128), [S.dres("modrow")], [g2b], g2b)
        dma("sp", cg2b[:], MODROW[1, 5120:6144].partition_broadcast(128), [S.dres("modrow")], [cg2b], cg2b)
        dma("sp", l2g[:], LN2G[l].partition_broadcast(128), [S.dres("ln2g")], [l2g], l2g)
        dma("sp", l2b[:], LN2B[l].partition_broadcast(128), [S.dres("ln2b")], [l2b], l2b)
        xmt = [ph("xmt%d" % i, [128, D]) for i in range(2)]
        fft = [ph("fft%d" % i, [128, D]) for i in range(2)]
        ot = [ph("ot%d" % i, [128, D]) for i in range(2)]
        y2 = [ph("y2%d" % i, [128, D]) for i in range(2)]
        st2 = [(ph("stats", [128, 2, 6]), ph("mv", [128, 2]), ph("rstd", [128, 1]), ph("nmr", [128, 1])) for _ in range(2)]

        def gen_ln2(T):
            xm_ = xmt[T % 2]; o_ = ot[T % 2]; y2_ = y2[T % 2]
            stats, mv, rstd, nmr = st2[T % 2]
            dma("sp", xm_[:], XMID[T * 128:(T + 1) * 128, :], [xmid_res[T]], [xm_], xm_)
            if T < 2:
                tt(y2_[:], acc[:, T, :], cg2b[:], ALU.mult, [accr[T], cg2b], [y2_])
            else:
                f_ = fft[T % 2]
                dma("sp", f_[:], FFN[T * 128:(T + 1) * 128, :], [S.dres("ffn")], [f_], f_)
                tt(y2_[:], f_[:], g2b[:], ALU.mult, [f_, g2b], [y2_])
            yield
            stt(y2_[:], xm_[:], ALPHA, y2_[:], ALU.mult, ALU.add, [xm_, y2_], [y2_])
            yield
            ln_stats(y2_, y2_, stats, mv, rstd, nmr)
            yield
            act(y2_[:], y2_[:], AF.Identity, [y2_, rstd, nmr], [y2_], bias=nmr[:, 0:1], scale=rstd[:, 0:1])
            yield
            tt(y2_[:], y2_[:], l2g[:], ALU.mult, [y2_, l2g], [y2_], eng="pool")
            yield
            tt(o_[:], y2_[:], l2b[:], ALU.add, [y2_, l2b], [o_], eng="pool")
            if last:
                ev = dma("sp", Y[(T - 2) * 128:(T - 1) * 128, :], o_[:], [o_], [y_res[T - 2]], o_)
                final.append(ev)
            else:
                dma("sp", XCUR[T * 128:(T + 1) * 128, :], o_[:], [o_], [xcur_res[T]], o_)
            yield

        tl_ = list(range(T0, NT))
        for j in range(0, len(tl_), 2):
            for _ in interleave([gen_ln2(T) for T in tl_[j:j + 2]]):
                pass

    S.emit(final_waits=final, reorder=reorder, only=only)
    if dbg:
        print("instr counts", {e: len(v) for e, v in S.ins.items()}, "waits", S.nwaits, flush=True)
    return nc


def _rope_tables():
    t = np.arange(8192)
    row = (t // 64).astype(np.float32); col = (t % 64).astype(np.float32)
    inv = (10000.0 ** (-np.arange(0, 32, 2, dtype=np.float32) / 32)).astype(np.float32)
    cs = np.ones((64, TOK), np.float32); sn = np.zeros((64, TOK), np.float32)
    for a, pos in enumerate((row, col)):
        ang = (pos[:, None] * inv[None, :]).astype(np.float32)
        c = np.cos(ang).T; s = np.sin(ang).T
        cs[a * 32:a * 32 + 16, 256:] = c; cs[a * 32 + 16:a * 32 + 32, 256:] = c
        sn[a * 32:a * 32 + 16, 256:] = -s; sn[a * 32 + 16:a * 32 + 32, 256:] = s
    cs = np.concatenate([cs, cs], 0); sn = np.concatenate([sn, sn], 0)
    return np.stack([cs * 0.125, sn * 0.125, cs, sn]).astype(np.float32)


def _win_ext(w_in):
    qa = w_in[:, :, 0:384]; ka = w_in[:, :, 384:512]; va = w_in[:, :, 512:640]
    bx = w_in[:, :, 640:896]; bb = w_in[:, :, 896:1152]; bc = w_in[:, :, 1152:1408]
    qn = w_in[:, :, 1408:1792]; kn = w_in[:, :, 1792:2176]; vn = w_in[:, :, 2176:2560]
    sw = np.concatenate([np.arange(16, 32), np.arange(0, 16), np.arange(48, 64), np.arange(32, 48)])

    def heads_sw(w, nh):
        idx = np.concatenate([h * 64 + sw for h in range(nh)])
        return w[:, :, idx]

    def qperm(w):
        idx = np.concatenate([np.concatenate([np.arange(c * 64, c * 64 + 64), np.arange((3 + c) * 64, (3 + c) * 64 + 64)])
                              for c in range(3)])
        return w[:, :, idx]

    return np.ascontiguousarray(np.concatenate(
        [qperm(qa), qperm(heads_sw(qa, 6)), ka, heads_sw(ka, 2), bx, bb, bc, qn, kn, va, vn], axis=2))


def _na_bias(rpb):
    NEG = -30000.0
    out = np.full((DEPTH, 5, 6, 5, 128, 128), NEG, np.float32)
    cq = np.arange(64)
    col_start = np.clip(cq - 8, 0, 48)
    col_ok = (cq[None, :] >= col_start[:, None]) & (cq[None, :] < col_start[:, None] + 16)
    coff = np.clip(cq[None, :] - cq[:, None], -15, 15) + 15
    variants = [10, 0, 1, 62, 63]
    for vi, P in enumerate(variants):
        base = min(max(P - 2, 0), 59)
        for rho in range(2):
            r = 2 * P + rho
            rs_ = min(max(r - 4, 0), 120)
            for j in range(5):
                for kap in range(2):
                    kr = 2 * (base + j) + kap
                    if not (rs_ <= kr < rs_ + 8):
                        continue
                    roff = kr - r + 7
                    b = rpb[:, :, roff, :][:, :, coff]
                    b = np.where(col_ok[None, None], b, NEG)
                    out[:, vi, :, j, kap * 64:(kap + 1) * 64, rho * 64:(rho + 1) * 64] = np.transpose(b, (0, 1, 3, 2))
    out = np.transpose(out, (0, 1, 4, 2, 3, 5)).reshape(DEPTH, 5, 128, 6, 640)
    return np.ascontiguousarray(out)


def _amask():
    k = np.arange(128)[:, None]; q = np.arange(128)[None, :]
    mp = (k >= q).astype(np.float32); mn = (k <= q).astype(np.float32)
    return np.ascontiguousarray(np.stack([np.tile(mp, (1, 3)), np.tile(mn, (1, 3))], axis=1))


def make_in_maps(x, c, ctx, c_ctx, w_mod, b_mod, w_in, conv_w, attn_sink, na_rpb, w_out,
                 ln1_g, ln1_b, w_router, w_gate, w_up, w_down, ln2_g, ln2_b):
    f = lambda a: np.ascontiguousarray(np.asarray(a, dtype=np.float32))
    shared = dict(
        w_mod=f(w_mod), b_mod=f(b_mod), w_in=_win_ext(f(w_in)), rope=_rope_tables(),
        convw=np.ascontiguousarray(np.transpose(f(conv_w).reshape(DEPTH, 3, 2, 128), (0, 3, 2, 1))),
        sink=f(attn_sink), nab=_na_bias(f(na_rpb)), amask=_amask(), w_out=f(w_out),
        ln1_g=f(ln1_g), ln1_b=f(ln1_b), ln2_g=f(ln2_g), ln2_b=f(ln2_b), w_router=f(w_router),
        w_gate=f(w_gate), w_up=f(w_up), w_down=f(w_down))
    x = f(x); ctx = f(ctx); c = f(c); c_ctx = f(c_ctx)
    maps = []
    for b in range(N_CORES):
        m = dict(shared)
        m["xin"] = np.ascontiguousarray(np.concatenate([ctx[b], x[b]], axis=0))
        m["cvec"] = np.ascontiguousarray(np.stack([c[b], c_ctx], axis=0))
        maps.append(m)
    return maps


_NC = {}


def kernel(**inputs):
    if "nc" not in _NC:
        _NC["nc"] = build_nc(reorder=False)
    maps = make_in_maps(**inputs)
    res = run_bass_kernel_spmd(_NC["nc"], maps, core_ids=list(range(N_CORES)))
    return np.stack([np.asarray(r["y"], dtype=np.float32) for r in res.results], axis=0)
```

```python
import numpy as np
import concourse.bass as bass
import concourse.mybir as mybir
from concourse.bass_utils import run_bass_kernel_spmd

F32 = mybir.dt.float32
BF16 = mybir.dt.bfloat16
I32 = mybir.dt.int32
AF = mybir.ActivationFunctionType
ALU = mybir.AluOpType
AX = mybir.AxisListType

D = 1024
NT = 66
TOK = NT * 128
DEPTH = 2
ALPHA = float((2 * DEPTH) ** 0.25)
NE = 16
RW = 1024 + 2 + 32
COMPUTE = ("pe", "dve", "act", "pool")
N_CORES = 4


class Res:
    __slots__ = ("name", "w", "r", "dsem", "wg")

    def __init__(self, name="r"):
        self.name = name
        self.w = None
        self.r = []
        self.dsem = {}
        self.wg = None


class Sched:
    def __init__(self, nc):
        self.nc = nc
        self.ins = {e: [] for e in ("pe", "dve", "act", "pool", "sp")}
        self.dram = {}
        self.ndsem = 0
        self.owners = []
        self.free = {"sp": [], "pool": []}
        self.phase = 0
        self.phase_ev = {0: []}
        self.last_dma = {}
        self.pool_dmas = []
        self.last_pew = {}

    def dres(self, *key):
        r = self.dram.get(key)
        if r is None:
            r = self.dram[key] = Res(str(key))
        return r

    def _deps(self, eng, reads, writes, pe_acc, group=None):
        deps = []
        for r in reads:
            if r.w is not None:
                if isinstance(r.w, list):
                    deps.extend(r.w)
                else:
                    deps.append(r.w)
        for r in writes:
            if r.w is not None:
                if isinstance(r.w, list):
                    if not (group is not None and r.wg == group):
                        deps.extend(r.w)
                elif not (pe_acc and r.w[0] == "E" and r.w[1] == "pe" and eng == "pe"):
                    deps.append(r.w)
            deps.extend(r.r)
        return deps

    def _post(self, ev, reads, writes, group=None):
        for r in reads:
            r.r.append(ev)
        for r in writes:
            if group is not None and r.wg == group and isinstance(r.w, list):
                r.w.append(ev)
            else:
                r.w = [ev] if group is not None else ev
                r.wg = group
                r.r = []

    def op(self, eng, fn, reads=(), writes=(), pe_acc=False, cost=0.5):
        deps = self._deps(eng, reads, writes, pe_acc)
        idx = len(self.ins[eng])
        order = []
        if eng == "pe":
            for r in writes:
                p = self.last_pew.get(id(r))
                if p is not None:
                    order.append(p)
                self.last_pew[id(r)] = idx
        ev = ("E", eng, idx)
        self.ins[eng].append([fn, deps, None, self.phase, cost, order, cost])
        self._post(ev, reads, writes)
        return ev

    def dma(self, q, fn, reads=(), writes=(), owner=None, group=None, cost=0.1, lat=3.0):
        deps = self._deps(q, reads, writes, False, group)
        sc = owner.dsem.get(q)
        if sc is None:
            if self.free[q]:
                sc = list(self.free[q].pop())
            else:
                sc = [self.ndsem, 0]
                self.ndsem += 1
            owner.dsem[q] = sc
            self.owners.append((owner, q))
        sc[1] += 16
        ev = ("D", sc[0], sc[1])
        if q == "pool":
            if len(self.pool_dmas) >= 24:
                deps.append(self.pool_dmas[-24])
            self.pool_dmas.append(ev)
        idx = len(self.ins[q])
        order = []
        p = self.last_dma.get(sc[0])
        if p is not None:
            order.append(p)
        self.last_dma[sc[0]] = idx
        self.ins[q].append([fn, deps, sc[0], self.phase, cost, order, cost + lat, ev])
        self._post(ev, reads, writes, group)
        return ev

    def barrier(self):
        evs = []
        for e in COMPUTE:
            for i in range(len(self.ins[e]) - 1, -1, -1):
                if self.ins[e][i][2] is None:
                    evs.append(("E", e, i))
                    break
        for (o, q) in self.owners:
            sc = o.dsem.pop(q)
            evs.append(("D", sc[0], sc[1]))
            self.free[q].append((sc[0], sc[1]))
        self.owners = []
        self.phase += 1
        self.phase_ev[self.phase] = evs

    def _schedule(self, only=None):
        import heapq
        engs = list(self.ins.keys())
        dprod = {}
        for q in ("sp", "pool"):
            for i, rec in enumerate(self.ins[q]):
                if rec[2] is not None:
                    dprod[(rec[7][1], rec[7][2])] = (q, i)
        fin = {}
        issue = {}
        order = {e: [] for e in engs}
        ptr0 = {e: 0 for e in engs}
        tnow = 0.0
        for ph in range(self.phase + 1):
            nodes = []
            for e in engs:
                lst = self.ins[e]
                i = ptr0[e]
                while i < len(lst) and lst[i][3] == ph:
                    nodes.append((e, i))
                    i += 1
                ptr0[e] = i
            if not nodes:
                continue
            if only is not None and ph not in only:
                for (e, i) in nodes:
                    order[e].append(i)
                continue
            inph = set(nodes)
            ndep = {}
            users = {}
            for (e, i) in nodes:
                rec = self.ins[e][i]
                preds = set()
                for d in rec[1]:
                    p = (d[1], d[2]) if d[0] == "E" else dprod.get((d[1], d[2]))
                    if p is not None and p in inph and p != (e, i):
                        preds.add((p, 0))
                for j in rec[5]:
                    if (e, j) in inph:
                        preds.add(((e, j), 1))
                ndep[(e, i)] = len(preds)
                for pk in preds:
                    users.setdefault(pk[0], []).append(((e, i), pk[1]))
            ready = {}
            heaps = {e: [] for e in engs}
            avail = {e: [] for e in engs}
            efree = {e: tnow for e in engs}
            for n in nodes:
                ready[n] = tnow
                if ndep[n] == 0:
                    heapq.heappush(heaps[n[0]], (tnow, n[1]))
            left = len(nodes)
            tmax = tnow
            while left:
                best = None
                for e in engs:
                    if avail[e]:
                        st_ = efree[e]
                    elif heaps[e]:
                        st_ = max(heaps[e][0][0], efree[e])
                    else:
                        continue
                    if best is None or st_ < best[0]:
                        best = (st_, e)
                st_, e = best
                while heaps[e] and heaps[e][0][0] <= st_:
                    heapq.heappush(avail[e], heapq.heappop(heaps[e])[1])
                i = heapq.heappop(avail[e])
                rec = self.ins[e][i]
                issue[(e, i)] = st_
                efree[e] = st_ + rec[4]
                f_ = st_ + rec[6]
                fin[(e, i)] = f_
                tmax = max(tmax, f_)
                order[e].append(i)
                left -= 1
                for (u, kind) in users.get((e, i), ()):
                    t_ = f_ if kind == 0 else st_
                    if t_ > ready[u]:
                        ready[u] = t_
                    ndep[u] -= 1
                    if ndep[u] == 0:
                        heapq.heappush(heaps[u[0]], (ready[u], u[1]))
            tnow = tmax
        self.sim_time = tnow
        return order

    def _check(self, order, val):
        sems = {}
        pos = {e: 0 for e in self.ins}
        curph = {e: 0 for e in self.ins}
        progress = True
        total = sum(len(v) for v in self.ins.values())
        done = 0
        while progress:
            progress = False
            for e in self.ins:
                while pos[e] < len(order[e]):
                    i = order[e][pos[e]]
                    rec = self.ins[e][i]
                    deps = list(rec[1])
                    if rec[3] != curph[e]:
                        for p in range(curph[e] + 1, rec[3] + 1):
                            deps.extend(self.phase_ev.get(p, ()))
                    ok = True
                    for d in deps:
                        if d[0] == "E":
                            if sems.get(("E", d[1]), 0) < val[d[1]][d[2]]:
                                ok = False
                                break
                        elif sems.get(("D", d[1]), 0) < d[2]:
                            ok = False
                            break
                    if not ok:
                        break
                    curph[e] = rec[3]
                    if rec[2] is not None:
                        sems[("D", rec[2])] = sems.get(("D", rec[2]), 0) + 16
                    elif i in val.get(e, {}):
                        sems[("E", e)] = val[e][i]
                    pos[e] += 1
                    done += 1
                    progress = True
        if done != total:
            msg = []
            for e in self.ins:
                if pos[e] < len(order[e]):
                    i = order[e][pos[e]]
                    msg.append((e, pos[e], i, self.ins[e][i][3], self.ins[e][i][1][:6]))
            raise RuntimeError("schedule deadlock: %s" % msg)

    def emit(self, final_waits=(), reorder=True, only=None):
        import contextlib
        nc = self.nc
        if reorder:
            order = self._schedule(only)
        else:
            order = {e: list(range(len(l))) for e, l in self.ins.items()}
        for e in self.ins:
            assert sorted(order[e]) == list(range(len(self.ins[e]))), e
        lastc = {}
        for e in COMPUTE:
            cur = None
            per = {}
            for i in order[e]:
                if self.ins[e][i][2] is None:
                    per[self.ins[e][i][3]] = i
            lastc[e] = per
        for p in list(self.phase_ev.keys()):
            evs = [d for d in self.phase_ev[p] if d[0] == "D"]
            for e in COMPUTE:
                qs = [q for q in lastc[e] if q < p]
                if qs:
                    evs.append(("E", e, lastc[e][max(qs)]))
            self.phase_ev[p] = evs
        need = {e: set() for e in COMPUTE}
        for e, lst in self.ins.items():
            for rec in lst:
                for d in rec[1]:
                    if d[0] == "E":
                        need[d[1]].add(d[2])
        for evs in self.phase_ev.values():
            for d in evs:
                if d[0] == "E":
                    need[d[1]].add(d[2])
        val = {}
        for e in COMPUTE:
            c = 0
            v = {}
            for i in order[e]:
                if i in need[e]:
                    c += 1
                    v[i] = c
            val[e] = v
        self._check(order, val)
        self.nwaits = {}
        with contextlib.ExitStack() as st:
            esem = {e: st.enter_context(nc.semaphore("s_" + e)) for e in COMPUTE}
            dsem = [st.enter_context(nc.semaphore("d%d" % i)) for i in range(self.ndsem)]
            block = st.enter_context(nc.Block())

            def run(ename, eng):
                waited = {}
                lst = self.ins[ename]
                cur_ph = 0
                for i in order[ename]:
                    rec = lst[i]
                    deps = rec[1]
                    if rec[3] != cur_ph:
                        deps = list(deps)
                        for p in range(cur_ph + 1, rec[3] + 1):
                            deps.extend(self.phase_ev.get(p, ()))
                        cur_ph = rec[3]
                    tg = {}
                    for d in deps:
                        if d[0] == "E":
                            key = ("E", d[1]); v = val[d[1]][d[2]]; sem = esem[d[1]]
                        else:
                            key = ("D", d[1]); v = d[2]; sem = dsem[d[1]]
                        if tg.get(key, (None, 0))[1] < v:
                            tg[key] = (sem, v)
                    for key, (sem, v) in tg.items():
                        if waited.get(key, 0) >= v:
                            continue
                        eng.wait_ge(sem, v)
                        waited[key] = v
                        self.nwaits[ename] = self.nwaits.get(ename, 0) + 1
                    ins = rec[0](eng)
                    if rec[2] is not None:
                        ins.then_inc(dsem[rec[2]], 16)
                    elif i in need[ename]:
                        ins.then_inc(esem[ename], 1)
                if ename == "sp":
                    for d in final_waits:
                        eng.wait_ge(dsem[d[1]], d[2])

            block.tensor(lambda e: run("pe", e))
            block.vector(lambda e: run("dve", e))
            block.scalar(lambda e: run("act", e))
            block.gpsimd(lambda e: run("pool", e))
            block.sync(lambda e: run("sp", e))


def interleave(gens):
    gens = [g for g in gens if g is not None]
    while gens:
        nxt = []
        for g in gens:
            try:
                next(g)
                nxt.append(g)
            except StopIteration:
                pass
            yield
        gens = nxt


class Tl:
    __slots__ = ("t", "r")

    def __init__(self, t, name):
        self.t = t
        self.r = Res(name)

    def __getitem__(self, k):
        return self.t[k]


def build_nc(dbg=False, depth_run=DEPTH, reorder=True, only=None):
    nc = bass.Bass("TRN2", target_bir_lowering=False)
    S = Sched(nc)

    def din(name, shape, dt=F32):
        return nc.dram_tensor(name, list(shape), dt, kind="ExternalInput").ap()

    def dscr(name, shape, dt):
        return nc.dram_tensor(name, list(shape), dt, kind="ExternalOutput" if dbg else "Internal").ap()

    XIN = din("xin", [TOK, D])
    CVEC = din("cvec", [2, D])
    WMOD = din("w_mod", [DEPTH, D, 6 * D])
    BMOD = din("b_mod", [DEPTH, 6 * D])
    WIN = din("w_in", [DEPTH, D, 3072])
    ROPE = din("rope", [4, 128, TOK])
    CONVW = din("convw", [DEPTH, 128, 2, 3])
    SINK = din("sink", [DEPTH, 6])
    NAB = din("nab", [DEPTH, 5, 128, 6, 640])
    AMASK = din("amask", [128, 2, 384])
    WOUT = din("w_out", [DEPTH, D, D])
    LN1G = din("ln1_g", [DEPTH, D]); LN1B = din("ln1_b", [DEPTH, D])
    LN2G = din("ln2_g", [DEPTH, D]); LN2B = din("ln2_b", [DEPTH, D])
    WR = din("w_router", [DEPTH, D, NE])
    WG = din("w_gate", [DEPTH, NE, D, 512]); WU = din("w_up", [DEPTH, NE, D, 512])
    WD = din("w_down", [DEPTH, NE, 512, D])
    Y = nc.dram_tensor("y", [8192, D], F32, kind="ExternalOutput").ap()

    MODROW = dscr("modrow", [2, 6 * D], F32)
    FM = dscr("fm", [128, 14, TOK], BF16)
    VV = dscr("vv", [TOK, 8, 65], BF16)
    XMID = dscr("xmid", [TOK, D], F32)
    H2T = dscr("h2t", [128, 8, TOK], BF16)
    XCUR = dscr("xcur", [TOK, D], F32)
    XH2 = dscr("xh2", [TOK, RW], BF16)
    XE = [dscr("xe%d" % e, [1024, RW], BF16) for e in range(NE)]
    FFN = dscr("ffn", [TOK, D], F32)

    SB_LO = 16512
    SB_HI = 229344
    st = {"pers": SB_LO, "ph": None}

    def _alloc(name, shape, dt, key):
        nb = int(np.prod(shape[1:])) * (2 if dt == BF16 else 4)
        nb = (nb + 31) // 32 * 32
        off = st[key]
        assert off + nb <= SB_HI, (name, off, nb)
        st[key] = off + nb
        return Tl(nc.alloc_sbuf_tensor_at(name, list(shape), dt, offset=off), name)

    def pers(name, shape, dt=F32):
        return _alloc(name, shape, dt, "pers")

    cnt = [0]

    def ph(name, shape, dt=F32):
        cnt[0] += 1
        return _alloc("%s_%d" % (name, cnt[0]), shape, dt, "ph")

    def new_phase():
        S.barrier()
        st["ph"] = st["pers_end"]

    pbig = Tl(nc.alloc_psum_tensor("pbig", [128, 6, 512], F32), "pbig")
    ptr = [Tl(nc.alloc_psum_tensor("ptr%d" % i, [128, 8, 128], BF16), "ptr%d" % i) for i in range(2)]
    pbr = [Res("pb%d" % i) for i in range(6)]

    ident = pers("ident", [128, 128], BF16)
    identf = pers("identf", [128, 128], F32)
    onesf = pers("onesf", [128, 128], F32)
    aff = pers("aff", [128, NT, NE], F32)
    wsel = pers("wsel", [128, NT, NE], F32)
    modT = pers("modT", [128, 48], F32)
    modcT = pers("modcT", [128, 48], F32)
    esink = pers("esink", [128, 6], F32)
    convw = pers("convw", [128, 2, 3], F32)
    amask = pers("amask", [128, 2, 384], BF16)
    eps_t = pers("eps", [128, 1], F32)
    ltri = pers("ltri", [128, 128], F32)
    idxT = pers("idxT", [128, NE, 64], I32)
    st["pers_end"] = st["pers"]
    st["ph"] = st["pers_end"]

    _breg = {}

    def breg(e):
        if "r" not in _breg:
            _breg["r"] = e.to_reg(1023)
        return _breg["r"]

    def rs(lst):
        return [x.r if isinstance(x, Tl) else x for x in lst]

    def fsz(ap):
        try:
            return float(ap.free_size())
        except Exception:
            return 512.0

    def mm(out, lhsT, rhs, start, stop, reads, writes, tr=False):
        if tr:
            S.op("pe", lambda e: e.matmul(out, lhsT=lhsT, rhs=rhs, is_transpose=True),
                 reads=rs(reads), writes=rs(writes), pe_acc=True, cost=0.08)
        else:
            c = max(fsz(rhs), 64.0) / 2000.0 * (4.0 if lhsT.dtype == F32 else 1.0) + 0.03
            S.op("pe", lambda e: e.matmul(out, lhsT=lhsT, rhs=rhs, start=start, stop=stop),
                 reads=rs(reads), writes=rs(writes), pe_acc=True, cost=c)

    def act(out, in_, func, reads, writes, bias=0.0, scale=1.0, accum=None):
        if accum is None:
            S.op("act", lambda e: e.activation(out=out, in_=in_, func=func, bias=bias, scale=scale),
                 reads=rs(reads), writes=rs(writes), cost=0.2 + fsz(out) / 1300.0)
        else:
            S.op("act", lambda e: e.activation(out=out, in_=in_, func=func, bias=bias, scale=scale,
                                               accum_out=accum), reads=rs(reads), writes=rs(writes))

    def vcost(eng, ap):
        return (0.1 + fsz(ap) / 900.0) if eng == "dve" else (0.2 + fsz(ap) / 450.0)

    def tt(out, in0, in1, op, reads, writes, eng="dve"):
        S.op(eng, lambda e: e.tensor_tensor(out=out, in0=in0, in1=in1, op=op), reads=rs(reads), writes=rs(writes),
             cost=vcost(eng, out))

    def ts(out, in0, s1, s2, op0, op1, reads, writes, eng="dve"):
        if op1 is None:
            S.op(eng, lambda e: e.tensor_scalar(out=out, in0=in0, scalar1=s1, scalar2=None, op0=op0),
                 reads=rs(reads), writes=rs(writes), cost=vcost(eng, out))
        else:
            S.op(eng, lambda e: e.tensor_scalar(out=out, in0=in0, scalar1=s1, scalar2=s2, op0=op0, op1=op1),
                 reads=rs(reads), writes=rs(writes), cost=vcost(eng, out))

    def stt(out, in0, scalar, in1, op0, op1, reads, writes):
        S.op("dve", lambda e: e.scalar_tensor_tensor(out=out, in0=in0, scalar=scalar, in1=in1, op0=op0, op1=op1),
             reads=rs(reads), writes=rs(writes), cost=vcost("dve", out))

    def cp(out, in_, reads, writes, eng="dve"):
        S.op(eng, lambda e: e.tensor_copy(out=out, in_=in_), reads=rs(reads), writes=rs(writes), cost=vcost(eng, out))

    def recip(out, in_, reads, writes):
        S.op("dve", lambda e: e.reciprocal(out=out, in_=in_), reads=rs(reads), writes=rs(writes))

    def reduce(out, in_, op, reads, writes, negate=False):
        S.op("dve", lambda e: e.tensor_reduce(out=out, in_=in_, axis=AX.X, op=op, negate=negate),
             reads=rs(reads), writes=rs(writes), cost=vcost("dve", in_))

    def memset(eng, ap, val, writes):
        S.op(eng, lambda e: e.memset(ap, val), writes=rs(writes), cost=vcost(eng, ap))

    def single(out, in_, scalar, op, reads, writes):
        S.op("dve", lambda e: e.tensor_single_scalar(out=out, in_=in_, scalar=scalar, op=op),
             reads=rs(reads), writes=rs(writes))

    def scan(out, d0, d1, reads, writes):
        S.op("dve", lambda e: e.tensor_tensor_scan(out=out, data0=d0, data1=d1, initial=0.0, op0=ALU.add, op1=ALU.add),
             reads=rs(reads), writes=rs(writes))

    def iota_tail(ap, base, writes):
        S.op("pool", lambda e: e.iota(ap, pattern=[[0, 1]], base=base, channel_multiplier=1), writes=rs(writes))

    def dma(q, out, in_, reads, writes, owner, slow=False, group=None):
        try:
            nbytes = float(out.nbytes())
        except Exception:
            nbytes = 1.0e5
        lat = 2.5 + nbytes / 1.5e5
        cost = 0.1 if q == "sp" else 1.0
        ow = owner.r if isinstance(owner, Tl) else owner
        if group is not None:
            return S.dma(q, lambda e: e.dma_start(out=out, in_=in_), reads=rs(reads), writes=rs(writes),
                         owner=ow, group=group, cost=cost, lat=lat)
        if slow:
            return S.dma(q, lambda e: e.dma_start(out=out, in_=in_, allow_slow_non_contiguous=True),
                         reads=rs(reads), writes=rs(writes), owner=ow, cost=cost, lat=lat + 3.0)
        return S.dma(q, lambda e: e.dma_start(out=out, in_=in_),
                     reads=rs(reads), writes=rs(writes), owner=ow, cost=cost, lat=lat)

    def ln_stats(src_ap, src_res, stats, mv, rstd, nmr=None):
        for h in range(2):
            S.op("dve", lambda e, h=h: e.bn_stats(out=stats[:, h, :], in_=src_ap[:, h * 512:(h + 1) * 512]),
                 reads=rs([src_res]), writes=rs([stats]))
        S.op("dve", lambda e: e.bn_aggr(out=mv[:], in_=stats[:].rearrange("p a b -> p (a b)")),
             reads=rs([stats]), writes=rs([mv]))
        act(rstd[:], mv[:, 1:2], AF.Sqrt, [mv, eps_t], [rstd], bias=eps_t[:, 0:1])
        S.op("dve", lambda e: e.reciprocal(out=rstd[:], in_=rstd[:]), reads=rs([rstd]), writes=rs([rstd]))
        if nmr is not None:
            stt(nmr[:], mv[:, 0:1], -1.0, rstd[:], ALU.mult, ALU.mult, [mv, rstd], [nmr])

    S.op("pool", lambda e: e.iota(identf[:], pattern=[[1, 128]], base=0, channel_multiplier=-1,
                                  allow_small_or_imprecise_dtypes=True), writes=rs([identf]))
    S.op("dve", lambda e: e.tensor_single_scalar(out=ident[:], in_=identf[:], scalar=0.0, op=ALU.is_equal),
         reads=rs([identf]), writes=rs([ident]))
    S.op("dve", lambda e: e.memset(onesf[:], 1.0), writes=rs([onesf]))
    S.op("dve", lambda e: e.tensor_single_scalar(out=ltri[:], in_=identf[:], scalar=0.0, op=ALU.is_gt),
         reads=rs([identf]), writes=rs([ltri]))
    S.op("dve", lambda e: e.memset(eps_t[:], 1e-6), writes=rs([eps_t]))
    dma("pool", amask[:], AMASK, [S.dres("amask")], [amask], amask)

    fm_res = [S.dres("fm", t) for t in range(NT)]
    vv_res = [S.dres("vv", t) for t in range(NT)]
    xmid_res = [S.dres("xmid", t) for t in range(NT)]
    h2t_res = [S.dres("h2t", t) for t in range(NT)]
    xcur_res = [S.dres("xcur", t) for t in range(NT)]
    xh2_res = [S.dres("xh2", t) for t in range(NT)]
    y_res = [S.dres("y", t) for t in range(64)]
    final = []

    for l in range(depth_run):
        last = l == DEPTH - 1
        T0 = 2 if last else 0
        new_phase()
        cT = ph("cT", [128, 8, 2])
        bm = ph("bm", [2, 6 * D])
        mrow = ph("mrow", [2, 6 * D])
        wm = [ph("wm%d" % i, [128, 8, 512]) for i in range(2)]
        for m_ in range(2):
            dma("sp", cT[:, :, m_], CVEC[m_].rearrange("(k p) -> p k", p=128), [S.dres("cvec")], [cT], cT, slow=True)
        dma("sp", bm[:], BMOD[l].partition_broadcast(2), [S.dres("bmod")], [bm], bm)
        dma("sp", esink[:], SINK[l].partition_broadcast(128), [S.dres("sink")], [esink], esink)
        dma("sp", convw[:], CONVW[l], [S.dres("convw")], [convw], convw)
        act(cT[:], cT[:], AF.Silu, [cT], [cT])
        act(esink[:], esink[:], AF.Exp, [esink], [esink])
        wmv = WMOD[l].rearrange("(k p) n -> p k n", p=128)
        for cc in range(12):
            w_ = wm[cc % 2]
            dma("sp", w_[:], wmv[:, :, cc * 512:(cc + 1) * 512], [S.dres("wmod")], [w_], w_)
            pb = pbig[0:2, cc % 2, :]
            for k in range(8):
                mm(pb, cT[:, k, :], w_[:, k, :], k == 0, k == 7, [cT, w_], [pbr[cc % 2]])
            tt(mrow[:, cc * 512:(cc + 1) * 512], pb, bm[:, cc * 512:(cc + 1) * 512], ALU.add,
               [pbr[cc % 2], bm], [mrow])
        dma("sp", MODROW, mrow[:], [mrow], [S.dres("modrow")], mrow)
        dma("sp", modT[:], MODROW[0].rearrange("(j p) -> p j", p=128), [S.dres("modrow")], [modT], modT, slow=True)
        dma("sp", modcT[:], MODROW[1].rearrange("(j p) -> p j", p=128), [S.dres("modrow")], [modcT], modcT, slow=True)
        for m_ in (modT, modcT):
            ts(m_[:, 8:16], m_[:, 8:16], 1.0, None, ALU.add, None, [m_], [m_])
            ts(m_[:, 32:40], m_[:, 32:40], 1.0, None, ALU.add, None, [m_], [m_])

        new_phase()
        win = ph("win", [128, 8, 3072], BF16)
        wiv = WIN[l].rearrange("(k p) n -> p k n", p=128)
        for k in range(8):
            dma("pool", win[:, k, :], wiv[:, k, :], [S.dres("win")], [win], win)
        xt = [ph("xt%d" % i, [128, D]) for i in range(2)]
        xh = [ph("xh%d" % i, [128, D], BF16) for i in range(2)]
        hT = [ph("hT%d" % i, [128, 8, 512], BF16) for i in range(2)]
        fmo = [ph("fmo%d" % i, [128, 14, 512], BF16) for i in range(2)]
        vo = [ph("vo%d" % i, [128, 4, 8, 65], BF16) for i in range(2)]
        rp = [ph("rp%d" % i, [128, 4, 512]) for i in range(2)]
        tmp = [ph("tmp%d" % i, [128, 512]) for i in range(3)]
        stats = ph("stats", [128, 2, 6]); mv = ph("mv", [128, 2]); rstd = ph("rstd", [128, 1])
        for v_ in vo:
            S.op("pool", lambda e, v_=v_: e.memset(v_[:], 1.0), writes=rs([v_]))
        src = XIN if l == 0 else XCUR
        src_res = (lambda t: S.dres("xin", t)) if l == 0 else (lambda t: xcur_res[t])
        groups = [(0, 2)] + [(2 + 4 * i, 4) for i in range(16)]
        bank = [0]

        def nextbank():
            b = bank[0]
            bank[0] = (b + 1) % 6
            return b

        def gen_L(gi, t0, nt):
            N = nt * 128
            sl = gi % 2
            mT = modcT if gi == 0 else modT
            rpt = rp[sl]
            dma("sp", rpt[:, :, :N], ROPE[:, :, t0 * 128:t0 * 128 + N].rearrange("a p n -> p a n"),
                [S.dres("rope")], [rpt], rpt)
            for s in range(nt):
                tl = t0 + s
                x_ = xt[tl % 2]; xh_ = xh[tl % 2]; pt_ = ptr[tl % 2]
                dma("sp", x_[:], src[tl * 128:(tl + 1) * 128, :], [src_res(tl)], [x_], x_)
                ln_stats(x_, x_, stats, mv, rstd)
                yield
                ts(xh_[:], x_[:], mv[:, 0:1], rstd[:, 0:1], ALU.subtract, ALU.mult, [x_, mv, rstd], [xh_])
                for k in range(8):
                    mm(pt_[:, k, :], xh_[:, k * 128:(k + 1) * 128], ident[:], True, True, [xh_, ident], [pt_], tr=True)
                yield
                for k in range(8):
                    act(hT[sl][:, k, s * 128:(s + 1) * 128], pt_[:, k, :], AF.Identity, [pt_, mT], [hT[sl]],
                        bias=mT[:, k:k + 1], scale=mT[:, 8 + k:9 + k])
                    if k % 4 == 3:
                        yield

        def gen_P(gi, t0, nt):
            N = nt * 128
            sl = gi % 2
            rpt = rp[sl]
            h_ = hT[sl]; fo = fmo[sl]

            def proj(ch):
                b = nextbank()
                for k in range(8):
                    mm(pbig[:, b, :N], win[:, k, ch * 128:(ch + 1) * 128], h_[:, k, :N], k == 0, k == 7,
                       [win, h_], [pbr[b]])
                return b

            def rope(chq, chs, tq, tsn, dst):
                b1 = proj(chq); b2 = proj(chs)
                tt(tmp[0][:, :N], pbig[:, b1, :N], rpt[:, tq, :N], ALU.mult, [pbr[b1], rpt], [tmp[0]])
                tt(tmp[1][:, :N], pbig[:, b2, :N], rpt[:, tsn, :N], ALU.mult, [pbr[b2], rpt], [tmp[1]])
                tt(fo[:, dst, :N], tmp[0][:, :N], tmp[1][:, :N], ALU.add, [tmp[0], tmp[1]], [fo], eng="pool")

            for c_ in range(3):
                rope(c_, 3 + c_, 0, 1, c_)
                yield
            rope(6, 7, 2, 3, 6)
            yield
            for c_ in range(2):
                b1 = proj(8 + c_)
                act(tmp[2][:, :N], pbig[:, b1, :N], AF.Identity, [pbr[b1]], [tmp[2]])
                b2 = proj(12 + c_)
                tt(fo[:, 10 + c_, :N], pbig[:, b2, :N], tmp[2][:, :N], ALU.mult, [pbr[b2], tmp[2]], [fo])
                yield
                b3 = proj(10 + c_)
                act(fo[:, 12 + c_, :N], pbig[:, b3, :N], AF.Identity, [pbr[b3]], [fo])
                yield
            for c_ in range(3):
                b1 = proj(14 + c_)
                act(fo[:, 3 + c_, :N], pbig[:, b1, :N], AF.Identity, [pbr[b1]], [fo], scale=0.125)
                yield
                b2 = proj(17 + c_)
                cp(fo[:, 7 + c_, :N], pbig[:, b2, :N], [pbr[b2]], [fo])
                yield
            for s in range(nt):
                b = nextbank()
                for k in range(8):
                    mm(pbig[:, b, :], h_[:, k, s * 128:(s + 1) * 128], win[:, k, 2560:3072], k == 0, k == 7,
                       [win, h_], [pbr[b]])
                act(vo[sl][:, s, :, 0:64], pbig[:, b, :].rearrange("p (h d) -> p h d", h=8), AF.Identity,
                    [pbr[b]], [vo[sl]])
                yield
            dma("sp", FM[:, :, t0 * 128:t0 * 128 + N], fo[:, :, :N], [fo], fm_res[t0:t0 + nt], fo)
            dma("sp", VV[t0 * 128:t0 * 128 + N].rearrange("(s p) h d -> p s h d", p=128), vo[sl][:, :nt],
                [vo[sl]], vv_res[t0:t0 + nt], vo[sl])
            yield

        prevP = None
        for gi, (t0, nt) in enumerate(groups):
            for _ in interleave([gen_L(gi, t0, nt), prevP]):
                pass
            prevP = gen_P(gi, t0, nt)
        for _ in prevP:
            pass

        new_phase()
        wout = ph("wout", [128, 8, D], BF16)
        wov = WOUT[l].rearrange("(k p) n -> p k n", p=128)
        for k in range(0, 8, 2):
            dma("pool", wout[:, k:k + 2, :], wov[:, k:k + 2, :], [S.dres("wout")], [wout], wout)
        wr = ph("wr", [128, 8, NE], BF16)
        dma("pool", wr[:], WR[l].rearrange("(k p) n -> p k n", p=128), [S.dres("wr")], [wr], wr)
        nabi = ph("nabi", [128, 6, 640], BF16)
        nabe = ph("nabe", [128, 6, 640], BF16)
        dma("pool", nabi[:], NAB[l, 0], [S.dres("nab")], [nabi], nabi)
        kctx = ph("kctx", [128, 4, 256], BF16)
        vctx = ph("vctx", [128, 2, 8, 65], BF16)
        dma("sp", kctx[:], FM[:, 6:10, 0:256], fm_res[0:2], [kctx], kctx)
        dma("sp", vctx[:], VV[0:256].rearrange("(s p) h d -> p s h d", p=128), vv_res[0:2], [vctx], vctx)
        g1b = ph("g1b", [128, D]); cg1b = ph("cg1b", [128, D])
        l1g = ph("l1g", [128, D]); l1b = ph("l1b", [128, D])
        dma("sp", g1b[:], MODROW[0, 2048:3072].partition_broadcast(128), [S.dres("modrow")], [g1b], g1b)
        dma("sp", cg1b[:], MODROW[1, 2048:3072].partition_broadcast(128), [S.dres("modrow")], [cg1b], cg1b)
        dma("sp", l1g[:], LN1G[l].partition_broadcast(128), [S.dres("ln1g")], [l1g], l1g)
        dma("sp", l1b[:], LN1B[l].partition_broadcast(128), [S.dres("ln1b")], [l1b], l1b)
        qw = [ph("qw%d" % i, [128, 6, 128], BF16) for i in range(2)]
        kw = [ph("kw%d" % i, [128, 4, 640], BF16) for i in range(2)]
        vw = [ph("vw%d" % i, [128, 5, 8, 65], BF16) for i in range(2)]
        uw = [ph("uw%d" % i, [128, 2, 130], BF16) for i in range(2)]
        bbw = [ph("bbw%d" % i, [128, 2, 128], BF16) for i in range(2)]
        xa = [ph("xa%d" % i, [128, D]) for i in range(2)]
        pta = [ph("pta%d" % i, [128, 5, 384], BF16) for i in range(2)]
        ptn = [ph("ptn%d" % i, [128, 896], BF16) for i in range(2)]
        sfn = [ph("sfn%d" % i, [128, 640]) for i in range(2)]
        mixc = ph("mixc", [128, 768], BF16)
        mixT = ph("mixT", [128, 8, 128], BF16)
        ctmp = ph("ctmp", [128, 128])
        den = ph("den", [128, 6]);
        t1 = ph("t1", [128, D]); y1 = ph("y1", [128, D]); xm = [ph("xm%d" % i, [128, D]) for i in range(2)]
        xh2 = ph("xh2", [128, RW], BF16)
        h2o = [ph("h2o%d" % i, [128, 8, 128], BF16) for i in range(2)]
        stats = ph("stats", [128, 2, 6]); mv = ph("mv", [128, 2]); rstd = ph("rstd", [128, 1]); nmr = ph("nmr", [128, 1])
        rmx = ph("rmx", [128, 1]); rsum = ph("rsum", [128, 1]); rexp = ph("rexp", [128, NE])

        mixT2 = [mixT, ph("mixTb", [128, 8, 128], BF16)]

        def gen_att(T):
            sl = T % 2
            is_ctx = T < 2
            i = T - 2
            q_ = qw[sl]; k_ = kw[sl]; v_ = vw[sl]; u_ = uw[sl]; bb_ = bbw[sl]
            mT_ = mixT2[sl]
            dma("sp", q_[:], FM[:, 0:6, T * 128:(T + 1) * 128], [fm_res[T]], [q_], q_)
            nb = nabi
            base = 0
            if not is_ctx:
                base = min(max(i - 2, 0), 59) + 2
                dma("sp", k_[:], FM[:, 6:10, base * 128:(base + 5) * 128], fm_res[base:base + 5], [k_], k_)
                dma("sp", v_[:], VV[base * 128:(base + 5) * 128].rearrange("(s p) h d -> p s h d", p=128),
                    vv_res[base:base + 5], [v_], v_)
                var = 0 if 2 <= i <= 61 else (1 + i if i < 2 else i - 59)
                if var != 0:
                    dma("pool", nabe[:], NAB[l, var], [S.dres("nab")], [nabe], nabe)
                    nb = nabe
            lo_pad = T in (0, 2); hi_pad = T in (1, NT - 1)
            if lo_pad or hi_pad:
                memset("pool", u_[:], 0.0, [u_])
            c0 = T * 128 - (0 if lo_pad else 1); c1 = (T + 1) * 128 + (0 if hi_pad else 1)
            o0 = 1 if lo_pad else 0
            fr = fm_res[max(T - 1, 0):min(T + 2, NT)]
            dma("sp", u_[:, :, o0:o0 + (c1 - c0)], FM[:, 10:12, c0:c1], fr, [u_], u_)
            dma("sp", bb_[:], FM[:, 12:14, T * 128:(T + 1) * 128], [fm_res[T]], [bb_], bb_)
            yield
            if is_ctx:
                akeys = [(("c", 0), None), (("c", 1), None)]
                nkeys = [("c", 0), ("c", 1)]
            else:
                akeys = []
                if i > 0:
                    akeys.append((("w", T - 1 - base), 0))
                akeys.append((("w", T - base), None))
                if i < 63:
                    akeys.append((("w", T + 1 - base), 1))
                akeys += [(("c", 0), None), (("c", 1), None)]
                nkeys = [("w", j) for j in range(5)] + [("c", 0), ("c", 1)]

            def kap(kt, ch, p0):
                if kt[0] == "c":
                    return kctx[p0:p0 + 64, ch, kt[1] * 128:(kt[1] + 1) * 128], kctx
                return k_[p0:p0 + 64, ch, kt[1] * 128:(kt[1] + 1) * 128], k_

            def vap(kt, head):
                if kt[0] == "c":
                    return vctx[:, kt[1], head, :], vctx
                return v_[:, kt[1], head, :], v_

            def gen_A():
                for g in range(2):
                    p0 = g * 64
                    pa_ = pta[g]
                    for ki, (kt, mk) in enumerate(akeys):
                        ka_, kr = kap(kt, 0, p0)
                        if mk is not None:
                            mm(pbig[:, 0, 0:384], ident[:], amask[:, mk, :], True, False, [ident, amask], [pbr[0]])
                        mm(pbig[:, 0, 0:384], ka_, q_[p0:p0 + 64, 0:3, :].rearrange("p a b -> p (a b)"), mk is None, True,
                           [kr, q_], [pbr[0]])
                        act(pa_[:, ki, :], pbig[:, 0, 0:384], AF.Exp, [pbr[0]], [pa_])
                        yield
                    po = pbig[:, 2, 0:195].rearrange("p (c d) -> p c d", c=3)
                    for c_ in range(3):
                        for ki, (kt, mk) in enumerate(akeys):
                            va_, vr = vap(kt, g)
                            mm(po[:, c_, :], pa_[:, ki, c_ * 128:(c_ + 1) * 128], va_, ki == 0, ki == len(akeys) - 1,
                               [pa_, vr], [pbr[2]])
                        yield
                    tt(den[:, 0:3], po[:, :, 64], esink[:, 3 * g:3 * g + 3], ALU.add, [pbr[2], esink], [den])
                    recip(den[:, 0:3], den[:, 0:3], [den], [den])
                    tt(mixc[:, g * 192:(g + 1) * 192].rearrange("p (c d) -> p c d", c=3), po[:, :, 0:64],
                       den[:, 0:3].unsqueeze(2).to_broadcast([128, 3, 64]), ALU.mult, [pbr[2], den], [mixcA])
                    yield

            def gen_N():
                po2 = pbig[:, 3, 0:390].rearrange("p (c d) -> p c d", c=6)
                nk = len(nkeys)
                for h in range(6):
                    ch = h // 2; p0 = (h % 2) * 64
                    pn_ = ptn[h % 2]; sf_ = sfn[h % 2]
                    ps2 = pbig[:, 4:6, :].rearrange("p a b -> p (a b)")
                    if is_ctx:
                        for j, kt in enumerate(nkeys):
                            ka_, kr = kap(kt, 1 + ch, p0)
                            mm(ps2[:, j * 128:(j + 1) * 128], ka_, q_[p0:p0 + 64, 3 + ch, :], True, True,
                               [kr, q_], [pbr[4], pbr[5]])
                        act(pn_[:, 0:256], ps2[:, 0:256], AF.Exp, [pbr[4], pbr[5]], [pn_])
                    else:
                        for j in (5, 6):
                            ka_, kr = kap(nkeys[j], 1 + ch, p0)
                            mm(ps2[:, j * 128:(j + 1) * 128], ka_, q_[p0:p0 + 64, 3 + ch, :], True, True,
                               [kr, q_], [pbr[4], pbr[5]])
                        mm(ps2[:, 512:640], ident[:], nb[:, h, 512:640], True, False, [ident, nb], [pbr[4], pbr[5]])
                        mm(ps2[:, 0:512], ident[:], nb[:, h, 0:512], True, False, [ident, nb], [pbr[4], pbr[5]])
                        for j in range(5):
                            ka_, kr = kap(nkeys[j], 1 + ch, p0)
                            mm(ps2[:, j * 128:(j + 1) * 128], ka_, q_[p0:p0 + 64, 3 + ch, :], False, j in (3, 4),
                               [kr, q_], [pbr[4], pbr[5]])
                        act(pn_[:, 0:896], ps2[:, 0:896], AF.Exp, [pbr[4], pbr[5]], [pn_])
                    yield
                    for j, kt in enumerate(nkeys):
                        va_, vr = vap(kt, 2 + h)
                        mm(po2[:, h, :], pn_[:, j * 128:(j + 1) * 128], va_, j == 0, j == nk - 1, [pn_, vr], [pbr[3]])
                    yield
                recip(den2[:], po2[:, :, 64], [pbr[3]], [den2])
                tt(mixc[:, 384:768].rearrange("p (c d) -> p c d", c=6), po2[:, :, 0:64],
                   den2[:].unsqueeze(2).to_broadcast([128, 6, 64]), ALU.mult, [pbr[3], den2], [mixcN])
                yield

            def gen_B():
                for c_ in range(2):
                    ts(ctmp[:], u_[:, c_, 0:128], convw[:, c_, 0:1], None, ALU.mult, None, [u_, convw], [ctmp])
                    stt(ctmp[:], u_[:, c_, 1:129], convw[:, c_, 1:2], ctmp[:], ALU.mult, ALU.add, [u_, convw, ctmp], [ctmp])
                    stt(ctmp[:], u_[:, c_, 2:130], convw[:, c_, 2:3], ctmp[:], ALU.mult, ALU.add, [u_, convw, ctmp], [ctmp])
                    tt(mT_[:, 3 + c_, :], ctmp[:], bb_[:, c_, :], ALU.mult, [ctmp, bb_], [mT_])
                    yield

            for _ in interleave([gen_A(), gen_N(), gen_B()]):
                yield
            pt_ = ptr[0]
            for c_ in range(6):
                mm(pt_[:, c_, :], mixc[:, c_ * 128:(c_ + 1) * 128], ident[:], True, True, [mixcA, mixcN, ident], [pt_], tr=True)
            cp(mT_[:, 0:3, :], pt_[:, 0:3, :], [pt_], [mT_])
            act(mT_[:, 5:8, :], pt_[:, 3:6, :], AF.Identity, [pt_], [mT_])
            yield

        def gen_epi(T):
            sl = T % 2
            is_ctx = T < 2
            x_ = xa[sl]; mT_ = mixT2[sl]
            dma("sp", x_[:], src[T * 128:(T + 1) * 128, :], [src_res(T)], [x_], x_)
            gb = cg1b if is_ctx else g1b
            for hf in range(2):
                for k in range(8):
                    mm(pbig[:, 1, :], mT_[:, k, :], wout[:, k, hf * 512:(hf + 1) * 512], k == 0, k == 7,
                       [mT_, wout], [pbr[1]])
                tt(t1[:, hf * 512:(hf + 1) * 512], pbig[:, 1, :], gb[:, hf * 512:(hf + 1) * 512], ALU.mult,
                   [pbr[1], gb], [t1])
                yield
            stt(y1[:], x_[:], ALPHA, t1[:], ALU.mult, ALU.add, [x_, t1], [y1])
            yield
            ln_stats(y1, y1, stats, mv, rstd, nmr)
            yield
            xm_ = xm[sl]
            act(t1[:], y1[:], AF.Identity, [y1, rstd, nmr], [t1], bias=nmr[:, 0:1], scale=rstd[:, 0:1])
            yield
            tt(t1[:], t1[:], l1g[:], ALU.mult, [t1, l1g], [t1], eng="pool")
            yield
            tt(xm_[:], t1[:], l1b[:], ALU.add, [t1, l1b], [xm_], eng="pool")
            dma("sp", XMID[T * 128:(T + 1) * 128, :], xm_[:], [xm_], [xmid_res[T]], xm_)
            yield
            ln_stats(xm_, xm_, stats, mv, rstd)
            yield
            ts(xh2[:, 0:D], xm_[:], mv[:, 0:1], rstd[:, 0:1], ALU.subtract, ALU.mult, [xm_, mv, rstd], [xh2])
            yield
            pt2 = ptr[1]
            for k in range(8):
                mm(pt2[:, k, :], xh2[:, k * 128:(k + 1) * 128], ident[:], True, True, [xh2, ident], [pt2], tr=True)
            mT = modcT if is_ctx else modT
            h2_ = h2o[sl]
            for k in range(8):
                act(h2_[:, k, :], pt2[:, k, :], AF.Identity, [pt2, mT], [h2_],
                    bias=mT[:, 24 + k:25 + k], scale=mT[:, 32 + k:33 + k])
                if k % 4 == 3:
                    yield
            if is_ctx:
                dma("sp", H2T[:, :, T * 128:(T + 1) * 128], h2_[:], [h2_], [h2t_res[T]], h2_)
            pr = pbig[:, 1, 0:NE]
            for k in range(8):
                mm(pr, h2_[:, k, :], wr[:, k, :], k == 0, k == 7, [h2_, wr], [pbr[1]])
            reduce(rmx[:], pr, ALU.max, [pbr[1]], [rmx], negate=True)
            act(rexp[:], pr, AF.Exp, [pbr[1], rmx], [rexp], bias=rmx[:, 0:1])
            yield
            reduce(rsum[:], rexp[:], ALU.add, [rexp], [rsum])
            recip(rsum[:], rsum[:], [rsum], [rsum])
            ts(aff[:, T, :], rexp[:], rsum[:, 0:1], None, ALU.mult, None, [rexp, rsum], [aff])
            if not is_ctx:
                ts(xh2[:, 1026:1042], rexp[:], rsum[:, 0:1], None, ALU.mult, None, [rexp, rsum], [xh2])
                stt(xh2[:, 1042:1058], rexp[:], rsum[:, 0:1], xh2[:, 1026:1042], ALU.mult, ALU.subtract,
                    [rexp, rsum, xh2], [xh2])
                iota_tail(xh2[:, 1024:1026].bitcast(I32), T * 128, [xh2])
                dma("sp", XH2[T * 128:(T + 1) * 128, :], xh2[:], [xh2], [xh2_res[T]], xh2)
            yield

        mixcA = Res("mixcA"); mixcN = Res("mixcN")
        den2 = ph("den2", [128, 6])
        prev = None
        for T in range(T0, NT):
            for _ in interleave([gen_att(T), prev]):
                pass
            prev = gen_epi(T)
        for _ in prev:
            pass

        new_phase()
        lo_t = ph("lo", [128, NE]); mid_t = ph("mid", [128, NE]); cntp = ph("cntp", [128, NE]); sel = ph("sel", [128, NE])
        cmp = ph("cmp", [128, NE, 64])
        incl = ph("incl", [128, NE, 64])
        zt = ph("zt", [128, 64])
        offs = ph("offs", [128, NE])
        zbig = ph("zbig", [128, 4096])
        memset("pool", zbig[:], 0.0, [zbig])
        memset("pool", zt[:], 0.0, [zt])
        for j in range(16):
            dma("sp", FFN[256 + j * 512:256 + (j + 1) * 512, :].rearrange("(p a) d -> p (a d)", p=128), zbig[:],
                [zbig], [S.dres("ffn")], zbig, group=("ffnz", l))
        sets = [(2, 64, 1024.0)] if last else [(0, 2, 32.0), (2, 64, 1024.0)]
        for (ta, tn, cap) in sets:
            av = aff[:, ta:ta + tn, :].rearrange("p t e -> p e t")
            memset("dve", lo_t[:], 0.0, [lo_t])
            for it in range(30):
                w_ = 0.5 ** (it + 1)
                ts(mid_t[:], lo_t[:], w_, None, ALU.add, None, [lo_t], [mid_t])
                tt(cmp[:, :, :tn], av, mid_t[:].unsqueeze(2).to_broadcast([128, NE, tn]), ALU.is_ge, [aff, mid_t], [cmp])
                reduce(cntp[:], cmp[:, :, :tn], ALU.add, [cmp], [cntp])
                mm(pbig[:, 0, 0:NE], onesf[:], cntp[:], True, True, [onesf, cntp], [pbr[0]])
                single(sel[:], pbig[:, 0, 0:NE], cap - 0.5, ALU.is_ge, [pbr[0]], [sel])
                stt(lo_t[:], sel[:], w_, lo_t[:], ALU.mult, ALU.add, [sel, lo_t], [lo_t])
            tt(cmp[:, :, :tn], av, lo_t[:].unsqueeze(2).to_broadcast([128, NE, tn]), ALU.is_ge, [aff, lo_t], [cmp])
            if dbg:
                LDBG = nc.dram_tensor("ldbg%d_%d" % (l, tn), [128, NE], F32, kind="ExternalOutput").ap()
                dma("sp", LDBG, lo_t[:], [lo_t], [S.dres("ldbg", tn)], lo_t)
                ADBG = nc.dram_tensor("adbg%d_%d" % (l, tn), [128, NT * NE], F32, kind="ExternalOutput").ap()
                dma("sp", ADBG, aff[:].rearrange("p a b -> p (a b)"), [aff], [S.dres("adbg", tn)], aff)
            if tn == 2:
                wv = wsel[:, ta:ta + tn, :].rearrange("p t e -> p e t")
                tt(wv, av, cmp[:, :, :tn], ALU.mult, [aff, cmp], [wsel])
                continue
            for ex in range(NE):
                scan(incl[:, ex, :], cmp[:, ex, :], zt[:], [cmp, zt], [incl])
            cp(cntp[:], incl[:, :, 63], [incl], [cntp])
            mm(pbig[:, 0, 0:NE], ltri[:], cntp[:], True, True, [ltri, cntp], [pbr[0]])
            cp(offs[:], pbig[:, 0, 0:NE], [pbr[0]], [offs])
            tt(incl[:], incl[:], cmp[:], ALU.subtract, [incl, cmp], [incl])
            tt(incl[:], incl[:], offs[:].unsqueeze(2).to_broadcast([128, NE, 64]), ALU.add, [incl, offs], [incl])
            stt(incl[:].rearrange("p a b -> p (a b)"), incl[:].rearrange("p a b -> p (a b)"), -1.0e6,
                cmp[:].rearrange("p a b -> p (a b)"), ALU.add, ALU.mult, [incl, cmp], [incl])
            ts(idxT[:], incl[:], 1.0e6, None, ALU.add, None, [incl], [idxT])
            if dbg:
                IDBG = nc.dram_tensor("idbg%d" % l, [128, NE * 64], I32, kind="ExternalOutput").ap()
                dma("sp", IDBG, idxT[:].rearrange("p a b -> p (a b)"), [idxT], [S.dres("idbg")], idxT)
                ODBG = nc.dram_tensor("odbg%d" % l, [128, NE], F32, kind="ExternalOutput").ap()
                dma("sp", ODBG, offs[:], [offs], [S.dres("odbg")], offs)
                CDBG = nc.dram_tensor("cdbg%d" % l, [128, NE * 64], F32, kind="ExternalOutput").ap()
                dma("sp", CDBG, cmp[:].rearrange("p a b -> p (a b)"), [cmp], [S.dres("cdbg")], cmp)

        new_phase()
        wgt = [ph("wg%d" % i, [128, 8, 512], BF16) for i in range(2)]
        wut = [ph("wu%d" % i, [128, 8, 512], BF16) for i in range(2)]
        wdt = [ph("wd%d" % i, [128, 4, D], BF16) for i in range(2)]
        tokc = [ph("tokc%d" % i, [128, 8, RW], BF16) for i in range(2)]
        xet = [ph("xet%d" % i, [128, RW], BF16) for i in range(2)]
        h2e = [ph("h2e%d" % i, [128, 8, 512], BF16) for i in range(2)]
        gT = [ph("gT%d" % i, [128, 4, 512], BF16) for i in range(2)]
        sa = [ph("sa%d" % i, [128, 512], BF16) for i in range(2)]
        yo = [ph("yo%d" % i, [128, D]) for i in range(2)]
        idxe = [ph("idxe%d" % i, [128, 1], I32) for i in range(8)]
        gate = ph("gate", [128, 8])
        rmx = ph("rmx", [128, 1]); rsum = ph("rsum", [128, 1]); rexp = ph("rexp", [128, NE])
        wr = ph("wr", [128, 8, NE], BF16)
        dma("pool", wr[:], WR[l].rearrange("(k p) n -> p k n", p=128), [S.dres("wr")], [wr], wr)
        h2g = ph("h2g", [128, 8, 256], BF16)
        acc = ph("acc", [128, 2, D])
        accr = [Res("acc%d" % i) for i in range(2)]
        if not last:
            dma("sp", h2g[:], H2T[:, :, 0:256], h2t_res[0:2], [h2g], h2g)
        xe_res = [S.dres("xe", e) for e in range(NE)]
        tcnt = [0]

        def load_w(ex):
            sl = ex % 2
            dma("pool", wgt[sl][:], WG[l, ex].rearrange("(k p) f -> p k f", p=128), [S.dres("wg")], [wgt[sl]], wgt[sl])
            dma("pool", wut[sl][:], WU[l, ex].rearrange("(k p) f -> p k f", p=128), [S.dres("wu")], [wut[sl]], wut[sl])
            dma("pool", wdt[sl][:], WD[l, ex].rearrange("(k p) f -> p k f", p=128), [S.dres("wd")], [wdt[sl]], wdt[sl])

        def dispatch(exs):
            for cg in range(8):
                tk = tokc[tcnt[0] % 2]
                tcnt[0] += 1
                dma("sp", tk[:], XH2[(2 + cg * 8) * 128:(2 + cg * 8 + 8) * 128, :].rearrange("(s p) d -> p s d", p=128),
                    xh2_res[2 + cg * 8:2 + cg * 8 + 8], [tk], tk)
                for s in range(8):
                    T = 2 + cg * 8 + s
                    for ex in exs:
                        S.dma("pool", lambda e, tk=tk, s=s, ex=ex, T=T: e.indirect_dma_start(
                            out=XE[ex][:, :], out_offset=bass.IndirectOffsetOnAxis(
                                ap=idxT[:].rearrange("p a b -> p (a b)")[:, ex * 64 + T - 2:ex * 64 + T - 1], axis=0),
                            in_=tk[:, s, :], in_offset=None, bounds_check=breg(e), oob_is_err=False),
                            reads=rs([tk, idxT]), writes=[xe_res[ex]], owner=tk.r, group=("xe", l, ex), cost=1.2, lat=4.0)

        egroups = [[0, 1], [2, 3, 4, 5], [6, 7, 8, 9], [10, 11, 12, 13], [14, 15]]
        gstart = {g[0]: gi for gi, g in enumerate(egroups)}
        gater = [Res("gate%d" % i) for i in range(8)]
        dispatch(egroups[0])
        load_w(0)
        load_w(1)

        def ctx_dense(ex, wg_, wu_, wd_):
                def ffn_chunk(h_src, N, g_):
                    for fc in range(4):
                        ba = 2 * (fc % 2); bu = ba + 1
                        for k in range(8):
                            mm(pbig[:, ba, :N], wg_[:, k, fc * 128:(fc + 1) * 128], h_src[0][:, k, :N], k == 0, k == 7,
                               [wg_, h_src[1]], [pbr[ba]])
                        for k in range(8):
                            mm(pbig[:, bu, :N], wu_[:, k, fc * 128:(fc + 1) * 128], h_src[0][:, k, :N], k == 0, k == 7,
                               [wu_, h_src[1]], [pbr[bu]])
                        s_ = sa[fc % 2]
                        act(s_[:, :N], pbig[:, ba, :N], AF.Silu, [pbr[ba]], [s_])
                        tt(g_[:, fc, :N], pbig[:, bu, :N], s_[:, :N], ALU.mult, [pbr[bu], s_], [g_])

                def down(g_, s):
                    for hf in range(2):
                        for fc in range(4):
                            mm(pbig[:, 4 + hf, :], g_[:, fc, s * 128:(s + 1) * 128], wd_[:, fc, hf * 512:(hf + 1) * 512],
                               fc == 0, fc == 3, [g_, wd_], [pbr[4 + hf]])
                    return pbig[:, 4:6, :]

                if not last:
                    g_ = gT[0]
                    ffn_chunk((h2g, h2g), 256, g_)
                    for s in range(2):
                        py = down(g_, s)
                        av_ = acc[:, s, :].rearrange("p (a b) -> p a b", a=2)
                        if ex == 0:
                            ts(av_, py, wsel[:, s, ex:ex + 1], None, ALU.mult, None, [pbr[4], pbr[5], wsel], [accr[s]])
                        else:
                            stt(av_, py, wsel[:, s, ex:ex + 1], av_, ALU.mult, ALU.add,
                                [pbr[4], pbr[5], wsel, accr[s]], [accr[s]])

        def gen_prep(ex, ci):
            h_ = h2e[ci]
            for s in range(4):
                st_ = ci * 4 + s
                x_ = xet[st_ % 2]
                dma("sp", x_[:], XE[ex][st_ * 128:(st_ + 1) * 128, :], [xe_res[ex]], [x_], x_)
                cp(idxe[st_][:], x_[:, 1024:1026].bitcast(I32), [x_], [idxe[st_]])
                tt(gate[:, st_:st_ + 1], x_[:, 1026 + ex:1027 + ex], x_[:, 1042 + ex:1043 + ex], ALU.add, [x_], [gater[st_]])
                pt_ = ptr[st_ % 2]
                for k in range(8):
                    mm(pt_[:, k, :], x_[:, k * 128:(k + 1) * 128], ident[:], True, True, [x_, ident], [pt_], tr=True)
                yield
                for k in range(8):
                    act(h_[:, k, s * 128:(s + 1) * 128], pt_[:, k, :], AF.Identity, [pt_, modT], [h_],
                        bias=modT[:, 24 + k:25 + k], scale=modT[:, 32 + k:33 + k])
                    if k % 4 == 3:
                        yield

        def gen_ffn(ex, ci, wg_, wu_, wd_):
            h_ = h2e[ci]
            g_ = gT[ci]
            for fc in range(4):
                ba = 2 * (fc % 2); bu = ba + 1
                for k in range(8):
                    mm(pbig[:, ba, :], wg_[:, k, fc * 128:(fc + 1) * 128], h_[:, k, :], k == 0, k == 7, [wg_, h_], [pbr[ba]])
                for k in range(8):
                    mm(pbig[:, bu, :], wu_[:, k, fc * 128:(fc + 1) * 128], h_[:, k, :], k == 0, k == 7, [wu_, h_], [pbr[bu]])
                s_ = sa[fc % 2]
                act(s_[:], pbig[:, ba, :], AF.Silu, [pbr[ba]], [s_])
                tt(g_[:, fc, :], pbig[:, bu, :], s_[:], ALU.mult, [pbr[bu], s_], [g_])
                yield
            for s in range(4):
                st_ = ci * 4 + s
                for hf in range(2):
                    for fc in range(4):
                        mm(pbig[:, 4 + hf, :], g_[:, fc, s * 128:(s + 1) * 128], wd_[:, fc, hf * 512:(hf + 1) * 512],
                           fc == 0, fc == 3, [g_, wd_], [pbr[4 + hf]])
                y_ = yo[st_ % 2]
                act(y_[:].rearrange("p (a b) -> p a b", a=2), pbig[:, 4:6, :], AF.Identity, [pbr[4], pbr[5], gater[st_]], [y_],
                    scale=gate[:, st_:st_ + 1])
                S.dma("pool", lambda e, y_=y_, ie_=idxe[st_]: e.indirect_dma_start(
                    out=FFN[:, :], out_offset=bass.IndirectOffsetOnAxis(ap=ie_[:, :], axis=0),
                    in_=y_[:, :], in_offset=None, compute_op=ALU.add),
                    reads=rs([y_, idxe[st_]]), writes=[S.dres("ffn")], owner=y_.r, group=("ffn", l, ex), cost=1.2, lat=8.0)
                yield

        prev = None
        for ex in range(NE):
            sl = ex % 2
            if ex in gstart and gstart[ex] + 1 < len(egroups):
                dispatch(egroups[gstart[ex] + 1])
            ctx_dense(ex, wgt[sl], wut[sl], wdt[sl])
            for ci in range(2):
                for _ in interleave([gen_prep(ex, ci), prev]):
                    pass
                if ci == 0 and ex >= 1 and ex + 1 < NE:
                    load_w(ex + 1)
                prev = gen_ffn(ex, ci, wgt[sl], wut[sl], wdt[sl])
        for _ in prev:
            pass

        g2b = ph("g2b", [128, D]); cg2b = ph("cg2b", [128, D])
        l2g = ph("l2g", [128, D]); l2b = ph("l2b", [128, D])
        dma("sp", g2b[:], MODROW[0, 5120:6144].partition_broadcast(128), [S.dres("modrow")], [g2b], g2b)
        dma("sp", cg2b[:], MODROW[1, 5120:6144].partition_broadcast(128), [S.dres("modrow")], [cg2b], cg2b)
        dma("sp", l2g[:], LN2G[l].partition_broadcast(128), [S.dres("ln2g")], [l2g], l2g)
        dma("sp", l2b[:], LN2B[l].partition_broadcast(128), [S.dres("ln2b")], [l2b], l2b)
        xmt = [ph("xmt%d" % i, [128, D]) for i in range(2)]
        fft = [ph("fft%d" % i, [128, D]) for i in range(2)]
        ot = [ph("ot%d" % i, [128, D]) for i in range(2)]
        y2 = [ph("y2%d" % i, [128, D]) for i in range(2)]
        st2 = [(ph("stats", [128, 2, 6]), ph("mv", [128, 2]), ph("rstd", [128, 1]), ph("nmr", [128, 1])) for _ in range(2)]

        def gen_ln2(T):
            xm_ = xmt[T % 2]; o_ = ot[T % 2]; y2_ = y2[T % 2]
            stats, mv, rstd, nmr = st2[T % 2]
            dma("sp", xm_[:], XMID[T * 128:(T + 1) * 128, :], [xmid_res[T]], [xm_], xm_)
            if T < 2:
                tt(y2_[:], acc[:, T, :], cg2b[:], ALU.mult, [accr[T], cg2b], [y2_])
            else:
                f_ = fft[T % 2]
                dma("sp", f_[:], FFN[T * 128:(T + 1) * 128, :], [S.dres("ffn")], [f_], f_)
                tt(y2_[:], f_[:], g2b[:], ALU.mult, [f_, g2b], [y2_])
            yield
            stt(y2_[:], xm_[:], ALPHA, y2_[:], ALU.mult, ALU.add, [xm_, y2_], [y2_])
            yield
            ln_stats(y2_, y2_, stats, mv, rstd, nmr)
            yield
            act(y2_[:], y2_[:], AF.Identity, [y2_, rstd, nmr], [y2_], bias=nmr[:, 0:1], scale=rstd[:, 0:1])
            yield
            tt(y2_[:], y2_[:], l2g[:], ALU.mult, [y2_, l2g], [y2_], eng="pool")
            yield
            tt(o_[:], y2_[:], l2b[:], ALU.add, [y2_, l2b], [o_], eng="pool")
            if last:
                ev = dma("sp", Y[(T - 2) * 128:(T - 1) * 128, :], o_[:], [o_], [y_res[T - 2]], o_)
                final.append(ev)
            else:
                dma("sp", XCUR[T * 128:(T + 1) * 128, :], o_[:], [o_], [xcur_res[T]], o_)
            yield

        tl_ = list(range(T0, NT))
        for j in range(0, len(tl_), 2):
            for _ in interleave([gen_ln2(T) for T in tl_[j:j + 2]]):
                pass

    S.emit(final_waits=final, reorder=reorder, only=only)
    if dbg:
        print("instr counts", {e: len(v) for e, v in S.ins.items()}, "waits", S.nwaits, flush=True)
    return nc


def _rope_tables():
    t = np.arange(8192)
    row = (t // 64).astype(np.float32); col = (t % 64).astype(np.float32)
    inv = (10000.0 ** (-np.arange(0, 32, 2, dtype=np.float32) / 32)).astype(np.float32)
    cs = np.ones((64, TOK), np.float32); sn = np.zeros((64, TOK), np.float32)
    for a, pos in enumerate((row, col)):
        ang = (pos[:, None] * inv[None, :]).astype(np.float32)
        c = np.cos(ang).T; s = np.sin(ang).T
        cs[a * 32:a * 32 + 16, 256:] = c; cs[a * 32 + 16:a * 32 + 32, 256:] = c
        sn[a * 32:a * 32 + 16, 256:] = -s; sn[a * 32 + 16:a * 32 + 32, 256:] = s
    cs = np.concatenate([cs, cs], 0); sn = np.concatenate([sn, sn], 0)
    return np.stack([cs * 0.125, sn * 0.125, cs, sn]).astype(np.float32)


def _win_ext(w_in):
    qa = w_in[:, :, 0:384]; ka = w_in[:, :, 384:512]; va = w_in[:, :, 512:640]
    bx = w_in[:, :, 640:896]; bb = w_in[:, :, 896:1152]; bc = w_in[:, :, 1152:1408]
    qn = w_in[:, :, 1408:1792]; kn = w_in[:, :, 1792:2176]; vn = w_in[:, :, 2176:2560]
    sw = np.concatenate([np.arange(16, 32), np.arange(0, 16), np.arange(48, 64), np.arange(32, 48)])

    def heads_sw(w, nh):
        idx = np.concatenate([h * 64 + sw for h in range(nh)])
        return w[:, :, idx]

    def qperm(w):
        idx = np.concatenate([np.concatenate([np.arange(c * 64, c * 64 + 64), np.arange((3 + c) * 64, (3 + c) * 64 + 64)])
                              for c in range(3)])
        return w[:, :, idx]

    return np.ascontiguousarray(np.concatenate(
        [qperm(qa), qperm(heads_sw(qa, 6)), ka, heads_sw(ka, 2), bx, bb, bc, qn, kn, va, vn], axis=2))


def _na_bias(rpb):
    NEG = -30000.0
    out = np.full((DEPTH, 5, 6, 5, 128, 128), NEG, np.float32)
    cq = np.arange(64)
    col_start = np.clip(cq - 8, 0, 48)
    col_ok = (cq[None, :] >= col_start[:, None]) & (cq[None, :] < col_start[:, None] + 16)
    coff = np.clip(cq[None, :] - cq[:, None], -15, 15) + 15
    variants = [10, 0, 1, 62, 63]
    for vi, P in enumerate(variants):
        base = min(max(P - 2, 0), 59)
        for rho in range(2):
            r = 2 * P + rho
            rs_ = min(max(r - 4, 0), 120)
            for j in range(5):
                for kap in range(2):
                    kr = 2 * (base + j) + kap
                    if not (rs_ <= kr < rs_ + 8):
                        continue
                    roff = kr - r + 7
                    b = rpb[:, :, roff, :][:, :, coff]
                    b = np.where(col_ok[None, None], b, NEG)
                    out[:, vi, :, j, kap * 64:(kap + 1) * 64, rho * 64:(rho + 1) * 64] = np.transpose(b, (0, 1, 3, 2))
    out = np.transpose(out, (0, 1, 4, 2, 3, 5)).reshape(DEPTH, 5, 128, 6, 640)
    return np.ascontiguousarray(out)


def _amask():
    k = np.arange(128)[:, None]; q = np.arange(128)[None, :]
    mp = np.where(k >= q, 0.0, -30000.0).astype(np.float32); mn = np.where(k <= q, 0.0, -30000.0).astype(np.float32)
    return np.ascontiguousarray(np.stack([np.tile(mp, (1, 3)), np.tile(mn, (1, 3))], axis=1))


def make_in_maps(x, c, ctx, c_ctx, w_mod, b_mod, w_in, conv_w, attn_sink, na_rpb, w_out,
                 ln1_g, ln1_b, w_router, w_gate, w_up, w_down, ln2_g, ln2_b):
    f = lambda a: np.ascontiguousarray(np.asarray(a, dtype=np.float32))
    shared = dict(
        w_mod=f(w_mod), b_mod=f(b_mod), w_in=_win_ext(f(w_in)), rope=_rope_tables(),
        convw=np.ascontiguousarray(np.transpose(f(conv_w).reshape(DEPTH, 3, 2, 128), (0, 3, 2, 1))),
        sink=f(attn_sink), nab=_na_bias(f(na_rpb)), amask=_amask(), w_out=f(w_out),
        ln1_g=f(ln1_g), ln1_b=f(ln1_b), ln2_g=f(ln2_g), ln2_b=f(ln2_b), w_router=f(w_router),
        w_gate=f(w_gate), w_up=f(w_up), w_down=f(w_down))
    x = f(x); ctx = f(ctx); c = f(c); c_ctx = f(c_ctx)
    maps = []
    for b in range(N_CORES):
        m = dict(shared)
        m["xin"] = np.ascontiguousarray(np.concatenate([ctx[b], x[b]], axis=0))
        m["cvec"] = np.ascontiguousarray(np.stack([c[b], c_ctx], axis=0))
        maps.append(m)
    return maps


_NC = {}


def kernel(**inputs):
    if "nc" not in _NC:
        _NC["nc"] = build_nc(reorder=False)
    maps = make_in_maps(**inputs)
    res = run_bass_kernel_spmd(_NC["nc"], maps, core_ids=list(range(N_CORES)))
    return np.stack([np.asarray(r["y"], dtype=np.float32) for r in res.results], axis=0)
```

```python
import numpy as np
import concourse.bass as bass
import concourse.mybir as mybir
from concourse.bass_utils import run_bass_kernel_spmd

F32 = mybir.dt.float32
BF16 = mybir.dt.bfloat16
I32 = mybir.dt.int32
AF = mybir.ActivationFunctionType
ALU = mybir.AluOpType
AX = mybir.AxisListType

D = 1024
NT = 66
TOK = NT * 128
DEPTH = 2
ALPHA = float((2 * DEPTH) ** 0.25)
NE = 16
RW = 1024 + 2 + 32
COMPUTE = ("pe", "dve", "act", "pool")
N_CORES = 4


class Res:
    __slots__ = ("name", "w", "r", "dsem", "wg")

    def __init__(self, name="r"):
        self.name = name
        self.w = None
        self.r = []
        self.dsem = {}
        self.wg = None


class Sched:
    def __init__(self, nc):
        self.nc = nc
        self.ins = {e: [] for e in ("pe", "dve", "act", "pool", "sp")}
        self.dram = {}
        self.ndsem = 0
        self.owners = []
        self.free = {"sp": [], "pool": []}
        self.phase = 0
        self.phase_ev = {0: []}
        self.last_dma = {}
        self.pool_dmas = []
        self.pool_throttle = 0
        self.last_pew = {}

    def dres(self, *key):
        r = self.dram.get(key)
        if r is None:
            r = self.dram[key] = Res(str(key))
        return r

    def _deps(self, eng, reads, writes, pe_acc, group=None):
        deps = []
        for r in reads:
            if r.w is not None:
                if isinstance(r.w, list):
                    deps.extend(r.w)
                else:
                    deps.append(r.w)
        for r in writes:
            if r.w is not None:
                if isinstance(r.w, list):
                    if not (group is not None and r.wg == group):
                        deps.extend(r.w)
                elif not (pe_acc and r.w[0] == "E" and r.w[1] == "pe" and eng == "pe"):
                    deps.append(r.w)
            deps.extend(r.r)
        return deps

    def _post(self, ev, reads, writes, group=None):
        for r in reads:
            r.r.append(ev)
        for r in writes:
            if group is not None and r.wg == group and isinstance(r.w, list):
                r.w.append(ev)
            else:
                r.w = [ev] if group is not None else ev
                r.wg = group
                r.r = []

    def op(self, eng, fn, reads=(), writes=(), pe_acc=False, cost=0.5):
        deps = self._deps(eng, reads, writes, pe_acc)
        idx = len(self.ins[eng])
        order = []
        if eng == "pe":
            for r in writes:
                p = self.last_pew.get(id(r))
                if p is not None:
                    order.append(p)
                self.last_pew[id(r)] = idx
        ev = ("E", eng, idx)
        self.ins[eng].append([fn, deps, None, self.phase, cost, order, cost])
        self._post(ev, reads, writes)
        return ev

    def dma(self, q, fn, reads=(), writes=(), owner=None, group=None, cost=0.1, lat=3.0):
        deps = self._deps(q, reads, writes, False, group)
        sc = owner.dsem.get(q)
        if sc is None:
            if self.free[q]:
                sc = list(self.free[q].pop())
            else:
                sc = [self.ndsem, 0]
                self.ndsem += 1
            owner.dsem[q] = sc
            self.owners.append((owner, q))
        sc[1] += 16
        ev = ("D", sc[0], sc[1])
        if q == "pool" and self.pool_throttle:
            if len(self.pool_dmas) >= self.pool_throttle:
                deps.append(self.pool_dmas[-self.pool_throttle])
            self.pool_dmas.append(ev)
        idx = len(self.ins[q])
        order = []
        p = self.last_dma.get(sc[0])
        if p is not None:
            order.append(p)
        self.last_dma[sc[0]] = idx
        self.ins[q].append([fn, deps, sc[0], self.phase, cost, order, cost + lat, ev])
        self._post(ev, reads, writes, group)
        return ev

    def barrier(self):
        evs = []
        for e in COMPUTE:
            for i in range(len(self.ins[e]) - 1, -1, -1):
                if self.ins[e][i][2] is None:
                    evs.append(("E", e, i))
                    break
        for (o, q) in self.owners:
            sc = o.dsem.pop(q)
            evs.append(("D", sc[0], sc[1]))
            self.free[q].append((sc[0], sc[1]))
        self.owners = []
        self.phase += 1
        self.phase_ev[self.phase] = evs

    def _schedule(self, only=None):
        import heapq
        engs = list(self.ins.keys())
        dprod = {}
        for q in ("sp", "pool"):
            for i, rec in enumerate(self.ins[q]):
                if rec[2] is not None:
                    dprod[(rec[7][1], rec[7][2])] = (q, i)
        fin = {}
        issue = {}
        order = {e: [] for e in engs}
        ptr0 = {e: 0 for e in engs}
        tnow = 0.0
        for ph in range(self.phase + 1):
            nodes = []
            for e in engs:
                lst = self.ins[e]
                i = ptr0[e]
                while i < len(lst) and lst[i][3] == ph:
                    nodes.append((e, i))
                    i += 1
                ptr0[e] = i
            if not nodes:
                continue
            if only is not None and ph not in only:
                for (e, i) in nodes:
                    order[e].append(i)
                continue
            inph = set(nodes)
            ndep = {}
            users = {}
            for (e, i) in nodes:
                rec = self.ins[e][i]
                preds = set()
                for d in rec[1]:
                    p = (d[1], d[2]) if d[0] == "E" else dprod.get((d[1], d[2]))
                    if p is not None and p in inph and p != (e, i):
                        preds.add((p, 0))
                for j in rec[5]:
                    if (e, j) in inph:
                        preds.add(((e, j), 1))
                ndep[(e, i)] = len(preds)
                for pk in preds:
                    users.setdefault(pk[0], []).append(((e, i), pk[1]))
            ready = {}
            heaps = {e: [] for e in engs}
            avail = {e: [] for e in engs}
            efree = {e: tnow for e in engs}
            for n in nodes:
                ready[n] = tnow
                if ndep[n] == 0:
                    heapq.heappush(heaps[n[0]], (tnow, n[1]))
            left = len(nodes)
            tmax = tnow
            while left:
                best = None
                for e in engs:
                    if avail[e]:
                        st_ = efree[e]
                    elif heaps[e]:
                        st_ = max(heaps[e][0][0], efree[e])
                    else:
                        continue
                    if best is None or st_ < best[0]:
                        best = (st_, e)
                st_, e = best
                while heaps[e] and heaps[e][0][0] <= st_:
                    heapq.heappush(avail[e], heapq.heappop(heaps[e])[1])
                i = heapq.heappop(avail[e])
                rec = self.ins[e][i]
                issue[(e, i)] = st_
                efree[e] = st_ + rec[4]
                f_ = st_ + rec[6]
                fin[(e, i)] = f_
                tmax = max(tmax, f_)
                order[e].append(i)
                left -= 1
                for (u, kind) in users.get((e, i), ()):
                    t_ = f_ if kind == 0 else st_
                    if t_ > ready[u]:
                        ready[u] = t_
                    ndep[u] -= 1
                    if ndep[u] == 0:
                        heapq.heappush(heaps[u[0]], (ready[u], u[1]))
            tnow = tmax
        self.sim_time = tnow
        return order

    def _check(self, order, val):
        sems = {}
        pos = {e: 0 for e in self.ins}
        curph = {e: 0 for e in self.ins}
        progress = True
        total = sum(len(v) for v in self.ins.values())
        done = 0
        while progress:
            progress = False
            for e in self.ins:
                while pos[e] < len(order[e]):
                    i = order[e][pos[e]]
                    rec = self.ins[e][i]
                    deps = list(rec[1])
                    if rec[3] != curph[e]:
                        for p in range(curph[e] + 1, rec[3] + 1):
                            deps.extend(self.phase_ev.get(p, ()))
                    ok = True
                    for d in deps:
                        if d[0] == "E":
                            if sems.get(("E", d[1]), 0) < val[d[1]][d[2]]:
                                ok = False
                                break
                        elif sems.get(("D", d[1]), 0) < d[2]:
                            ok = False
                            break
                    if not ok:
                        break
                    curph[e] = rec[3]
                    if rec[2] is not None:
                        sems[("D", rec[2])] = sems.get(("D", rec[2]), 0) + 16
                    elif i in val.get(e, {}):
                        sems[("E", e)] = val[e][i]
                    pos[e] += 1
                    done += 1
                    progress = True
        if done != total:
            msg = []
            for e in self.ins:
                if pos[e] < len(order[e]):
                    i = order[e][pos[e]]
                    msg.append((e, pos[e], i, self.ins[e][i][3], self.ins[e][i][1][:6]))
            raise RuntimeError("schedule deadlock: %s" % msg)

    def emit(self, final_waits=(), reorder=True, only=None):
        import contextlib
        nc = self.nc
        if reorder:
            order = self._schedule(only)
        else:
            order = {e: list(range(len(l))) for e, l in self.ins.items()}
        for e in self.ins:
            assert sorted(order[e]) == list(range(len(self.ins[e]))), e
        lastc = {}
        for e in COMPUTE:
            cur = None
            per = {}
            for i in order[e]:
                if self.ins[e][i][2] is None:
                    per[self.ins[e][i][3]] = i
            lastc[e] = per
        for p in list(self.phase_ev.keys()):
            evs = [d for d in self.phase_ev[p] if d[0] == "D"]
            for e in COMPUTE:
                qs = [q for q in lastc[e] if q < p]
                if qs:
                    evs.append(("E", e, lastc[e][max(qs)]))
            self.phase_ev[p] = evs
        need = {e: set() for e in COMPUTE}
        for e, lst in self.ins.items():
            for rec in lst:
                for d in rec[1]:
                    if d[0] == "E":
                        need[d[1]].add(d[2])
        for evs in self.phase_ev.values():
            for d in evs:
                if d[0] == "E":
                    need[d[1]].add(d[2])
        val = {}
        for e in COMPUTE:
            c = 0
            v = {}
            for i in order[e]:
                if i in need[e]:
                    c += 1
                    v[i] = c
            val[e] = v
        self._check(order, val)
        self.nwaits = {}
        with contextlib.ExitStack() as st:
            esem = {e: st.enter_context(nc.semaphore("s_" + e)) for e in COMPUTE}
            dsem = [st.enter_context(nc.semaphore("d%d" % i)) for i in range(self.ndsem)]
            block = st.enter_context(nc.Block())

            def run(ename, eng):
                waited = {}
                lst = self.ins[ename]
                cur_ph = 0
                for i in order[ename]:
                    rec = lst[i]
                    deps = rec[1]
                    if rec[3] != cur_ph:
                        deps = list(deps)
                        for p in range(cur_ph + 1, rec[3] + 1):
                            deps.extend(self.phase_ev.get(p, ()))
                        cur_ph = rec[3]
                    tg = {}
                    for d in deps:
                        if d[0] == "E":
                            key = ("E", d[1]); v = val[d[1]][d[2]]; sem = esem[d[1]]
                        else:
                            key = ("D", d[1]); v = d[2]; sem = dsem[d[1]]
                        if tg.get(key, (None, 0))[1] < v:
                            tg[key] = (sem, v)
                    for key, (sem, v) in tg.items():
                        if waited.get(key, 0) >= v:
                            continue
                        eng.wait_ge(sem, v)
                        waited[key] = v
                        self.nwaits[ename] = self.nwaits.get(ename, 0) + 1
                    ins = rec[0](eng)
                    if rec[2] is not None:
                        ins.then_inc(dsem[rec[2]], 16)
                    elif i in need[ename]:
                        ins.then_inc(esem[ename], 1)
                if ename == "sp":
                    for d in final_waits:
                        eng.wait_ge(dsem[d[1]], d[2])

            block.tensor(lambda e: run("pe", e))
            block.vector(lambda e: run("dve", e))
            block.scalar(lambda e: run("act", e))
            block.gpsimd(lambda e: run("pool", e))
            block.sync(lambda e: run("sp", e))


def interleave(gens):
    gens = [g for g in gens if g is not None]
    while gens:
        nxt = []
        for g in gens:
            try:
                next(g)
                nxt.append(g)
            except StopIteration:
                pass
            yield
        gens = nxt


class Tl:
    __slots__ = ("t", "r")

    def __init__(self, t, name):
        self.t = t
        self.r = Res(name)

    def __getitem__(self, k):
        return self.t[k]


def build_nc(dbg=False, depth_run=DEPTH, reorder=True, only=None):
    nc = bass.Bass("TRN2", target_bir_lowering=False)
    S = Sched(nc)

    def din(name, shape, dt=F32):
        return nc.dram_tensor(name, list(shape), dt, kind="ExternalInput").ap()

    def dscr(name, shape, dt):
        return nc.dram_tensor(name, list(shape), dt, kind="ExternalOutput" if dbg else "Internal").ap()

    XIN = din("xin", [TOK, D])
    CVEC = din("cvec", [2, D])
    WMOD = din("w_mod", [DEPTH, D, 6 * D])
    BMOD = din("b_mod", [DEPTH, 6 * D])
    WIN = din("w_in", [DEPTH, D, 3072])
    ROPE = din("rope", [4, 128, TOK])
    CONVW = din("convw", [DEPTH, 128, 2, 3])
    SINK = din("sink", [DEPTH, 6])
    NAB = din("nab", [DEPTH, 5, 128, 6, 640])
    AMASK = din("amask", [128, 2, 384])
    WOUT = din("w_out", [DEPTH, D, D])
    LN1G = din("ln1_g", [DEPTH, D]); LN1B = din("ln1_b", [DEPTH, D])
    LN2G = din("ln2_g", [DEPTH, D]); LN2B = din("ln2_b", [DEPTH, D])
    WR = din("w_router", [DEPTH, D, NE])
    WG = din("w_gate", [DEPTH, NE, D, 512]); WU = din("w_up", [DEPTH, NE, D, 512])
    WD = din("w_down", [DEPTH, NE, 512, D])
    Y = nc.dram_tensor("y", [8192, D], F32, kind="ExternalOutput").ap()

    MODROW = dscr("modrow", [2, 6 * D], F32)
    FM = dscr("fm", [128, 14, TOK], BF16)
    VV = dscr("vv", [TOK, 8, 65], BF16)
    XMID = dscr("xmid", [TOK, D], F32)
    H2T = dscr("h2t", [128, 8, TOK], BF16)
    XCUR = dscr("xcur", [TOK, D], F32)
    XH2 = dscr("xh2", [TOK, RW], BF16)
    XE = [dscr("xe%d" % e, [1024, RW], BF16) for e in range(NE)]
    FFN = dscr("ffn", [TOK, D], F32)

    SB_LO = 16512
    SB_HI = 229344
    st = {"pers": SB_LO, "ph": None}

    def _alloc(name, shape, dt, key):
        nb = int(np.prod(shape[1:])) * (2 if dt == BF16 else 4)
        nb = (nb + 31) // 32 * 32
        off = st[key]
        assert off + nb <= SB_HI, (name, off, nb)
        st[key] = off + nb
        return Tl(nc.alloc_sbuf_tensor_at(name, list(shape), dt, offset=off), name)

    def pers(name, shape, dt=F32):
        return _alloc(name, shape, dt, "pers")

    cnt = [0]

    def ph(name, shape, dt=F32):
        cnt[0] += 1
        return _alloc("%s_%d" % (name, cnt[0]), shape, dt, "ph")

    def new_phase():
        S.barrier()
        st["ph"] = st["pers_end"]

    pbig = Tl(nc.alloc_psum_tensor("pbig", [128, 6, 512], F32), "pbig")
    ptr = [Tl(nc.alloc_psum_tensor("ptr%d" % i, [128, 8, 128], BF16), "ptr%d" % i) for i in range(2)]
    pbr = [Res("pb%d" % i) for i in range(6)]

    ident = pers("ident", [128, 128], BF16)
    identf = pers("identf", [128, 128], F32)
    onesf = pers("onesf", [128, 128], F32)
    aff = pers("aff", [128, NT, NE], F32)
    wsel = pers("wsel", [128, NT, NE], F32)
    modT = pers("modT", [128, 48], F32)
    modcT = pers("modcT", [128, 48], F32)
    esink = pers("esink", [128, 6], F32)
    convw = pers("convw", [128, 2, 3], F32)
    amask = pers("amask", [128, 2, 384], BF16)
    eps_t = pers("eps", [128, 1], F32)
    ltri = pers("ltri", [128, 128], F32)
    idxT = pers("idxT", [128, NE, 64], I32)
    st["pers_end"] = st["pers"]
    st["ph"] = st["pers_end"]

    _breg = {}

    def breg(e):
        if "r" not in _breg:
            _breg["r"] = e.to_reg(1023)
        return _breg["r"]

    def rs(lst):
        return [x.r if isinstance(x, Tl) else x for x in lst]

    def fsz(ap):
        try:
            return float(ap.free_size())
        except Exception:
            return 512.0

    def mm(out, lhsT, rhs, start, stop, reads, writes, tr=False):
        if tr:
            S.op("pe", lambda e: e.matmul(out, lhsT=lhsT, rhs=rhs, is_transpose=True),
                 reads=rs(reads), writes=rs(writes), pe_acc=True, cost=0.08)
        else:
            c = max(fsz(rhs), 64.0) / 2000.0 * (4.0 if lhsT.dtype == F32 else 1.0) + 0.03
            S.op("pe", lambda e: e.matmul(out, lhsT=lhsT, rhs=rhs, start=start, stop=stop),
                 reads=rs(reads), writes=rs(writes), pe_acc=True, cost=c)

    def act(out, in_, func, reads, writes, bias=0.0, scale=1.0, accum=None):
        if accum is None:
            S.op("act", lambda e: e.activation(out=out, in_=in_, func=func, bias=bias, scale=scale),
                 reads=rs(reads), writes=rs(writes), cost=0.2 + fsz(out) / 1300.0)
        else:
            S.op("act", lambda e: e.activation(out=out, in_=in_, func=func, bias=bias, scale=scale,
                                               accum_out=accum), reads=rs(reads), writes=rs(writes))

    def vcost(eng, ap):
        return (0.1 + fsz(ap) / 900.0) if eng == "dve" else (0.2 + fsz(ap) / 450.0)

    def tt(out, in0, in1, op, reads, writes, eng="dve"):
        S.op(eng, lambda e: e.tensor_tensor(out=out, in0=in0, in1=in1, op=op), reads=rs(reads), writes=rs(writes),
             cost=vcost(eng, out))

    def ts(out, in0, s1, s2, op0, op1, reads, writes, eng="dve"):
        if op1 is None:
            S.op(eng, lambda e: e.tensor_scalar(out=out, in0=in0, scalar1=s1, scalar2=None, op0=op0),
                 reads=rs(reads), writes=rs(writes), cost=vcost(eng, out))
        else:
            S.op(eng, lambda e: e.tensor_scalar(out=out, in0=in0, scalar1=s1, scalar2=s2, op0=op0, op1=op1),
                 reads=rs(reads), writes=rs(writes), cost=vcost(eng, out))

    def stt(out, in0, scalar, in1, op0, op1, reads, writes):
        S.op("dve", lambda e: e.scalar_tensor_tensor(out=out, in0=in0, scalar=scalar, in1=in1, op0=op0, op1=op1),
             reads=rs(reads), writes=rs(writes), cost=vcost("dve", out))

    def cp(out, in_, reads, writes, eng="dve"):
        S.op(eng, lambda e: e.tensor_copy(out=out, in_=in_), reads=rs(reads), writes=rs(writes), cost=vcost(eng, out))

    def recip(out, in_, reads, writes):
        S.op("dve", lambda e: e.reciprocal(out=out, in_=in_), reads=rs(reads), writes=rs(writes))

    def reduce(out, in_, op, reads, writes, negate=False):
        S.op("dve", lambda e: e.tensor_reduce(out=out, in_=in_, axis=AX.X, op=op, negate=negate),
             reads=rs(reads), writes=rs(writes), cost=vcost("dve", in_))

    def memset(eng, ap, val, writes):
        S.op(eng, lambda e: e.memset(ap, val), writes=rs(writes), cost=vcost(eng, ap))

    def single(out, in_, scalar, op, reads, writes):
        S.op("dve", lambda e: e.tensor_single_scalar(out=out, in_=in_, scalar=scalar, op=op),
             reads=rs(reads), writes=rs(writes))

    def scan(out, d0, d1, reads, writes):
        S.op("dve", lambda e: e.tensor_tensor_scan(out=out, data0=d0, data1=d1, initial=0.0, op0=ALU.add, op1=ALU.add),
             reads=rs(reads), writes=rs(writes))

    def iota_tail(ap, base, writes):
        S.op("pool", lambda e: e.iota(ap, pattern=[[0, 1]], base=base, channel_multiplier=1), writes=rs(writes))

    def dma(q, out, in_, reads, writes, owner, slow=False, group=None):
        try:
            nbytes = float(out.nbytes())
        except Exception:
            nbytes = 1.0e5
        lat = 2.5 + nbytes / 1.5e5
        cost = 0.1 if q == "sp" else 1.0
        ow = owner.r if isinstance(owner, Tl) else owner
        if group is not None:
            return S.dma(q, lambda e: e.dma_start(out=out, in_=in_), reads=rs(reads), writes=rs(writes),
                         owner=ow, group=group, cost=cost, lat=lat)
        if slow:
            return S.dma(q, lambda e: e.dma_start(out=out, in_=in_, allow_slow_non_contiguous=True),
                         reads=rs(reads), writes=rs(writes), owner=ow, cost=cost, lat=lat + 3.0)
        return S.dma(q, lambda e: e.dma_start(out=out, in_=in_),
                     reads=rs(reads), writes=rs(writes), owner=ow, cost=cost, lat=lat)

    def ln_stats(src_ap, src_res, stats, mv, rstd, nmr=None):
        for h in range(2):
            S.op("dve", lambda e, h=h: e.bn_stats(out=stats[:, h, :], in_=src_ap[:, h * 512:(h + 1) * 512]),
                 reads=rs([src_res]), writes=rs([stats]))
        S.op("dve", lambda e: e.bn_aggr(out=mv[:], in_=stats[:].rearrange("p a b -> p (a b)")),
             reads=rs([stats]), writes=rs([mv]))
        act(rstd[:], mv[:, 1:2], AF.Sqrt, [mv, eps_t], [rstd], bias=eps_t[:, 0:1])
        S.op("dve", lambda e: e.reciprocal(out=rstd[:], in_=rstd[:]), reads=rs([rstd]), writes=rs([rstd]))
        if nmr is not None:
            stt(nmr[:], mv[:, 0:1], -1.0, rstd[:], ALU.mult, ALU.mult, [mv, rstd], [nmr])

    S.op("pool", lambda e: e.iota(identf[:], pattern=[[1, 128]], base=0, channel_multiplier=-1,
                                  allow_small_or_imprecise_dtypes=True), writes=rs([identf]))
    S.op("dve", lambda e: e.tensor_single_scalar(out=ident[:], in_=identf[:], scalar=0.0, op=ALU.is_equal),
         reads=rs([identf]), writes=rs([ident]))
    S.op("dve", lambda e: e.memset(onesf[:], 1.0), writes=rs([onesf]))
    S.op("dve", lambda e: e.tensor_single_scalar(out=ltri[:], in_=identf[:], scalar=0.0, op=ALU.is_gt),
         reads=rs([identf]), writes=rs([ltri]))
    S.op("dve", lambda e: e.memset(eps_t[:], 1e-6), writes=rs([eps_t]))
    dma("pool", amask[:], AMASK, [S.dres("amask")], [amask], amask)

    fm_res = [S.dres("fm", t) for t in range(NT)]
    vv_res = [S.dres("vv", t) for t in range(NT)]
    xmid_res = [S.dres("xmid", t) for t in range(NT)]
    h2t_res = [S.dres("h2t", t) for t in range(NT)]
    xcur_res = [S.dres("xcur", t) for t in range(NT)]
    xh2_res = [S.dres("xh2", t) for t in range(NT)]
    y_res = [S.dres("y", t) for t in range(64)]
    final = []

    for l in range(depth_run):
        last = l == DEPTH - 1
        T0 = 2 if last else 0
        new_phase()
        cT = ph("cT", [128, 8, 2])
        bm = ph("bm", [2, 6 * D])
        mrow = ph("mrow", [2, 6 * D])
        wm = [ph("wm%d" % i, [128, 8, 512]) for i in range(2)]
        for m_ in range(2):
            dma("sp", cT[:, :, m_], CVEC[m_].rearrange("(k p) -> p k", p=128), [S.dres("cvec")], [cT], cT, slow=True)
        dma("sp", bm[:], BMOD[l].partition_broadcast(2), [S.dres("bmod")], [bm], bm)
        dma("sp", esink[:], SINK[l].partition_broadcast(128), [S.dres("sink")], [esink], esink)
        dma("sp", convw[:], CONVW[l], [S.dres("convw")], [convw], convw)
        act(cT[:], cT[:], AF.Silu, [cT], [cT])
        act(esink[:], esink[:], AF.Exp, [esink], [esink])
        wmv = WMOD[l].rearrange("(k p) n -> p k n", p=128)
        for cc in range(12):
            w_ = wm[cc % 2]
            dma("sp", w_[:], wmv[:, :, cc * 512:(cc + 1) * 512], [S.dres("wmod")], [w_], w_)
            pb = pbig[0:2, cc % 2, :]
            for k in range(8):
                mm(pb, cT[:, k, :], w_[:, k, :], k == 0, k == 7, [cT, w_], [pbr[cc % 2]])
            tt(mrow[:, cc * 512:(cc + 1) * 512], pb, bm[:, cc * 512:(cc + 1) * 512], ALU.add,
               [pbr[cc % 2], bm], [mrow])
        dma("sp", MODROW, mrow[:], [mrow], [S.dres("modrow")], mrow)
        dma("sp", modT[:], MODROW[0].rearrange("(j p) -> p j", p=128), [S.dres("modrow")], [modT], modT, slow=True)
        dma("sp", modcT[:], MODROW[1].rearrange("(j p) -> p j", p=128), [S.dres("modrow")], [modcT], modcT, slow=True)
        for m_ in (modT, modcT):
            ts(m_[:, 8:16], m_[:, 8:16], 1.0, None, ALU.add, None, [m_], [m_])
            ts(m_[:, 32:40], m_[:, 32:40], 1.0, None, ALU.add, None, [m_], [m_])

        new_phase()
        win = ph("win", [128, 8, 3072], BF16)
        wiv = WIN[l].rearrange("(k p) n -> p k n", p=128)
        for k in range(8):
            dma("pool", win[:, k, :], wiv[:, k, :], [S.dres("win")], [win], win)
        xt = [ph("xt%d" % i, [128, D]) for i in range(2)]
        xh = [ph("xh%d" % i, [128, D], BF16) for i in range(2)]
        hT = [ph("hT%d" % i, [128, 8, 512], BF16) for i in range(2)]
        fmo = [ph("fmo%d" % i, [128, 14, 512], BF16) for i in range(2)]
        vo = [ph("vo%d" % i, [128, 4, 8, 65], BF16) for i in range(2)]
        rp = [ph("rp%d" % i, [128, 4, 512]) for i in range(2)]
        tmp = [ph("tmp%d" % i, [128, 512]) for i in range(3)]
        stats = ph("stats", [128, 2, 6]); mv = ph("mv", [128, 2]); rstd = ph("rstd", [128, 1])
        for v_ in vo:
            S.op("pool", lambda e, v_=v_: e.memset(v_[:], 1.0), writes=rs([v_]))
        src = XIN if l == 0 else XCUR
        src_res = (lambda t: S.dres("xin", t)) if l == 0 else (lambda t: xcur_res[t])
        groups = [(0, 2)] + [(2 + 4 * i, 4) for i in range(16)]
        bank = [0]

        def nextbank():
            b = bank[0]
            bank[0] = (b + 1) % 6
            return b

        def gen_L(gi, t0, nt):
            N = nt * 128
            sl = gi % 2
            mT = modcT if gi == 0 else modT
            rpt = rp[sl]
            dma("sp", rpt[:, :, :N], ROPE[:, :, t0 * 128:t0 * 128 + N].rearrange("a p n -> p a n"),
                [S.dres("rope")], [rpt], rpt)
            for s in range(nt):
                tl = t0 + s
                x_ = xt[tl % 2]; xh_ = xh[tl % 2]; pt_ = ptr[tl % 2]
                dma("sp", x_[:], src[tl * 128:(tl + 1) * 128, :], [src_res(tl)], [x_], x_)
                ln_stats(x_, x_, stats, mv, rstd)
                yield
                ts(xh_[:], x_[:], mv[:, 0:1], rstd[:, 0:1], ALU.subtract, ALU.mult, [x_, mv, rstd], [xh_])
                for k in range(8):
                    mm(pt_[:, k, :], xh_[:, k * 128:(k + 1) * 128], ident[:], True, True, [xh_, ident], [pt_], tr=True)
                yield
                for k in range(8):
                    act(hT[sl][:, k, s * 128:(s + 1) * 128], pt_[:, k, :], AF.Identity, [pt_, mT], [hT[sl]],
                        bias=mT[:, k:k + 1], scale=mT[:, 8 + k:9 + k])
                    if k % 4 == 3:
                        yield

        def gen_P(gi, t0, nt):
            N = nt * 128
            sl = gi % 2
            rpt = rp[sl]
            h_ = hT[sl]; fo = fmo[sl]

            def proj(ch):
                b = nextbank()
                for k in range(8):
                    mm(pbig[:, b, :N], win[:, k, ch * 128:(ch + 1) * 128], h_[:, k, :N], k == 0, k == 7,
                       [win, h_], [pbr[b]])
                return b

            def rope(chq, chs, tq, tsn, dst):
                b1 = proj(chq); b2 = proj(chs)
                tt(tmp[0][:, :N], pbig[:, b1, :N], rpt[:, tq, :N], ALU.mult, [pbr[b1], rpt], [tmp[0]])
                tt(tmp[1][:, :N], pbig[:, b2, :N], rpt[:, tsn, :N], ALU.mult, [pbr[b2], rpt], [tmp[1]])
                tt(fo[:, dst, :N], tmp[0][:, :N], tmp[1][:, :N], ALU.add, [tmp[0], tmp[1]], [fo], eng="pool")

            for c_ in range(3):
                rope(c_, 3 + c_, 0, 1, c_)
                yield
            rope(6, 7, 2, 3, 6)
            yield
            for c_ in range(2):
                b1 = proj(8 + c_)
                act(tmp[2][:, :N], pbig[:, b1, :N], AF.Identity, [pbr[b1]], [tmp[2]])
                b2 = proj(12 + c_)
                tt(fo[:, 10 + c_, :N], pbig[:, b2, :N], tmp[2][:, :N], ALU.mult, [pbr[b2], tmp[2]], [fo])
                yield
                b3 = proj(10 + c_)
                act(fo[:, 12 + c_, :N], pbig[:, b3, :N], AF.Identity, [pbr[b3]], [fo])
                yield
            for c_ in range(3):
                b1 = proj(14 + c_)
                act(fo[:, 3 + c_, :N], pbig[:, b1, :N], AF.Identity, [pbr[b1]], [fo], scale=0.125)
                yield
                b2 = proj(17 + c_)
                cp(fo[:, 7 + c_, :N], pbig[:, b2, :N], [pbr[b2]], [fo])
                yield
            for s in range(nt):
                b = nextbank()
                for k in range(8):
                    mm(pbig[:, b, :], h_[:, k, s * 128:(s + 1) * 128], win[:, k, 2560:3072], k == 0, k == 7,
                       [win, h_], [pbr[b]])
                act(vo[sl][:, s, :, 0:64], pbig[:, b, :].rearrange("p (h d) -> p h d", h=8), AF.Identity,
                    [pbr[b]], [vo[sl]])
                yield
            dma("sp", FM[:, :, t0 * 128:t0 * 128 + N], fo[:, :, :N], [fo], fm_res[t0:t0 + nt], fo)
            dma("sp", VV[t0 * 128:t0 * 128 + N].rearrange("(s p) h d -> p s h d", p=128), vo[sl][:, :nt],
                [vo[sl]], vv_res[t0:t0 + nt], vo[sl])
            yield

        prevP = None
        for gi, (t0, nt) in enumerate(groups):
            for _ in interleave([gen_L(gi, t0, nt), prevP]):
                pass
            prevP = gen_P(gi, t0, nt)
        for _ in prevP:
            pass

        new_phase()
        wout = ph("wout", [128, 8, D], BF16)
        wov = WOUT[l].rearrange("(k p) n -> p k n", p=128)
        for k in range(0, 8, 2):
            dma("pool", wout[:, k:k + 2, :], wov[:, k:k + 2, :], [S.dres("wout")], [wout], wout)
        wr = ph("wr", [128, 8, NE], BF16)
        dma("pool", wr[:], WR[l].rearrange("(k p) n -> p k n", p=128), [S.dres("wr")], [wr], wr)
        nabi = ph("nabi", [128, 6, 640], BF16)
        nabe = ph("nabe", [128, 6, 640], BF16)
        dma("pool", nabi[:], NAB[l, 0], [S.dres("nab")], [nabi], nabi)
        kctx = ph("kctx", [128, 4, 256], BF16)
        vctx = ph("vctx", [128, 2, 8, 65], BF16)
        dma("sp", kctx[:], FM[:, 6:10, 0:256], fm_res[0:2], [kctx], kctx)
        dma("sp", vctx[:], VV[0:256].rearrange("(s p) h d -> p s h d", p=128), vv_res[0:2], [vctx], vctx)
        g1b = ph("g1b", [128, D]); cg1b = ph("cg1b", [128, D])
        l1g = ph("l1g", [128, D]); l1b = ph("l1b", [128, D])
        dma("sp", g1b[:], MODROW[0, 2048:3072].partition_broadcast(128), [S.dres("modrow")], [g1b], g1b)
        dma("sp", cg1b[:], MODROW[1, 2048:3072].partition_broadcast(128), [S.dres("modrow")], [cg1b], cg1b)
        dma("sp", l1g[:], LN1G[l].partition_broadcast(128), [S.dres("ln1g")], [l1g], l1g)
        dma("sp", l1b[:], LN1B[l].partition_broadcast(128), [S.dres("ln1b")], [l1b], l1b)
        qw = [ph("qw%d" % i, [128, 6, 128], BF16) for i in range(2)]
        kw = [ph("kw%d" % i, [128, 4, 640], BF16) for i in range(2)]
        vw = [ph("vw%d" % i, [128, 5, 8, 65], BF16) for i in range(2)]
        uw = [ph("uw%d" % i, [128, 2, 130], BF16) for i in range(2)]
        bbw = [ph("bbw%d" % i, [128, 2, 128], BF16) for i in range(2)]
        xa = [ph("xa%d" % i, [128, D]) for i in range(2)]
        pta = [ph("pta%d" % i, [128, 5, 384], BF16) for i in range(2)]
        ptn = [ph("ptn%d" % i, [128, 896], BF16) for i in range(2)]
        sfn = [ph("sfn%d" % i, [128, 640]) for i in range(2)]
        mixc = ph("mixc", [128, 768], BF16)
        mixT = ph("mixT", [128, 8, 128], BF16)
        ctmp = ph("ctmp", [128, 128])
        den = ph("den", [128, 6]);
        t1 = ph("t1", [128, D]); y1 = ph("y1", [128, D]); xm = [ph("xm%d" % i, [128, D]) for i in range(2)]
        xh2 = ph("xh2", [128, RW], BF16)
        h2o = [ph("h2o%d" % i, [128, 8, 128], BF16) for i in range(2)]
        stats = ph("stats", [128, 2, 6]); mv = ph("mv", [128, 2]); rstd = ph("rstd", [128, 1]); nmr = ph("nmr", [128, 1])
        rmx = ph("rmx", [128, 1]); rsum = ph("rsum", [128, 1]); rexp = ph("rexp", [128, NE])

        mixT2 = [mixT, ph("mixTb", [128, 8, 128], BF16)]

        def gen_att(T):
            sl = T % 2
            is_ctx = T < 2
            i = T - 2
            q_ = qw[sl]; k_ = kw[sl]; v_ = vw[sl]; u_ = uw[sl]; bb_ = bbw[sl]
            mT_ = mixT2[sl]
            dma("sp", q_[:], FM[:, 0:6, T * 128:(T + 1) * 128], [fm_res[T]], [q_], q_)
            nb = nabi
            base = 0
            if not is_ctx:
                base = min(max(i - 2, 0), 59) + 2
                dma("sp", k_[:], FM[:, 6:10, base * 128:(base + 5) * 128], fm_res[base:base + 5], [k_], k_)
                dma("sp", v_[:], VV[base * 128:(base + 5) * 128].rearrange("(s p) h d -> p s h d", p=128),
                    vv_res[base:base + 5], [v_], v_)
                var = 0 if 2 <= i <= 61 else (1 + i if i < 2 else i - 59)
                if var != 0:
                    dma("pool", nabe[:], NAB[l, var], [S.dres("nab")], [nabe], nabe)
                    nb = nabe
            lo_pad = T in (0, 2); hi_pad = T in (1, NT - 1)
            if lo_pad or hi_pad:
                memset("pool", u_[:], 0.0, [u_])
            c0 = T * 128 - (0 if lo_pad else 1); c1 = (T + 1) * 128 + (0 if hi_pad else 1)
            o0 = 1 if lo_pad else 0
            fr = fm_res[max(T - 1, 0):min(T + 2, NT)]
            dma("sp", u_[:, :, o0:o0 + (c1 - c0)], FM[:, 10:12, c0:c1], fr, [u_], u_)
            dma("sp", bb_[:], FM[:, 12:14, T * 128:(T + 1) * 128], [fm_res[T]], [bb_], bb_)
            yield
            if is_ctx:
                akeys = [(("c", 0), None), (("c", 1), None)]
                nkeys = [("c", 0), ("c", 1)]
            else:
                akeys = []
                if i > 0:
                    akeys.append((("w", T - 1 - base), 0))
                akeys.append((("w", T - base), None))
                if i < 63:
                    akeys.append((("w", T + 1 - base), 1))
                akeys += [(("c", 0), None), (("c", 1), None)]
                nkeys = [("w", j) for j in range(5)] + [("c", 0), ("c", 1)]

            def kap(kt, ch, p0):
                if kt[0] == "c":
                    return kctx[p0:p0 + 64, ch, kt[1] * 128:(kt[1] + 1) * 128], kctx
                return k_[p0:p0 + 64, ch, kt[1] * 128:(kt[1] + 1) * 128], k_

            def vap(kt, head):
                if kt[0] == "c":
                    return vctx[:, kt[1], head, :], vctx
                return v_[:, kt[1], head, :], v_

            def gen_A():
                for g in range(2):
                    p0 = g * 64
                    pa_ = pta[g]
                    for ki, (kt, mk) in enumerate(akeys):
                        ka_, kr = kap(kt, 0, p0)
                        if mk is not None:
                            mm(pbig[:, 0, 0:384], ident[:], amask[:, mk, :], True, False, [ident, amask], [pbr[0]])
                        mm(pbig[:, 0, 0:384], ka_, q_[p0:p0 + 64, 0:3, :].rearrange("p a b -> p (a b)"), mk is None, True,
                           [kr, q_], [pbr[0]])
                        act(pa_[:, ki, :], pbig[:, 0, 0:384], AF.Exp, [pbr[0]], [pa_])
                        yield
                    po = pbig[:, 2, 0:195].rearrange("p (c d) -> p c d", c=3)
                    for c_ in range(3):
                        for ki, (kt, mk) in enumerate(akeys):
                            va_, vr = vap(kt, g)
                            mm(po[:, c_, :], pa_[:, ki, c_ * 128:(c_ + 1) * 128], va_, ki == 0, ki == len(akeys) - 1,
                               [pa_, vr], [pbr[2]])
                        yield
                    tt(den[:, 0:3], po[:, :, 64], esink[:, 3 * g:3 * g + 3], ALU.add, [pbr[2], esink], [den])
                    recip(den[:, 0:3], den[:, 0:3], [den], [den])
                    tt(mixc[:, g * 192:(g + 1) * 192].rearrange("p (c d) -> p c d", c=3), po[:, :, 0:64],
                       den[:, 0:3].unsqueeze(2).to_broadcast([128, 3, 64]), ALU.mult, [pbr[2], den], [mixcA])
                    yield

            def gen_N():
                po2 = pbig[:, 3, 0:390].rearrange("p (c d) -> p c d", c=6)
                nk = len(nkeys)
                for h in range(6):
                    ch = h // 2; p0 = (h % 2) * 64
                    pn_ = ptn[h % 2]; sf_ = sfn[h % 2]
                    ps2 = pbig[:, 4:6, :].rearrange("p a b -> p (a b)")
                    if is_ctx:
                        for j, kt in enumerate(nkeys):
                            ka_, kr = kap(kt, 1 + ch, p0)
                            mm(ps2[:, j * 128:(j + 1) * 128], ka_, q_[p0:p0 + 64, 3 + ch, :], True, True,
                               [kr, q_], [pbr[4], pbr[5]])
                        act(pn_[:, 0:256], ps2[:, 0:256], AF.Exp, [pbr[4], pbr[5]], [pn_])
                    else:
                        for j in (5, 6):
                            ka_, kr = kap(nkeys[j], 1 + ch, p0)
                            mm(ps2[:, j * 128:(j + 1) * 128], ka_, q_[p0:p0 + 64, 3 + ch, :], True, True,
                               [kr, q_], [pbr[4], pbr[5]])
                        mm(ps2[:, 512:640], ident[:], nb[:, h, 512:640], True, False, [ident, nb], [pbr[4], pbr[5]])
                        mm(ps2[:, 0:512], ident[:], nb[:, h, 0:512], True, False, [ident, nb], [pbr[4], pbr[5]])
                        for j in range(5):
                            ka_, kr = kap(nkeys[j], 1 + ch, p0)
                            mm(ps2[:, j * 128:(j + 1) * 128], ka_, q_[p0:p0 + 64, 3 + ch, :], False, j in (3, 4),
                               [kr, q_], [pbr[4], pbr[5]])
                        act(pn_[:, 0:896], ps2[:, 0:896], AF.Exp, [pbr[4], pbr[5]], [pn_])
                    yield
                    for j, kt in enumerate(nkeys):
                        va_, vr = vap(kt, 2 + h)
                        mm(po2[:, h, :], pn_[:, j * 128:(j + 1) * 128], va_, j == 0, j == nk - 1, [pn_, vr], [pbr[3]])
                    yield
                recip(den2[:], po2[:, :, 64], [pbr[3]], [den2])
                tt(mixc[:, 384:768].rearrange("p (c d) -> p c d", c=6), po2[:, :, 0:64],
                   den2[:].unsqueeze(2).to_broadcast([128, 6, 64]), ALU.mult, [pbr[3], den2], [mixcN])
                yield

            def gen_B():
                for c_ in range(2):
                    ts(ctmp[:], u_[:, c_, 0:128], convw[:, c_, 0:1], None, ALU.mult, None, [u_, convw], [ctmp])
                    stt(ctmp[:], u_[:, c_, 1:129], convw[:, c_, 1:2], ctmp[:], ALU.mult, ALU.add, [u_, convw, ctmp], [ctmp])
                    stt(ctmp[:], u_[:, c_, 2:130], convw[:, c_, 2:3], ctmp[:], ALU.mult, ALU.add, [u_, convw, ctmp], [ctmp])
                    tt(mT_[:, 3 + c_, :], ctmp[:], bb_[:, c_, :], ALU.mult, [ctmp, bb_], [mT_])
                    yield

            for _ in interleave([gen_A(), gen_N(), gen_B()]):
                yield
            pt_ = ptr[0]
            for c_ in range(6):
                mm(pt_[:, c_, :], mixc[:, c_ * 128:(c_ + 1) * 128], ident[:], True, True, [mixcA, mixcN, ident], [pt_], tr=True)
            cp(mT_[:, 0:3, :], pt_[:, 0:3, :], [pt_], [mT_])
            act(mT_[:, 5:8, :], pt_[:, 3:6, :], AF.Identity, [pt_], [mT_])
            yield

        def gen_epi(T):
            sl = T % 2
            is_ctx = T < 2
            x_ = xa[sl]; mT_ = mixT2[sl]
            dma("sp", x_[:], src[T * 128:(T + 1) * 128, :], [src_res(T)], [x_], x_)
            gb = cg1b if is_ctx else g1b
            for hf in range(2):
                for k in range(8):
                    mm(pbig[:, 1, :], mT_[:, k, :], wout[:, k, hf * 512:(hf + 1) * 512], k == 0, k == 7,
                       [mT_, wout], [pbr[1]])
                tt(t1[:, hf * 512:(hf + 1) * 512], pbig[:, 1, :], gb[:, hf * 512:(hf + 1) * 512], ALU.mult,
                   [pbr[1], gb], [t1])
                yield
            stt(y1[:], x_[:], ALPHA, t1[:], ALU.mult, ALU.add, [x_, t1], [y1])
            yield
            ln_stats(y1, y1, stats, mv, rstd, nmr)
            yield
            xm_ = xm[sl]
            act(t1[:], y1[:], AF.Identity, [y1, rstd, nmr], [t1], bias=nmr[:, 0:1], scale=rstd[:, 0:1])
            yield
            tt(t1[:], t1[:], l1g[:], ALU.mult, [t1, l1g], [t1], eng="pool")
            yield
            tt(xm_[:], t1[:], l1b[:], ALU.add, [t1, l1b], [xm_], eng="pool")
            dma("sp", XMID[T * 128:(T + 1) * 128, :], xm_[:], [xm_], [xmid_res[T]], xm_)
            yield
            ln_stats(xm_, xm_, stats, mv, rstd)
            yield
            ts(xh2[:, 0:D], xm_[:], mv[:, 0:1], rstd[:, 0:1], ALU.subtract, ALU.mult, [xm_, mv, rstd], [xh2])
            yield
            pt2 = ptr[1]
            for k in range(8):
                mm(pt2[:, k, :], xh2[:, k * 128:(k + 1) * 128], ident[:], True, True, [xh2, ident], [pt2], tr=True)
            mT = modcT if is_ctx else modT
            h2_ = h2o[sl]
            for k in range(8):
                act(h2_[:, k, :], pt2[:, k, :], AF.Identity, [pt2, mT], [h2_],
                    bias=mT[:, 24 + k:25 + k], scale=mT[:, 32 + k:33 + k])
                if k % 4 == 3:
                    yield
            if is_ctx:
                dma("sp", H2T[:, :, T * 128:(T + 1) * 128], h2_[:], [h2_], [h2t_res[T]], h2_)
            pr = pbig[:, 1, 0:NE]
            for k in range(8):
                mm(pr, h2_[:, k, :], wr[:, k, :], k == 0, k == 7, [h2_, wr], [pbr[1]])
            reduce(rmx[:], pr, ALU.max, [pbr[1]], [rmx], negate=True)
            act(rexp[:], pr, AF.Exp, [pbr[1], rmx], [rexp], bias=rmx[:, 0:1])
            yield
            reduce(rsum[:], rexp[:], ALU.add, [rexp], [rsum])
            recip(rsum[:], rsum[:], [rsum], [rsum])
            ts(aff[:, T, :], rexp[:], rsum[:, 0:1], None, ALU.mult, None, [rexp, rsum], [aff])
            if not is_ctx:
                ts(xh2[:, 1026:1042], rexp[:], rsum[:, 0:1], None, ALU.mult, None, [rexp, rsum], [xh2])
                stt(xh2[:, 1042:1058], rexp[:], rsum[:, 0:1], xh2[:, 1026:1042], ALU.mult, ALU.subtract,
                    [rexp, rsum, xh2], [xh2])
                iota_tail(xh2[:, 1024:1026].bitcast(I32), T * 128, [xh2])
                dma("sp", XH2[T * 128:(T + 1) * 128, :], xh2[:], [xh2], [xh2_res[T]], xh2)
            yield

        mixcA = Res("mixcA"); mixcN = Res("mixcN")
        den2 = ph("den2", [128, 6])
        prev = None
        for T in range(T0, NT):
            for _ in interleave([gen_att(T), prev]):
                pass
            prev = gen_epi(T)
        for _ in prev:
            pass

        new_phase()
        lo_t = ph("lo", [128, NE]); mid_t = ph("mid", [128, NE]); cntp = ph("cntp", [128, NE]); sel = ph("sel", [128, NE])
        cmp = ph("cmp", [128, NE, 64])
        incl = ph("incl", [128, NE, 64])
        zt = ph("zt", [128, 64])
        offs = ph("offs", [128, NE])
        zbig = ph("zbig", [128, 4096])
        memset("pool", zbig[:], 0.0, [zbig])
        memset("pool", zt[:], 0.0, [zt])
        for j in range(16):
            dma("sp", FFN[256 + j * 512:256 + (j + 1) * 512, :].rearrange("(p a) d -> p (a d)", p=128), zbig[:],
                [zbig], [S.dres("ffn")], zbig, group=("ffnz", l))
        sets = [(2, 64, 1024.0)] if last else [(0, 2, 32.0), (2, 64, 1024.0)]
        for (ta, tn, cap) in sets:
            av = aff[:, ta:ta + tn, :].rearrange("p t e -> p e t")
            memset("dve", lo_t[:], 0.0, [lo_t])
            for it in range(30):
                w_ = 0.5 ** (it + 1)
                ts(mid_t[:], lo_t[:], w_, None, ALU.add, None, [lo_t], [mid_t])
                tt(cmp[:, :, :tn], av, mid_t[:].unsqueeze(2).to_broadcast([128, NE, tn]), ALU.is_ge, [aff, mid_t], [cmp])
                reduce(cntp[:], cmp[:, :, :tn], ALU.add, [cmp], [cntp])
                mm(pbig[:, 0, 0:NE], onesf[:], cntp[:], True, True, [onesf, cntp], [pbr[0]])
                single(sel[:], pbig[:, 0, 0:NE], cap - 0.5, ALU.is_ge, [pbr[0]], [sel])
                stt(lo_t[:], sel[:], w_, lo_t[:], ALU.mult, ALU.add, [sel, lo_t], [lo_t])
            tt(cmp[:, :, :tn], av, lo_t[:].unsqueeze(2).to_broadcast([128, NE, tn]), ALU.is_ge, [aff, lo_t], [cmp])
            if dbg:
                LDBG = nc.dram_tensor("ldbg%d_%d" % (l, tn), [128, NE], F32, kind="ExternalOutput").ap()
                dma("sp", LDBG, lo_t[:], [lo_t], [S.dres("ldbg", tn)], lo_t)
                ADBG = nc.dram_tensor("adbg%d_%d" % (l, tn), [128, NT * NE], F32, kind="ExternalOutput").ap()
                dma("sp", ADBG, aff[:].rearrange("p a b -> p (a b)"), [aff], [S.dres("adbg", tn)], aff)
            if tn == 2:
                wv = wsel[:, ta:ta + tn, :].rearrange("p t e -> p e t")
                tt(wv, av, cmp[:, :, :tn], ALU.mult, [aff, cmp], [wsel])
                continue
            for ex in range(NE):
                scan(incl[:, ex, :], cmp[:, ex, :], zt[:], [cmp, zt], [incl])
            cp(cntp[:], incl[:, :, 63], [incl], [cntp])
            mm(pbig[:, 0, 0:NE], ltri[:], cntp[:], True, True, [ltri, cntp], [pbr[0]])
            cp(offs[:], pbig[:, 0, 0:NE], [pbr[0]], [offs])
            tt(incl[:], incl[:], cmp[:], ALU.subtract, [incl, cmp], [incl])
            tt(incl[:], incl[:], offs[:].unsqueeze(2).to_broadcast([128, NE, 64]), ALU.add, [incl, offs], [incl])
            stt(incl[:].rearrange("p a b -> p (a b)"), incl[:].rearrange("p a b -> p (a b)"), -1.0e6,
                cmp[:].rearrange("p a b -> p (a b)"), ALU.add, ALU.mult, [incl, cmp], [incl])
            ts(idxT[:], incl[:], 1.0e6, None, ALU.add, None, [incl], [idxT])
            if dbg:
                IDBG = nc.dram_tensor("idbg%d" % l, [128, NE * 64], I32, kind="ExternalOutput").ap()
                dma("sp", IDBG, idxT[:].rearrange("p a b -> p (a b)"), [idxT], [S.dres("idbg")], idxT)
                ODBG = nc.dram_tensor("odbg%d" % l, [128, NE], F32, kind="ExternalOutput").ap()
                dma("sp", ODBG, offs[:], [offs], [S.dres("odbg")], offs)
                CDBG = nc.dram_tensor("cdbg%d" % l, [128, NE * 64], F32, kind="ExternalOutput").ap()
                dma("sp", CDBG, cmp[:].rearrange("p a b -> p (a b)"), [cmp], [S.dres("cdbg")], cmp)

        new_phase()
        wgt = [ph("wg%d" % i, [128, 8, 512], BF16) for i in range(2)]
        wut = [ph("wu%d" % i, [128, 8, 512], BF16) for i in range(2)]
        wdt = [ph("wd%d" % i, [128, 4, D], BF16) for i in range(2)]
        tokc = [ph("tokc%d" % i, [128, 8, RW], BF16) for i in range(2)]
        xet = [ph("xet%d" % i, [128, RW], BF16) for i in range(2)]
        h2e = [ph("h2e%d" % i, [128, 8, 512], BF16) for i in range(2)]
        gT = [ph("gT%d" % i, [128, 4, 512], BF16) for i in range(2)]
        sa = [ph("sa%d" % i, [128, 512], BF16) for i in range(2)]
        yo = [ph("yo%d" % i, [128, D]) for i in range(2)]
        idxe = [ph("idxe%d" % i, [128, 1], I32) for i in range(8)]
        gate = ph("gate", [128, 8])
        rmx = ph("rmx", [128, 1]); rsum = ph("rsum", [128, 1]); rexp = ph("rexp", [128, NE])
        wr = ph("wr", [128, 8, NE], BF16)
        dma("pool", wr[:], WR[l].rearrange("(k p) n -> p k n", p=128), [S.dres("wr")], [wr], wr)
        h2g = ph("h2g", [128, 8, 256], BF16)
        acc = ph("acc", [128, 2, D])
        accr = [Res("acc%d" % i) for i in range(2)]
        if not last:
            dma("sp", h2g[:], H2T[:, :, 0:256], h2t_res[0:2], [h2g], h2g)
        xe_res = [S.dres("xe", e) for e in range(NE)]
        tcnt = [0]

        def load_w(ex):
            sl = ex % 2
            dma("pool", wgt[sl][:], WG[l, ex].rearrange("(k p) f -> p k f", p=128), [S.dres("wg")], [wgt[sl]], wgt[sl])
            dma("pool", wut[sl][:], WU[l, ex].rearrange("(k p) f -> p k f", p=128), [S.dres("wu")], [wut[sl]], wut[sl])
            dma("pool", wdt[sl][:], WD[l, ex].rearrange("(k p) f -> p k f", p=128), [S.dres("wd")], [wdt[sl]], wdt[sl])

        disp_own = [[Res("disp%d_%d" % (e_, j_)) for j_ in range(2)] for e_ in range(NE)]

        def dispatch(exs):
            for cg in range(8):
                tk = tokc[tcnt[0] % 2]
                tcnt[0] += 1
                dma("sp", tk[:], XH2[(2 + cg * 8) * 128:(2 + cg * 8 + 8) * 128, :].rearrange("(s p) d -> p s d", p=128),
                    xh2_res[2 + cg * 8:2 + cg * 8 + 8], [tk], tk)
                for s in range(8):
                    T = 2 + cg * 8 + s
                    for ex in exs:
                        S.dma("pool", lambda e, tk=tk, s=s, ex=ex, T=T: e.indirect_dma_start(
                            out=XE[ex][:, :], out_offset=bass.IndirectOffsetOnAxis(
                                ap=idxT[:].rearrange("p a b -> p (a b)")[:, ex * 64 + T - 2:ex * 64 + T - 1], axis=0),
                            in_=tk[:, s, :], in_offset=None, bounds_check=breg(e), oob_is_err=False),
                            reads=rs([tk, idxT]), writes=[xe_res[ex]], owner=disp_own[ex][cg % 2], group=("xe", l, ex),
                            cost=1.2, lat=4.0)

        egroups = [[0, 1], [2, 3, 4, 5], [6, 7, 8, 9], [10, 11, 12, 13], [14, 15]]
        gstart = {g[0]: gi for gi, g in enumerate(egroups)}
        gater = [Res("gate%d" % i) for i in range(8)]
        dispatch(egroups[0])
        load_w(0)
        load_w(1)

        def ctx_dense(ex, wg_, wu_, wd_):
                def ffn_chunk(h_src, N, g_):
                    for fc in range(4):
                        ba = 2 * (fc % 2); bu = ba + 1
                        for k in range(8):
                            mm(pbig[:, ba, :N], wg_[:, k, fc * 128:(fc + 1) * 128], h_src[0][:, k, :N], k == 0, k == 7,
                               [wg_, h_src[1]], [pbr[ba]])
                        for k in range(8):
                            mm(pbig[:, bu, :N], wu_[:, k, fc * 128:(fc + 1) * 128], h_src[0][:, k, :N], k == 0, k == 7,
                               [wu_, h_src[1]], [pbr[bu]])
                        s_ = sa[fc % 2]
                        act(s_[:, :N], pbig[:, ba, :N], AF.Silu, [pbr[ba]], [s_])
                        tt(g_[:, fc, :N], pbig[:, bu, :N], s_[:, :N], ALU.mult, [pbr[bu], s_], [g_])

                def down(g_, s):
                    for hf in range(2):
                        for fc in range(4):
                            mm(pbig[:, 4 + hf, :], g_[:, fc, s * 128:(s + 1) * 128], wd_[:, fc, hf * 512:(hf + 1) * 512],
                               fc == 0, fc == 3, [g_, wd_], [pbr[4 + hf]])
                    return pbig[:, 4:6, :]

                if not last:
                    g_ = gT[0]
                    ffn_chunk((h2g, h2g), 256, g_)
                    for s in range(2):
                        py = down(g_, s)
                        av_ = acc[:, s, :].rearrange("p (a b) -> p a b", a=2)
                        if ex == 0:
                            ts(av_, py, wsel[:, s, ex:ex + 1], None, ALU.mult, None, [pbr[4], pbr[5], wsel], [accr[s]])
                        else:
                            stt(av_, py, wsel[:, s, ex:ex + 1], av_, ALU.mult, ALU.add,
                                [pbr[4], pbr[5], wsel, accr[s]], [accr[s]])

        def gen_prep(ex, ci):
            h_ = h2e[ci]
            for s in range(4):
                st_ = ci * 4 + s
                x_ = xet[st_ % 2]
                dma("sp", x_[:], XE[ex][st_ * 128:(st_ + 1) * 128, :], [xe_res[ex]], [x_], x_)
                cp(idxe[st_][:], x_[:, 1024:1026].bitcast(I32), [x_], [idxe[st_]])
                tt(gate[:, st_:st_ + 1], x_[:, 1026 + ex:1027 + ex], x_[:, 1042 + ex:1043 + ex], ALU.add, [x_], [gater[st_]])
                pt_ = ptr[st_ % 2]
                for k in range(8):
                    mm(pt_[:, k, :], x_[:, k * 128:(k + 1) * 128], ident[:], True, True, [x_, ident], [pt_], tr=True)
                yield
                for k in range(8):
                    act(h_[:, k, s * 128:(s + 1) * 128], pt_[:, k, :], AF.Identity, [pt_, modT], [h_],
                        bias=modT[:, 24 + k:25 + k], scale=modT[:, 32 + k:33 + k])
                    if k % 4 == 3:
                        yield

        def gen_ffn(ex, ci, wg_, wu_, wd_):
            h_ = h2e[ci]
            g_ = gT[ci]
            for fc in range(4):
                ba = 2 * (fc % 2); bu = ba + 1
                for k in range(8):
                    mm(pbig[:, ba, :], wg_[:, k, fc * 128:(fc + 1) * 128], h_[:, k, :], k == 0, k == 7, [wg_, h_], [pbr[ba]])
                for k in range(8):
                    mm(pbig[:, bu, :], wu_[:, k, fc * 128:(fc + 1) * 128], h_[:, k, :], k == 0, k == 7, [wu_, h_], [pbr[bu]])
                s_ = sa[fc % 2]
                act(s_[:], pbig[:, ba, :], AF.Silu, [pbr[ba]], [s_])
                tt(g_[:, fc, :], pbig[:, bu, :], s_[:], ALU.mult, [pbr[bu], s_], [g_])
                yield
            for s in range(4):
                st_ = ci * 4 + s
                for hf in range(2):
                    for fc in range(4):
                        mm(pbig[:, 4 + hf, :], g_[:, fc, s * 128:(s + 1) * 128], wd_[:, fc, hf * 512:(hf + 1) * 512],
                           fc == 0, fc == 3, [g_, wd_], [pbr[4 + hf]])
                y_ = yo[st_ % 2]
                act(y_[:].rearrange("p (a b) -> p a b", a=2), pbig[:, 4:6, :], AF.Identity, [pbr[4], pbr[5], gater[st_]], [y_],
                    scale=gate[:, st_:st_ + 1])
                S.dma("pool", lambda e, y_=y_, ie_=idxe[st_]: e.indirect_dma_start(
                    out=FFN[:, :], out_offset=bass.IndirectOffsetOnAxis(ap=ie_[:, :], axis=0),
                    in_=y_[:, :], in_offset=None, compute_op=ALU.add),
                    reads=rs([y_, idxe[st_]]), writes=[S.dres("ffn")], owner=y_.r, group=("ffn", l, ex), cost=1.2, lat=8.0)
                yield

        prev = None
        for ex in range(NE):
            sl = ex % 2
            if ex in gstart and gstart[ex] + 1 < len(egroups):
                dispatch(egroups[gstart[ex] + 1])
            ctx_dense(ex, wgt[sl], wut[sl], wdt[sl])
            for ci in range(2):
                for _ in interleave([gen_prep(ex, ci), prev]):
                    pass
                if ci == 0 and ex >= 1 and ex + 1 < NE:
                    load_w(ex + 1)
                prev = gen_ffn(ex, ci, wgt[sl], wut[sl], wdt[sl])
        for _ in prev:
            pass

        g2b = ph("g2b", [128, D]); cg2b = ph("cg2b", [128, D])
        l2g = ph("l2g", [128, D]); l2b = ph("l2b", [128, D])
        dma("sp", g2b[:], MODROW[0, 5120:6144].partition_broadcast(128), [S.dres("modrow")], [g2b], g2b)
        dma("sp", cg2b[:], MODROW[1, 5120:6144].partition_broadcast(128), [S.dres("modrow")], [cg2b], cg2b)
        dma("sp", l2g[:], LN2G[l].partition_broadcast(128), [S.dres("ln2g")], [l2g], l2g)
        dma("sp", l2b[:], LN2B[l].partition_broadcast(128), [S.dres("ln2b")], [l2b], l2b)
        xmt = [ph("xmt%d" % i, [128, D]) for i in range(2)]
        fft = [ph("fft%d" % i, [128, D]) for i in range(2)]
        ot = [ph("ot%d" % i, [128, D]) for i in range(2)]
        y2 = [ph("y2%d" % i, [128, D]) for i in range(2)]
        st2 = [(ph("stats", [128, 2, 6]), ph("mv", [128, 2]), ph("rstd", [128, 1]), ph("nmr", [128, 1])) for _ in range(2)]

        def gen_ln2(T):
            xm_ = xmt[T % 2]; o_ = ot[T % 2]; y2_ = y2[T % 2]
            stats, mv, rstd, nmr = st2[T % 2]
            dma("sp", xm_[:], XMID[T * 128:(T + 1) * 128, :], [xmid_res[T]], [xm_], xm_)
            if T < 2:
                tt(y2_[:], acc[:, T, :], cg2b[:], ALU.mult, [accr[T], cg2b], [y2_])
            else:
                f_ = fft[T % 2]
                dma("sp", f_[:], FFN[T * 128:(T + 1) * 128, :], [S.dres("ffn")], [f_], f_)
                tt(y2_[:], f_[:], g2b[:], ALU.mult, [f_, g2b], [y2_])
            yield
            stt(y2_[:], xm_[:], ALPHA, y2_[:], ALU.mult, ALU.add, [xm_, y2_], [y2_])
            yield
            ln_stats(y2_, y2_, stats, mv, rstd, nmr)
            yield
            act(y2_[:], y2_[:], AF.Identity, [y2_, rstd, nmr], [y2_], bias=nmr[:, 0:1], scale=rstd[:, 0:1])
            yield
            tt(y2_[:], y2_[:], l2g[:], ALU.mult, [y2_, l2g], [y2_], eng="pool")
            yield
            tt(o_[:], y2_[:], l2b[:], ALU.add, [y2_, l2b], [o_], eng="pool")
            if last:
                ev = dma("sp", Y[(T - 2) * 128:(T - 1) * 128, :], o_[:], [o_], [y_res[T - 2]], o_)
                final.append(ev)
            else:
                dma("sp", XCUR[T * 128:(T + 1) * 128, :], o_[:], [o_], [xcur_res[T]], o_)
            yield

        tl_ = list(range(T0, NT))
        for j in range(0, len(tl_), 2):
            for _ in interleave([gen_ln2(T) for T in tl_[j:j + 2]]):
                pass

    S.emit(final_waits=final, reorder=reorder, only=only)
    if dbg:
        print("instr counts", {e: len(v) for e, v in S.ins.items()}, "waits", S.nwaits, flush=True)
    return nc


def _rope_tables():
    t = np.arange(8192)
    row = (t // 64).astype(np.float32); col = (t % 64).astype(np.float32)
    inv = (10000.0 ** (-np.arange(0, 32, 2, dtype=np.float32) / 32)).astype(np.float32)
    cs = np.ones((64, TOK), np.float32); sn = np.zeros((64, TOK), np.float32)
    for a, pos in enumerate((row, col)):
        ang = (pos[:, None] * inv[None, :]).astype(np.float32)
        c = np.cos(ang).T; s = np.sin(ang).T
        cs[a * 32:a * 32 + 16, 256:] = c; cs[a * 32 + 16:a * 32 + 32, 256:] = c
        sn[a * 32:a * 32 + 16, 256:] = -s; sn[a * 32 + 16:a * 32 + 32, 256:] = s
    cs = np.concatenate([cs, cs], 0); sn = np.concatenate([sn, sn], 0)
    return np.stack([cs * 0.125, sn * 0.125, cs, sn]).astype(np.float32)


def _win_ext(w_in):
    qa = w_in[:, :, 0:384]; ka = w_in[:, :, 384:512]; va = w_in[:, :, 512:640]
    bx = w_in[:, :, 640:896]; bb = w_in[:, :, 896:1152]; bc = w_in[:, :, 1152:1408]
    qn = w_in[:, :, 1408:1792]; kn = w_in[:, :, 1792:2176]; vn = w_in[:, :, 2176:2560]
    sw = np.concatenate([np.arange(16, 32), np.arange(0, 16), np.arange(48, 64), np.arange(32, 48)])

    def heads_sw(w, nh):
        idx = np.concatenate([h * 64 + sw for h in range(nh)])
        return w[:, :, idx]

    def qperm(w):
        idx = np.concatenate([np.concatenate([np.arange(c * 64, c * 64 + 64), np.arange((3 + c) * 64, (3 + c) * 64 + 64)])
                              for c in range(3)])
        return w[:, :, idx]

    return np.ascontiguousarray(np.concatenate(
        [qperm(qa), qperm(heads_sw(qa, 6)), ka, heads_sw(ka, 2), bx, bb, bc, qn, kn, va, vn], axis=2))


def _na_bias(rpb):
    NEG = -30000.0
    out = np.full((DEPTH, 5, 6, 5, 128, 128), NEG, np.float32)
    cq = np.arange(64)
    col_start = np.clip(cq - 8, 0, 48)
    col_ok = (cq[None, :] >= col_start[:, None]) & (cq[None, :] < col_start[:, None] + 16)
    coff = np.clip(cq[None, :] - cq[:, None], -15, 15) + 15
    variants = [10, 0, 1, 62, 63]
    for vi, P in enumerate(variants):
        base = min(max(P - 2, 0), 59)
        for rho in range(2):
            r = 2 * P + rho
            rs_ = min(max(r - 4, 0), 120)
            for j in range(5):
                for kap in range(2):
                    kr = 2 * (base + j) + kap
                    if not (rs_ <= kr < rs_ + 8):
                        continue
                    roff = kr - r + 7
                    b = rpb[:, :, roff, :][:, :, coff]
                    b = np.where(col_ok[None, None], b, NEG)
                    out[:, vi, :, j, kap * 64:(kap + 1) * 64, rho * 64:(rho + 1) * 64] = np.transpose(b, (0, 1, 3, 2))
    out = np.transpose(out, (0, 1, 4, 2, 3, 5)).reshape(DEPTH, 5, 128, 6, 640)
    return np.ascontiguousarray(out)


def _amask():
    k = np.arange(128)[:, None]; q = np.arange(128)[None, :]
    mp = np.where(k >= q, 0.0, -30000.0).astype(np.float32); mn = np.where(k <= q, 0.0, -30000.0).astype(np.float32)
    return np.ascontiguousarray(np.stack([np.tile(mp, (1, 3)), np.tile(mn, (1, 3))], axis=1))


def make_in_maps(x, c, ctx, c_ctx, w_mod, b_mod, w_in, conv_w, attn_sink, na_rpb, w_out,
                 ln1_g, ln1_b, w_router, w_gate, w_up, w_down, ln2_g, ln2_b):
    f = lambda a: np.ascontiguousarray(np.asarray(a, dtype=np.float32))
    shared = dict(
        w_mod=f(w_mod), b_mod=f(b_mod), w_in=_win_ext(f(w_in)), rope=_rope_tables(),
        convw=np.ascontiguousarray(np.transpose(f(conv_w).reshape(DEPTH, 3, 2, 128), (0, 3, 2, 1))),
        sink=f(attn_sink), nab=_na_bias(f(na_rpb)), amask=_amask(), w_out=f(w_out),
        ln1_g=f(ln1_g), ln1_b=f(ln1_b), ln2_g=f(ln2_g), ln2_b=f(ln2_b), w_router=f(w_router),
        w_gate=f(w_gate), w_up=f(w_up), w_down=f(w_down))
    x = f(x); ctx = f(ctx); c = f(c); c_ctx = f(c_ctx)
    maps = []
    for b in range(N_CORES):
        m = dict(shared)
        m["xin"] = np.ascontiguousarray(np.concatenate([ctx[b], x[b]], axis=0))
        m["cvec"] = np.ascontiguousarray(np.stack([c[b], c_ctx], axis=0))
        maps.append(m)
    return maps


_NC = {}


def kernel(**inputs):
    if "nc" not in _NC:
        _NC["nc"] = build_nc(reorder=False)
    maps = make_in_maps(**inputs)
    res = run_bass_kernel_spmd(_NC["nc"], maps, core_ids=list(range(N_CORES)))
    return np.stack([np.asarray(r["y"], dtype=np.float32) for r in res.results], axis=0)
```

```python
import numpy as np
import concourse.bass as bass
import concourse.mybir as mybir
from concourse.bass_utils import run_bass_kernel_spmd

F32 = mybir.dt.float32
BF16 = mybir.dt.bfloat16
I32 = mybir.dt.int32
AF = mybir.ActivationFunctionType
ALU = mybir.AluOpType
AX = mybir.AxisListType

D = 1024
NT = 66
TOK = NT * 128
DEPTH = 2
ALPHA = float((2 * DEPTH) ** 0.25)
NE = 16
RW = 1024 + 2 + 32
COMPUTE = ("pe", "dve", "act", "pool")
N_CORES = 4


class Res:
    __slots__ = ("name", "w", "r", "dsem", "wg")

    def __init__(self, name="r"):
        self.name = name
        self.w = None
        self.r = []
        self.dsem = {}
        self.wg = None


class Sched:
    def __init__(self, nc):
        self.nc = nc
        self.ins = {e: [] for e in ("pe", "dve", "act", "pool", "sp")}
        self.dram = {}
        self.ndsem = 0
        self.owners = []
        self.free = {"sp": [], "pool": []}
        self.phase = 0
        self.phase_ev = {0: []}
        self.last_dma = {}
        self.pool_dmas = []
        self.pool_throttle = 0
        self.last_pew = {}

    def dres(self, *key):
        r = self.dram.get(key)
        if r is None:
            r = self.dram[key] = Res(str(key))
        return r

    def _deps(self, eng, reads, writes, pe_acc, group=None):
        deps = []
        for r in reads:
            if r.w is not None:
                if isinstance(r.w, list):
                    deps.extend(r.w)
                else:
                    deps.append(r.w)
        for r in writes:
            if r.w is not None:
                if isinstance(r.w, list):
                    if not (group is not None and r.wg == group):
                        deps.extend(r.w)
                elif not (pe_acc and r.w[0] == "E" and r.w[1] == "pe" and eng == "pe"):
                    deps.append(r.w)
            deps.extend(r.r)
        return deps

    def _post(self, ev, reads, writes, group=None):
        for r in reads:
            r.r.append(ev)
        for r in writes:
            if group is not None and r.wg == group and isinstance(r.w, list):
                r.w.append(ev)
            else:
                r.w = [ev] if group is not None else ev
                r.wg = group
                r.r = []

    def op(self, eng, fn, reads=(), writes=(), pe_acc=False, cost=0.5):
        deps = self._deps(eng, reads, writes, pe_acc)
        idx = len(self.ins[eng])
        order = []
        if eng == "pe":
            for r in writes:
                p = self.last_pew.get(id(r))
                if p is not None:
                    order.append(p)
                self.last_pew[id(r)] = idx
        ev = ("E", eng, idx)
        self.ins[eng].append([fn, deps, None, self.phase, cost, order, cost])
        self._post(ev, reads, writes)
        return ev

    def dma(self, q, fn, reads=(), writes=(), owner=None, group=None, cost=0.1, lat=3.0):
        deps = self._deps(q, reads, writes, False, group)
        sc = owner.dsem.get(q)
        if sc is None:
            if self.free[q]:
                sc = list(self.free[q].pop())
            else:
                sc = [self.ndsem, 0]
                self.ndsem += 1
            owner.dsem[q] = sc
            self.owners.append((owner, q))
        sc[1] += 16
        ev = ("D", sc[0], sc[1])
        if q == "pool" and self.pool_throttle:
            if len(self.pool_dmas) >= self.pool_throttle:
                deps.append(self.pool_dmas[-self.pool_throttle])
            self.pool_dmas.append(ev)
        idx = len(self.ins[q])
        order = []
        p = self.last_dma.get(sc[0])
        if p is not None:
            order.append(p)
        self.last_dma[sc[0]] = idx
        self.ins[q].append([fn, deps, sc[0], self.phase, cost, order, cost + lat, ev])
        self._post(ev, reads, writes, group)
        return ev

    def barrier(self):
        evs = []
        for e in COMPUTE:
            for i in range(len(self.ins[e]) - 1, -1, -1):
                if self.ins[e][i][2] is None:
                    evs.append(("E", e, i))
                    break
        for (o, q) in self.owners:
            sc = o.dsem.pop(q)
            evs.append(("D", sc[0], sc[1]))
            self.free[q].append((sc[0], sc[1]))
        self.owners = []
        self.phase += 1
        self.phase_ev[self.phase] = evs

    def _schedule(self, only=None):
        import heapq
        engs = list(self.ins.keys())
        dprod = {}
        for q in ("sp", "pool"):
            for i, rec in enumerate(self.ins[q]):
                if rec[2] is not None:
                    dprod[(rec[7][1], rec[7][2])] = (q, i)
        fin = {}
        issue = {}
        order = {e: [] for e in engs}
        ptr0 = {e: 0 for e in engs}
        tnow = 0.0
        for ph in range(self.phase + 1):
            nodes = []
            for e in engs:
                lst = self.ins[e]
                i = ptr0[e]
                while i < len(lst) and lst[i][3] == ph:
                    nodes.append((e, i))
                    i += 1
                ptr0[e] = i
            if not nodes:
                continue
            if only is not None and ph not in only:
                for (e, i) in nodes:
                    order[e].append(i)
                continue
            inph = set(nodes)
            ndep = {}
            users = {}
            for (e, i) in nodes:
                rec = self.ins[e][i]
                preds = set()
                for d in rec[1]:
                    p = (d[1], d[2]) if d[0] == "E" else dprod.get((d[1], d[2]))
                    if p is not None and p in inph and p != (e, i):
                        preds.add((p, 0))
                for j in rec[5]:
                    if (e, j) in inph:
                        preds.add(((e, j), 1))
                ndep[(e, i)] = len(preds)
                for pk in preds:
                    users.setdefault(pk[0], []).append(((e, i), pk[1]))
            ready = {}
            heaps = {e: [] for e in engs}
            avail = {e: [] for e in engs}
            efree = {e: tnow for e in engs}
            for n in nodes:
                ready[n] = tnow
                if ndep[n] == 0:
                    heapq.heappush(heaps[n[0]], (tnow, n[1]))
            left = len(nodes)
            tmax = tnow
            while left:
                best = None
                for e in engs:
                    if avail[e]:
                        st_ = efree[e]
                    elif heaps[e]:
                        st_ = max(heaps[e][0][0], efree[e])
                    else:
                        continue
                    if best is None or st_ < best[0]:
                        best = (st_, e)
                st_, e = best
                while heaps[e] and heaps[e][0][0] <= st_:
                    heapq.heappush(avail[e], heapq.heappop(heaps[e])[1])
                i = heapq.heappop(avail[e])
                rec = self.ins[e][i]
                issue[(e, i)] = st_
                efree[e] = st_ + rec[4]
                f_ = st_ + rec[6]
                fin[(e, i)] = f_
                tmax = max(tmax, f_)
                order[e].append(i)
                left -= 1
                for (u, kind) in users.get((e, i), ()):
                    t_ = f_ if kind == 0 else st_
                    if t_ > ready[u]:
                        ready[u] = t_
                    ndep[u] -= 1
                    if ndep[u] == 0:
                        heapq.heappush(heaps[u[0]], (ready[u], u[1]))
            tnow = tmax
        self.sim_time = tnow
        return order

    def _check(self, order, val):
        sems = {}
        pos = {e: 0 for e in self.ins}
        curph = {e: 0 for e in self.ins}
        progress = True
        total = sum(len(v) for v in self.ins.values())
        done = 0
        while progress:
            progress = False
            for e in self.ins:
                while pos[e] < len(order[e]):
                    i = order[e][pos[e]]
                    rec = self.ins[e][i]
                    deps = list(rec[1])
                    if rec[3] != curph[e]:
                        for p in range(curph[e] + 1, rec[3] + 1):
                            deps.extend(self.phase_ev.get(p, ()))
                    ok = True
                    for d in deps:
                        if d[0] == "E":
                            if sems.get(("E", d[1]), 0) < val[d[1]][d[2]]:
                                ok = False
                                break
                        elif sems.get(("D", d[1]), 0) < d[2]:
                            ok = False
                            break
                    if not ok:
                        break
                    curph[e] = rec[3]
                    if rec[2] is not None:
                        sems[("D", rec[2])] = sems.get(("D", rec[2]), 0) + 16
                    elif i in val.get(e, {}):
                        sems[("E", e)] = val[e][i]
                    pos[e] += 1
                    done += 1
                    progress = True
        if done != total:
            msg = []
            for e in self.ins:
                if pos[e] < len(order[e]):
                    i = order[e][pos[e]]
                    msg.append((e, pos[e], i, self.ins[e][i][3], self.ins[e][i][1][:6]))
            raise RuntimeError("schedule deadlock: %s" % msg)

    def emit(self, final_waits=(), reorder=True, only=None):
        import contextlib
        nc = self.nc
        if reorder:
            order = self._schedule(only)
        else:
            order = {e: list(range(len(l))) for e, l in self.ins.items()}
        for e in self.ins:
            assert sorted(order[e]) == list(range(len(self.ins[e]))), e
        lastc = {}
        for e in COMPUTE:
            cur = None
            per = {}
            for i in order[e]:
                if self.ins[e][i][2] is None:
                    per[self.ins[e][i][3]] = i
            lastc[e] = per
        for p in list(self.phase_ev.keys()):
            evs = [d for d in self.phase_ev[p] if d[0] == "D"]
            for e in COMPUTE:
                qs = [q for q in lastc[e] if q < p]
                if qs:
                    evs.append(("E", e, lastc[e][max(qs)]))
            self.phase_ev[p] = evs
        need = {e: set() for e in COMPUTE}
        for e, lst in self.ins.items():
            for rec in lst:
                for d in rec[1]:
                    if d[0] == "E":
                        need[d[1]].add(d[2])
        for evs in self.phase_ev.values():
            for d in evs:
                if d[0] == "E":
                    need[d[1]].add(d[2])
        val = {}
        for e in COMPUTE:
            c = 0
            v = {}
            for i in order[e]:
                if i in need[e]:
                    c += 1
                    v[i] = c
            val[e] = v
        self._check(order, val)
        self.nwaits = {}
        with contextlib.ExitStack() as st:
            esem = {e: st.enter_context(nc.semaphore("s_" + e)) for e in COMPUTE}
            dsem = [st.enter_context(nc.semaphore("d%d" % i)) for i in range(self.ndsem)]
            block = st.enter_context(nc.Block())

            def run(ename, eng):
                waited = {}
                lst = self.ins[ename]
                cur_ph = 0
                for i in order[ename]:
                    rec = lst[i]
                    deps = rec[1]
                    if rec[3] != cur_ph:
                        deps = list(deps)
                        for p in range(cur_ph + 1, rec[3] + 1):
                            deps.extend(self.phase_ev.get(p, ()))
                        cur_ph = rec[3]
                    tg = {}
                    for d in deps:
                        if d[0] == "E":
                            key = ("E", d[1]); v = val[d[1]][d[2]]; sem = esem[d[1]]
                        else:
                            key = ("D", d[1]); v = d[2]; sem = dsem[d[1]]
                        if tg.get(key, (None, 0))[1] < v:
                            tg[key] = (sem, v)
                    for key, (sem, v) in tg.items():
                        if waited.get(key, 0) >= v:
                            continue
                        eng.wait_ge(sem, v)
                        waited[key] = v
                        self.nwaits[ename] = self.nwaits.get(ename, 0) + 1
                    ins = rec[0](eng)
                    if rec[2] is not None:
                        ins.then_inc(dsem[rec[2]], 16)
                    elif i in need[ename]:
                        ins.then_inc(esem[ename], 1)
                if ename == "sp":
                    for d in final_waits:
                        eng.wait_ge(dsem[d[1]], d[2])

            block.tensor(lambda e: run("pe", e))
            block.vector(lambda e: run("dve", e))
            block.scalar(lambda e: run("act", e))
            block.gpsimd(lambda e: run("pool", e))
            block.sync(lambda e: run("sp", e))


def interleave(gens):
    gens = [g for g in gens if g is not None]
    while gens:
        nxt = []
        for g in gens:
            try:
                next(g)
                nxt.append(g)
            except StopIteration:
                pass
            yield
        gens = nxt


class Tl:
    __slots__ = ("t", "r")

    def __init__(self, t, name):
        self.t = t
        self.r = Res(name)

    def __getitem__(self, k):
        return self.t[k]


def build_nc(dbg=False, depth_run=DEPTH, reorder=True, only=None):
    nc = bass.Bass("TRN2", target_bir_lowering=False)
    S = Sched(nc)

    def din(name, shape, dt=F32):
        return nc.dram_tensor(name, list(shape), dt, kind="ExternalInput").ap()

    def dscr(name, shape, dt):
        return nc.dram_tensor(name, list(shape), dt, kind="ExternalOutput" if dbg else "Internal").ap()

    XIN = din("xin", [TOK, D])
    CVEC = din("cvec", [2, D])
    WMOD = din("w_mod", [DEPTH, D, 6 * D])
    BMOD = din("b_mod", [DEPTH, 6 * D])
    WIN = din("w_in", [DEPTH, D, 3072])
    ROPE = din("rope", [4, 128, TOK])
    CONVW = din("convw", [DEPTH, 128, 2, 3])
    SINK = din("sink", [DEPTH, 6])
    NAB = din("nab", [DEPTH, 5, 128, 6, 640])
    AMASK = din("amask", [128, 2, 384])
    WOUT = din("w_out", [DEPTH, D, D])
    LN1G = din("ln1_g", [DEPTH, D]); LN1B = din("ln1_b", [DEPTH, D])
    LN2G = din("ln2_g", [DEPTH, D]); LN2B = din("ln2_b", [DEPTH, D])
    WR = din("w_router", [DEPTH, D, NE])
    WG = din("w_gate", [DEPTH, NE, D, 512]); WU = din("w_up", [DEPTH, NE, D, 512])
    WD = din("w_down", [DEPTH, NE, 512, D])
    Y = nc.dram_tensor("y", [8192, D], F32, kind="ExternalOutput").ap()

    MODROW = dscr("modrow", [2, 6 * D], F32)
    FM = dscr("fm", [128, 14, TOK], BF16)
    VV = dscr("vv", [TOK, 8, 65], BF16)
    XMID = dscr("xmid", [TOK, D], F32)
    H2T = dscr("h2t", [128, 8, TOK], BF16)
    XCUR = dscr("xcur", [TOK, D], F32)
    XH2 = dscr("xh2", [TOK, RW], BF16)
    XE = [dscr("xe%d" % e, [1024, RW], BF16) for e in range(NE)]
    FFN = dscr("ffn", [TOK, D], F32)

    SB_LO = 16512
    SB_HI = 229344
    st = {"pers": SB_LO, "ph": None}

    def _alloc(name, shape, dt, key):
        nb = int(np.prod(shape[1:])) * (2 if dt == BF16 else 4)
        nb = (nb + 31) // 32 * 32
        off = st[key]
        assert off + nb <= SB_HI, (name, off, nb)
        st[key] = off + nb
        return Tl(nc.alloc_sbuf_tensor_at(name, list(shape), dt, offset=off), name)

    def pers(name, shape, dt=F32):
        return _alloc(name, shape, dt, "pers")

    cnt = [0]

    def ph(name, shape, dt=F32):
        cnt[0] += 1
        return _alloc("%s_%d" % (name, cnt[0]), shape, dt, "ph")

    def new_phase():
        S.barrier()
        st["ph"] = st["pers_end"]

    pbig = Tl(nc.alloc_psum_tensor("pbig", [128, 6, 512], F32), "pbig")
    ptr = [Tl(nc.alloc_psum_tensor("ptr%d" % i, [128, 8, 128], BF16), "ptr%d" % i) for i in range(2)]
    pbr = [Res("pb%d" % i) for i in range(6)]

    ident = pers("ident", [128, 128], BF16)
    identf = pers("identf", [128, 128], F32)
    onesf = pers("onesf", [128, 128], F32)
    aff = pers("aff", [128, NT, NE], F32)
    wsel = pers("wsel", [128, NT, NE], F32)
    modT = pers("modT", [128, 48], F32)
    modcT = pers("modcT", [128, 48], F32)
    esink = pers("esink", [128, 6], F32)
    convw = pers("convw", [128, 2, 3], F32)
    amask = pers("amask", [128, 2, 384], BF16)
    eps_t = pers("eps", [128, 1], F32)
    ltri = pers("ltri", [128, 128], F32)
    idxT = pers("idxT", [128, NE, 64], I32)
    acc = pers("acc", [128, 2, D], F32)
    st["pers_end"] = st["pers"]
    st["ph"] = st["pers_end"]

    _breg = {}

    def breg(e):
        if "r" not in _breg:
            _breg["r"] = e.to_reg(1023)
        return _breg["r"]

    def rs(lst):
        return [x.r if isinstance(x, Tl) else x for x in lst]

    def fsz(ap):
        try:
            return float(ap.free_size())
        except Exception:
            return 512.0

    def mm(out, lhsT, rhs, start, stop, reads, writes, tr=False):
        if tr:
            S.op("pe", lambda e: e.matmul(out, lhsT=lhsT, rhs=rhs, is_transpose=True),
                 reads=rs(reads), writes=rs(writes), pe_acc=True, cost=0.08)
        else:
            c = max(fsz(rhs), 64.0) / 2000.0 * (4.0 if lhsT.dtype == F32 else 1.0) + 0.03
            S.op("pe", lambda e: e.matmul(out, lhsT=lhsT, rhs=rhs, start=start, stop=stop),
                 reads=rs(reads), writes=rs(writes), pe_acc=True, cost=c)

    def act(out, in_, func, reads, writes, bias=0.0, scale=1.0, accum=None):
        if accum is None:
            S.op("act", lambda e: e.activation(out=out, in_=in_, func=func, bias=bias, scale=scale),
                 reads=rs(reads), writes=rs(writes), cost=0.2 + fsz(out) / 1300.0)
        else:
            S.op("act", lambda e: e.activation(out=out, in_=in_, func=func, bias=bias, scale=scale,
                                               accum_out=accum), reads=rs(reads), writes=rs(writes))

    def vcost(eng, ap):
        return (0.1 + fsz(ap) / 900.0) if eng == "dve" else (0.2 + fsz(ap) / 450.0)

    def tt(out, in0, in1, op, reads, writes, eng="dve"):
        S.op(eng, lambda e: e.tensor_tensor(out=out, in0=in0, in1=in1, op=op), reads=rs(reads), writes=rs(writes),
             cost=vcost(eng, out))

    def ts(out, in0, s1, s2, op0, op1, reads, writes, eng="dve"):
        if op1 is None:
            S.op(eng, lambda e: e.tensor_scalar(out=out, in0=in0, scalar1=s1, scalar2=None, op0=op0),
                 reads=rs(reads), writes=rs(writes), cost=vcost(eng, out))
        else:
            S.op(eng, lambda e: e.tensor_scalar(out=out, in0=in0, scalar1=s1, scalar2=s2, op0=op0, op1=op1),
                 reads=rs(reads), writes=rs(writes), cost=vcost(eng, out))

    def stt(out, in0, scalar, in1, op0, op1, reads, writes):
        S.op("dve", lambda e: e.scalar_tensor_tensor(out=out, in0=in0, scalar=scalar, in1=in1, op0=op0, op1=op1),
             reads=rs(reads), writes=rs(writes), cost=vcost("dve", out))

    def cp(out, in_, reads, writes, eng="dve"):
        S.op(eng, lambda e: e.tensor_copy(out=out, in_=in_), reads=rs(reads), writes=rs(writes), cost=vcost(eng, out))

    def recip(out, in_, reads, writes):
        S.op("dve", lambda e: e.reciprocal(out=out, in_=in_), reads=rs(reads), writes=rs(writes))

    def reduce(out, in_, op, reads, writes, negate=False):
        S.op("dve", lambda e: e.tensor_reduce(out=out, in_=in_, axis=AX.X, op=op, negate=negate),
             reads=rs(reads), writes=rs(writes), cost=vcost("dve", in_))

    def memset(eng, ap, val, writes):
        S.op(eng, lambda e: e.memset(ap, val), writes=rs(writes), cost=vcost(eng, ap))

    def single(out, in_, scalar, op, reads, writes):
        S.op("dve", lambda e: e.tensor_single_scalar(out=out, in_=in_, scalar=scalar, op=op),
             reads=rs(reads), writes=rs(writes))

    def scan(out, d0, d1, reads, writes):
        S.op("dve", lambda e: e.tensor_tensor_scan(out=out, data0=d0, data1=d1, initial=0.0, op0=ALU.add, op1=ALU.add),
             reads=rs(reads), writes=rs(writes))

    def iota_tail(ap, base, writes):
        S.op("pool", lambda e: e.iota(ap, pattern=[[0, 1]], base=base, channel_multiplier=1), writes=rs(writes))

    def dma(q, out, in_, reads, writes, owner, slow=False, group=None):
        try:
            nbytes = float(out.nbytes())
        except Exception:
            nbytes = 1.0e5
        lat = 2.5 + nbytes / 1.5e5
        cost = 0.1 if q == "sp" else 1.0
        ow = owner.r if isinstance(owner, Tl) else owner
        if group is not None:
            return S.dma(q, lambda e: e.dma_start(out=out, in_=in_), reads=rs(reads), writes=rs(writes),
                         owner=ow, group=group, cost=cost, lat=lat)
        if slow:
            return S.dma(q, lambda e: e.dma_start(out=out, in_=in_, allow_slow_non_contiguous=True),
                         reads=rs(reads), writes=rs(writes), owner=ow, cost=cost, lat=lat + 3.0)
        return S.dma(q, lambda e: e.dma_start(out=out, in_=in_),
                     reads=rs(reads), writes=rs(writes), owner=ow, cost=cost, lat=lat)

    def ln_stats(src_ap, src_res, stats, mv, rstd, nmr=None):
        for h in range(2):
            S.op("dve", lambda e, h=h: e.bn_stats(out=stats[:, h, :], in_=src_ap[:, h * 512:(h + 1) * 512]),
                 reads=rs([src_res]), writes=rs([stats]))
        S.op("dve", lambda e: e.bn_aggr(out=mv[:], in_=stats[:].rearrange("p a b -> p (a b)")),
             reads=rs([stats]), writes=rs([mv]))
        act(rstd[:], mv[:, 1:2], AF.Sqrt, [mv, eps_t], [rstd], bias=eps_t[:, 0:1])
        S.op("dve", lambda e: e.reciprocal(out=rstd[:], in_=rstd[:]), reads=rs([rstd]), writes=rs([rstd]))
        if nmr is not None:
            stt(nmr[:], mv[:, 0:1], -1.0, rstd[:], ALU.mult, ALU.mult, [mv, rstd], [nmr])

    S.op("pool", lambda e: e.iota(identf[:], pattern=[[1, 128]], base=0, channel_multiplier=-1,
                                  allow_small_or_imprecise_dtypes=True), writes=rs([identf]))
    S.op("dve", lambda e: e.tensor_single_scalar(out=ident[:], in_=identf[:], scalar=0.0, op=ALU.is_equal),
         reads=rs([identf]), writes=rs([ident]))
    S.op("dve", lambda e: e.memset(onesf[:], 1.0), writes=rs([onesf]))
    S.op("dve", lambda e: e.tensor_single_scalar(out=ltri[:], in_=identf[:], scalar=0.0, op=ALU.is_gt),
         reads=rs([identf]), writes=rs([ltri]))
    S.op("dve", lambda e: e.memset(eps_t[:], 1e-6), writes=rs([eps_t]))
    dma("pool", amask[:], AMASK, [S.dres("amask")], [amask], amask)

    fm_res = [S.dres("fm", t) for t in range(NT)]
    vv_res = [S.dres("vv", t) for t in range(NT)]
    xmid_res = [S.dres("xmid", t) for t in range(NT)]
    h2t_res = [S.dres("h2t", t) for t in range(NT)]
    xcur_res = [S.dres("xcur", t) for t in range(NT)]
    xh2_res = [S.dres("xh2", t) for t in range(NT)]
    y_res = [S.dres("y", t) for t in range(64)]
    final = []

    for l in range(depth_run):
        last = l == DEPTH - 1
        T0 = 2 if last else 0
        new_phase()
        cT = ph("cT", [128, 8, 2])
        bm = ph("bm", [2, 6 * D])
        mrow = ph("mrow", [2, 6 * D])
        wm = [ph("wm%d" % i, [128, 8, 512]) for i in range(2)]
        for m_ in range(2):
            dma("sp", cT[:, :, m_], CVEC[m_].rearrange("(k p) -> p k", p=128), [S.dres("cvec")], [cT], cT, slow=True)
        dma("sp", bm[:], BMOD[l].partition_broadcast(2), [S.dres("bmod")], [bm], bm)
        dma("sp", esink[:], SINK[l].partition_broadcast(128), [S.dres("sink")], [esink], esink)
        dma("sp", convw[:], CONVW[l], [S.dres("convw")], [convw], convw)
        act(cT[:], cT[:], AF.Silu, [cT], [cT])
        act(esink[:], esink[:], AF.Exp, [esink], [esink])
        wmv = WMOD[l].rearrange("(k p) n -> p k n", p=128)
        for cc in range(12):
            w_ = wm[cc % 2]
            dma("sp", w_[:], wmv[:, :, cc * 512:(cc + 1) * 512], [S.dres("wmod")], [w_], w_)
            pb = pbig[0:2, cc % 2, :]
            for k in range(8):
                mm(pb, cT[:, k, :], w_[:, k, :], k == 0, k == 7, [cT, w_], [pbr[cc % 2]])
            tt(mrow[:, cc * 512:(cc + 1) * 512], pb, bm[:, cc * 512:(cc + 1) * 512], ALU.add,
               [pbr[cc % 2], bm], [mrow])
        dma("sp", MODROW, mrow[:], [mrow], [S.dres("modrow")], mrow)
        dma("sp", modT[:], MODROW[0].rearrange("(j p) -> p j", p=128), [S.dres("modrow")], [modT], modT, slow=True)
        dma("sp", modcT[:], MODROW[1].rearrange("(j p) -> p j", p=128), [S.dres("modrow")], [modcT], modcT, slow=True)
        for m_ in (modT, modcT):
            ts(m_[:, 8:16], m_[:, 8:16], 1.0, None, ALU.add, None, [m_], [m_])
            ts(m_[:, 32:40], m_[:, 32:40], 1.0, None, ALU.add, None, [m_], [m_])

        new_phase()
        win = ph("win", [128, 8, 3072], BF16)
        wiv = WIN[l].rearrange("(k p) n -> p k n", p=128)
        for k in range(8):
            dma("pool", win[:, k, :], wiv[:, k, :], [S.dres("win")], [win], win)
        xt = [ph("xt%d" % i, [128, D]) for i in range(2)]
        xh = [ph("xh%d" % i, [128, D], BF16) for i in range(2)]
        hT = [ph("hT%d" % i, [128, 8, 512], BF16) for i in range(2)]
        fmo = [ph("fmo%d" % i, [128, 14, 512], BF16) for i in range(2)]
        vo = [ph("vo%d" % i, [128, 4, 8, 65], BF16) for i in range(2)]
        rp = [ph("rp%d" % i, [128, 4, 512]) for i in range(2)]
        tmp = [ph("tmp%d" % i, [128, 512]) for i in range(3)]
        stats = ph("stats", [128, 2, 6]); mv = ph("mv", [128, 2]); rstd = ph("rstd", [128, 1])
        for v_ in vo:
            S.op("pool", lambda e, v_=v_: e.memset(v_[:], 1.0), writes=rs([v_]))
        src = XIN if l == 0 else XCUR
        src_res = (lambda t: S.dres("xin", t)) if l == 0 else (lambda t: xcur_res[t])
        groups = [(0, 2)] + [(2 + 4 * i, 4) for i in range(16)]
        bank = [0]

        def nextbank():
            b = bank[0]
            bank[0] = (b + 1) % 6
            return b

        def gen_L(gi, t0, nt):
            N = nt * 128
            sl = gi % 2
            mT = modcT if gi == 0 else modT
            rpt = rp[sl]
            dma("sp", rpt[:, :, :N], ROPE[:, :, t0 * 128:t0 * 128 + N].rearrange("a p n -> p a n"),
                [S.dres("rope")], [rpt], rpt)
            for s in range(nt):
                tl = t0 + s
                x_ = xt[tl % 2]; xh_ = xh[tl % 2]; pt_ = ptr[tl % 2]
                dma("sp", x_[:], src[tl * 128:(tl + 1) * 128, :], [src_res(tl)], [x_], x_)
                ln_stats(x_, x_, stats, mv, rstd)
                yield
                ts(xh_[:], x_[:], mv[:, 0:1], rstd[:, 0:1], ALU.subtract, ALU.mult, [x_, mv, rstd], [xh_])
                for k in range(8):
                    mm(pt_[:, k, :], xh_[:, k * 128:(k + 1) * 128], ident[:], True, True, [xh_, ident], [pt_], tr=True)
                yield
                for k in range(8):
                    act(hT[sl][:, k, s * 128:(s + 1) * 128], pt_[:, k, :], AF.Identity, [pt_, mT], [hT[sl]],
                        bias=mT[:, k:k + 1], scale=mT[:, 8 + k:9 + k])
                    if k % 4 == 3:
                        yield

        def gen_P(gi, t0, nt):
            N = nt * 128
            sl = gi % 2
            rpt = rp[sl]
            h_ = hT[sl]; fo = fmo[sl]

            def proj(ch):
                b = nextbank()
                for k in range(8):
                    mm(pbig[:, b, :N], win[:, k, ch * 128:(ch + 1) * 128], h_[:, k, :N], k == 0, k == 7,
                       [win, h_], [pbr[b]])
                return b

            def rope(chq, chs, tq, tsn, dst):
                b1 = proj(chq); b2 = proj(chs)
                tt(tmp[0][:, :N], pbig[:, b1, :N], rpt[:, tq, :N], ALU.mult, [pbr[b1], rpt], [tmp[0]])
                tt(tmp[1][:, :N], pbig[:, b2, :N], rpt[:, tsn, :N], ALU.mult, [pbr[b2], rpt], [tmp[1]])
                tt(fo[:, dst, :N], tmp[0][:, :N], tmp[1][:, :N], ALU.add, [tmp[0], tmp[1]], [fo], eng="pool")

            for c_ in range(3):
                rope(c_, 3 + c_, 0, 1, c_)
                yield
            rope(6, 7, 2, 3, 6)
            yield
            for c_ in range(2):
                b1 = proj(8 + c_)
                act(tmp[2][:, :N], pbig[:, b1, :N], AF.Identity, [pbr[b1]], [tmp[2]])
                b2 = proj(12 + c_)
                tt(fo[:, 10 + c_, :N], pbig[:, b2, :N], tmp[2][:, :N], ALU.mult, [pbr[b2], tmp[2]], [fo])
                yield
                b3 = proj(10 + c_)
                act(fo[:, 12 + c_, :N], pbig[:, b3, :N], AF.Identity, [pbr[b3]], [fo])
                yield
            for c_ in range(3):
                b1 = proj(14 + c_)
                act(fo[:, 3 + c_, :N], pbig[:, b1, :N], AF.Identity, [pbr[b1]], [fo], scale=0.125)
                yield
                b2 = proj(17 + c_)
                cp(fo[:, 7 + c_, :N], pbig[:, b2, :N], [pbr[b2]], [fo])
                yield
            for s in range(nt):
                b = nextbank()
                for k in range(8):
                    mm(pbig[:, b, :], h_[:, k, s * 128:(s + 1) * 128], win[:, k, 2560:3072], k == 0, k == 7,
                       [win, h_], [pbr[b]])
                act(vo[sl][:, s, :, 0:64], pbig[:, b, :].rearrange("p (h d) -> p h d", h=8), AF.Identity,
                    [pbr[b]], [vo[sl]])
                yield
            dma("sp", FM[:, :, t0 * 128:t0 * 128 + N], fo[:, :, :N], [fo], fm_res[t0:t0 + nt], fo)
            dma("sp", VV[t0 * 128:t0 * 128 + N].rearrange("(s p) h d -> p s h d", p=128), vo[sl][:, :nt],
                [vo[sl]], vv_res[t0:t0 + nt], vo[sl])
            yield

        prevP = None
        for gi, (t0, nt) in enumerate(groups):
            for _ in interleave([gen_L(gi, t0, nt), prevP]):
                pass
            prevP = gen_P(gi, t0, nt)
        for _ in prevP:
            pass

        new_phase()
        wout = ph("wout", [128, 8, D], BF16)
        wov = WOUT[l].rearrange("(k p) n -> p k n", p=128)
        for k in range(0, 8, 2):
            dma("pool", wout[:, k:k + 2, :], wov[:, k:k + 2, :], [S.dres("wout")], [wout], wout)
        wr = ph("wr", [128, 8, NE], BF16)
        dma("pool", wr[:], WR[l].rearrange("(k p) n -> p k n", p=128), [S.dres("wr")], [wr], wr)
        nabi = ph("nabi", [128, 6, 640], BF16)
        nabe = ph("nabe", [128, 6, 640], BF16)
        dma("pool", nabi[:], NAB[l, 0], [S.dres("nab")], [nabi], nabi)
        kctx = ph("kctx", [128, 4, 256], BF16)
        vctx = ph("vctx", [128, 2, 8, 65], BF16)
        dma("sp", kctx[:], FM[:, 6:10, 0:256], fm_res[0:2], [kctx], kctx)
        dma("sp", vctx[:], VV[0:256].rearrange("(s p) h d -> p s h d", p=128), vv_res[0:2], [vctx], vctx)
        g1b = ph("g1b", [128, D]); cg1b = ph("cg1b", [128, D])
        l1g = ph("l1g", [128, D]); l1b = ph("l1b", [128, D])
        dma("sp", g1b[:], MODROW[0, 2048:3072].partition_broadcast(128), [S.dres("modrow")], [g1b], g1b)
        dma("sp", cg1b[:], MODROW[1, 2048:3072].partition_broadcast(128), [S.dres("modrow")], [cg1b], cg1b)
        dma("sp", l1g[:], LN1G[l].partition_broadcast(128), [S.dres("ln1g")], [l1g], l1g)
        dma("sp", l1b[:], LN1B[l].partition_broadcast(128), [S.dres("ln1b")], [l1b], l1b)
        qw = [ph("qw%d" % i, [128, 6, 128], BF16) for i in range(2)]
        kw = [ph("kw%d" % i, [128, 4, 640], BF16) for i in range(2)]
        vw = [ph("vw%d" % i, [128, 5, 8, 65], BF16) for i in range(2)]
        uw = [ph("uw%d" % i, [128, 2, 130], BF16) for i in range(2)]
        bbw = [ph("bbw%d" % i, [128, 2, 128], BF16) for i in range(2)]
        xa = [ph("xa%d" % i, [128, D]) for i in range(2)]
        pta = [ph("pta%d" % i, [128, 5, 384], BF16) for i in range(2)]
        ptn = [ph("ptn%d" % i, [128, 896], BF16) for i in range(2)]
        sfn = [ph("sfn%d" % i, [128, 640]) for i in range(2)]
        mixc = ph("mixc", [128, 768], BF16)
        mixT = ph("mixT", [128, 8, 128], BF16)
        ctmp = ph("ctmp", [128, 128])
        den = ph("den", [128, 6]);
        t1 = ph("t1", [128, D]); y1 = ph("y1", [128, D]); xm = [ph("xm%d" % i, [128, D]) for i in range(2)]
        xh2 = ph("xh2", [128, RW], BF16)
        h2o = [ph("h2o%d" % i, [128, 8, 128], BF16) for i in range(2)]
        stats = ph("stats", [128, 2, 6]); mv = ph("mv", [128, 2]); rstd = ph("rstd", [128, 1]); nmr = ph("nmr", [128, 1])
        rmx = ph("rmx", [128, 1]); rsum = ph("rsum", [128, 1]); rexp = ph("rexp", [128, NE])

        mixT2 = [mixT, ph("mixTb", [128, 8, 128], BF16)]

        def gen_att(T):
            sl = T % 2
            is_ctx = T < 2
            i = T - 2
            q_ = qw[sl]; k_ = kw[sl]; v_ = vw[sl]; u_ = uw[sl]; bb_ = bbw[sl]
            mT_ = mixT2[sl]
            dma("sp", q_[:], FM[:, 0:6, T * 128:(T + 1) * 128], [fm_res[T]], [q_], q_)
            nb = nabi
            base = 0
            if not is_ctx:
                base = min(max(i - 2, 0), 59) + 2
                dma("sp", k_[:], FM[:, 6:10, base * 128:(base + 5) * 128], fm_res[base:base + 5], [k_], k_)
                dma("sp", v_[:], VV[base * 128:(base + 5) * 128].rearrange("(s p) h d -> p s h d", p=128),
                    vv_res[base:base + 5], [v_], v_)
                var = 0 if 2 <= i <= 61 else (1 + i if i < 2 else i - 59)
                if var != 0:
                    dma("pool", nabe[:], NAB[l, var], [S.dres("nab")], [nabe], nabe)
                    nb = nabe
            lo_pad = T in (0, 2); hi_pad = T in (1, NT - 1)
            if lo_pad or hi_pad:
                memset("pool", u_[:], 0.0, [u_])
            c0 = T * 128 - (0 if lo_pad else 1); c1 = (T + 1) * 128 + (0 if hi_pad else 1)
            o0 = 1 if lo_pad else 0
            fr = fm_res[max(T - 1, 0):min(T + 2, NT)]
            dma("sp", u_[:, :, o0:o0 + (c1 - c0)], FM[:, 10:12, c0:c1], fr, [u_], u_)
            dma("sp", bb_[:], FM[:, 12:14, T * 128:(T + 1) * 128], [fm_res[T]], [bb_], bb_)
            yield
            if is_ctx:
                akeys = [(("c", 0), None), (("c", 1), None)]
                nkeys = [("c", 0), ("c", 1)]
            else:
                akeys = []
                if i > 0:
                    akeys.append((("w", T - 1 - base), 0))
                akeys.append((("w", T - base), None))
                if i < 63:
                    akeys.append((("w", T + 1 - base), 1))
                akeys += [(("c", 0), None), (("c", 1), None)]
                nkeys = [("w", j) for j in range(5)] + [("c", 0), ("c", 1)]

            def kap(kt, ch, p0):
                if kt[0] == "c":
                    return kctx[p0:p0 + 64, ch, kt[1] * 128:(kt[1] + 1) * 128], kctx
                return k_[p0:p0 + 64, ch, kt[1] * 128:(kt[1] + 1) * 128], k_

            def vap(kt, head):
                if kt[0] == "c":
                    return vctx[:, kt[1], head, :], vctx
                return v_[:, kt[1], head, :], v_

            def gen_A():
                for g in range(2):
                    p0 = g * 64
                    pa_ = pta[g]
                    for ki, (kt, mk) in enumerate(akeys):
                        ka_, kr = kap(kt, 0, p0)
                        if mk is not None:
                            mm(pbig[:, 0, 0:384], ident[:], amask[:, mk, :], True, False, [ident, amask], [pbr[0]])
                        mm(pbig[:, 0, 0:384], ka_, q_[p0:p0 + 64, 0:3, :].rearrange("p a b -> p (a b)"), mk is None, True,
                           [kr, q_], [pbr[0]])
                        act(pa_[:, ki, :], pbig[:, 0, 0:384], AF.Exp, [pbr[0]], [pa_])
                        yield
                    po = pbig[:, 2, 0:195].rearrange("p (c d) -> p c d", c=3)
                    for c_ in range(3):
                        for ki, (kt, mk) in enumerate(akeys):
                            va_, vr = vap(kt, g)
                            mm(po[:, c_, :], pa_[:, ki, c_ * 128:(c_ + 1) * 128], va_, ki == 0, ki == len(akeys) - 1,
                               [pa_, vr], [pbr[2]])
                        yield
                    tt(den[:, 0:3], po[:, :, 64], esink[:, 3 * g:3 * g + 3], ALU.add, [pbr[2], esink], [den])
                    recip(den[:, 0:3], den[:, 0:3], [den], [den])
                    tt(mixc[:, g * 192:(g + 1) * 192].rearrange("p (c d) -> p c d", c=3), po[:, :, 0:64],
                       den[:, 0:3].unsqueeze(2).to_broadcast([128, 3, 64]), ALU.mult, [pbr[2], den], [mixcA])
                    yield

            def gen_N():
                po2 = pbig[:, 3, 0:390].rearrange("p (c d) -> p c d", c=6)
                nk = len(nkeys)
                for h in range(6):
                    ch = h // 2; p0 = (h % 2) * 64
                    pn_ = ptn[h % 2]; sf_ = sfn[h % 2]
                    ps2 = pbig[:, 4:6, :].rearrange("p a b -> p (a b)")
                    if is_ctx:
                        for j, kt in enumerate(nkeys):
                            ka_, kr = kap(kt, 1 + ch, p0)
                            mm(ps2[:, j * 128:(j + 1) * 128], ka_, q_[p0:p0 + 64, 3 + ch, :], True, True,
                               [kr, q_], [pbr[4], pbr[5]])
                        act(pn_[:, 0:256], ps2[:, 0:256], AF.Exp, [pbr[4], pbr[5]], [pn_])
                    else:
                        for j in (5, 6):
                            ka_, kr = kap(nkeys[j], 1 + ch, p0)
                            mm(ps2[:, j * 128:(j + 1) * 128], ka_, q_[p0:p0 + 64, 3 + ch, :], True, True,
                               [kr, q_], [pbr[4], pbr[5]])
                        mm(ps2[:, 512:640], ident[:], nb[:, h, 512:640], True, False, [ident, nb], [pbr[4], pbr[5]])
                        mm(ps2[:, 0:512], ident[:], nb[:, h, 0:512], True, False, [ident, nb], [pbr[4], pbr[5]])
                        for j in range(5):
                            ka_, kr = kap(nkeys[j], 1 + ch, p0)
                            mm(ps2[:, j * 128:(j + 1) * 128], ka_, q_[p0:p0 + 64, 3 + ch, :], False, j in (3, 4),
                               [kr, q_], [pbr[4], pbr[5]])
                        act(pn_[:, 0:896], ps2[:, 0:896], AF.Exp, [pbr[4], pbr[5]], [pn_])
                    yield
                    for j, kt in enumerate(nkeys):
                        va_, vr = vap(kt, 2 + h)
                        mm(po2[:, h, :], pn_[:, j * 128:(j + 1) * 128], va_, j == 0, j == nk - 1, [pn_, vr], [pbr[3]])
                    yield
                recip(den2[:], po2[:, :, 64], [pbr[3]], [den2])
                tt(mixc[:, 384:768].rearrange("p (c d) -> p c d", c=6), po2[:, :, 0:64],
                   den2[:].unsqueeze(2).to_broadcast([128, 6, 64]), ALU.mult, [pbr[3], den2], [mixcN])
                yield

            def gen_B():
                for c_ in range(2):
                    ts(ctmp[:], u_[:, c_, 0:128], convw[:, c_, 0:1], None, ALU.mult, None, [u_, convw], [ctmp])
                    stt(ctmp[:], u_[:, c_, 1:129], convw[:, c_, 1:2], ctmp[:], ALU.mult, ALU.add, [u_, convw, ctmp], [ctmp])
                    stt(ctmp[:], u_[:, c_, 2:130], convw[:, c_, 2:3], ctmp[:], ALU.mult, ALU.add, [u_, convw, ctmp], [ctmp])
                    tt(mT_[:, 3 + c_, :], ctmp[:], bb_[:, c_, :], ALU.mult, [ctmp, bb_], [mT_])
                    yield

            for _ in interleave([gen_A(), gen_N(), gen_B()]):
                yield
            pt_ = ptr[0]
            for c_ in range(6):
                mm(pt_[:, c_, :], mixc[:, c_ * 128:(c_ + 1) * 128], ident[:], True, True, [mixcA, mixcN, ident], [pt_], tr=True)
            cp(mT_[:, 0:3, :], pt_[:, 0:3, :], [pt_], [mT_])
            act(mT_[:, 5:8, :], pt_[:, 3:6, :], AF.Identity, [pt_], [mT_])
            yield

        def gen_epi(T):
            sl = T % 2
            is_ctx = T < 2
            x_ = xa[sl]; mT_ = mixT2[sl]
            dma("sp", x_[:], src[T * 128:(T + 1) * 128, :], [src_res(T)], [x_], x_)
            gb = cg1b if is_ctx else g1b
            for hf in range(2):
                for k in range(8):
                    mm(pbig[:, 1, :], mT_[:, k, :], wout[:, k, hf * 512:(hf + 1) * 512], k == 0, k == 7,
                       [mT_, wout], [pbr[1]])
                tt(t1[:, hf * 512:(hf + 1) * 512], pbig[:, 1, :], gb[:, hf * 512:(hf + 1) * 512], ALU.mult,
                   [pbr[1], gb], [t1])
                yield
            stt(y1[:], x_[:], ALPHA, t1[:], ALU.mult, ALU.add, [x_, t1], [y1])
            yield
            ln_stats(y1, y1, stats, mv, rstd, nmr)
            yield
            xm_ = xm[sl]
            act(t1[:], y1[:], AF.Identity, [y1, rstd, nmr], [t1], bias=nmr[:, 0:1], scale=rstd[:, 0:1])
            yield
            tt(t1[:], t1[:], l1g[:], ALU.mult, [t1, l1g], [t1], eng="pool")
            yield
            tt(xm_[:], t1[:], l1b[:], ALU.add, [t1, l1b], [xm_], eng="pool")
            dma("sp", XMID[T * 128:(T + 1) * 128, :], xm_[:], [xm_], [xmid_res[T]], xm_)
            yield
            ln_stats(xm_, xm_, stats, mv, rstd)
            yield
            ts(xh2[:, 0:D], xm_[:], mv[:, 0:1], rstd[:, 0:1], ALU.subtract, ALU.mult, [xm_, mv, rstd], [xh2])
            yield
            pt2 = ptr[1]
            for k in range(8):
                mm(pt2[:, k, :], xh2[:, k * 128:(k + 1) * 128], ident[:], True, True, [xh2, ident], [pt2], tr=True)
            mT = modcT if is_ctx else modT
            h2_ = h2o[sl]
            for k in range(8):
                act(h2_[:, k, :], pt2[:, k, :], AF.Identity, [pt2, mT], [h2_],
                    bias=mT[:, 24 + k:25 + k], scale=mT[:, 32 + k:33 + k])
                if k % 4 == 3:
                    yield
            if is_ctx:
                dma("sp", H2T[:, :, T * 128:(T + 1) * 128], h2_[:], [h2_], [h2t_res[T]], h2_)
            pr = pbig[:, 1, 0:NE]
            for k in range(8):
                mm(pr, h2_[:, k, :], wr[:, k, :], k == 0, k == 7, [h2_, wr], [pbr[1]])
            reduce(rmx[:], pr, ALU.max, [pbr[1]], [rmx], negate=True)
            act(rexp[:], pr, AF.Exp, [pbr[1], rmx], [rexp], bias=rmx[:, 0:1])
            yield
            reduce(rsum[:], rexp[:], ALU.add, [rexp], [rsum])
            recip(rsum[:], rsum[:], [rsum], [rsum])
            ts(aff[:, T, :], rexp[:], rsum[:, 0:1], None, ALU.mult, None, [rexp, rsum], [aff])
            if not is_ctx:
                ts(xh2[:, 1026:1042], rexp[:], rsum[:, 0:1], None, ALU.mult, None, [rexp, rsum], [xh2])
                stt(xh2[:, 1042:1058], rexp[:], rsum[:, 0:1], xh2[:, 1026:1042], ALU.mult, ALU.subtract,
                    [rexp, rsum, xh2], [xh2])
                iota_tail(xh2[:, 1024:1026].bitcast(I32), T * 128, [xh2])
                dma("sp", XH2[T * 128:(T + 1) * 128, :], xh2[:], [xh2], [xh2_res[T]], xh2)
            yield

        mixcA = Res("mixcA"); mixcN = Res("mixcN")
        den2 = ph("den2", [128, 6])
        prev = None
        for T in range(T0, NT):
            for _ in interleave([gen_att(T), prev]):
                pass
            prev = gen_epi(T)
        for _ in prev:
            pass

        new_phase()
        lo_t = ph("lo", [128, NE]); mid_t = ph("mid", [128, NE]); cntp = ph("cntp", [128, NE]); sel = ph("sel", [128, NE])
        cmp = ph("cmp", [128, NE, 64])
        incl = ph("incl", [128, NE, 64])
        zt = ph("zt", [128, 64])
        offs = ph("offs", [128, NE])
        zbig = ph("zbig", [128, 4096])
        memset("pool", zbig[:], 0.0, [zbig])
        memset("pool", zt[:], 0.0, [zt])
        for j in range(16):
            dma("sp", FFN[256 + j * 512:256 + (j + 1) * 512, :].rearrange("(p a) d -> p (a d)", p=128), zbig[:],
                [zbig], [S.dres("ffn")], zbig, group=("ffnz", l))
        sets = [(2, 64, 1024.0)] if last else [(0, 2, 32.0), (2, 64, 1024.0)]
        for (ta, tn, cap) in sets:
            av = aff[:, ta:ta + tn, :].rearrange("p t e -> p e t")
            memset("dve", lo_t[:], 0.0, [lo_t])
            for it in range(30):
                w_ = 0.5 ** (it + 1)
                ts(mid_t[:], lo_t[:], w_, None, ALU.add, None, [lo_t], [mid_t])
                tt(cmp[:, :, :tn], av, mid_t[:].unsqueeze(2).to_broadcast([128, NE, tn]), ALU.is_ge, [aff, mid_t], [cmp])
                reduce(cntp[:], cmp[:, :, :tn], ALU.add, [cmp], [cntp])
                mm(pbig[:, 0, 0:NE], onesf[:], cntp[:], True, True, [onesf, cntp], [pbr[0]])
                single(sel[:], pbig[:, 0, 0:NE], cap - 0.5, ALU.is_ge, [pbr[0]], [sel])
                stt(lo_t[:], sel[:], w_, lo_t[:], ALU.mult, ALU.add, [sel, lo_t], [lo_t])
            tt(cmp[:, :, :tn], av, lo_t[:].unsqueeze(2).to_broadcast([128, NE, tn]), ALU.is_ge, [aff, lo_t], [cmp])
            if dbg:
                LDBG = nc.dram_tensor("ldbg%d_%d" % (l, tn), [128, NE], F32, kind="ExternalOutput").ap()
                dma("sp", LDBG, lo_t[:], [lo_t], [S.dres("ldbg", tn)], lo_t)
                ADBG = nc.dram_tensor("adbg%d_%d" % (l, tn), [128, NT * NE], F32, kind="ExternalOutput").ap()
                dma("sp", ADBG, aff[:].rearrange("p a b -> p (a b)"), [aff], [S.dres("adbg", tn)], aff)
            if tn == 2:
                wv = wsel[:, ta:ta + tn, :].rearrange("p t e -> p e t")
                tt(wv, av, cmp[:, :, :tn], ALU.mult, [aff, cmp], [wsel])
                continue
            for ex in range(NE):
                scan(incl[:, ex, :], cmp[:, ex, :], zt[:], [cmp, zt], [incl])
            cp(cntp[:], incl[:, :, 63], [incl], [cntp])
            mm(pbig[:, 0, 0:NE], ltri[:], cntp[:], True, True, [ltri, cntp], [pbr[0]])
            cp(offs[:], pbig[:, 0, 0:NE], [pbr[0]], [offs])
            tt(incl[:], incl[:], cmp[:], ALU.subtract, [incl, cmp], [incl])
            tt(incl[:], incl[:], offs[:].unsqueeze(2).to_broadcast([128, NE, 64]), ALU.add, [incl, offs], [incl])
            stt(incl[:].rearrange("p a b -> p (a b)"), incl[:].rearrange("p a b -> p (a b)"), -1.0e6,
                cmp[:].rearrange("p a b -> p (a b)"), ALU.add, ALU.mult, [incl, cmp], [incl])
            ts(idxT[:], incl[:], 1.0e6, None, ALU.add, None, [incl], [idxT])
            if dbg:
                IDBG = nc.dram_tensor("idbg%d" % l, [128, NE * 64], I32, kind="ExternalOutput").ap()
                dma("sp", IDBG, idxT[:].rearrange("p a b -> p (a b)"), [idxT], [S.dres("idbg")], idxT)
                ODBG = nc.dram_tensor("odbg%d" % l, [128, NE], F32, kind="ExternalOutput").ap()
                dma("sp", ODBG, offs[:], [offs], [S.dres("odbg")], offs)
                CDBG = nc.dram_tensor("cdbg%d" % l, [128, NE * 64], F32, kind="ExternalOutput").ap()
                dma("sp", CDBG, cmp[:].rearrange("p a b -> p (a b)"), [cmp], [S.dres("cdbg")], cmp)

        new_phase()
        wgt = [ph("wg%d" % i, [128, 8, 512], BF16) for i in range(2)]
        wut = [ph("wu%d" % i, [128, 8, 512], BF16) for i in range(2)]
        wdt = [ph("wd%d" % i, [128, 4, D], BF16) for i in range(2)]
        tokc = [ph("tokc%d" % i, [128, 8, RW], BF16) for i in range(2)]
        xet = [ph("xet%d" % i, [128, RW], BF16) for i in range(2)]
        h2e = [ph("h2e%d" % i, [128, 8, 512], BF16) for i in range(2)]
        gT = [ph("gT%d" % i, [128, 4, 512], BF16) for i in range(2)]
        sa = [ph("sa%d" % i, [128, 512], BF16) for i in range(2)]
        yo = [ph("yo%d" % i, [128, D]) for i in range(2)]
        idxe = [ph("idxe%d" % i, [128, 1], I32) for i in range(8)]
        gate = ph("gate", [128, 8])
        rmx = ph("rmx", [128, 1]); rsum = ph("rsum", [128, 1]); rexp = ph("rexp", [128, NE])
        wr = ph("wr", [128, 8, NE], BF16)
        dma("pool", wr[:], WR[l].rearrange("(k p) n -> p k n", p=128), [S.dres("wr")], [wr], wr)
        h2g = ph("h2g", [128, 8, 256], BF16)
        accr = [Res("acc%d" % i) for i in range(2)]
        if not last:
            dma("sp", h2g[:], H2T[:, :, 0:256], h2t_res[0:2], [h2g], h2g)
        xe_res = [S.dres("xe", e) for e in range(NE)]
        tcnt = [0]

        def load_w(ex):
            sl = ex % 2
            dma("pool", wgt[sl][:], WG[l, ex].rearrange("(k p) f -> p k f", p=128), [S.dres("wg")], [wgt[sl]], wgt[sl])
            dma("pool", wut[sl][:], WU[l, ex].rearrange("(k p) f -> p k f", p=128), [S.dres("wu")], [wut[sl]], wut[sl])
            dma("pool", wdt[sl][:], WD[l, ex].rearrange("(k p) f -> p k f", p=128), [S.dres("wd")], [wdt[sl]], wdt[sl])

        disp_own = [[Res("disp%d_%d" % (e_, j_)) for j_ in range(2)] for e_ in range(NE)]

        def dispatch(exs):
            for cg in range(8):
                tk = tokc[tcnt[0] % 2]
                tcnt[0] += 1
                dma("sp", tk[:], XH2[(2 + cg * 8) * 128:(2 + cg * 8 + 8) * 128, :].rearrange("(s p) d -> p s d", p=128),
                    xh2_res[2 + cg * 8:2 + cg * 8 + 8], [tk], tk)
                for s in range(8):
                    T = 2 + cg * 8 + s
                    for ex in exs:
                        S.dma("pool", lambda e, tk=tk, s=s, ex=ex, T=T: e.indirect_dma_start(
                            out=XE[ex][:, :], out_offset=bass.IndirectOffsetOnAxis(
                                ap=idxT[:].rearrange("p a b -> p (a b)")[:, ex * 64 + T - 2:ex * 64 + T - 1], axis=0),
                            in_=tk[:, s, :], in_offset=None, bounds_check=breg(e), oob_is_err=False),
                            reads=rs([tk, idxT]), writes=[xe_res[ex]], owner=disp_own[ex][cg % 2], group=("xe", l, ex),
                            cost=1.2, lat=4.0)

        egroups = [[0, 1], [2, 3, 4, 5], [6, 7, 8, 9], [10, 11, 12, 13], [14, 15]]
        gstart = {g[0]: gi for gi, g in enumerate(egroups)}
        gater = [Res("gate%d" % i) for i in range(8)]
        dispatch(egroups[0])
        load_w(0)
        load_w(1)

        def ctx_dense(ex, wg_, wu_, wd_):
                def ffn_chunk(h_src, N, g_):
                    for fc in range(4):
                        ba = 2 * (fc % 2); bu = ba + 1
                        for k in range(8):
                            mm(pbig[:, ba, :N], wg_[:, k, fc * 128:(fc + 1) * 128], h_src[0][:, k, :N], k == 0, k == 7,
                               [wg_, h_src[1]], [pbr[ba]])
                        for k in range(8):
                            mm(pbig[:, bu, :N], wu_[:, k, fc * 128:(fc + 1) * 128], h_src[0][:, k, :N], k == 0, k == 7,
                               [wu_, h_src[1]], [pbr[bu]])
                        s_ = sa[fc % 2]
                        act(s_[:, :N], pbig[:, ba, :N], AF.Silu, [pbr[ba]], [s_])
                        tt(g_[:, fc, :N], pbig[:, bu, :N], s_[:, :N], ALU.mult, [pbr[bu], s_], [g_])

                def down(g_, s):
                    for hf in range(2):
                        for fc in range(4):
                            mm(pbig[:, 4 + hf, :], g_[:, fc, s * 128:(s + 1) * 128], wd_[:, fc, hf * 512:(hf + 1) * 512],
                               fc == 0, fc == 3, [g_, wd_], [pbr[4 + hf]])
                    return pbig[:, 4:6, :]

                if not last:
                    g_ = gT[0]
                    ffn_chunk((h2g, h2g), 256, g_)
                    for s in range(2):
                        py = down(g_, s)
                        av_ = acc[:, s, :].rearrange("p (a b) -> p a b", a=2)
                        if ex == 0:
                            ts(av_, py, wsel[:, s, ex:ex + 1], None, ALU.mult, None, [pbr[4], pbr[5], wsel], [accr[s]])
                        else:
                            stt(av_, py, wsel[:, s, ex:ex + 1], av_, ALU.mult, ALU.add,
                                [pbr[4], pbr[5], wsel, accr[s]], [accr[s]])

        def gen_prep(ex, ci):
            h_ = h2e[ci]
            for s in range(4):
                st_ = ci * 4 + s
                x_ = xet[st_ % 2]
                dma("sp", x_[:], XE[ex][st_ * 128:(st_ + 1) * 128, :], [xe_res[ex]], [x_], x_)
                cp(idxe[st_][:], x_[:, 1024:1026].bitcast(I32), [x_], [idxe[st_]])
                tt(gate[:, st_:st_ + 1], x_[:, 1026 + ex:1027 + ex], x_[:, 1042 + ex:1043 + ex], ALU.add, [x_], [gater[st_]])
                pt_ = ptr[st_ % 2]
                for k in range(8):
                    mm(pt_[:, k, :], x_[:, k * 128:(k + 1) * 128], ident[:], True, True, [x_, ident], [pt_], tr=True)
                yield
                for k in range(8):
                    act(h_[:, k, s * 128:(s + 1) * 128], pt_[:, k, :], AF.Identity, [pt_, modT], [h_],
                        bias=modT[:, 24 + k:25 + k], scale=modT[:, 32 + k:33 + k])
                    if k % 4 == 3:
                        yield

        def gen_ffn(ex, ci, wg_, wu_, wd_):
            h_ = h2e[ci]
            g_ = gT[ci]
            for fc in range(4):
                ba = 2 * (fc % 2); bu = ba + 1
                for k in range(8):
                    mm(pbig[:, ba, :], wg_[:, k, fc * 128:(fc + 1) * 128], h_[:, k, :], k == 0, k == 7, [wg_, h_], [pbr[ba]])
                for k in range(8):
                    mm(pbig[:, bu, :], wu_[:, k, fc * 128:(fc + 1) * 128], h_[:, k, :], k == 0, k == 7, [wu_, h_], [pbr[bu]])
                s_ = sa[fc % 2]
                act(s_[:], pbig[:, ba, :], AF.Silu, [pbr[ba]], [s_])
                tt(g_[:, fc, :], pbig[:, bu, :], s_[:], ALU.mult, [pbr[bu], s_], [g_])
                yield
            for s in range(4):
                st_ = ci * 4 + s
                for hf in range(2):
                    for fc in range(4):
                        mm(pbig[:, 4 + hf, :], g_[:, fc, s * 128:(s + 1) * 128], wd_[:, fc, hf * 512:(hf + 1) * 512],
                           fc == 0, fc == 3, [g_, wd_], [pbr[4 + hf]])
                y_ = yo[st_ % 2]
                act(y_[:].rearrange("p (a b) -> p a b", a=2), pbig[:, 4:6, :], AF.Identity, [pbr[4], pbr[5], gater[st_]], [y_],
                    scale=gate[:, st_:st_ + 1])
                S.dma("pool", lambda e, y_=y_, ie_=idxe[st_]: e.indirect_dma_start(
                    out=FFN[:, :], out_offset=bass.IndirectOffsetOnAxis(ap=ie_[:, :], axis=0),
                    in_=y_[:, :], in_offset=None, compute_op=ALU.add),
                    reads=rs([y_, idxe[st_]]), writes=[S.dres("ffn")], owner=y_.r, group=("ffn", l, ex), cost=1.2, lat=8.0)
                yield

        prev = None
        for ex in range(NE):
            sl = ex % 2
            if ex in gstart and gstart[ex] + 1 < len(egroups):
                dispatch(egroups[gstart[ex] + 1])
            ctx_dense(ex, wgt[sl], wut[sl], wdt[sl])
            for ci in range(2):
                for _ in interleave([gen_prep(ex, ci), prev]):
                    pass
                if ci == 0 and ex >= 1 and ex + 1 < NE:
                    load_w(ex + 1)
                prev = gen_ffn(ex, ci, wgt[sl], wut[sl], wdt[sl])
        for _ in prev:
            pass

        new_phase()
        g2b = ph("g2b", [128, D]); cg2b = ph("cg2b", [128, D])
        l2g = ph("l2g", [128, D]); l2b = ph("l2b", [128, D])
        dma("sp", g2b[:], MODROW[0, 5120:6144].partition_broadcast(128), [S.dres("modrow")], [g2b], g2b)
        dma("sp", cg2b[:], MODROW[1, 5120:6144].partition_broadcast(128), [S.dres("modrow")], [cg2b], cg2b)
        dma("sp", l2g[:], LN2G[l].partition_broadcast(128), [S.dres("ln2g")], [l2g], l2g)
        dma("sp", l2b[:], LN2B[l].partition_broadcast(128), [S.dres("ln2b")], [l2b], l2b)
        xmt = [ph("xmt%d" % i, [128, D]) for i in range(4)]
        fft = [ph("fft%d" % i, [128, D]) for i in range(4)]
        ot = [ph("ot%d" % i, [128, D]) for i in range(4)]
        y2 = [ph("y2%d" % i, [128, D]) for i in range(4)]
        st2 = [(ph("stats", [128, 2, 6]), ph("mv", [128, 2]), ph("rstd", [128, 1]), ph("nmr", [128, 1])) for _ in range(4)]

        def gen_ln2(T):
            xm_ = xmt[T % 4]; o_ = ot[T % 4]; y2_ = y2[T % 4]
            stats, mv, rstd, nmr = st2[T % 4]
            dma("sp", xm_[:], XMID[T * 128:(T + 1) * 128, :], [xmid_res[T]], [xm_], xm_)
            if T < 2:
                tt(y2_[:], acc[:, T, :], cg2b[:], ALU.mult, [accr[T], cg2b], [y2_])
            else:
                f_ = fft[T % 4]
                dma("sp", f_[:], FFN[T * 128:(T + 1) * 128, :], [S.dres("ffn")], [f_], f_)
                tt(y2_[:], f_[:], g2b[:], ALU.mult, [f_, g2b], [y2_])
            yield
            stt(y2_[:], xm_[:], ALPHA, y2_[:], ALU.mult, ALU.add, [xm_, y2_], [y2_])
            yield
            ln_stats(y2_, y2_, stats, mv, rstd, nmr)
            yield
            act(y2_[:], y2_[:], AF.Identity, [y2_, rstd, nmr], [y2_], bias=nmr[:, 0:1], scale=rstd[:, 0:1])
            yield
            tt(y2_[:], y2_[:], l2g[:], ALU.mult, [y2_, l2g], [y2_], eng="pool")
            yield
            tt(o_[:], y2_[:], l2b[:], ALU.add, [y2_, l2b], [o_], eng="pool")
            if last:
                ev = dma("sp", Y[(T - 2) * 128:(T - 1) * 128, :], o_[:], [o_], [y_res[T - 2]], o_)
                final.append(ev)
            else:
                dma("sp", XCUR[T * 128:(T + 1) * 128, :], o_[:], [o_], [xcur_res[T]], o_)
            yield

        tl_ = list(range(T0, NT))
        for j in range(0, len(tl_), 4):
            for _ in interleave([gen_ln2(T) for T in tl_[j:j + 4]]):
                pass

    S.emit(final_waits=final, reorder=reorder, only=only)
    if dbg:
        print("instr counts", {e: len(v) for e, v in S.ins.items()}, "waits", S.nwaits, flush=True)
    return nc


def _rope_tables():
    t = np.arange(8192)
    row = (t // 64).astype(np.float32); col = (t % 64).astype(np.float32)
    inv = (10000.0 ** (-np.arange(0, 32, 2, dtype=np.float32) / 32)).astype(np.float32)
    cs = np.ones((64, TOK), np.float32); sn = np.zeros((64, TOK), np.float32)
    for a, pos in enumerate((row, col)):
        ang = (pos[:, None] * inv[None, :]).astype(np.float32)
        c = np.cos(ang).T; s = np.sin(ang).T
        cs[a * 32:a * 32 + 16, 256:] = c; cs[a * 32 + 16:a * 32 + 32, 256:] = c
        sn[a * 32:a * 32 + 16, 256:] = -s; sn[a * 32 + 16:a * 32 + 32, 256:] = s
    cs = np.concatenate([cs, cs], 0); sn = np.concatenate([sn, sn], 0)
    return np.stack([cs * 0.125, sn * 0.125, cs, sn]).astype(np.float32)


def _win_ext(w_in):
    qa = w_in[:, :, 0:384]; ka = w_in[:, :, 384:512]; va = w_in[:, :, 512:640]
    bx = w_in[:, :, 640:896]; bb = w_in[:, :, 896:1152]; bc = w_in[:, :, 1152:1408]
    qn = w_in[:, :, 1408:1792]; kn = w_in[:, :, 1792:2176]; vn = w_in[:, :, 2176:2560]
    sw = np.concatenate([np.arange(16, 32), np.arange(0, 16), np.arange(48, 64), np.arange(32, 48)])

    def heads_sw(w, nh):
        idx = np.concatenate([h * 64 + sw for h in range(nh)])
        return w[:, :, idx]

    def qperm(w):
        idx = np.concatenate([np.concatenate([np.arange(c * 64, c * 64 + 64), np.arange((3 + c) * 64, (3 + c) * 64 + 64)])
                              for c in range(3)])
        return w[:, :, idx]

    return np.ascontiguousarray(np.concatenate(
        [qperm(qa), qperm(heads_sw(qa, 6)), ka, heads_sw(ka, 2), bx, bb, bc, qn, kn, va, vn], axis=2))


def _na_bias(rpb):
    NEG = -30000.0
    out = np.full((DEPTH, 5, 6, 5, 128, 128), NEG, np.float32)
    cq = np.arange(64)
    col_start = np.clip(cq - 8, 0, 48)
    col_ok = (cq[None, :] >= col_start[:, None]) & (cq[None, :] < col_start[:, None] + 16)
    coff = np.clip(cq[None, :] - cq[:, None], -15, 15) + 15
    variants = [10, 0, 1, 62, 63]
    for vi, P in enumerate(variants):
        base = min(max(P - 2, 0), 59)
        for rho in range(2):
            r = 2 * P + rho
            rs_ = min(max(r - 4, 0), 120)
            for j in range(5):
                for kap in range(2):
                    kr = 2 * (base + j) + kap
                    if not (rs_ <= kr < rs_ + 8):
                        continue
                    roff = kr - r + 7
                    b = rpb[:, :, roff, :][:, :, coff]
                    b = np.where(col_ok[None, None], b, NEG)
                    out[:, vi, :, j, kap * 64:(kap + 1) * 64, rho * 64:(rho + 1) * 64] = np.transpose(b, (0, 1, 3, 2))
    out = np.transpose(out, (0, 1, 4, 2, 3, 5)).reshape(DEPTH, 5, 128, 6, 640)
    return np.ascontiguousarray(out)


def _amask():
    k = np.arange(128)[:, None]; q = np.arange(128)[None, :]
    mp = np.where(k >= q, 0.0, -30000.0).astype(np.float32); mn = np.where(k <= q, 0.0, -30000.0).astype(np.float32)
    return np.ascontiguousarray(np.stack([np.tile(mp, (1, 3)), np.tile(mn, (1, 3))], axis=1))


def make_in_maps(x, c, ctx, c_ctx, w_mod, b_mod, w_in, conv_w, attn_sink, na_rpb, w_out,
                 ln1_g, ln1_b, w_router, w_gate, w_up, w_down, ln2_g, ln2_b):
    f = lambda a: np.ascontiguousarray(np.asarray(a, dtype=np.float32))
    shared = dict(
        w_mod=f(w_mod), b_mod=f(b_mod), w_in=_win_ext(f(w_in)), rope=_rope_tables(),
        convw=np.ascontiguousarray(np.transpose(f(conv_w).reshape(DEPTH, 3, 2, 128), (0, 3, 2, 1))),
        sink=f(attn_sink), nab=_na_bias(f(na_rpb)), amask=_amask(), w_out=f(w_out),
        ln1_g=f(ln1_g), ln1_b=f(ln1_b), ln2_g=f(ln2_g), ln2_b=f(ln2_b), w_router=f(w_router),
        w_gate=f(w_gate), w_up=f(w_up), w_down=f(w_down))
    x = f(x); ctx = f(ctx); c = f(c); c_ctx = f(c_ctx)
    maps = []
    for b in range(N_CORES):
        m = dict(shared)
        m["xin"] = np.ascontiguousarray(np.concatenate([ctx[b], x[b]], axis=0))
        m["cvec"] = np.ascontiguousarray(np.stack([c[b], c_ctx], axis=0))
        maps.append(m)
    return maps


_NC = {}


def kernel(**inputs):
    if "nc" not in _NC:
        _NC["nc"] = build_nc(reorder=False)
    maps = make_in_maps(**inputs)
    res = run_bass_kernel_spmd(_NC["nc"], maps, core_ids=list(range(N_CORES)))
    return np.stack([np.asarray(r["y"], dtype=np.float32) for r in res.results], axis=0)
```

```python
import numpy as np
import concourse.bass as bass
import concourse.mybir as mybir
from concourse.bass_utils import run_bass_kernel_spmd

F32 = mybir.dt.float32
BF16 = mybir.dt.bfloat16
I32 = mybir.dt.int32
AF = mybir.ActivationFunctionType
ALU = mybir.AluOpType
AX = mybir.AxisListType

D = 1024
NT = 66
TOK = NT * 128
DEPTH = 2
ALPHA = float((2 * DEPTH) ** 0.25)
NE = 16
RW = 1024 + 2 + 32
COMPUTE = ("pe", "dve", "act", "pool")
N_CORES = 4


class Res:
    __slots__ = ("name", "w", "r", "dsem", "wg")

    def __init__(self, name="r"):
        self.name = name
        self.w = None
        self.r = []
        self.dsem = {}
        self.wg = None


class Sched:
    def __init__(self, nc):
        self.nc = nc
        self.ins = {e: [] for e in ("pe", "dve", "act", "pool", "sp")}
        self.dram = {}
        self.ndsem = 0
        self.owners = []
        self.free = {"sp": [], "pool": []}
        self.phase = 0
        self.phase_ev = {0: []}
        self.last_dma = {}
        self.pool_dmas = []
        self.pool_throttle = 0
        self.last_pew = {}

    def dres(self, *key):
        r = self.dram.get(key)
        if r is None:
            r = self.dram[key] = Res(str(key))
        return r

    def _deps(self, eng, reads, writes, pe_acc, group=None):
        deps = []
        for r in reads:
            if r.w is not None:
                if isinstance(r.w, list):
                    deps.extend(r.w)
                else:
                    deps.append(r.w)
        for r in writes:
            if r.w is not None:
                if isinstance(r.w, list):
                    if not (group is not None and r.wg == group):
                        deps.extend(r.w)
                elif not (pe_acc and r.w[0] == "E" and r.w[1] == "pe" and eng == "pe"):
                    deps.append(r.w)
            deps.extend(r.r)
        return deps

    def _post(self, ev, reads, writes, group=None):
        for r in reads:
            r.r.append(ev)
        for r in writes:
            if group is not None and r.wg == group and isinstance(r.w, list):
                r.w.append(ev)
            else:
                r.w = [ev] if group is not None else ev
                r.wg = group
                r.r = []

    def op(self, eng, fn, reads=(), writes=(), pe_acc=False, cost=0.5):
        deps = self._deps(eng, reads, writes, pe_acc)
        idx = len(self.ins[eng])
        order = []
        if eng == "pe":
            for r in writes:
                p = self.last_pew.get(id(r))
                if p is not None:
                    order.append(p)
                self.last_pew[id(r)] = idx
        ev = ("E", eng, idx)
        self.ins[eng].append([fn, deps, None, self.phase, cost, order, cost])
        self._post(ev, reads, writes)
        return ev

    def dma(self, q, fn, reads=(), writes=(), owner=None, group=None, cost=0.1, lat=3.0):
        deps = self._deps(q, reads, writes, False, group)
        sc = owner.dsem.get(q)
        if sc is None:
            if self.free[q]:
                sc = list(self.free[q].pop())
            else:
                sc = [self.ndsem, 0]
                self.ndsem += 1
            owner.dsem[q] = sc
            self.owners.append((owner, q))
        sc[1] += 16
        ev = ("D", sc[0], sc[1])
        if q == "pool" and self.pool_throttle:
            if len(self.pool_dmas) >= self.pool_throttle:
                deps.append(self.pool_dmas[-self.pool_throttle])
            self.pool_dmas.append(ev)
        idx = len(self.ins[q])
        order = []
        p = self.last_dma.get(sc[0])
        if p is not None:
            order.append(p)
        self.last_dma[sc[0]] = idx
        self.ins[q].append([fn, deps, sc[0], self.phase, cost, order, cost + lat, ev])
        self._post(ev, reads, writes, group)
        return ev

    def barrier(self):
        evs = []
        for e in COMPUTE:
            for i in range(len(self.ins[e]) - 1, -1, -1):
                if self.ins[e][i][2] is None:
                    evs.append(("E", e, i))
                    break
        for (o, q) in self.owners:
            sc = o.dsem.pop(q)
            evs.append(("D", sc[0], sc[1]))
            self.free[q].append((sc[0], sc[1]))
        self.owners = []
        self.phase += 1
        self.phase_ev[self.phase] = evs

    def _schedule(self, only=None):
        import heapq
        engs = list(self.ins.keys())
        dprod = {}
        for q in ("sp", "pool"):
            for i, rec in enumerate(self.ins[q]):
                if rec[2] is not None:
                    dprod[(rec[7][1], rec[7][2])] = (q, i)
        fin = {}
        issue = {}
        order = {e: [] for e in engs}
        ptr0 = {e: 0 for e in engs}
        tnow = 0.0
        for ph in range(self.phase + 1):
            nodes = []
            for e in engs:
                lst = self.ins[e]
                i = ptr0[e]
                while i < len(lst) and lst[i][3] == ph:
                    nodes.append((e, i))
                    i += 1
                ptr0[e] = i
            if not nodes:
                continue
            if only is not None and ph not in only:
                for (e, i) in nodes:
                    order[e].append(i)
                continue
            inph = set(nodes)
            ndep = {}
            users = {}
            for (e, i) in nodes:
                rec = self.ins[e][i]
                preds = set()
                for d in rec[1]:
                    p = (d[1], d[2]) if d[0] == "E" else dprod.get((d[1], d[2]))
                    if p is not None and p in inph and p != (e, i):
                        preds.add((p, 0))
                for j in rec[5]:
                    if (e, j) in inph:
                        preds.add(((e, j), 1))
                ndep[(e, i)] = len(preds)
                for pk in preds:
                    users.setdefault(pk[0], []).append(((e, i), pk[1]))
            ready = {}
            heaps = {e: [] for e in engs}
            avail = {e: [] for e in engs}
            efree = {e: tnow for e in engs}
            for n in nodes:
                ready[n] = tnow
                if ndep[n] == 0:
                    heapq.heappush(heaps[n[0]], (tnow, n[1]))
            left = len(nodes)
            tmax = tnow
            while left:
                best = None
                for e in engs:
                    if avail[e]:
                        st_ = efree[e]
                    elif heaps[e]:
                        st_ = max(heaps[e][0][0], efree[e])
                    else:
                        continue
                    if best is None or st_ < best[0]:
                        best = (st_, e)
                st_, e = best
                while heaps[e] and heaps[e][0][0] <= st_:
                    heapq.heappush(avail[e], heapq.heappop(heaps[e])[1])
                i = heapq.heappop(avail[e])
                rec = self.ins[e][i]
                issue[(e, i)] = st_
                efree[e] = st_ + rec[4]
                f_ = st_ + rec[6]
                fin[(e, i)] = f_
                tmax = max(tmax, f_)
                order[e].append(i)
                left -= 1
                for (u, kind) in users.get((e, i), ()):
                    t_ = f_ if kind == 0 else st_
                    if t_ > ready[u]:
                        ready[u] = t_
                    ndep[u] -= 1
                    if ndep[u] == 0:
                        heapq.heappush(heaps[u[0]], (ready[u], u[1]))
            tnow = tmax
        self.sim_time = tnow
        return order

    def _check(self, order, val):
        sems = {}
        pos = {e: 0 for e in self.ins}
        curph = {e: 0 for e in self.ins}
        progress = True
        total = sum(len(v) for v in self.ins.values())
        done = 0
        while progress:
            progress = False
            for e in self.ins:
                while pos[e] < len(order[e]):
                    i = order[e][pos[e]]
                    rec = self.ins[e][i]
                    deps = list(rec[1])
                    if rec[3] != curph[e]:
                        for p in range(curph[e] + 1, rec[3] + 1):
                            deps.extend(self.phase_ev.get(p, ()))
                    ok = True
                    for d in deps:
                        if d[0] == "E":
                            if sems.get(("E", d[1]), 0) < val[d[1]][d[2]]:
                                ok = False
                                break
                        elif sems.get(("D", d[1]), 0) < d[2]:
                            ok = False
                            break
                    if not ok:
                        break
                    curph[e] = rec[3]
                    if rec[2] is not None:
                        sems[("D", rec[2])] = sems.get(("D", rec[2]), 0) + 16
                    elif i in val.get(e, {}):
                        sems[("E", e)] = val[e][i]
                    pos[e] += 1
                    done += 1
                    progress = True
        if done != total:
            msg = []
            for e in self.ins:
                if pos[e] < len(order[e]):
                    i = order[e][pos[e]]
                    msg.append((e, pos[e], i, self.ins[e][i][3], self.ins[e][i][1][:6]))
            raise RuntimeError("schedule deadlock: %s" % msg)

    def emit(self, final_waits=(), reorder=True, only=None):
        import contextlib
        nc = self.nc
        if reorder:
            order = self._schedule(only)
        else:
            order = {e: list(range(len(l))) for e, l in self.ins.items()}
        for e in self.ins:
            assert sorted(order[e]) == list(range(len(self.ins[e]))), e
        lastc = {}
        for e in COMPUTE:
            cur = None
            per = {}
            for i in order[e]:
                if self.ins[e][i][2] is None:
                    per[self.ins[e][i][3]] = i
            lastc[e] = per
        for p in list(self.phase_ev.keys()):
            evs = [d for d in self.phase_ev[p] if d[0] == "D"]
            for e in COMPUTE:
                qs = [q for q in lastc[e] if q < p]
                if qs:
                    evs.append(("E", e, lastc[e][max(qs)]))
            self.phase_ev[p] = evs
        need = {e: set() for e in COMPUTE}
        for e, lst in self.ins.items():
            for rec in lst:
                for d in rec[1]:
                    if d[0] == "E":
                        need[d[1]].add(d[2])
        for evs in self.phase_ev.values():
            for d in evs:
                if d[0] == "E":
                    need[d[1]].add(d[2])
        val = {}
        for e in COMPUTE:
            c = 0
            v = {}
            for i in order[e]:
                if i in need[e]:
                    c += 1
                    v[i] = c
            val[e] = v
        self._check(order, val)
        self.nwaits = {}
        with contextlib.ExitStack() as st:
            esem = {e: st.enter_context(nc.semaphore("s_" + e)) for e in COMPUTE}
            dsem = [st.enter_context(nc.semaphore("d%d" % i)) for i in range(self.ndsem)]
            block = st.enter_context(nc.Block())

            def run(ename, eng):
                waited = {}
                lst = self.ins[ename]
                cur_ph = 0
                for i in order[ename]:
                    rec = lst[i]
                    deps = rec[1]
                    if rec[3] != cur_ph:
                        deps = list(deps)
                        for p in range(cur_ph + 1, rec[3] + 1):
                            deps.extend(self.phase_ev.get(p, ()))
                        cur_ph = rec[3]
                    tg = {}
                    for d in deps:
                        if d[0] == "E":
                            key = ("E", d[1]); v = val[d[1]][d[2]]; sem = esem[d[1]]
                        else:
                            key = ("D", d[1]); v = d[2]; sem = dsem[d[1]]
                        if tg.get(key, (None, 0))[1] < v:
                            tg[key] = (sem, v)
                    for key, (sem, v) in tg.items():
                        if waited.get(key, 0) >= v:
                            continue
                        eng.wait_ge(sem, v)
                        waited[key] = v
                        self.nwaits[ename] = self.nwaits.get(ename, 0) + 1
                    ins = rec[0](eng)
                    if rec[2] is not None:
                        ins.then_inc(dsem[rec[2]], 16)
                    elif i in need[ename]:
                        ins.then_inc(esem[ename], 1)
                if ename == "sp":
                    for d in final_waits:
                        eng.wait_ge(dsem[d[1]], d[2])

            block.tensor(lambda e: run("pe", e))
            block.vector(lambda e: run("dve", e))
            block.scalar(lambda e: run("act", e))
            block.gpsimd(lambda e: run("pool", e))
            block.sync(lambda e: run("sp", e))


def interleave(gens):
    gens = [g for g in gens if g is not None]
    while gens:
        nxt = []
        for g in gens:
            try:
                next(g)
                nxt.append(g)
            except StopIteration:
                pass
            yield
        gens = nxt


class Tl:
    __slots__ = ("t", "r")

    def __init__(self, t, name):
        self.t = t
        self.r = Res(name)

    def __getitem__(self, k):
        return self.t[k]


def build_nc(dbg=False, depth_run=DEPTH, reorder=True, only=None):
    nc = bass.Bass("TRN2", target_bir_lowering=False)
    S = Sched(nc)

    def din(name, shape, dt=F32):
        return nc.dram_tensor(name, list(shape), dt, kind="ExternalInput").ap()

    def dscr(name, shape, dt):
        return nc.dram_tensor(name, list(shape), dt, kind="ExternalOutput" if dbg else "Internal").ap()

    XIN = din("xin", [TOK, D])
    CVEC = din("cvec", [2, D])
    WMOD = din("w_mod", [DEPTH, D, 6 * D])
    BMOD = din("b_mod", [DEPTH, 6 * D])
    WIN = din("w_in", [DEPTH, D, 3072])
    ROPE = din("rope", [4, 128, TOK])
    CONVW = din("convw", [DEPTH, 128, 2, 3])
    SINK = din("sink", [DEPTH, 6])
    NAB = din("nab", [DEPTH, 5, 128, 6, 640])
    AMASK = din("amask", [128, 2, 384])
    WOUT = din("w_out", [DEPTH, D, D])
    LN1G = din("ln1_g", [DEPTH, D]); LN1B = din("ln1_b", [DEPTH, D])
    LN2G = din("ln2_g", [DEPTH, D]); LN2B = din("ln2_b", [DEPTH, D])
    WR = din("w_router", [DEPTH, D, NE])
    WG = din("w_gate", [DEPTH, NE, D, 512]); WU = din("w_up", [DEPTH, NE, D, 512])
    WD = din("w_down", [DEPTH, NE, 512, D])
    Y = nc.dram_tensor("y", [8192, D], F32, kind="ExternalOutput").ap()

    MODROW = dscr("modrow", [2, 6 * D], F32)
    FM = dscr("fm", [128, 14, TOK], BF16)
    VV = dscr("vv", [TOK, 8, 65], BF16)
    XMID = dscr("xmid", [TOK, D], F32)
    H2T = dscr("h2t", [128, 8, TOK], BF16)
    XCUR = dscr("xcur", [TOK, D], F32)
    XH2 = dscr("xh2", [TOK, RW], BF16)
    XE = [dscr("xe%d" % e, [1024, RW], BF16) for e in range(NE)]
    FFN = dscr("ffn", [TOK, D], F32)

    SB_LO = 16512
    SB_HI = 229344
    st = {"pers": SB_LO, "ph": None}

    def _alloc(name, shape, dt, key):
        nb = int(np.prod(shape[1:])) * (2 if dt == BF16 else 4)
        nb = (nb + 31) // 32 * 32
        off = st[key]
        assert off + nb <= SB_HI, (name, off, nb)
        st[key] = off + nb
        return Tl(nc.alloc_sbuf_tensor_at(name, list(shape), dt, offset=off), name)

    def pers(name, shape, dt=F32):
        return _alloc(name, shape, dt, "pers")

    cnt = [0]

    def ph(name, shape, dt=F32):
        cnt[0] += 1
        return _alloc("%s_%d" % (name, cnt[0]), shape, dt, "ph")

    def new_phase():
        S.barrier()
        st["ph"] = st["pers_end"]

    pbig = Tl(nc.alloc_psum_tensor("pbig", [128, 6, 512], F32), "pbig")
    ptr = [Tl(nc.alloc_psum_tensor("ptr%d" % i, [128, 8, 128], BF16), "ptr%d" % i) for i in range(2)]
    pbr = [Res("pb%d" % i) for i in range(6)]

    ident = pers("ident", [128, 128], BF16)
    identf = pers("identf", [128, 128], F32)
    onesf = pers("onesf", [128, 128], F32)
    aff = pers("aff", [128, NT, NE], F32)
    wsel = pers("wsel", [128, NT, NE], F32)
    modT = pers("modT", [128, 48], F32)
    modcT = pers("modcT", [128, 48], F32)
    esink = pers("esink", [128, 6], F32)
    convw = pers("convw", [128, 2, 3], F32)
    amask = pers("amask", [128, 2, 384], BF16)
    eps_t = pers("eps", [128, 1], F32)
    ltri = pers("ltri", [128, 128], F32)
    idxT = pers("idxT", [128, NE, 64], I32)
    acc = pers("acc", [128, 2, D], F32)
    st["pers_end"] = st["pers"]
    st["ph"] = st["pers_end"]

    _breg = {}

    def breg(e):
        if "r" not in _breg:
            _breg["r"] = e.to_reg(1023)
        return _breg["r"]

    def rs(lst):
        return [x.r if isinstance(x, Tl) else x for x in lst]

    def fsz(ap):
        try:
            return float(ap.free_size())
        except Exception:
            return 512.0

    def mm(out, lhsT, rhs, start, stop, reads, writes, tr=False):
        if tr:
            S.op("pe", lambda e: e.matmul(out, lhsT=lhsT, rhs=rhs, is_transpose=True),
                 reads=rs(reads), writes=rs(writes), pe_acc=True, cost=0.08)
        else:
            c = max(fsz(rhs), 64.0) / 2000.0 * (4.0 if lhsT.dtype == F32 else 1.0) + 0.03
            S.op("pe", lambda e: e.matmul(out, lhsT=lhsT, rhs=rhs, start=start, stop=stop),
                 reads=rs(reads), writes=rs(writes), pe_acc=True, cost=c)

    def act(out, in_, func, reads, writes, bias=0.0, scale=1.0, accum=None):
        if accum is None:
            S.op("act", lambda e: e.activation(out=out, in_=in_, func=func, bias=bias, scale=scale),
                 reads=rs(reads), writes=rs(writes), cost=0.2 + fsz(out) / 1300.0)
        else:
            S.op("act", lambda e: e.activation(out=out, in_=in_, func=func, bias=bias, scale=scale,
                                               accum_out=accum), reads=rs(reads), writes=rs(writes))

    def vcost(eng, ap):
        return (0.1 + fsz(ap) / 900.0) if eng == "dve" else (0.2 + fsz(ap) / 450.0)

    def tt(out, in0, in1, op, reads, writes, eng="dve"):
        S.op(eng, lambda e: e.tensor_tensor(out=out, in0=in0, in1=in1, op=op), reads=rs(reads), writes=rs(writes),
             cost=vcost(eng, out))

    def ts(out, in0, s1, s2, op0, op1, reads, writes, eng="dve"):
        if op1 is None:
            S.op(eng, lambda e: e.tensor_scalar(out=out, in0=in0, scalar1=s1, scalar2=None, op0=op0),
                 reads=rs(reads), writes=rs(writes), cost=vcost(eng, out))
        else:
            S.op(eng, lambda e: e.tensor_scalar(out=out, in0=in0, scalar1=s1, scalar2=s2, op0=op0, op1=op1),
                 reads=rs(reads), writes=rs(writes), cost=vcost(eng, out))

    def stt(out, in0, scalar, in1, op0, op1, reads, writes):
        S.op("dve", lambda e: e.scalar_tensor_tensor(out=out, in0=in0, scalar=scalar, in1=in1, op0=op0, op1=op1),
             reads=rs(reads), writes=rs(writes), cost=vcost("dve", out))

    def cp(out, in_, reads, writes, eng="dve"):
        S.op(eng, lambda e: e.tensor_copy(out=out, in_=in_), reads=rs(reads), writes=rs(writes), cost=vcost(eng, out))

    def recip(out, in_, reads, writes):
        S.op("dve", lambda e: e.reciprocal(out=out, in_=in_), reads=rs(reads), writes=rs(writes))

    def reduce(out, in_, op, reads, writes, negate=False):
        S.op("dve", lambda e: e.tensor_reduce(out=out, in_=in_, axis=AX.X, op=op, negate=negate),
             reads=rs(reads), writes=rs(writes), cost=vcost("dve", in_))

    def memset(eng, ap, val, writes):
        S.op(eng, lambda e: e.memset(ap, val), writes=rs(writes), cost=vcost(eng, ap))

    def single(out, in_, scalar, op, reads, writes):
        S.op("dve", lambda e: e.tensor_single_scalar(out=out, in_=in_, scalar=scalar, op=op),
             reads=rs(reads), writes=rs(writes))

    def scan(out, d0, d1, reads, writes):
        S.op("dve", lambda e: e.tensor_tensor_scan(out=out, data0=d0, data1=d1, initial=0.0, op0=ALU.add, op1=ALU.add),
             reads=rs(reads), writes=rs(writes))

    def iota_tail(ap, base, writes):
        S.op("pool", lambda e: e.iota(ap, pattern=[[0, 1]], base=base, channel_multiplier=1), writes=rs(writes))

    def dma(q, out, in_, reads, writes, owner, slow=False, group=None):
        try:
            nbytes = float(out.nbytes())
        except Exception:
            nbytes = 1.0e5
        lat = 2.5 + nbytes / 1.5e5
        cost = 0.1 if q == "sp" else 1.0
        ow = owner.r if isinstance(owner, Tl) else owner
        if group is not None:
            return S.dma(q, lambda e: e.dma_start(out=out, in_=in_), reads=rs(reads), writes=rs(writes),
                         owner=ow, group=group, cost=cost, lat=lat)
        if slow:
            return S.dma(q, lambda e: e.dma_start(out=out, in_=in_, allow_slow_non_contiguous=True),
                         reads=rs(reads), writes=rs(writes), owner=ow, cost=cost, lat=lat + 3.0)
        return S.dma(q, lambda e: e.dma_start(out=out, in_=in_),
                     reads=rs(reads), writes=rs(writes), owner=ow, cost=cost, lat=lat)

    def ln_stats(src_ap, src_res, stats, mv, rstd, nmr=None):
        for h in range(2):
            S.op("dve", lambda e, h=h: e.bn_stats(out=stats[:, h, :], in_=src_ap[:, h * 512:(h + 1) * 512]),
                 reads=rs([src_res]), writes=rs([stats]))
        S.op("dve", lambda e: e.bn_aggr(out=mv[:], in_=stats[:].rearrange("p a b -> p (a b)")),
             reads=rs([stats]), writes=rs([mv]))
        act(rstd[:], mv[:, 1:2], AF.Ln, [mv, eps_t], [rstd], bias=eps_t[:, 0:1])
        act(rstd[:], rstd[:], AF.Exp, [rstd], [rstd], scale=-0.5)
        if nmr is not None:
            stt(nmr[:], mv[:, 0:1], -1.0, rstd[:], ALU.mult, ALU.mult, [mv, rstd], [nmr])

    S.op("pool", lambda e: e.iota(identf[:], pattern=[[1, 128]], base=0, channel_multiplier=-1,
                                  allow_small_or_imprecise_dtypes=True), writes=rs([identf]))
    S.op("dve", lambda e: e.tensor_single_scalar(out=ident[:], in_=identf[:], scalar=0.0, op=ALU.is_equal),
         reads=rs([identf]), writes=rs([ident]))
    S.op("dve", lambda e: e.memset(onesf[:], 1.0), writes=rs([onesf]))
    S.op("dve", lambda e: e.tensor_single_scalar(out=ltri[:], in_=identf[:], scalar=0.0, op=ALU.is_gt),
         reads=rs([identf]), writes=rs([ltri]))
    S.op("dve", lambda e: e.memset(eps_t[:], 1e-6), writes=rs([eps_t]))
    dma("pool", amask[:], AMASK, [S.dres("amask")], [amask], amask)

    fm_res = [S.dres("fm", t) for t in range(NT)]
    vv_res = [S.dres("vv", t) for t in range(NT)]
    xmid_res = [S.dres("xmid", t) for t in range(NT)]
    h2t_res = [S.dres("h2t", t) for t in range(NT)]
    xcur_res = [S.dres("xcur", t) for t in range(NT)]
    xh2_res = [S.dres("xh2", t) for t in range(NT)]
    y_res = [S.dres("y", t) for t in range(64)]
    final = []

    for l in range(depth_run):
        last = l == DEPTH - 1
        T0 = 2 if last else 0
        new_phase()
        cT = ph("cT", [128, 8, 2])
        bm = ph("bm", [2, 6 * D])
        mrow = ph("mrow", [2, 6 * D])
        wm = [ph("wm%d" % i, [128, 8, 512]) for i in range(2)]
        for m_ in range(2):
            dma("sp", cT[:, :, m_], CVEC[m_].rearrange("(k p) -> p k", p=128), [S.dres("cvec")], [cT], cT, slow=True)
        dma("sp", bm[:], BMOD[l].partition_broadcast(2), [S.dres("bmod")], [bm], bm)
        dma("sp", esink[:], SINK[l].partition_broadcast(128), [S.dres("sink")], [esink], esink)
        dma("sp", convw[:], CONVW[l], [S.dres("convw")], [convw], convw)
        act(cT[:], cT[:], AF.Silu, [cT], [cT])
        act(esink[:], esink[:], AF.Exp, [esink], [esink])
        wmv = WMOD[l].rearrange("(k p) n -> p k n", p=128)
        for cc in range(12):
            w_ = wm[cc % 2]
            dma("sp", w_[:], wmv[:, :, cc * 512:(cc + 1) * 512], [S.dres("wmod")], [w_], w_)
            pb = pbig[0:2, cc % 2, :]
            for k in range(8):
                mm(pb, cT[:, k, :], w_[:, k, :], k == 0, k == 7, [cT, w_], [pbr[cc % 2]])
            tt(mrow[:, cc * 512:(cc + 1) * 512], pb, bm[:, cc * 512:(cc + 1) * 512], ALU.add,
               [pbr[cc % 2], bm], [mrow])
        dma("sp", MODROW, mrow[:], [mrow], [S.dres("modrow")], mrow)
        dma("sp", modT[:], MODROW[0].rearrange("(j p) -> p j", p=128), [S.dres("modrow")], [modT], modT, slow=True)
        dma("sp", modcT[:], MODROW[1].rearrange("(j p) -> p j", p=128), [S.dres("modrow")], [modcT], modcT, slow=True)
        for m_ in (modT, modcT):
            ts(m_[:, 8:16], m_[:, 8:16], 1.0, None, ALU.add, None, [m_], [m_])
            ts(m_[:, 32:40], m_[:, 32:40], 1.0, None, ALU.add, None, [m_], [m_])

        new_phase()
        win = ph("win", [128, 8, 3072], BF16)
        wiv = WIN[l].rearrange("(k p) n -> p k n", p=128)
        for k in range(8):
            dma("pool", win[:, k, :], wiv[:, k, :], [S.dres("win")], [win], win)
        xt = [ph("xt%d" % i, [128, D]) for i in range(2)]
        xh = [ph("xh%d" % i, [128, D], BF16) for i in range(2)]
        hT = [ph("hT%d" % i, [128, 8, 512], BF16) for i in range(2)]
        fmo = [ph("fmo%d" % i, [128, 14, 512], BF16) for i in range(2)]
        vo = [ph("vo%d" % i, [128, 4, 8, 65], BF16) for i in range(2)]
        rp = [ph("rp%d" % i, [128, 4, 512]) for i in range(2)]
        tmp = [ph("tmp%d" % i, [128, 512]) for i in range(3)]
        stats = ph("stats", [128, 2, 6]); mv = ph("mv", [128, 2]); rstd = ph("rstd", [128, 1])
        for v_ in vo:
            S.op("pool", lambda e, v_=v_: e.memset(v_[:], 1.0), writes=rs([v_]))
        src = XIN if l == 0 else XCUR
        src_res = (lambda t: S.dres("xin", t)) if l == 0 else (lambda t: xcur_res[t])
        groups = [(0, 2)] + [(2 + 4 * i, 4) for i in range(16)]
        bank = [0]

        def nextbank():
            b = bank[0]
            bank[0] = (b + 1) % 6
            return b

        def gen_L(gi, t0, nt):
            N = nt * 128
            sl = gi % 2
            mT = modcT if gi == 0 else modT
            rpt = rp[sl]
            dma("sp", rpt[:, :, :N], ROPE[:, :, t0 * 128:t0 * 128 + N].rearrange("a p n -> p a n"),
                [S.dres("rope")], [rpt], rpt)
            for s in range(nt):
                tl = t0 + s
                x_ = xt[tl % 2]; xh_ = xh[tl % 2]; pt_ = ptr[tl % 2]
                dma("sp", x_[:], src[tl * 128:(tl + 1) * 128, :], [src_res(tl)], [x_], x_)
                ln_stats(x_, x_, stats, mv, rstd)
                yield
                ts(xh_[:], x_[:], mv[:, 0:1], rstd[:, 0:1], ALU.subtract, ALU.mult, [x_, mv, rstd], [xh_])
                for k in range(8):
                    mm(pt_[:, k, :], xh_[:, k * 128:(k + 1) * 128], ident[:], True, True, [xh_, ident], [pt_], tr=True)
                yield
                for k in range(8):
                    act(hT[sl][:, k, s * 128:(s + 1) * 128], pt_[:, k, :], AF.Identity, [pt_, mT], [hT[sl]],
                        bias=mT[:, k:k + 1], scale=mT[:, 8 + k:9 + k])
                    if k % 4 == 3:
                        yield

        def gen_P(gi, t0, nt):
            N = nt * 128
            sl = gi % 2
            rpt = rp[sl]
            h_ = hT[sl]; fo = fmo[sl]

            def proj(ch):
                b = nextbank()
                for k in range(8):
                    mm(pbig[:, b, :N], win[:, k, ch * 128:(ch + 1) * 128], h_[:, k, :N], k == 0, k == 7,
                       [win, h_], [pbr[b]])
                return b

            def rope(chq, chs, tq, tsn, dst):
                b1 = proj(chq); b2 = proj(chs)
                tt(tmp[0][:, :N], pbig[:, b1, :N], rpt[:, tq, :N], ALU.mult, [pbr[b1], rpt], [tmp[0]])
                tt(tmp[1][:, :N], pbig[:, b2, :N], rpt[:, tsn, :N], ALU.mult, [pbr[b2], rpt], [tmp[1]])
                tt(fo[:, dst, :N], tmp[0][:, :N], tmp[1][:, :N], ALU.add, [tmp[0], tmp[1]], [fo], eng="pool")

            for c_ in range(3):
                rope(c_, 3 + c_, 0, 1, c_)
                yield
            rope(6, 7, 2, 3, 6)
            yield
            for c_ in range(2):
                b1 = proj(8 + c_)
                act(tmp[2][:, :N], pbig[:, b1, :N], AF.Identity, [pbr[b1]], [tmp[2]])
                b2 = proj(12 + c_)
                tt(fo[:, 10 + c_, :N], pbig[:, b2, :N], tmp[2][:, :N], ALU.mult, [pbr[b2], tmp[2]], [fo])
                yield
                b3 = proj(10 + c_)
                act(fo[:, 12 + c_, :N], pbig[:, b3, :N], AF.Identity, [pbr[b3]], [fo])
                yield
            for c_ in range(3):
                b1 = proj(14 + c_)
                act(fo[:, 3 + c_, :N], pbig[:, b1, :N], AF.Identity, [pbr[b1]], [fo], scale=0.125)
                yield
                b2 = proj(17 + c_)
                cp(fo[:, 7 + c_, :N], pbig[:, b2, :N], [pbr[b2]], [fo])
                yield
            for s in range(nt):
                b = nextbank()
                for k in range(8):
                    mm(pbig[:, b, :], h_[:, k, s * 128:(s + 1) * 128], win[:, k, 2560:3072], k == 0, k == 7,
                       [win, h_], [pbr[b]])
                act(vo[sl][:, s, :, 0:64], pbig[:, b, :].rearrange("p (h d) -> p h d", h=8), AF.Identity,
                    [pbr[b]], [vo[sl]])
                yield
            dma("sp", FM[:, :, t0 * 128:t0 * 128 + N], fo[:, :, :N], [fo], fm_res[t0:t0 + nt], fo)
            dma("sp", VV[t0 * 128:t0 * 128 + N].rearrange("(s p) h d -> p s h d", p=128), vo[sl][:, :nt],
                [vo[sl]], vv_res[t0:t0 + nt], vo[sl])
            yield

        prevP = None
        for gi, (t0, nt) in enumerate(groups):
            for _ in interleave([gen_L(gi, t0, nt), prevP]):
                pass
            prevP = gen_P(gi, t0, nt)
        for _ in prevP:
            pass

        new_phase()
        wout = ph("wout", [128, 8, D], BF16)
        wov = WOUT[l].rearrange("(k p) n -> p k n", p=128)
        for k in range(0, 8, 2):
            dma("pool", wout[:, k:k + 2, :], wov[:, k:k + 2, :], [S.dres("wout")], [wout], wout)
        wr = ph("wr", [128, 8, NE], BF16)
        dma("pool", wr[:], WR[l].rearrange("(k p) n -> p k n", p=128), [S.dres("wr")], [wr], wr)
        nabi = ph("nabi", [128, 6, 640], BF16)
        nabe = ph("nabe", [128, 6, 640], BF16)
        dma("pool", nabi[:], NAB[l, 0], [S.dres("nab")], [nabi], nabi)
        kctx = ph("kctx", [128, 4, 256], BF16)
        vctx = ph("vctx", [128, 2, 8, 65], BF16)
        dma("sp", kctx[:], FM[:, 6:10, 0:256], fm_res[0:2], [kctx], kctx)
        dma("sp", vctx[:], VV[0:256].rearrange("(s p) h d -> p s h d", p=128), vv_res[0:2], [vctx], vctx)
        g1b = ph("g1b", [128, D]); cg1b = ph("cg1b", [128, D])
        l1g = ph("l1g", [128, D]); l1b = ph("l1b", [128, D])
        dma("sp", g1b[:], MODROW[0, 2048:3072].partition_broadcast(128), [S.dres("modrow")], [g1b], g1b)
        dma("sp", cg1b[:], MODROW[1, 2048:3072].partition_broadcast(128), [S.dres("modrow")], [cg1b], cg1b)
        dma("sp", l1g[:], LN1G[l].partition_broadcast(128), [S.dres("ln1g")], [l1g], l1g)
        dma("sp", l1b[:], LN1B[l].partition_broadcast(128), [S.dres("ln1b")], [l1b], l1b)
        qw = [ph("qw%d" % i, [128, 6, 128], BF16) for i in range(2)]
        kw = [ph("kw%d" % i, [128, 4, 640], BF16) for i in range(2)]
        vw = [ph("vw%d" % i, [128, 5, 8, 65], BF16) for i in range(2)]
        uw = [ph("uw%d" % i, [128, 2, 130], BF16) for i in range(2)]
        bbw = [ph("bbw%d" % i, [128, 2, 128], BF16) for i in range(2)]
        xa = [ph("xa%d" % i, [128, D]) for i in range(2)]
        pta = [ph("pta%d" % i, [128, 5, 384], BF16) for i in range(2)]
        ptn = [ph("ptn%d" % i, [128, 896], BF16) for i in range(2)]
        sfn = [ph("sfn%d" % i, [128, 640]) for i in range(2)]
        mixc = ph("mixc", [128, 768], BF16)
        mixT = ph("mixT", [128, 8, 128], BF16)
        ctmp = ph("ctmp", [128, 128])
        den = ph("den", [128, 6]);
        t1 = ph("t1", [128, D]); y1 = ph("y1", [128, D]); xm = [ph("xm%d" % i, [128, D]) for i in range(2)]
        xh2 = ph("xh2", [128, RW], BF16)
        h2o = [ph("h2o%d" % i, [128, 8, 128], BF16) for i in range(2)]
        stats = ph("stats", [128, 2, 6]); mv = ph("mv", [128, 2]); rstd = ph("rstd", [128, 1]); nmr = ph("nmr", [128, 1])
        rmx = ph("rmx", [128, 1]); rsum = ph("rsum", [128, 1]); rexp = ph("rexp", [128, NE])

        mixT2 = [mixT, ph("mixTb", [128, 8, 128], BF16)]

        def gen_att(T):
            sl = T % 2
            is_ctx = T < 2
            i = T - 2
            q_ = qw[sl]; k_ = kw[sl]; v_ = vw[sl]; u_ = uw[sl]; bb_ = bbw[sl]
            mT_ = mixT2[sl]
            dma("sp", q_[:], FM[:, 0:6, T * 128:(T + 1) * 128], [fm_res[T]], [q_], q_)
            nb = nabi
            base = 0
            if not is_ctx:
                base = min(max(i - 2, 0), 59) + 2
                dma("sp", k_[:], FM[:, 6:10, base * 128:(base + 5) * 128], fm_res[base:base + 5], [k_], k_)
                dma("sp", v_[:], VV[base * 128:(base + 5) * 128].rearrange("(s p) h d -> p s h d", p=128),
                    vv_res[base:base + 5], [v_], v_)
                var = 0 if 2 <= i <= 61 else (1 + i if i < 2 else i - 59)
                if var != 0:
                    dma("pool", nabe[:], NAB[l, var], [S.dres("nab")], [nabe], nabe)
                    nb = nabe
            lo_pad = T in (0, 2); hi_pad = T in (1, NT - 1)
            if lo_pad or hi_pad:
                memset("pool", u_[:], 0.0, [u_])
            c0 = T * 128 - (0 if lo_pad else 1); c1 = (T + 1) * 128 + (0 if hi_pad else 1)
            o0 = 1 if lo_pad else 0
            fr = fm_res[max(T - 1, 0):min(T + 2, NT)]
            dma("sp", u_[:, :, o0:o0 + (c1 - c0)], FM[:, 10:12, c0:c1], fr, [u_], u_)
            dma("sp", bb_[:], FM[:, 12:14, T * 128:(T + 1) * 128], [fm_res[T]], [bb_], bb_)
            yield
            if is_ctx:
                akeys = [(("c", 0), None), (("c", 1), None)]
                nkeys = [("c", 0), ("c", 1)]
            else:
                akeys = []
                if i > 0:
                    akeys.append((("w", T - 1 - base), 0))
                akeys.append((("w", T - base), None))
                if i < 63:
                    akeys.append((("w", T + 1 - base), 1))
                akeys += [(("c", 0), None), (("c", 1), None)]
                nkeys = [("w", j) for j in range(5)] + [("c", 0), ("c", 1)]

            def kap(kt, ch, p0):
                if kt[0] == "c":
                    return kctx[p0:p0 + 64, ch, kt[1] * 128:(kt[1] + 1) * 128], kctx
                return k_[p0:p0 + 64, ch, kt[1] * 128:(kt[1] + 1) * 128], k_

            def vap(kt, head):
                if kt[0] == "c":
                    return vctx[:, kt[1], head, :], vctx
                return v_[:, kt[1], head, :], v_

            def gen_A():
                for g in range(2):
                    p0 = g * 64
                    pa_ = pta[g]
                    for ki, (kt, mk) in enumerate(akeys):
                        ka_, kr = kap(kt, 0, p0)
                        if mk is not None:
                            mm(pbig[:, 0, 0:384], ident[:], amask[:, mk, :], True, False, [ident, amask], [pbr[0]])
                        mm(pbig[:, 0, 0:384], ka_, q_[p0:p0 + 64, 0:3, :].rearrange("p a b -> p (a b)"), mk is None, True,
                           [kr, q_], [pbr[0]])
                        act(pa_[:, ki, :], pbig[:, 0, 0:384], AF.Exp, [pbr[0]], [pa_])
                        yield
                    po = pbig[:, 2, 0:195].rearrange("p (c d) -> p c d", c=3)
                    for c_ in range(3):
                        for ki, (kt, mk) in enumerate(akeys):
                            va_, vr = vap(kt, g)
                            mm(po[:, c_, :], pa_[:, ki, c_ * 128:(c_ + 1) * 128], va_, ki == 0, ki == len(akeys) - 1,
                               [pa_, vr], [pbr[2]])
                        yield
                    tt(den[:, 0:3], po[:, :, 64], esink[:, 3 * g:3 * g + 3], ALU.add, [pbr[2], esink], [den])
                    recip(den[:, 0:3], den[:, 0:3], [den], [den])
                    tt(mixc[:, g * 192:(g + 1) * 192].rearrange("p (c d) -> p c d", c=3), po[:, :, 0:64],
                       den[:, 0:3].unsqueeze(2).to_broadcast([128, 3, 64]), ALU.mult, [pbr[2], den], [mixcA])
                    yield

            def gen_N():
                po2 = pbig[:, 3, 0:390].rearrange("p (c d) -> p c d", c=6)
                nk = len(nkeys)
                for h in range(6):
                    ch = h // 2; p0 = (h % 2) * 64
                    pn_ = ptn[h % 2]; sf_ = sfn[h % 2]
                    ps2 = pbig[:, 4:6, :].rearrange("p a b -> p (a b)")
                    if is_ctx:
                        for j, kt in enumerate(nkeys):
                            ka_, kr = kap(kt, 1 + ch, p0)
                            mm(ps2[:, j * 128:(j + 1) * 128], ka_, q_[p0:p0 + 64, 3 + ch, :], True, True,
                               [kr, q_], [pbr[4], pbr[5]])
                        act(pn_[:, 0:256], ps2[:, 0:256], AF.Exp, [pbr[4], pbr[5]], [pn_])
                    else:
                        for j in (5, 6):
                            ka_, kr = kap(nkeys[j], 1 + ch, p0)
                            mm(ps2[:, j * 128:(j + 1) * 128], ka_, q_[p0:p0 + 64, 3 + ch, :], True, True,
                               [kr, q_], [pbr[4], pbr[5]])
                        mm(ps2[:, 512:640], ident[:], nb[:, h, 512:640], True, False, [ident, nb], [pbr[4], pbr[5]])
                        mm(ps2[:, 0:512], ident[:], nb[:, h, 0:512], True, False, [ident, nb], [pbr[4], pbr[5]])
                        for j in range(5):
                            ka_, kr = kap(nkeys[j], 1 + ch, p0)
                            mm(ps2[:, j * 128:(j + 1) * 128], ka_, q_[p0:p0 + 64, 3 + ch, :], False, j in (3, 4),
                               [kr, q_], [pbr[4], pbr[5]])
                        act(pn_[:, 0:896], ps2[:, 0:896], AF.Exp, [pbr[4], pbr[5]], [pn_])
                    yield
                    for j, kt in enumerate(nkeys):
                        va_, vr = vap(kt, 2 + h)
                        mm(po2[:, h, :], pn_[:, j * 128:(j + 1) * 128], va_, j == 0, j == nk - 1, [pn_, vr], [pbr[3]])
                    yield
                recip(den2[:], po2[:, :, 64], [pbr[3]], [den2])
                tt(mixc[:, 384:768].rearrange("p (c d) -> p c d", c=6), po2[:, :, 0:64],
                   den2[:].unsqueeze(2).to_broadcast([128, 6, 64]), ALU.mult, [pbr[3], den2], [mixcN])
                yield

            def gen_B():
                for c_ in range(2):
                    ts(ctmp[:], u_[:, c_, 0:128], convw[:, c_, 0:1], None, ALU.mult, None, [u_, convw], [ctmp])
                    stt(ctmp[:], u_[:, c_, 1:129], convw[:, c_, 1:2], ctmp[:], ALU.mult, ALU.add, [u_, convw, ctmp], [ctmp])
                    stt(ctmp[:], u_[:, c_, 2:130], convw[:, c_, 2:3], ctmp[:], ALU.mult, ALU.add, [u_, convw, ctmp], [ctmp])
                    tt(mT_[:, 3 + c_, :], ctmp[:], bb_[:, c_, :], ALU.mult, [ctmp, bb_], [mT_])
                    yield

            for _ in interleave([gen_A(), gen_N(), gen_B()]):
                yield
            pt_ = ptr[0]
            for c_ in range(6):
                mm(pt_[:, c_, :], mixc[:, c_ * 128:(c_ + 1) * 128], ident[:], True, True, [mixcA, mixcN, ident], [pt_], tr=True)
            cp(mT_[:, 0:3, :], pt_[:, 0:3, :], [pt_], [mT_])
            act(mT_[:, 5:8, :], pt_[:, 3:6, :], AF.Identity, [pt_], [mT_])
            yield

        def gen_epi(T):
            sl = T % 2
            is_ctx = T < 2
            x_ = xa[sl]; mT_ = mixT2[sl]
            dma("sp", x_[:], src[T * 128:(T + 1) * 128, :], [src_res(T)], [x_], x_)
            gb = cg1b if is_ctx else g1b
            for hf in range(2):
                for k in range(8):
                    mm(pbig[:, 1, :], mT_[:, k, :], wout[:, k, hf * 512:(hf + 1) * 512], k == 0, k == 7,
                       [mT_, wout], [pbr[1]])
                tt(t1[:, hf * 512:(hf + 1) * 512], pbig[:, 1, :], gb[:, hf * 512:(hf + 1) * 512], ALU.mult,
                   [pbr[1], gb], [t1])
                yield
            stt(y1[:], x_[:], ALPHA, t1[:], ALU.mult, ALU.add, [x_, t1], [y1])
            yield
            ln_stats(y1, y1, stats, mv, rstd, nmr)
            yield
            xm_ = xm[sl]
            act(t1[:], y1[:], AF.Identity, [y1, rstd, nmr], [t1], bias=nmr[:, 0:1], scale=rstd[:, 0:1])
            yield
            tt(t1[:], t1[:], l1g[:], ALU.mult, [t1, l1g], [t1], eng="pool")
            yield
            tt(xm_[:], t1[:], l1b[:], ALU.add, [t1, l1b], [xm_], eng="pool")
            dma("sp", XMID[T * 128:(T + 1) * 128, :], xm_[:], [xm_], [xmid_res[T]], xm_)
            yield
            ln_stats(xm_, xm_, stats, mv, rstd)
            yield
            ts(xh2[:, 0:D], xm_[:], mv[:, 0:1], rstd[:, 0:1], ALU.subtract, ALU.mult, [xm_, mv, rstd], [xh2])
            yield
            pt2 = ptr[1]
            for k in range(8):
                mm(pt2[:, k, :], xh2[:, k * 128:(k + 1) * 128], ident[:], True, True, [xh2, ident], [pt2], tr=True)
            mT = modcT if is_ctx else modT
            h2_ = h2o[sl]
            for k in range(8):
                act(h2_[:, k, :], pt2[:, k, :], AF.Identity, [pt2, mT], [h2_],
                    bias=mT[:, 24 + k:25 + k], scale=mT[:, 32 + k:33 + k])
                if k % 4 == 3:
                    yield
            if is_ctx:
                dma("sp", H2T[:, :, T * 128:(T + 1) * 128], h2_[:], [h2_], [h2t_res[T]], h2_)
            pr = pbig[:, 1, 0:NE]
            for k in range(8):
                mm(pr, h2_[:, k, :], wr[:, k, :], k == 0, k == 7, [h2_, wr], [pbr[1]])
            reduce(rmx[:], pr, ALU.max, [pbr[1]], [rmx], negate=True)
            act(rexp[:], pr, AF.Exp, [pbr[1], rmx], [rexp], bias=rmx[:, 0:1])
            yield
            reduce(rsum[:], rexp[:], ALU.add, [rexp], [rsum])
            recip(rsum[:], rsum[:], [rsum], [rsum])
            ts(aff[:, T, :], rexp[:], rsum[:, 0:1], None, ALU.mult, None, [rexp, rsum], [aff])
            if not is_ctx:
                ts(xh2[:, 1026:1042], rexp[:], rsum[:, 0:1], None, ALU.mult, None, [rexp, rsum], [xh2])
                stt(xh2[:, 1042:1058], rexp[:], rsum[:, 0:1], xh2[:, 1026:1042], ALU.mult, ALU.subtract,
                    [rexp, rsum, xh2], [xh2])
                iota_tail(xh2[:, 1024:1026].bitcast(I32), T * 128, [xh2])
                dma("sp", XH2[T * 128:(T + 1) * 128, :], xh2[:], [xh2], [xh2_res[T]], xh2)
            yield

        mixcA = Res("mixcA"); mixcN = Res("mixcN")
        den2 = ph("den2", [128, 6])
        prev = None
        for T in range(T0, NT):
            for _ in interleave([gen_att(T), prev]):
                pass
            prev = gen_epi(T)
        for _ in prev:
            pass

        new_phase()
        lo_t = ph("lo", [128, NE]); mid_t = ph("mid", [128, NE]); cntp = ph("cntp", [128, NE]); sel = ph("sel", [128, NE])
        cmp = ph("cmp", [128, NE, 64])
        incl = ph("incl", [128, NE, 64])
        zt = ph("zt", [128, 64])
        offs = ph("offs", [128, NE])
        zbig = ph("zbig", [128, 4096])
        memset("pool", zbig[:], 0.0, [zbig])
        memset("pool", zt[:], 0.0, [zt])
        for j in range(16):
            dma("sp", FFN[256 + j * 512:256 + (j + 1) * 512, :].rearrange("(p a) d -> p (a d)", p=128), zbig[:],
                [zbig], [S.dres("ffn")], zbig, group=("ffnz", l))
        sets = [(2, 64, 1024.0)] if last else [(0, 2, 32.0), (2, 64, 1024.0)]
        for (ta, tn, cap) in sets:
            av = aff[:, ta:ta + tn, :].rearrange("p t e -> p e t")
            memset("dve", lo_t[:], 0.0, [lo_t])
            for it in range(30):
                w_ = 0.5 ** (it + 1)
                ts(mid_t[:], lo_t[:], w_, None, ALU.add, None, [lo_t], [mid_t])
                tt(cmp[:, :, :tn], av, mid_t[:].unsqueeze(2).to_broadcast([128, NE, tn]), ALU.is_ge, [aff, mid_t], [cmp])
                reduce(cntp[:], cmp[:, :, :tn], ALU.add, [cmp], [cntp])
                mm(pbig[:, 0, 0:NE], onesf[:], cntp[:], True, True, [onesf, cntp], [pbr[0]])
                single(sel[:], pbig[:, 0, 0:NE], cap - 0.5, ALU.is_ge, [pbr[0]], [sel])
                stt(lo_t[:], sel[:], w_, lo_t[:], ALU.mult, ALU.add, [sel, lo_t], [lo_t])
            tt(cmp[:, :, :tn], av, lo_t[:].unsqueeze(2).to_broadcast([128, NE, tn]), ALU.is_ge, [aff, lo_t], [cmp])
            if dbg:
                LDBG = nc.dram_tensor("ldbg%d_%d" % (l, tn), [128, NE], F32, kind="ExternalOutput").ap()
                dma("sp", LDBG, lo_t[:], [lo_t], [S.dres("ldbg", tn)], lo_t)
                ADBG = nc.dram_tensor("adbg%d_%d" % (l, tn), [128, NT * NE], F32, kind="ExternalOutput").ap()
                dma("sp", ADBG, aff[:].rearrange("p a b -> p (a b)"), [aff], [S.dres("adbg", tn)], aff)
            if tn == 2:
                wv = wsel[:, ta:ta + tn, :].rearrange("p t e -> p e t")
                tt(wv, av, cmp[:, :, :tn], ALU.mult, [aff, cmp], [wsel])
                continue
            for ex in range(NE):
                scan(incl[:, ex, :], cmp[:, ex, :], zt[:], [cmp, zt], [incl])
            cp(cntp[:], incl[:, :, 63], [incl], [cntp])
            mm(pbig[:, 0, 0:NE], ltri[:], cntp[:], True, True, [ltri, cntp], [pbr[0]])
            cp(offs[:], pbig[:, 0, 0:NE], [pbr[0]], [offs])
            tt(incl[:], incl[:], cmp[:], ALU.subtract, [incl, cmp], [incl])
            tt(incl[:], incl[:], offs[:].unsqueeze(2).to_broadcast([128, NE, 64]), ALU.add, [incl, offs], [incl])
            stt(incl[:].rearrange("p a b -> p (a b)"), incl[:].rearrange("p a b -> p (a b)"), -1.0e6,
                cmp[:].rearrange("p a b -> p (a b)"), ALU.add, ALU.mult, [incl, cmp], [incl])
            ts(idxT[:], incl[:], 1.0e6, None, ALU.add, None, [incl], [idxT])
            if dbg:
                IDBG = nc.dram_tensor("idbg%d" % l, [128, NE * 64], I32, kind="ExternalOutput").ap()
                dma("sp", IDBG, idxT[:].rearrange("p a b -> p (a b)"), [idxT], [S.dres("idbg")], idxT)
                ODBG = nc.dram_tensor("odbg%d" % l, [128, NE], F32, kind="ExternalOutput").ap()
                dma("sp", ODBG, offs[:], [offs], [S.dres("odbg")], offs)
                CDBG = nc.dram_tensor("cdbg%d" % l, [128, NE * 64], F32, kind="ExternalOutput").ap()
                dma("sp", CDBG, cmp[:].rearrange("p a b -> p (a b)"), [cmp], [S.dres("cdbg")], cmp)

        new_phase()
        wgt = [ph("wg%d" % i, [128, 8, 512], BF16) for i in range(2)]
        wut = [ph("wu%d" % i, [128, 8, 512], BF16) for i in range(2)]
        wdt = [ph("wd%d" % i, [128, 4, D], BF16) for i in range(2)]
        tokc = [ph("tokc%d" % i, [128, 8, RW], BF16) for i in range(2)]
        xet = [ph("xet%d" % i, [128, RW], BF16) for i in range(2)]
        h2e = [ph("h2e%d" % i, [128, 8, 512], BF16) for i in range(2)]
        gT = [ph("gT%d" % i, [128, 4, 512], BF16) for i in range(2)]
        sa = [ph("sa%d" % i, [128, 512], BF16) for i in range(2)]
        yo = [ph("yo%d" % i, [128, D]) for i in range(2)]
        idxe = [ph("idxe%d" % i, [128, 1], I32) for i in range(8)]
        gate = ph("gate", [128, 8])
        rmx = ph("rmx", [128, 1]); rsum = ph("rsum", [128, 1]); rexp = ph("rexp", [128, NE])
        wr = ph("wr", [128, 8, NE], BF16)
        dma("pool", wr[:], WR[l].rearrange("(k p) n -> p k n", p=128), [S.dres("wr")], [wr], wr)
        h2g = ph("h2g", [128, 8, 256], BF16)
        accr = [Res("acc%d" % i) for i in range(2)]
        if not last:
            dma("sp", h2g[:], H2T[:, :, 0:256], h2t_res[0:2], [h2g], h2g)
        xe_res = [S.dres("xe", e) for e in range(NE)]
        tcnt = [0]

        def load_w(ex):
            sl = ex % 2
            dma("pool", wgt[sl][:], WG[l, ex].rearrange("(k p) f -> p k f", p=128), [S.dres("wg")], [wgt[sl]], wgt[sl])
            dma("pool", wut[sl][:], WU[l, ex].rearrange("(k p) f -> p k f", p=128), [S.dres("wu")], [wut[sl]], wut[sl])
            dma("pool", wdt[sl][:], WD[l, ex].rearrange("(k p) f -> p k f", p=128), [S.dres("wd")], [wdt[sl]], wdt[sl])

        disp_own = [[Res("disp%d_%d" % (e_, j_)) for j_ in range(2)] for e_ in range(NE)]

        def dispatch(exs):
            for cg in range(8):
                tk = tokc[tcnt[0] % 2]
                tcnt[0] += 1
                dma("sp", tk[:], XH2[(2 + cg * 8) * 128:(2 + cg * 8 + 8) * 128, :].rearrange("(s p) d -> p s d", p=128),
                    xh2_res[2 + cg * 8:2 + cg * 8 + 8], [tk], tk)
                for s in range(8):
                    T = 2 + cg * 8 + s
                    for ex in exs:
                        S.dma("pool", lambda e, tk=tk, s=s, ex=ex, T=T: e.indirect_dma_start(
                            out=XE[ex][:, :], out_offset=bass.IndirectOffsetOnAxis(
                                ap=idxT[:].rearrange("p a b -> p (a b)")[:, ex * 64 + T - 2:ex * 64 + T - 1], axis=0),
                            in_=tk[:, s, :], in_offset=None, bounds_check=breg(e), oob_is_err=False),
                            reads=rs([tk, idxT]), writes=[xe_res[ex]], owner=disp_own[ex][cg % 2], group=("xe", l, ex),
                            cost=1.2, lat=4.0)

        egroups = [[0, 1], [2, 3, 4, 5], [6, 7, 8, 9], [10, 11, 12, 13], [14, 15]]
        gstart = {g[0]: gi for gi, g in enumerate(egroups)}
        gater = [Res("gate%d" % i) for i in range(8)]
        dispatch(egroups[0])
        load_w(0)
        load_w(1)

        def ctx_dense(ex, wg_, wu_, wd_):
                def ffn_chunk(h_src, N, g_):
                    for fc in range(4):
                        ba = 2 * (fc % 2); bu = ba + 1
                        for k in range(8):
                            mm(pbig[:, ba, :N], wg_[:, k, fc * 128:(fc + 1) * 128], h_src[0][:, k, :N], k == 0, k == 7,
                               [wg_, h_src[1]], [pbr[ba]])
                        for k in range(8):
                            mm(pbig[:, bu, :N], wu_[:, k, fc * 128:(fc + 1) * 128], h_src[0][:, k, :N], k == 0, k == 7,
                               [wu_, h_src[1]], [pbr[bu]])
                        s_ = sa[fc % 2]
                        act(s_[:, :N], pbig[:, ba, :N], AF.Silu, [pbr[ba]], [s_])
                        tt(g_[:, fc, :N], pbig[:, bu, :N], s_[:, :N], ALU.mult, [pbr[bu], s_], [g_])

                def down(g_, s):
                    for hf in range(2):
                        for fc in range(4):
                            mm(pbig[:, 4 + hf, :], g_[:, fc, s * 128:(s + 1) * 128], wd_[:, fc, hf * 512:(hf + 1) * 512],
                               fc == 0, fc == 3, [g_, wd_], [pbr[4 + hf]])
                    return pbig[:, 4:6, :]

                if not last:
                    g_ = gT[0]
                    ffn_chunk((h2g, h2g), 256, g_)
                    for s in range(2):
                        py = down(g_, s)
                        av_ = acc[:, s, :].rearrange("p (a b) -> p a b", a=2)
                        if ex == 0:
                            ts(av_, py, wsel[:, s, ex:ex + 1], None, ALU.mult, None, [pbr[4], pbr[5], wsel], [accr[s]])
                        else:
                            stt(av_, py, wsel[:, s, ex:ex + 1], av_, ALU.mult, ALU.add,
                                [pbr[4], pbr[5], wsel, accr[s]], [accr[s]])

        def gen_prep(ex, ci):
            h_ = h2e[ci]
            for s in range(4):
                st_ = ci * 4 + s
                x_ = xet[st_ % 2]
                dma("sp", x_[:], XE[ex][st_ * 128:(st_ + 1) * 128, :], [xe_res[ex]], [x_], x_)
                cp(idxe[st_][:], x_[:, 1024:1026].bitcast(I32), [x_], [idxe[st_]])
                tt(gate[:, st_:st_ + 1], x_[:, 1026 + ex:1027 + ex], x_[:, 1042 + ex:1043 + ex], ALU.add, [x_], [gater[st_]])
                pt_ = ptr[st_ % 2]
                for k in range(8):
                    mm(pt_[:, k, :], x_[:, k * 128:(k + 1) * 128], ident[:], True, True, [x_, ident], [pt_], tr=True)
                yield
                for k in range(8):
                    act(h_[:, k, s * 128:(s + 1) * 128], pt_[:, k, :], AF.Identity, [pt_, modT], [h_],
                        bias=modT[:, 24 + k:25 + k], scale=modT[:, 32 + k:33 + k])
                    if k % 4 == 3:
                        yield

        def gen_ffn(ex, ci, wg_, wu_, wd_):
            h_ = h2e[ci]
            g_ = gT[ci]
            for fc in range(4):
                ba = 2 * (fc % 2); bu = ba + 1
                for k in range(8):
                    mm(pbig[:, ba, :], wg_[:, k, fc * 128:(fc + 1) * 128], h_[:, k, :], k == 0, k == 7, [wg_, h_], [pbr[ba]])
                for k in range(8):
                    mm(pbig[:, bu, :], wu_[:, k, fc * 128:(fc + 1) * 128], h_[:, k, :], k == 0, k == 7, [wu_, h_], [pbr[bu]])
                s_ = sa[fc % 2]
                act(s_[:], pbig[:, ba, :], AF.Silu, [pbr[ba]], [s_])
                tt(g_[:, fc, :], pbig[:, bu, :], s_[:], ALU.mult, [pbr[bu], s_], [g_])
                yield
            for s in range(4):
                st_ = ci * 4 + s
                for hf in range(2):
                    for fc in range(4):
                        mm(pbig[:, 4 + hf, :], g_[:, fc, s * 128:(s + 1) * 128], wd_[:, fc, hf * 512:(hf + 1) * 512],
                           fc == 0, fc == 3, [g_, wd_], [pbr[4 + hf]])
                y_ = yo[st_ % 2]
                act(y_[:].rearrange("p (a b) -> p a b", a=2), pbig[:, 4:6, :], AF.Identity, [pbr[4], pbr[5], gater[st_]], [y_],
                    scale=gate[:, st_:st_ + 1])
                S.dma("pool", lambda e, y_=y_, ie_=idxe[st_]: e.indirect_dma_start(
                    out=FFN[:, :], out_offset=bass.IndirectOffsetOnAxis(ap=ie_[:, :], axis=0),
                    in_=y_[:, :], in_offset=None, compute_op=ALU.add),
                    reads=rs([y_, idxe[st_]]), writes=[S.dres("ffn")], owner=y_.r, group=("ffn", l, ex), cost=1.2, lat=8.0)
                yield

        prev = None
        for ex in range(NE):
            sl = ex % 2
            if ex in gstart and gstart[ex] + 1 < len(egroups):
                dispatch(egroups[gstart[ex] + 1])
            ctx_dense(ex, wgt[sl], wut[sl], wdt[sl])
            for ci in range(2):
                for _ in interleave([gen_prep(ex, ci), prev]):
                    pass
                if ci == 0 and ex >= 1 and ex + 1 < NE:
                    load_w(ex + 1)
                prev = gen_ffn(ex, ci, wgt[sl], wut[sl], wdt[sl])
        for _ in prev:
            pass

        new_phase()
        g2b = ph("g2b", [128, D]); cg2b = ph("cg2b", [128, D])
        l2g = ph("l2g", [128, D]); l2b = ph("l2b", [128, D])
        dma("sp", g2b[:], MODROW[0, 5120:6144].partition_broadcast(128), [S.dres("modrow")], [g2b], g2b)
        dma("sp", cg2b[:], MODROW[1, 5120:6144].partition_broadcast(128), [S.dres("modrow")], [cg2b], cg2b)
        dma("sp", l2g[:], LN2G[l].partition_broadcast(128), [S.dres("ln2g")], [l2g], l2g)
        dma("sp", l2b[:], LN2B[l].partition_broadcast(128), [S.dres("ln2b")], [l2b], l2b)
        xmt = [ph("xmt%d" % i, [128, D]) for i in range(4)]
        fft = [ph("fft%d" % i, [128, D]) for i in range(4)]
        ot = [ph("ot%d" % i, [128, D]) for i in range(4)]
        y2 = [ph("y2%d" % i, [128, D]) for i in range(4)]
        st2 = [(ph("stats", [128, 2, 6]), ph("mv", [128, 2]), ph("rstd", [128, 1]), ph("nmr", [128, 1])) for _ in range(4)]

        def gen_ln2(T):
            xm_ = xmt[T % 4]; o_ = ot[T % 4]; y2_ = y2[T % 4]
            stats, mv, rstd, nmr = st2[T % 4]
            dma("sp", xm_[:], XMID[T * 128:(T + 1) * 128, :], [xmid_res[T]], [xm_], xm_)
            if T < 2:
                tt(y2_[:], acc[:, T, :], cg2b[:], ALU.mult, [accr[T], cg2b], [y2_])
            else:
                f_ = fft[T % 4]
                dma("sp", f_[:], FFN[T * 128:(T + 1) * 128, :], [S.dres("ffn")], [f_], f_)
                tt(y2_[:], f_[:], g2b[:], ALU.mult, [f_, g2b], [y2_])
            yield
            stt(y2_[:], xm_[:], ALPHA, y2_[:], ALU.mult, ALU.add, [xm_, y2_], [y2_])
            yield
            ln_stats(y2_, y2_, stats, mv, rstd, nmr)
            yield
            act(y2_[:], y2_[:], AF.Identity, [y2_, rstd, nmr], [y2_], bias=nmr[:, 0:1], scale=rstd[:, 0:1])
            yield
            tt(y2_[:], y2_[:], l2g[:], ALU.mult, [y2_, l2g], [y2_], eng="pool")
            yield
            tt(o_[:], y2_[:], l2b[:], ALU.add, [y2_, l2b], [o_], eng="pool")
            if last:
                ev = dma("sp", Y[(T - 2) * 128:(T - 1) * 128, :], o_[:], [o_], [y_res[T - 2]], o_)
                final.append(ev)
            else:
                dma("sp", XCUR[T * 128:(T + 1) * 128, :], o_[:], [o_], [xcur_res[T]], o_)
            yield

        tl_ = list(range(T0, NT))
        for j in range(0, len(tl_), 4):
            for _ in interleave([gen_ln2(T) for T in tl_[j:j + 4]]):
                pass

    S.emit(final_waits=final, reorder=reorder, only=only)
    if dbg:
        print("instr counts", {e: len(v) for e, v in S.ins.items()}, "waits", S.nwaits, flush=True)
    return nc


def _rope_tables():
    t = np.arange(8192)
    row = (t // 64).astype(np.float32); col = (t % 64).astype(np.float32)
    inv = (10000.0 ** (-np.arange(0, 32, 2, dtype=np.float32) / 32)).astype(np.float32)
    cs = np.ones((64, TOK), np.float32); sn = np.zeros((64, TOK), np.float32)
    for a, pos in enumerate((row, col)):
        ang = (pos[:, None] * inv[None, :]).astype(np.float32)
        c = np.cos(ang).T; s = np.sin(ang).T
        cs[a * 32:a * 32 + 16, 256:] = c; cs[a * 32 + 16:a * 32 + 32, 256:] = c
        sn[a * 32:a * 32 + 16, 256:] = -s; sn[a * 32 + 16:a * 32 + 32, 256:] = s
    cs = np.concatenate([cs, cs], 0); sn = np.concatenate([sn, sn], 0)
    return np.stack([cs * 0.125, sn * 0.125, cs, sn]).astype(np.float32)


def _win_ext(w_in):
    qa = w_in[:, :, 0:384]; ka = w_in[:, :, 384:512]; va = w_in[:, :, 512:640]
    bx = w_in[:, :, 640:896]; bb = w_in[:, :, 896:1152]; bc = w_in[:, :, 1152:1408]
    qn = w_in[:, :, 1408:1792]; kn = w_in[:, :, 1792:2176]; vn = w_in[:, :, 2176:2560]
    sw = np.concatenate([np.arange(16, 32), np.arange(0, 16), np.arange(48, 64), np.arange(32, 48)])

    def heads_sw(w, nh):
        idx = np.concatenate([h * 64 + sw for h in range(nh)])
        return w[:, :, idx]

    def qperm(w):
        idx = np.concatenate([np.concatenate([np.arange(c * 64, c * 64 + 64), np.arange((3 + c) * 64, (3 + c) * 64 + 64)])
                              for c in range(3)])
        return w[:, :, idx]

    return np.ascontiguousarray(np.concatenate(
        [qperm(qa), qperm(heads_sw(qa, 6)), ka, heads_sw(ka, 2), bx, bb, bc, qn, kn, va, vn], axis=2))


def _na_bias(rpb):
    NEG = -30000.0
    out = np.full((DEPTH, 5, 6, 5, 128, 128), NEG, np.float32)
    cq = np.arange(64)
    col_start = np.clip(cq - 8, 0, 48)
    col_ok = (cq[None, :] >= col_start[:, None]) & (cq[None, :] < col_start[:, None] + 16)
    coff = np.clip(cq[None, :] - cq[:, None], -15, 15) + 15
    variants = [10, 0, 1, 62, 63]
    for vi, P in enumerate(variants):
        base = min(max(P - 2, 0), 59)
        for rho in range(2):
            r = 2 * P + rho
            rs_ = min(max(r - 4, 0), 120)
            for j in range(5):
                for kap in range(2):
                    kr = 2 * (base + j) + kap
                    if not (rs_ <= kr < rs_ + 8):
                        continue
                    roff = kr - r + 7
                    b = rpb[:, :, roff, :][:, :, coff]
                    b = np.where(col_ok[None, None], b, NEG)
                    out[:, vi, :, j, kap * 64:(kap + 1) * 64, rho * 64:(rho + 1) * 64] = np.transpose(b, (0, 1, 3, 2))
    out = np.transpose(out, (0, 1, 4, 2, 3, 5)).reshape(DEPTH, 5, 128, 6, 640)
    return np.ascontiguousarray(out)


def _amask():
    k = np.arange(128)[:, None]; q = np.arange(128)[None, :]
    mp = np.where(k >= q, 0.0, -30000.0).astype(np.float32); mn = np.where(k <= q, 0.0, -30000.0).astype(np.float32)
    return np.ascontiguousarray(np.stack([np.tile(mp, (1, 3)), np.tile(mn, (1, 3))], axis=1))


def make_in_maps(x, c, ctx, c_ctx, w_mod, b_mod, w_in, conv_w, attn_sink, na_rpb, w_out,
                 ln1_g, ln1_b, w_router, w_gate, w_up, w_down, ln2_g, ln2_b):
    f = lambda a: np.ascontiguousarray(np.asarray(a, dtype=np.float32))
    shared = dict(
        w_mod=f(w_mod), b_mod=f(b_mod), w_in=_win_ext(f(w_in)), rope=_rope_tables(),
        convw=np.ascontiguousarray(np.transpose(f(conv_w).reshape(DEPTH, 3, 2, 128), (0, 3, 2, 1))),
        sink=f(attn_sink), nab=_na_bias(f(na_rpb)), amask=_amask(), w_out=f(w_out),
        ln1_g=f(ln1_g), ln1_b=f(ln1_b), ln2_g=f(ln2_g), ln2_b=f(ln2_b), w_router=f(w_router),
        w_gate=f(w_gate), w_up=f(w_up), w_down=f(w_down))
    x = f(x); ctx = f(ctx); c = f(c); c_ctx = f(c_ctx)
    maps = []
    for b in range(N_CORES):
        m = dict(shared)
        m["xin"] = np.ascontiguousarray(np.concatenate([ctx[b], x[b]], axis=0))
        m["cvec"] = np.ascontiguousarray(np.stack([c[b], c_ctx], axis=0))
        maps.append(m)
    return maps


_NC = {}


def kernel(**inputs):
    if "nc" not in _NC:
        _NC["nc"] = build_nc(reorder=False)
    maps = make_in_maps(**inputs)
    res = run_bass_kernel_spmd(_NC["nc"], maps, core_ids=list(range(N_CORES)))
    return np.stack([np.asarray(r["y"], dtype=np.float32) for r in res.results], axis=0)
```

```python
import numpy as np
import concourse.bass as bass
import concourse.mybir as mybir
from concourse.bass_utils import run_bass_kernel_spmd

F32 = mybir.dt.float32
BF16 = mybir.dt.bfloat16
I32 = mybir.dt.int32
AF = mybir.ActivationFunctionType
ALU = mybir.AluOpType
AX = mybir.AxisListType

D = 1024
NT = 66
TOK = NT * 128
DEPTH = 2
ALPHA = float((2 * DEPTH) ** 0.25)
NE = 16
RW = 1024 + 2 + 32
COMPUTE = ("pe", "dve", "act", "pool")
N_CORES = 4


class Res:
    __slots__ = ("name", "w", "r", "dsem", "wg")

    def __init__(self, name="r"):
        self.name = name
        self.w = None
        self.r = []
        self.dsem = {}
        self.wg = None


class Sched:
    def __init__(self, nc):
        self.nc = nc
        self.ins = {e: [] for e in ("pe", "dve", "act", "pool", "sp")}
        self.dram = {}
        self.ndsem = 0
        self.owners = []
        self.free = {"sp": [], "pool": []}
        self.phase = 0
        self.phase_ev = {0: []}
        self.last_dma = {}
        self.pool_dmas = []
        self.pool_throttle = 0
        self.last_pew = {}

    def dres(self, *key):
        r = self.dram.get(key)
        if r is None:
            r = self.dram[key] = Res(str(key))
        return r

    def _deps(self, eng, reads, writes, pe_acc, group=None):
        deps = []
        for r in reads:
            if r.w is not None:
                if isinstance(r.w, list):
                    deps.extend(r.w)
                else:
                    deps.append(r.w)
        for r in writes:
            if r.w is not None:
                if isinstance(r.w, list):
                    if not (group is not None and r.wg == group):
                        deps.extend(r.w)
                elif not (pe_acc and r.w[0] == "E" and r.w[1] == "pe" and eng == "pe"):
                    deps.append(r.w)
            deps.extend(r.r)
        return deps

    def _post(self, ev, reads, writes, group=None):
        for r in reads:
            r.r.append(ev)
        for r in writes:
            if group is not None and r.wg == group and isinstance(r.w, list):
                r.w.append(ev)
            else:
                r.w = [ev] if group is not None else ev
                r.wg = group
                r.r = []

    def op(self, eng, fn, reads=(), writes=(), pe_acc=False, cost=0.5):
        deps = self._deps(eng, reads, writes, pe_acc)
        idx = len(self.ins[eng])
        order = []
        if eng == "pe":
            for r in writes:
                p = self.last_pew.get(id(r))
                if p is not None:
                    order.append(p)
                self.last_pew[id(r)] = idx
        ev = ("E", eng, idx)
        self.ins[eng].append([fn, deps, None, self.phase, cost, order, cost])
        self._post(ev, reads, writes)
        return ev

    def dma(self, q, fn, reads=(), writes=(), owner=None, group=None, cost=0.1, lat=3.0):
        deps = self._deps(q, reads, writes, False, group)
        sc = owner.dsem.get(q)
        if sc is None:
            if self.free[q]:
                sc = list(self.free[q].pop())
            else:
                sc = [self.ndsem, 0]
                self.ndsem += 1
            owner.dsem[q] = sc
            self.owners.append((owner, q))
        sc[1] += 16
        ev = ("D", sc[0], sc[1])
        if q == "pool" and self.pool_throttle:
            if len(self.pool_dmas) >= self.pool_throttle:
                deps.append(self.pool_dmas[-self.pool_throttle])
            self.pool_dmas.append(ev)
        idx = len(self.ins[q])
        order = []
        p = self.last_dma.get(sc[0])
        if p is not None:
            order.append(p)
        self.last_dma[sc[0]] = idx
        self.ins[q].append([fn, deps, sc[0], self.phase, cost, order, cost + lat, ev])
        self._post(ev, reads, writes, group)
        return ev

    def barrier(self):
        evs = []
        for e in COMPUTE:
            for i in range(len(self.ins[e]) - 1, -1, -1):
                if self.ins[e][i][2] is None:
                    evs.append(("E", e, i))
                    break
        for (o, q) in self.owners:
            sc = o.dsem.pop(q)
            evs.append(("D", sc[0], sc[1]))
            self.free[q].append((sc[0], sc[1]))
        self.owners = []
        self.phase += 1
        self.phase_ev[self.phase] = evs

    def _schedule(self, only=None):
        import heapq
        engs = list(self.ins.keys())
        dprod = {}
        for q in ("sp", "pool"):
            for i, rec in enumerate(self.ins[q]):
                if rec[2] is not None:
                    dprod[(rec[7][1], rec[7][2])] = (q, i)
        fin = {}
        issue = {}
        order = {e: [] for e in engs}
        ptr0 = {e: 0 for e in engs}
        tnow = 0.0
        for ph in range(self.phase + 1):
            nodes = []
            for e in engs:
                lst = self.ins[e]
                i = ptr0[e]
                while i < len(lst) and lst[i][3] == ph:
                    nodes.append((e, i))
                    i += 1
                ptr0[e] = i
            if not nodes:
                continue
            if only is not None and ph not in only:
                for (e, i) in nodes:
                    order[e].append(i)
                continue
            inph = set(nodes)
            ndep = {}
            users = {}
            for (e, i) in nodes:
                rec = self.ins[e][i]
                preds = set()
                for d in rec[1]:
                    p = (d[1], d[2]) if d[0] == "E" else dprod.get((d[1], d[2]))
                    if p is not None and p in inph and p != (e, i):
                        preds.add((p, 0))
                for j in rec[5]:
                    if (e, j) in inph:
                        preds.add(((e, j), 1))
                ndep[(e, i)] = len(preds)
                for pk in preds:
                    users.setdefault(pk[0], []).append(((e, i), pk[1]))
            ready = {}
            heaps = {e: [] for e in engs}
            avail = {e: [] for e in engs}
            efree = {e: tnow for e in engs}
            for n in nodes:
                ready[n] = tnow
                if ndep[n] == 0:
                    heapq.heappush(heaps[n[0]], (tnow, n[1]))
            left = len(nodes)
            tmax = tnow
            while left:
                best = None
                for e in engs:
                    if avail[e]:
                        st_ = efree[e]
                    elif heaps[e]:
                        st_ = max(heaps[e][0][0], efree[e])
                    else:
                        continue
                    if best is None or st_ < best[0]:
                        best = (st_, e)
                st_, e = best
                while heaps[e] and heaps[e][0][0] <= st_:
                    heapq.heappush(avail[e], heapq.heappop(heaps[e])[1])
                i = heapq.heappop(avail[e])
                rec = self.ins[e][i]
                issue[(e, i)] = st_
                efree[e] = st_ + rec[4]
                f_ = st_ + rec[6]
                fin[(e, i)] = f_
                tmax = max(tmax, f_)
                order[e].append(i)
                left -= 1
                for (u, kind) in users.get((e, i), ()):
                    t_ = f_ if kind == 0 else st_
                    if t_ > ready[u]:
                        ready[u] = t_
                    ndep[u] -= 1
                    if ndep[u] == 0:
                        heapq.heappush(heaps[u[0]], (ready[u], u[1]))
            tnow = tmax
        self.sim_time = tnow
        return order

    def _check(self, order, val):
        sems = {}
        pos = {e: 0 for e in self.ins}
        curph = {e: 0 for e in self.ins}
        progress = True
        total = sum(len(v) for v in self.ins.values())
        done = 0
        while progress:
            progress = False
            for e in self.ins:
                while pos[e] < len(order[e]):
                    i = order[e][pos[e]]
                    rec = self.ins[e][i]
                    deps = list(rec[1])
                    if rec[3] != curph[e]:
                        for p in range(curph[e] + 1, rec[3] + 1):
                            deps.extend(self.phase_ev.get(p, ()))
                    ok = True
                    for d in deps:
                        if d[0] == "E":
                            if sems.get(("E", d[1]), 0) < val[d[1]][d[2]]:
                                ok = False
                                break
                        elif sems.get(("D", d[1]), 0) < d[2]:
                            ok = False
                            break
                    if not ok:
                        break
                    curph[e] = rec[3]
                    if rec[2] is not None:
                        sems[("D", rec[2])] = sems.get(("D", rec[2]), 0) + 16
                    elif i in val.get(e, {}):
                        sems[("E", e)] = val[e][i]
                    pos[e] += 1
                    done += 1
                    progress = True
        if done != total:
            msg = []
            for e in self.ins:
                if pos[e] < len(order[e]):
                    i = order[e][pos[e]]
                    msg.append((e, pos[e], i, self.ins[e][i][3], self.ins[e][i][1][:6]))
            raise RuntimeError("schedule deadlock: %s" % msg)

    def emit(self, final_waits=(), reorder=True, only=None):
        import contextlib
        nc = self.nc
        if reorder:
            order = self._schedule(only)
        else:
            order = {e: list(range(len(l))) for e, l in self.ins.items()}
        for e in self.ins:
            assert sorted(order[e]) == list(range(len(self.ins[e]))), e
        lastc = {}
        for e in COMPUTE:
            cur = None
            per = {}
            for i in order[e]:
                if self.ins[e][i][2] is None:
                    per[self.ins[e][i][3]] = i
            lastc[e] = per
        for p in list(self.phase_ev.keys()):
            evs = [d for d in self.phase_ev[p] if d[0] == "D"]
            for e in COMPUTE:
                qs = [q for q in lastc[e] if q < p]
                if qs:
                    evs.append(("E", e, lastc[e][max(qs)]))
            self.phase_ev[p] = evs
        need = {e: set() for e in COMPUTE}
        for e, lst in self.ins.items():
            for rec in lst:
                for d in rec[1]:
                    if d[0] == "E":
                        need[d[1]].add(d[2])
        for evs in self.phase_ev.values():
            for d in evs:
                if d[0] == "E":
                    need[d[1]].add(d[2])
        val = {}
        for e in COMPUTE:
            c = 0
            v = {}
            for i in order[e]:
                if i in need[e]:
                    c += 1
                    v[i] = c
            val[e] = v
        self._check(order, val)
        self.nwaits = {}
        with contextlib.ExitStack() as st:
            esem = {e: st.enter_context(nc.semaphore("s_" + e)) for e in COMPUTE}
            dsem = [st.enter_context(nc.semaphore("d%d" % i)) for i in range(self.ndsem)]
            block = st.enter_context(nc.Block())

            def run(ename, eng):
                waited = {}
                lst = self.ins[ename]
                cur_ph = 0
                for i in order[ename]:
                    rec = lst[i]
                    deps = rec[1]
                    if rec[3] != cur_ph:
                        deps = list(deps)
                        for p in range(cur_ph + 1, rec[3] + 1):
                            deps.extend(self.phase_ev.get(p, ()))
                        cur_ph = rec[3]
                    tg = {}
                    for d in deps:
                        if d[0] == "E":
                            key = ("E", d[1]); v = val[d[1]][d[2]]; sem = esem[d[1]]
                        else:
                            key = ("D", d[1]); v = d[2]; sem = dsem[d[1]]
                        if tg.get(key, (None, 0))[1] < v:
                            tg[key] = (sem, v)
                    for key, (sem, v) in tg.items():
                        if waited.get(key, 0) >= v:
                            continue
                        eng.wait_ge(sem, v)
                        waited[key] = v
                        self.nwaits[ename] = self.nwaits.get(ename, 0) + 1
                    ins = rec[0](eng)
                    if rec[2] is not None:
                        ins.then_inc(dsem[rec[2]], 16)
                    elif i in need[ename]:
                        ins.then_inc(esem[ename], 1)
                if ename == "sp":
                    for d in final_waits:
                        eng.wait_ge(dsem[d[1]], d[2])

            block.tensor(lambda e: run("pe", e))
            block.vector(lambda e: run("dve", e))
            block.scalar(lambda e: run("act", e))
            block.gpsimd(lambda e: run("pool", e))
            block.sync(lambda e: run("sp", e))


def interleave(gens):
    gens = [g for g in gens if g is not None]
    while gens:
        nxt = []
        for g in gens:
            try:
                next(g)
                nxt.append(g)
            except StopIteration:
                pass
            yield
        gens = nxt


class Tl:
    __slots__ = ("t", "r")

    def __init__(self, t, name):
        self.t = t
        self.r = Res(name)

    def __getitem__(self, k):
        return self.t[k]


def build_nc(dbg=False, depth_run=DEPTH, reorder=True, only=None):
    nc = bass.Bass("TRN2", target_bir_lowering=False)
    S = Sched(nc)

    def din(name, shape, dt=F32):
        return nc.dram_tensor(name, list(shape), dt, kind="ExternalInput").ap()

    def dscr(name, shape, dt):
        return nc.dram_tensor(name, list(shape), dt, kind="ExternalOutput" if dbg else "Internal").ap()

    XIN = din("xin", [TOK, D])
    CVEC = din("cvec", [2, D])
    WMOD = din("w_mod", [DEPTH, D, 6 * D])
    BMOD = din("b_mod", [DEPTH, 6 * D])
    WIN = din("w_in", [DEPTH, D, 3072])
    ROPE = din("rope", [4, 128, TOK])
    CONVW = din("convw", [DEPTH, 128, 2, 3])
    SINK = din("sink", [DEPTH, 6])
    NAB = din("nab", [DEPTH, 5, 128, 6, 640])
    AMASK = din("amask", [128, 2, 384])
    WOUT = din("w_out", [DEPTH, D, D])
    LN1G = din("ln1_g", [DEPTH, D]); LN1B = din("ln1_b", [DEPTH, D])
    LN2G = din("ln2_g", [DEPTH, D]); LN2B = din("ln2_b", [DEPTH, D])
    WR = din("w_router", [DEPTH, D, NE])
    WG = din("w_gate", [DEPTH, NE, D, 512]); WU = din("w_up", [DEPTH, NE, D, 512])
    WD = din("w_down", [DEPTH, NE, 512, D])
    Y = nc.dram_tensor("y", [8192, D], F32, kind="ExternalOutput").ap()

    MODROW = dscr("modrow", [2, 6 * D], F32)
    FM = dscr("fm", [128, 14, TOK], BF16)
    VV = dscr("vv", [TOK, 8, 65], BF16)
    XMID = dscr("xmid", [TOK, D], F32)
    H2T = dscr("h2t", [128, 8, TOK], BF16)
    XCUR = dscr("xcur", [TOK, D], F32)
    XH2 = dscr("xh2", [TOK, RW], BF16)
    XE = [dscr("xe%d" % e, [1024, RW], BF16) for e in range(NE)]
    FFN = dscr("ffn", [TOK, D], F32)

    SB_LO = 16512
    SB_HI = 229344
    st = {"pers": SB_LO, "ph": None}

    def _alloc(name, shape, dt, key):
        nb = int(np.prod(shape[1:])) * (2 if dt == BF16 else 4)
        nb = (nb + 31) // 32 * 32
        off = st[key]
        assert off + nb <= SB_HI, (name, off, nb)
        st[key] = off + nb
        return Tl(nc.alloc_sbuf_tensor_at(name, list(shape), dt, offset=off), name)

    def pers(name, shape, dt=F32):
        return _alloc(name, shape, dt, "pers")

    cnt = [0]

    def ph(name, shape, dt=F32):
        cnt[0] += 1
        return _alloc("%s_%d" % (name, cnt[0]), shape, dt, "ph")

    def new_phase():
        S.barrier()
        st["ph"] = st["pers_end"]

    pbig = Tl(nc.alloc_psum_tensor("pbig", [128, 6, 512], F32), "pbig")
    ptr = [Tl(nc.alloc_psum_tensor("ptr%d" % i, [128, 8, 128], BF16), "ptr%d" % i) for i in range(2)]
    pbr = [Res("pb%d" % i) for i in range(6)]

    ident = pers("ident", [128, 128], BF16)
    identf = pers("identf", [128, 128], F32)
    onesf = pers("onesf", [128, 128], F32)
    aff = pers("aff", [128, NT, NE], F32)
    wsel = pers("wsel", [128, NT, NE], F32)
    modT = pers("modT", [128, 48], F32)
    modcT = pers("modcT", [128, 48], F32)
    esink = pers("esink", [128, 6], F32)
    convw = pers("convw", [128, 2, 3], F32)
    amask = pers("amask", [128, 2, 384], BF16)
    eps_t = pers("eps", [128, 1], F32)
    ltri = pers("ltri", [128, 128], F32)
    idxT = pers("idxT", [128, NE, 64], I32)
    acc = pers("acc", [128, 2, D], F32)
    st["pers_end"] = st["pers"]
    st["ph"] = st["pers_end"]

    _breg = {}

    def breg(e):
        if "r" not in _breg:
            _breg["r"] = e.to_reg(1023)
        return _breg["r"]

    def rs(lst):
        return [x.r if isinstance(x, Tl) else x for x in lst]

    def fsz(ap):
        try:
            return float(ap.free_size())
        except Exception:
            return 512.0

    def mm(out, lhsT, rhs, start, stop, reads, writes, tr=False):
        if tr:
            S.op("pe", lambda e: e.matmul(out, lhsT=lhsT, rhs=rhs, is_transpose=True),
                 reads=rs(reads), writes=rs(writes), pe_acc=True, cost=0.08)
        else:
            c = max(fsz(rhs), 64.0) / 2000.0 * (4.0 if lhsT.dtype == F32 else 1.0) + 0.03
            S.op("pe", lambda e: e.matmul(out, lhsT=lhsT, rhs=rhs, start=start, stop=stop),
                 reads=rs(reads), writes=rs(writes), pe_acc=True, cost=c)

    def act(out, in_, func, reads, writes, bias=0.0, scale=1.0, accum=None):
        if accum is None:
            S.op("act", lambda e: e.activation(out=out, in_=in_, func=func, bias=bias, scale=scale),
                 reads=rs(reads), writes=rs(writes), cost=0.2 + fsz(out) / 1300.0)
        else:
            S.op("act", lambda e: e.activation(out=out, in_=in_, func=func, bias=bias, scale=scale,
                                               accum_out=accum), reads=rs(reads), writes=rs(writes))

    def vcost(eng, ap):
        return (0.1 + fsz(ap) / 900.0) if eng == "dve" else (0.2 + fsz(ap) / 450.0)

    def tt(out, in0, in1, op, reads, writes, eng="dve"):
        S.op(eng, lambda e: e.tensor_tensor(out=out, in0=in0, in1=in1, op=op), reads=rs(reads), writes=rs(writes),
             cost=vcost(eng, out))

    def ts(out, in0, s1, s2, op0, op1, reads, writes, eng="dve"):
        if op1 is None:
            S.op(eng, lambda e: e.tensor_scalar(out=out, in0=in0, scalar1=s1, scalar2=None, op0=op0),
                 reads=rs(reads), writes=rs(writes), cost=vcost(eng, out))
        else:
            S.op(eng, lambda e: e.tensor_scalar(out=out, in0=in0, scalar1=s1, scalar2=s2, op0=op0, op1=op1),
                 reads=rs(reads), writes=rs(writes), cost=vcost(eng, out))

    def stt(out, in0, scalar, in1, op0, op1, reads, writes):
        S.op("dve", lambda e: e.scalar_tensor_tensor(out=out, in0=in0, scalar=scalar, in1=in1, op0=op0, op1=op1),
             reads=rs(reads), writes=rs(writes), cost=vcost("dve", out))

    def cp(out, in_, reads, writes, eng="dve"):
        S.op(eng, lambda e: e.tensor_copy(out=out, in_=in_), reads=rs(reads), writes=rs(writes), cost=vcost(eng, out))

    def recip(out, in_, reads, writes):
        S.op("dve", lambda e: e.reciprocal(out=out, in_=in_), reads=rs(reads), writes=rs(writes))

    def reduce(out, in_, op, reads, writes, negate=False):
        S.op("dve", lambda e: e.tensor_reduce(out=out, in_=in_, axis=AX.X, op=op, negate=negate),
             reads=rs(reads), writes=rs(writes), cost=vcost("dve", in_))

    def memset(eng, ap, val, writes):
        S.op(eng, lambda e: e.memset(ap, val), writes=rs(writes), cost=vcost(eng, ap))

    def single(out, in_, scalar, op, reads, writes):
        S.op("dve", lambda e: e.tensor_single_scalar(out=out, in_=in_, scalar=scalar, op=op),
             reads=rs(reads), writes=rs(writes))

    def scan(out, d0, d1, reads, writes):
        S.op("dve", lambda e: e.tensor_tensor_scan(out=out, data0=d0, data1=d1, initial=0.0, op0=ALU.add, op1=ALU.add),
             reads=rs(reads), writes=rs(writes))

    def iota_tail(ap, base, writes):
        S.op("pool", lambda e: e.iota(ap, pattern=[[0, 1]], base=base, channel_multiplier=1), writes=rs(writes))

    def dma(q, out, in_, reads, writes, owner, slow=False, group=None):
        try:
            nbytes = float(out.nbytes())
        except Exception:
            nbytes = 1.0e5
        lat = 2.5 + nbytes / 1.5e5
        cost = 0.1 if q == "sp" else 1.0
        ow = owner.r if isinstance(owner, Tl) else owner
        if group is not None:
            return S.dma(q, lambda e: e.dma_start(out=out, in_=in_), reads=rs(reads), writes=rs(writes),
                         owner=ow, group=group, cost=cost, lat=lat)
        if slow:
            return S.dma(q, lambda e: e.dma_start(out=out, in_=in_, allow_slow_non_contiguous=True),
                         reads=rs(reads), writes=rs(writes), owner=ow, cost=cost, lat=lat + 3.0)
        return S.dma(q, lambda e: e.dma_start(out=out, in_=in_),
                     reads=rs(reads), writes=rs(writes), owner=ow, cost=cost, lat=lat)

    def ln_stats(src_ap, src_res, stats, mv, rstd, nmr=None):
        for h in range(2):
            S.op("dve", lambda e, h=h: e.bn_stats(out=stats[:, h, :], in_=src_ap[:, h * 512:(h + 1) * 512]),
                 reads=rs([src_res]), writes=rs([stats]))
        S.op("dve", lambda e: e.bn_aggr(out=mv[:], in_=stats[:].rearrange("p a b -> p (a b)")),
             reads=rs([stats]), writes=rs([mv]))
        act(rstd[:], mv[:, 1:2], AF.Ln, [mv, eps_t], [rstd], bias=eps_t[:, 0:1])
        act(rstd[:], rstd[:], AF.Exp, [rstd], [rstd], scale=-0.5)
        if nmr is not None:
            stt(nmr[:], mv[:, 0:1], -1.0, rstd[:], ALU.mult, ALU.mult, [mv, rstd], [nmr])

    S.op("pool", lambda e: e.iota(identf[:], pattern=[[1, 128]], base=0, channel_multiplier=-1,
                                  allow_small_or_imprecise_dtypes=True), writes=rs([identf]))
    S.op("dve", lambda e: e.tensor_single_scalar(out=ident[:], in_=identf[:], scalar=0.0, op=ALU.is_equal),
         reads=rs([identf]), writes=rs([ident]))
    S.op("dve", lambda e: e.memset(onesf[:], 1.0), writes=rs([onesf]))
    S.op("dve", lambda e: e.tensor_single_scalar(out=ltri[:], in_=identf[:], scalar=0.0, op=ALU.is_gt),
         reads=rs([identf]), writes=rs([ltri]))
    S.op("dve", lambda e: e.memset(eps_t[:], 1e-6), writes=rs([eps_t]))
    dma("pool", amask[:], AMASK, [S.dres("amask")], [amask], amask)

    fm_res = [S.dres("fm", t) for t in range(NT)]
    vv_res = [S.dres("vv", t) for t in range(NT)]
    xmid_res = [S.dres("xmid", t) for t in range(NT)]
    h2t_res = [S.dres("h2t", t) for t in range(NT)]
    xcur_res = [S.dres("xcur", t) for t in range(NT)]
    xh2_res = [S.dres("xh2", t) for t in range(NT)]
    y_res = [S.dres("y", t) for t in range(64)]
    final = []

    for l in range(depth_run):
        last = l == DEPTH - 1
        T0 = 2 if last else 0
        new_phase()
        cT = ph("cT", [128, 8, 2])
        bm = ph("bm", [2, 6 * D])
        mrow = ph("mrow", [2, 6 * D])
        wm = [ph("wm%d" % i, [128, 8, 512]) for i in range(2)]
        for m_ in range(2):
            dma("sp", cT[:, :, m_], CVEC[m_].rearrange("(k p) -> p k", p=128), [S.dres("cvec")], [cT], cT, slow=True)
        dma("sp", bm[:], BMOD[l].partition_broadcast(2), [S.dres("bmod")], [bm], bm)
        dma("sp", esink[:], SINK[l].partition_broadcast(128), [S.dres("sink")], [esink], esink)
        dma("sp", convw[:], CONVW[l], [S.dres("convw")], [convw], convw)
        act(cT[:], cT[:], AF.Silu, [cT], [cT])
        act(esink[:], esink[:], AF.Exp, [esink], [esink])
        wmv = WMOD[l].rearrange("(k p) n -> p k n", p=128)
        for cc in range(12):
            w_ = wm[cc % 2]
            dma("sp", w_[:], wmv[:, :, cc * 512:(cc + 1) * 512], [S.dres("wmod")], [w_], w_)
            pb = pbig[0:2, cc % 2, :]
            for k in range(8):
                mm(pb, cT[:, k, :], w_[:, k, :], k == 0, k == 7, [cT, w_], [pbr[cc % 2]])
            tt(mrow[:, cc * 512:(cc + 1) * 512], pb, bm[:, cc * 512:(cc + 1) * 512], ALU.add,
               [pbr[cc % 2], bm], [mrow])
        dma("sp", MODROW, mrow[:], [mrow], [S.dres("modrow")], mrow)
        dma("sp", modT[:], MODROW[0].rearrange("(j p) -> p j", p=128), [S.dres("modrow")], [modT], modT, slow=True)
        dma("sp", modcT[:], MODROW[1].rearrange("(j p) -> p j", p=128), [S.dres("modrow")], [modcT], modcT, slow=True)
        for m_ in (modT, modcT):
            ts(m_[:, 8:16], m_[:, 8:16], 1.0, None, ALU.add, None, [m_], [m_])
            ts(m_[:, 32:40], m_[:, 32:40], 1.0, None, ALU.add, None, [m_], [m_])

        new_phase()
        win = ph("win", [128, 8, 3072], BF16)
        wiv = WIN[l].rearrange("(k p) n -> p k n", p=128)
        for k in range(8):
            dma("pool", win[:, k, :], wiv[:, k, :], [S.dres("win")], [win], win)
        xt = [ph("xt%d" % i, [128, D]) for i in range(2)]
        xh = [ph("xh%d" % i, [128, D], BF16) for i in range(2)]
        hT = [ph("hT%d" % i, [128, 8, 512], BF16) for i in range(2)]
        fmo = [ph("fmo%d" % i, [128, 14, 512], BF16) for i in range(2)]
        vo = [ph("vo%d" % i, [128, 4, 8, 65], BF16) for i in range(2)]
        rp = [ph("rp%d" % i, [128, 4, 512]) for i in range(2)]
        tmp = [ph("tmp%d" % i, [128, 512]) for i in range(3)]
        stats = ph("stats", [128, 2, 6]); mv = ph("mv", [128, 2]); rstd = ph("rstd", [128, 1])
        for v_ in vo:
            S.op("pool", lambda e, v_=v_: e.memset(v_[:], 1.0), writes=rs([v_]))
        src = XIN if l == 0 else XCUR
        src_res = (lambda t: S.dres("xin", t)) if l == 0 else (lambda t: xcur_res[t])
        groups = [(0, 2)] + [(2 + 4 * i, 4) for i in range(16)]
        bank = [0]

        def nextbank():
            b = bank[0]
            bank[0] = (b + 1) % 6
            return b

        def gen_L(gi, t0, nt):
            N = nt * 128
            sl = gi % 2
            mT = modcT if gi == 0 else modT
            rpt = rp[sl]
            dma("sp", rpt[:, :, :N], ROPE[:, :, t0 * 128:t0 * 128 + N].rearrange("a p n -> p a n"),
                [S.dres("rope")], [rpt], rpt)
            for s in range(nt):
                tl = t0 + s
                x_ = xt[tl % 2]; xh_ = xh[tl % 2]; pt_ = ptr[tl % 2]
                dma("sp", x_[:], src[tl * 128:(tl + 1) * 128, :], [src_res(tl)], [x_], x_)
                ln_stats(x_, x_, stats, mv, rstd)
                yield
                ts(xh_[:], x_[:], mv[:, 0:1], rstd[:, 0:1], ALU.subtract, ALU.mult, [x_, mv, rstd], [xh_])
                for k in range(8):
                    mm(pt_[:, k, :], xh_[:, k * 128:(k + 1) * 128], ident[:], True, True, [xh_, ident], [pt_], tr=True)
                yield
                for k in range(8):
                    act(hT[sl][:, k, s * 128:(s + 1) * 128], pt_[:, k, :], AF.Identity, [pt_, mT], [hT[sl]],
                        bias=mT[:, k:k + 1], scale=mT[:, 8 + k:9 + k])
                    if k % 4 == 3:
                        yield

        def gen_P(gi, t0, nt):
            N = nt * 128
            sl = gi % 2
            rpt = rp[sl]
            h_ = hT[sl]; fo = fmo[sl]

            def proj(ch):
                b = nextbank()
                for k in range(8):
                    mm(pbig[:, b, :N], win[:, k, ch * 128:(ch + 1) * 128], h_[:, k, :N], k == 0, k == 7,
                       [win, h_], [pbr[b]])
                return b

            def rope(chq, chs, tq, tsn, dst):
                b1 = proj(chq); b2 = proj(chs)
                tt(tmp[0][:, :N], pbig[:, b1, :N], rpt[:, tq, :N], ALU.mult, [pbr[b1], rpt], [tmp[0]])
                tt(tmp[1][:, :N], pbig[:, b2, :N], rpt[:, tsn, :N], ALU.mult, [pbr[b2], rpt], [tmp[1]])
                tt(fo[:, dst, :N], tmp[0][:, :N], tmp[1][:, :N], ALU.add, [tmp[0], tmp[1]], [fo], eng="pool")

            for c_ in range(3):
                rope(c_, 3 + c_, 0, 1, c_)
                yield
            rope(6, 7, 2, 3, 6)
            yield
            for c_ in range(2):
                b1 = proj(8 + c_)
                act(tmp[2][:, :N], pbig[:, b1, :N], AF.Identity, [pbr[b1]], [tmp[2]])
                b2 = proj(12 + c_)
                tt(fo[:, 10 + c_, :N], pbig[:, b2, :N], tmp[2][:, :N], ALU.mult, [pbr[b2], tmp[2]], [fo])
                yield
                b3 = proj(10 + c_)
                act(fo[:, 12 + c_, :N], pbig[:, b3, :N], AF.Identity, [pbr[b3]], [fo])
                yield
            for c_ in range(3):
                b1 = proj(14 + c_)
                act(fo[:, 3 + c_, :N], pbig[:, b1, :N], AF.Identity, [pbr[b1]], [fo], scale=0.125)
                yield
                b2 = proj(17 + c_)
                cp(fo[:, 7 + c_, :N], pbig[:, b2, :N], [pbr[b2]], [fo])
                yield
            for s in range(nt):
                b = nextbank()
                for k in range(8):
                    mm(pbig[:, b, :], h_[:, k, s * 128:(s + 1) * 128], win[:, k, 2560:3072], k == 0, k == 7,
                       [win, h_], [pbr[b]])
                act(vo[sl][:, s, :, 0:64], pbig[:, b, :].rearrange("p (h d) -> p h d", h=8), AF.Identity,
                    [pbr[b]], [vo[sl]])
                yield
            dma("sp", FM[:, :, t0 * 128:t0 * 128 + N], fo[:, :, :N], [fo], fm_res[t0:t0 + nt], fo)
            dma("sp", VV[t0 * 128:t0 * 128 + N].rearrange("(s p) h d -> p s h d", p=128), vo[sl][:, :nt],
                [vo[sl]], vv_res[t0:t0 + nt], vo[sl])
            yield

        prevP = None
        for gi, (t0, nt) in enumerate(groups):
            for _ in interleave([gen_L(gi, t0, nt), prevP]):
                pass
            prevP = gen_P(gi, t0, nt)
        for _ in prevP:
            pass

        new_phase()
        wout = ph("wout", [128, 8, D], BF16)
        wov = WOUT[l].rearrange("(k p) n -> p k n", p=128)
        for k in range(0, 8, 2):
            dma("pool", wout[:, k:k + 2, :], wov[:, k:k + 2, :], [S.dres("wout")], [wout], wout)
        wr = ph("wr", [128, 8, NE], BF16)
        dma("pool", wr[:], WR[l].rearrange("(k p) n -> p k n", p=128), [S.dres("wr")], [wr], wr)
        nabi = ph("nabi", [128, 6, 640], BF16)
        nabe = ph("nabe", [128, 6, 640], BF16)
        dma("pool", nabi[:], NAB[l, 0], [S.dres("nab")], [nabi], nabi)
        kctx = ph("kctx", [128, 4, 256], BF16)
        vctx = ph("vctx", [128, 2, 8, 65], BF16)
        dma("sp", kctx[:], FM[:, 6:10, 0:256], fm_res[0:2], [kctx], kctx)
        dma("sp", vctx[:], VV[0:256].rearrange("(s p) h d -> p s h d", p=128), vv_res[0:2], [vctx], vctx)
        g1b = ph("g1b", [128, D]); cg1b = ph("cg1b", [128, D])
        l1g = ph("l1g", [128, D]); l1b = ph("l1b", [128, D])
        dma("sp", g1b[:], MODROW[0, 2048:3072].partition_broadcast(128), [S.dres("modrow")], [g1b], g1b)
        dma("sp", cg1b[:], MODROW[1, 2048:3072].partition_broadcast(128), [S.dres("modrow")], [cg1b], cg1b)
        dma("sp", l1g[:], LN1G[l].partition_broadcast(128), [S.dres("ln1g")], [l1g], l1g)
        dma("sp", l1b[:], LN1B[l].partition_broadcast(128), [S.dres("ln1b")], [l1b], l1b)
        qw = [ph("qw%d" % i, [128, 6, 128], BF16) for i in range(2)]
        kw = [ph("kw%d" % i, [128, 4, 640], BF16) for i in range(2)]
        vw = [ph("vw%d" % i, [128, 5, 8, 65], BF16) for i in range(2)]
        uw = [ph("uw%d" % i, [128, 2, 130], BF16) for i in range(2)]
        bbw = [ph("bbw%d" % i, [128, 2, 128], BF16) for i in range(2)]
        xa = [ph("xa%d" % i, [128, D]) for i in range(2)]
        pta = [ph("pta%d" % i, [128, 5, 384], BF16) for i in range(2)]
        ptn = [ph("ptn%d" % i, [128, 896], BF16) for i in range(2)]
        sfn = [ph("sfn%d" % i, [128, 640]) for i in range(2)]
        mixc = ph("mixc", [128, 768], BF16)
        mixT = ph("mixT", [128, 8, 128], BF16)
        ctmp = ph("ctmp", [128, 128])
        den = ph("den", [128, 6]);
        t1 = ph("t1", [128, D]); y1 = ph("y1", [128, D]); xm = [ph("xm%d" % i, [128, D]) for i in range(2)]
        xh2 = ph("xh2", [128, RW], BF16)
        h2o = [ph("h2o%d" % i, [128, 8, 128], BF16) for i in range(2)]
        stats = ph("stats", [128, 2, 6]); mv = ph("mv", [128, 2]); rstd = ph("rstd", [128, 1]); nmr = ph("nmr", [128, 1])
        rmx = ph("rmx", [128, 1]); rsum = ph("rsum", [128, 1]); rexp = ph("rexp", [128, NE])

        mixT2 = [mixT, ph("mixTb", [128, 8, 128], BF16)]

        def gen_att(T):
            sl = T % 2
            is_ctx = T < 2
            i = T - 2
            q_ = qw[sl]; k_ = kw[sl]; v_ = vw[sl]; u_ = uw[sl]; bb_ = bbw[sl]
            mT_ = mixT2[sl]
            dma("sp", q_[:], FM[:, 0:6, T * 128:(T + 1) * 128], [fm_res[T]], [q_], q_)
            nb = nabi
            base = 0
            if not is_ctx:
                base = min(max(i - 2, 0), 59) + 2
                dma("sp", k_[:], FM[:, 6:10, base * 128:(base + 5) * 128], fm_res[base:base + 5], [k_], k_)
                dma("sp", v_[:], VV[base * 128:(base + 5) * 128].rearrange("(s p) h d -> p s h d", p=128),
                    vv_res[base:base + 5], [v_], v_)
                var = 0 if 2 <= i <= 61 else (1 + i if i < 2 else i - 59)
                if var != 0:
                    dma("pool", nabe[:], NAB[l, var], [S.dres("nab")], [nabe], nabe)
                    nb = nabe
            lo_pad = T in (0, 2); hi_pad = T in (1, NT - 1)
            if lo_pad or hi_pad:
                memset("pool", u_[:], 0.0, [u_])
            c0 = T * 128 - (0 if lo_pad else 1); c1 = (T + 1) * 128 + (0 if hi_pad else 1)
            o0 = 1 if lo_pad else 0
            fr = fm_res[max(T - 1, 0):min(T + 2, NT)]
            dma("sp", u_[:, :, o0:o0 + (c1 - c0)], FM[:, 10:12, c0:c1], fr, [u_], u_)
            dma("sp", bb_[:], FM[:, 12:14, T * 128:(T + 1) * 128], [fm_res[T]], [bb_], bb_)
            yield
            if is_ctx:
                akeys = [(("c", 0), None), (("c", 1), None)]
                nkeys = [("c", 0), ("c", 1)]
            else:
                akeys = []
                if i > 0:
                    akeys.append((("w", T - 1 - base), 0))
                akeys.append((("w", T - base), None))
                if i < 63:
                    akeys.append((("w", T + 1 - base), 1))
                akeys += [(("c", 0), None), (("c", 1), None)]
                nkeys = [("w", j) for j in range(5)] + [("c", 0), ("c", 1)]

            def kap(kt, ch, p0):
                if kt[0] == "c":
                    return kctx[p0:p0 + 64, ch, kt[1] * 128:(kt[1] + 1) * 128], kctx
                return k_[p0:p0 + 64, ch, kt[1] * 128:(kt[1] + 1) * 128], k_

            def vap(kt, head):
                if kt[0] == "c":
                    return vctx[:, kt[1], head, :], vctx
                return v_[:, kt[1], head, :], v_

            def gen_A():
                for g in range(2):
                    p0 = g * 64
                    pa_ = pta[g]
                    for ki, (kt, mk) in enumerate(akeys):
                        ka_, kr = kap(kt, 0, p0)
                        if mk is not None:
                            mm(pbig[:, 0, 0:384], ident[:], amask[:, mk, :], True, False, [ident, amask], [pbr[0]])
                        mm(pbig[:, 0, 0:384], ka_, q_[p0:p0 + 64, 0:3, :].rearrange("p a b -> p (a b)"), mk is None, True,
                           [kr, q_], [pbr[0]])
                        act(pa_[:, ki, :], pbig[:, 0, 0:384], AF.Exp, [pbr[0]], [pa_])
                        yield
                    po = pbig[:, 2, 0:195].rearrange("p (c d) -> p c d", c=3)
                    for c_ in range(3):
                        for ki, (kt, mk) in enumerate(akeys):
                            va_, vr = vap(kt, g)
                            mm(po[:, c_, :], pa_[:, ki, c_ * 128:(c_ + 1) * 128], va_, ki == 0, ki == len(akeys) - 1,
                               [pa_, vr], [pbr[2]])
                        yield
                    tt(den[:, 0:3], po[:, :, 64], esink[:, 3 * g:3 * g + 3], ALU.add, [pbr[2], esink], [den])
                    recip(den[:, 0:3], den[:, 0:3], [den], [den])
                    tt(mixc[:, g * 192:(g + 1) * 192].rearrange("p (c d) -> p c d", c=3), po[:, :, 0:64],
                       den[:, 0:3].unsqueeze(2).to_broadcast([128, 3, 64]), ALU.mult, [pbr[2], den], [mixcA])
                    yield

            def gen_N():
                po2 = pbig[:, 3, 0:390].rearrange("p (c d) -> p c d", c=6)
                nk = len(nkeys)
                for h in range(6):
                    ch = h // 2; p0 = (h % 2) * 64
                    pn_ = ptn[h % 2]; sf_ = sfn[h % 2]
                    ps2 = pbig[:, 4:6, :].rearrange("p a b -> p (a b)")
                    if is_ctx:
                        for j, kt in enumerate(nkeys):
                            ka_, kr = kap(kt, 1 + ch, p0)
                            mm(ps2[:, j * 128:(j + 1) * 128], ka_, q_[p0:p0 + 64, 3 + ch, :], True, True,
                               [kr, q_], [pbr[4], pbr[5]])
                        act(pn_[:, 0:256], ps2[:, 0:256], AF.Exp, [pbr[4], pbr[5]], [pn_])
                    else:
                        for j in (5, 6):
                            ka_, kr = kap(nkeys[j], 1 + ch, p0)
                            mm(ps2[:, j * 128:(j + 1) * 128], ka_, q_[p0:p0 + 64, 3 + ch, :], True, True,
                               [kr, q_], [pbr[4], pbr[5]])
                        mm(ps2[:, 512:640], ident[:], nb[:, h, 512:640], True, False, [ident, nb], [pbr[4], pbr[5]])
                        mm(ps2[:, 0:512], ident[:], nb[:, h, 0:512], True, False, [ident, nb], [pbr[4], pbr[5]])
                        for j in range(5):
                            ka_, kr = kap(nkeys[j], 1 + ch, p0)
                            mm(ps2[:, j * 128:(j + 1) * 128], ka_, q_[p0:p0 + 64, 3 + ch, :], False, j in (3, 4),
                               [kr, q_], [pbr[4], pbr[5]])
                        act(pn_[:, 0:896], ps2[:, 0:896], AF.Exp, [pbr[4], pbr[5]], [pn_])
                    yield
                    for j, kt in enumerate(nkeys):
                        va_, vr = vap(kt, 2 + h)
                        mm(po2[:, h, :], pn_[:, j * 128:(j + 1) * 128], va_, j == 0, j == nk - 1, [pn_, vr], [pbr[3]])
                    yield
                recip(den2[:], po2[:, :, 64], [pbr[3]], [den2])
                tt(mixc[:, 384:768].rearrange("p (c d) -> p c d", c=6), po2[:, :, 0:64],
                   den2[:].unsqueeze(2).to_broadcast([128, 6, 64]), ALU.mult, [pbr[3], den2], [mixcN])
                yield

            def gen_B():
                for c_ in range(2):
                    ts(ctmp[:], u_[:, c_, 0:128], convw[:, c_, 0:1], None, ALU.mult, None, [u_, convw], [ctmp])
                    stt(ctmp[:], u_[:, c_, 1:129], convw[:, c_, 1:2], ctmp[:], ALU.mult, ALU.add, [u_, convw, ctmp], [ctmp])
                    stt(ctmp[:], u_[:, c_, 2:130], convw[:, c_, 2:3], ctmp[:], ALU.mult, ALU.add, [u_, convw, ctmp], [ctmp])
                    tt(mT_[:, 3 + c_, :], ctmp[:], bb_[:, c_, :], ALU.mult, [ctmp, bb_], [mT_])
                    yield

            for _ in interleave([gen_A(), gen_N(), gen_B()]):
                yield
            pt_ = ptr[0]
            for c_ in range(6):
                mm(pt_[:, c_, :], mixc[:, c_ * 128:(c_ + 1) * 128], ident[:], True, True, [mixcA, mixcN, ident], [pt_], tr=True)
            cp(mT_[:, 0:3, :], pt_[:, 0:3, :], [pt_], [mT_])
            act(mT_[:, 5:8, :], pt_[:, 3:6, :], AF.Identity, [pt_], [mT_])
            yield

        def gen_epi(T):
            sl = T % 2
            is_ctx = T < 2
            x_ = xa[sl]; mT_ = mixT2[sl]
            dma("sp", x_[:], src[T * 128:(T + 1) * 128, :], [src_res(T)], [x_], x_)
            gb = cg1b if is_ctx else g1b
            for hf in range(2):
                for k in range(8):
                    mm(pbig[:, 1, :], mT_[:, k, :], wout[:, k, hf * 512:(hf + 1) * 512], k == 0, k == 7,
                       [mT_, wout], [pbr[1]])
                tt(t1[:, hf * 512:(hf + 1) * 512], pbig[:, 1, :], gb[:, hf * 512:(hf + 1) * 512], ALU.mult,
                   [pbr[1], gb], [t1])
                yield
            stt(y1[:], x_[:], ALPHA, t1[:], ALU.mult, ALU.add, [x_, t1], [y1])
            yield
            ln_stats(y1, y1, stats, mv, rstd, nmr)
            yield
            xm_ = xm[sl]
            act(t1[:], y1[:], AF.Identity, [y1, rstd, nmr], [t1], bias=nmr[:, 0:1], scale=rstd[:, 0:1])
            yield
            tt(t1[:], t1[:], l1g[:], ALU.mult, [t1, l1g], [t1], eng="pool")
            yield
            tt(xm_[:], t1[:], l1b[:], ALU.add, [t1, l1b], [xm_], eng="pool")
            dma("sp", XMID[T * 128:(T + 1) * 128, :], xm_[:], [xm_], [xmid_res[T]], xm_)
            yield
            ln_stats(xm_, xm_, stats, mv, rstd)
            yield
            ts(xh2[:, 0:D], xm_[:], mv[:, 0:1], rstd[:, 0:1], ALU.subtract, ALU.mult, [xm_, mv, rstd], [xh2])
            yield
            pt2 = ptr[1]
            for k in range(8):
                mm(pt2[:, k, :], xh2[:, k * 128:(k + 1) * 128], ident[:], True, True, [xh2, ident], [pt2], tr=True)
            mT = modcT if is_ctx else modT
            h2_ = h2o[sl]
            for k in range(8):
                act(h2_[:, k, :], pt2[:, k, :], AF.Identity, [pt2, mT], [h2_],
                    bias=mT[:, 24 + k:25 + k], scale=mT[:, 32 + k:33 + k])
                if k % 4 == 3:
                    yield
            if is_ctx:
                dma("sp", H2T[:, :, T * 128:(T + 1) * 128], h2_[:], [h2_], [h2t_res[T]], h2_)
            pr = pbig[:, 1, 0:NE]
            for k in range(8):
                mm(pr, h2_[:, k, :], wr[:, k, :], k == 0, k == 7, [h2_, wr], [pbr[1]])
            reduce(rmx[:], pr, ALU.max, [pbr[1]], [rmx], negate=True)
            act(rexp[:], pr, AF.Exp, [pbr[1], rmx], [rexp], bias=rmx[:, 0:1])
            yield
            reduce(rsum[:], rexp[:], ALU.add, [rexp], [rsum])
            recip(rsum[:], rsum[:], [rsum], [rsum])
            ts(aff[:, T, :], rexp[:], rsum[:, 0:1], None, ALU.mult, None, [rexp, rsum], [aff])
            if not is_ctx:
                ts(xh2[:, 1026:1042], rexp[:], rsum[:, 0:1], None, ALU.mult, None, [rexp, rsum], [xh2])
                stt(xh2[:, 1042:1058], rexp[:], rsum[:, 0:1], xh2[:, 1026:1042], ALU.mult, ALU.subtract,
                    [rexp, rsum, xh2], [xh2])
                iota_tail(xh2[:, 1024:1026].bitcast(I32), T * 128, [xh2])
                dma("sp", XH2[T * 128:(T + 1) * 128, :], xh2[:], [xh2], [xh2_res[T]], xh2)
            yield

        mixcA = Res("mixcA"); mixcN = Res("mixcN")
        den2 = ph("den2", [128, 6])
        prev = None
        for T in range(T0, NT):
            for _ in interleave([gen_att(T), prev]):
                pass
            prev = gen_epi(T)
        for _ in prev:
            pass

        new_phase()
        lo_t = ph("lo", [128, NE]); mid_t = ph("mid", [128, NE]); cntp = ph("cntp", [128, NE]); sel = ph("sel", [128, NE])
        cmp = ph("cmp", [128, NE, 64])
        incl = ph("incl", [128, NE, 64])
        zt = ph("zt", [128, 64])
        offs = ph("offs", [128, NE])
        zbig = ph("zbig", [128, 4096])
        memset("pool", zbig[:], 0.0, [zbig])
        memset("pool", zt[:], 0.0, [zt])
        for j in range(16):
            dma("sp", FFN[256 + j * 512:256 + (j + 1) * 512, :].rearrange("(p a) d -> p (a d)", p=128), zbig[:],
                [zbig], [S.dres("ffn")], zbig, group=("ffnz", l))
        sets = [(2, 64, 1024.0)] if last else [(0, 2, 32.0), (2, 64, 1024.0)]
        for (ta, tn, cap) in sets:
            av = aff[:, ta:ta + tn, :].rearrange("p t e -> p e t")
            memset("dve", lo_t[:], 0.0, [lo_t])
            for it in range(30):
                w_ = 0.5 ** (it + 1)
                ts(mid_t[:], lo_t[:], w_, None, ALU.add, None, [lo_t], [mid_t])
                tt(cmp[:, :, :tn], av, mid_t[:].unsqueeze(2).to_broadcast([128, NE, tn]), ALU.is_ge, [aff, mid_t], [cmp])
                reduce(cntp[:], cmp[:, :, :tn], ALU.add, [cmp], [cntp])
                mm(pbig[:, 0, 0:NE], onesf[:], cntp[:], True, True, [onesf, cntp], [pbr[0]])
                single(sel[:], pbig[:, 0, 0:NE], cap - 0.5, ALU.is_ge, [pbr[0]], [sel])
                stt(lo_t[:], sel[:], w_, lo_t[:], ALU.mult, ALU.add, [sel, lo_t], [lo_t])
            tt(cmp[:, :, :tn], av, lo_t[:].unsqueeze(2).to_broadcast([128, NE, tn]), ALU.is_ge, [aff, lo_t], [cmp])
            if dbg:
                LDBG = nc.dram_tensor("ldbg%d_%d" % (l, tn), [128, NE], F32, kind="ExternalOutput").ap()
                dma("sp", LDBG, lo_t[:], [lo_t], [S.dres("ldbg", tn)], lo_t)
                ADBG = nc.dram_tensor("adbg%d_%d" % (l, tn), [128, NT * NE], F32, kind="ExternalOutput").ap()
                dma("sp", ADBG, aff[:].rearrange("p a b -> p (a b)"), [aff], [S.dres("adbg", tn)], aff)
            if tn == 2:
                wv = wsel[:, ta:ta + tn, :].rearrange("p t e -> p e t")
                tt(wv, av, cmp[:, :, :tn], ALU.mult, [aff, cmp], [wsel])
                continue
            for ex in range(NE):
                scan(incl[:, ex, :], cmp[:, ex, :], zt[:], [cmp, zt], [incl])
            cp(cntp[:], incl[:, :, 63], [incl], [cntp])
            mm(pbig[:, 0, 0:NE], ltri[:], cntp[:], True, True, [ltri, cntp], [pbr[0]])
            cp(offs[:], pbig[:, 0, 0:NE], [pbr[0]], [offs])
            tt(incl[:], incl[:], cmp[:], ALU.subtract, [incl, cmp], [incl])
            tt(incl[:], incl[:], offs[:].unsqueeze(2).to_broadcast([128, NE, 64]), ALU.add, [incl, offs], [incl])
            stt(incl[:].rearrange("p a b -> p (a b)"), incl[:].rearrange("p a b -> p (a b)"), -1.0e6,
                cmp[:].rearrange("p a b -> p (a b)"), ALU.add, ALU.mult, [incl, cmp], [incl])
            ts(idxT[:], incl[:], 1.0e6, None, ALU.add, None, [incl], [idxT])
            if dbg:
                IDBG = nc.dram_tensor("idbg%d" % l, [128, NE * 64], I32, kind="ExternalOutput").ap()
                dma("sp", IDBG, idxT[:].rearrange("p a b -> p (a b)"), [idxT], [S.dres("idbg")], idxT)
                ODBG = nc.dram_tensor("odbg%d" % l, [128, NE], F32, kind="ExternalOutput").ap()
                dma("sp", ODBG, offs[:], [offs], [S.dres("odbg")], offs)
                CDBG = nc.dram_tensor("cdbg%d" % l, [128, NE * 64], F32, kind="ExternalOutput").ap()
                dma("sp", CDBG, cmp[:].rearrange("p a b -> p (a b)"), [cmp], [S.dres("cdbg")], cmp)

        new_phase()
        wgt = [ph("wg%d" % i, [128, 8, 512], BF16) for i in range(2)]
        wut = [ph("wu%d" % i, [128, 8, 512], BF16) for i in range(2)]
        wdt = [ph("wd%d" % i, [128, 4, D], BF16) for i in range(2)]
        tokc = [ph("tokc%d" % i, [128, 8, RW], BF16) for i in range(2)]
        xet = [ph("xet%d" % i, [128, RW], BF16) for i in range(2)]
        h2e = [ph("h2e%d" % i, [128, 8, 512], BF16) for i in range(2)]
        gT = [ph("gT%d" % i, [128, 4, 512], BF16) for i in range(2)]
        sa = [ph("sa%d" % i, [128, 512], BF16) for i in range(2)]
        yo = [ph("yo%d" % i, [128, D]) for i in range(2)]
        idxe = [ph("idxe%d" % i, [128, 1], I32) for i in range(8)]
        gate = ph("gate", [128, 8])
        rmx = ph("rmx", [128, 1]); rsum = ph("rsum", [128, 1]); rexp = ph("rexp", [128, NE])
        wr = ph("wr", [128, 8, NE], BF16)
        dma("pool", wr[:], WR[l].rearrange("(k p) n -> p k n", p=128), [S.dres("wr")], [wr], wr)
        h2g = ph("h2g", [128, 8, 256], BF16)
        accr = [Res("acc%d" % i) for i in range(2)]
        if not last:
            dma("sp", h2g[:], H2T[:, :, 0:256], h2t_res[0:2], [h2g], h2g)
        xe_res = [S.dres("xe", e) for e in range(NE)]
        tcnt = [0]

        def load_w(ex):
            sl = ex % 2
            dma("pool", wgt[sl][:], WG[l, ex].rearrange("(k p) f -> p k f", p=128), [S.dres("wg")], [wgt[sl]], wgt[sl])
            dma("pool", wut[sl][:], WU[l, ex].rearrange("(k p) f -> p k f", p=128), [S.dres("wu")], [wut[sl]], wut[sl])
            dma("pool", wdt[sl][:], WD[l, ex].rearrange("(k p) f -> p k f", p=128), [S.dres("wd")], [wdt[sl]], wdt[sl])

        disp_own = [[Res("disp%d_%d" % (e_, j_)) for j_ in range(2)] for e_ in range(NE)]

        def dispatch_gen(exs):
            for cg in range(8):
                tk = tokc[tcnt[0] % 2]
                tcnt[0] += 1
                dma("sp", tk[:], XH2[(2 + cg * 8) * 128:(2 + cg * 8 + 8) * 128, :].rearrange("(s p) d -> p s d", p=128),
                    xh2_res[2 + cg * 8:2 + cg * 8 + 8], [tk], tk)
                for s in range(8):
                    T = 2 + cg * 8 + s
                    for ex in exs:
                        S.dma("pool", lambda e, tk=tk, s=s, ex=ex, T=T: e.indirect_dma_start(
                            out=XE[ex][:, :], out_offset=bass.IndirectOffsetOnAxis(
                                ap=idxT[:].rearrange("p a b -> p (a b)")[:, ex * 64 + T - 2:ex * 64 + T - 1], axis=0),
                            in_=tk[:, s, :], in_offset=None, bounds_check=breg(e), oob_is_err=False),
                            reads=rs([tk, idxT]), writes=[xe_res[ex]], owner=disp_own[ex][cg % 2], group=("xe", l, ex),
                            cost=1.2, lat=4.0)
                yield

        def take(g, n):
            for _ in range(n):
                try:
                    next(g)
                except StopIteration:
                    return
                yield

        egroups = [[0, 1], [2, 3, 4, 5], [6, 7, 8, 9], [10, 11, 12, 13], [14, 15]]
        gstart = {g[0]: gi for gi, g in enumerate(egroups)}
        gater = [Res("gate%d" % i) for i in range(8)]
        for _ in dispatch_gen(egroups[0]):
            pass
        load_w(0)
        load_w(1)

        def ctx_dense(ex, wg_, wu_, wd_):
                def ffn_chunk(h_src, N, g_):
                    for fc in range(4):
                        ba = 2 * (fc % 2); bu = ba + 1
                        for k in range(8):
                            mm(pbig[:, ba, :N], wg_[:, k, fc * 128:(fc + 1) * 128], h_src[0][:, k, :N], k == 0, k == 7,
                               [wg_, h_src[1]], [pbr[ba]])
                        for k in range(8):
                            mm(pbig[:, bu, :N], wu_[:, k, fc * 128:(fc + 1) * 128], h_src[0][:, k, :N], k == 0, k == 7,
                               [wu_, h_src[1]], [pbr[bu]])
                        s_ = sa[fc % 2]
                        act(s_[:, :N], pbig[:, ba, :N], AF.Silu, [pbr[ba]], [s_])
                        tt(g_[:, fc, :N], pbig[:, bu, :N], s_[:, :N], ALU.mult, [pbr[bu], s_], [g_])

                def down(g_, s):
                    for hf in range(2):
                        for fc in range(4):
                            mm(pbig[:, 4 + hf, :], g_[:, fc, s * 128:(s + 1) * 128], wd_[:, fc, hf * 512:(hf + 1) * 512],
                               fc == 0, fc == 3, [g_, wd_], [pbr[4 + hf]])
                    return pbig[:, 4:6, :]

                if not last:
                    g_ = gT[0]
                    ffn_chunk((h2g, h2g), 256, g_)
                    for s in range(2):
                        py = down(g_, s)
                        av_ = acc[:, s, :].rearrange("p (a b) -> p a b", a=2)
                        if ex == 0:
                            ts(av_, py, wsel[:, s, ex:ex + 1], None, ALU.mult, None, [pbr[4], pbr[5], wsel], [accr[s]])
                        else:
                            stt(av_, py, wsel[:, s, ex:ex + 1], av_, ALU.mult, ALU.add,
                                [pbr[4], pbr[5], wsel, accr[s]], [accr[s]])

        def gen_prep(ex, ci):
            h_ = h2e[ci]
            for s in range(4):
                st_ = ci * 4 + s
                x_ = xet[st_ % 2]
                dma("sp", x_[:], XE[ex][st_ * 128:(st_ + 1) * 128, :], [xe_res[ex]], [x_], x_)
                cp(idxe[st_][:], x_[:, 1024:1026].bitcast(I32), [x_], [idxe[st_]])
                tt(gate[:, st_:st_ + 1], x_[:, 1026 + ex:1027 + ex], x_[:, 1042 + ex:1043 + ex], ALU.add, [x_], [gater[st_]])
                pt_ = ptr[st_ % 2]
                for k in range(8):
                    mm(pt_[:, k, :], x_[:, k * 128:(k + 1) * 128], ident[:], True, True, [x_, ident], [pt_], tr=True)
                yield
                for k in range(8):
                    act(h_[:, k, s * 128:(s + 1) * 128], pt_[:, k, :], AF.Identity, [pt_, modT], [h_],
                        bias=modT[:, 24 + k:25 + k], scale=modT[:, 32 + k:33 + k])
                    if k % 4 == 3:
                        yield

        def gen_ffn(ex, ci, wg_, wu_, wd_):
            h_ = h2e[ci]
            g_ = gT[ci]
            for fc in range(4):
                ba = 2 * (fc % 2); bu = ba + 1
                for k in range(8):
                    mm(pbig[:, ba, :], wg_[:, k, fc * 128:(fc + 1) * 128], h_[:, k, :], k == 0, k == 7, [wg_, h_], [pbr[ba]])
                for k in range(8):
                    mm(pbig[:, bu, :], wu_[:, k, fc * 128:(fc + 1) * 128], h_[:, k, :], k == 0, k == 7, [wu_, h_], [pbr[bu]])
                s_ = sa[fc % 2]
                act(s_[:], pbig[:, ba, :], AF.Silu, [pbr[ba]], [s_])
                tt(g_[:, fc, :], pbig[:, bu, :], s_[:], ALU.mult, [pbr[bu], s_], [g_])
                yield
            for s in range(4):
                st_ = ci * 4 + s
                for hf in range(2):
                    for fc in range(4):
                        mm(pbig[:, 4 + hf, :], g_[:, fc, s * 128:(s + 1) * 128], wd_[:, fc, hf * 512:(hf + 1) * 512],
                           fc == 0, fc == 3, [g_, wd_], [pbr[4 + hf]])
                y_ = yo[st_ % 2]
                act(y_[:].rearrange("p (a b) -> p a b", a=2), pbig[:, 4:6, :], AF.Identity, [pbr[4], pbr[5], gater[st_]], [y_],
                    scale=gate[:, st_:st_ + 1])
                S.dma("pool", lambda e, y_=y_, ie_=idxe[st_]: e.indirect_dma_start(
                    out=FFN[:, :], out_offset=bass.IndirectOffsetOnAxis(ap=ie_[:, :], axis=0),
                    in_=y_[:, :], in_offset=None, compute_op=ALU.add),
                    reads=rs([y_, idxe[st_]]), writes=[S.dres("ffn")], owner=y_.r, group=("ffn", l, ex), cost=1.2, lat=8.0)
                yield

        prev = None
        dg = None
        per = 0
        gend = {g[-1] for g in egroups}
        for ex in range(NE):
            sl = ex % 2
            if ex in gstart and gstart[ex] + 1 < len(egroups):
                dg = dispatch_gen(egroups[gstart[ex] + 1])
                nch = 2 * len(egroups[gstart[ex]])
                per = (8 + nch - 1) // nch
            ctx_dense(ex, wgt[sl], wut[sl], wdt[sl])
            for ci in range(2):
                for _ in interleave([gen_prep(ex, ci), prev, take(dg, per) if dg is not None else None]):
                    pass
                if ci == 0 and ex >= 1 and ex + 1 < NE:
                    load_w(ex + 1)
                prev = gen_ffn(ex, ci, wgt[sl], wut[sl], wdt[sl])
            if ex in gend and dg is not None:
                for _ in dg:
                    pass
                dg = None
        for _ in prev:
            pass

        new_phase()
        g2b = ph("g2b", [128, D]); cg2b = ph("cg2b", [128, D])
        l2g = ph("l2g", [128, D]); l2b = ph("l2b", [128, D])
        dma("sp", g2b[:], MODROW[0, 5120:6144].partition_broadcast(128), [S.dres("modrow")], [g2b], g2b)
        dma("sp", cg2b[:], MODROW[1, 5120:6144].partition_broadcast(128), [S.dres("modrow")], [cg2b], cg2b)
        dma("sp", l2g[:], LN2G[l].partition_broadcast(128), [S.dres("ln2g")], [l2g], l2g)
        dma("sp", l2b[:], LN2B[l].partition_broadcast(128), [S.dres("ln2b")], [l2b], l2b)
        xmt = [ph("xmt%d" % i, [128, D]) for i in range(4)]
        fft = [ph("fft%d" % i, [128, D]) for i in range(4)]
        ot = [ph("ot%d" % i, [128, D]) for i in range(4)]
        y2 = [ph("y2%d" % i, [128, D]) for i in range(4)]
        st2 = [(ph("stats", [128, 2, 6]), ph("mv", [128, 2]), ph("rstd", [128, 1]), ph("nmr", [128, 1])) for _ in range(4)]

        def gen_ln2(T):
            xm_ = xmt[T % 4]; o_ = ot[T % 4]; y2_ = y2[T % 4]
            stats, mv, rstd, nmr = st2[T % 4]
            dma("sp", xm_[:], XMID[T * 128:(T + 1) * 128, :], [xmid_res[T]], [xm_], xm_)
            if T < 2:
                tt(y2_[:], acc[:, T, :], cg2b[:], ALU.mult, [accr[T], cg2b], [y2_])
            else:
                f_ = fft[T % 4]
                dma("sp", f_[:], FFN[T * 128:(T + 1) * 128, :], [S.dres("ffn")], [f_], f_)
                tt(y2_[:], f_[:], g2b[:], ALU.mult, [f_, g2b], [y2_])
            yield
            stt(y2_[:], xm_[:], ALPHA, y2_[:], ALU.mult, ALU.add, [xm_, y2_], [y2_])
            yield
            ln_stats(y2_, y2_, stats, mv, rstd, nmr)
            yield
            act(y2_[:], y2_[:], AF.Identity, [y2_, rstd, nmr], [y2_], bias=nmr[:, 0:1], scale=rstd[:, 0:1])
            yield
            tt(y2_[:], y2_[:], l2g[:], ALU.mult, [y2_, l2g], [y2_], eng="pool")
            yield
            tt(o_[:], y2_[:], l2b[:], ALU.add, [y2_, l2b], [o_], eng="pool")
            if last:
                ev = dma("sp", Y[(T - 2) * 128:(T - 1) * 128, :], o_[:], [o_], [y_res[T - 2]], o_)
                final.append(ev)
            else:
                dma("sp", XCUR[T * 128:(T + 1) * 128, :], o_[:], [o_], [xcur_res[T]], o_)
            yield

        tl_ = list(range(T0, NT))
        for j in range(0, len(tl_), 4):
            for _ in interleave([gen_ln2(T) for T in tl_[j:j + 4]]):
                pass

    S.emit(final_waits=final, reorder=reorder, only=only)
    if dbg:
        print("instr counts", {e: len(v) for e, v in S.ins.items()}, "waits", S.nwaits, flush=True)
    return nc


def _rope_tables():
    t = np.arange(8192)
    row = (t // 64).astype(np.float32); col = (t % 64).astype(np.float32)
    inv = (10000.0 ** (-np.arange(0, 32, 2, dtype=np.float32) / 32)).astype(np.float32)
    cs = np.ones((64, TOK), np.float32); sn = np.zeros((64, TOK), np.float32)
    for a, pos in enumerate((row, col)):
        ang = (pos[:, None] * inv[None, :]).astype(np.float32)
        c = np.cos(ang).T; s = np.sin(ang).T
        cs[a * 32:a * 32 + 16, 256:] = c; cs[a * 32 + 16:a * 32 + 32, 256:] = c
        sn[a * 32:a * 32 + 16, 256:] = -s; sn[a * 32 + 16:a * 32 + 32, 256:] = s
    cs = np.concatenate([cs, cs], 0); sn = np.concatenate([sn, sn], 0)
    return np.stack([cs * 0.125, sn * 0.125, cs, sn]).astype(np.float32)


def _win_ext(w_in):
    qa = w_in[:, :, 0:384]; ka = w_in[:, :, 384:512]; va = w_in[:, :, 512:640]
    bx = w_in[:, :, 640:896]; bb = w_in[:, :, 896:1152]; bc = w_in[:, :, 1152:1408]
    qn = w_in[:, :, 1408:1792]; kn = w_in[:, :, 1792:2176]; vn = w_in[:, :, 2176:2560]
    sw = np.concatenate([np.arange(16, 32), np.arange(0, 16), np.arange(48, 64), np.arange(32, 48)])

    def heads_sw(w, nh):
        idx = np.concatenate([h * 64 + sw for h in range(nh)])
        return w[:, :, idx]

    def qperm(w):
        idx = np.concatenate([np.concatenate([np.arange(c * 64, c * 64 + 64), np.arange((3 + c) * 64, (3 + c) * 64 + 64)])
                              for c in range(3)])
        return w[:, :, idx]

    return np.ascontiguousarray(np.concatenate(
        [qperm(qa), qperm(heads_sw(qa, 6)), ka, heads_sw(ka, 2), bx, bb, bc, qn, kn, va, vn], axis=2))


def _na_bias(rpb):
    NEG = -30000.0
    out = np.full((DEPTH, 5, 6, 5, 128, 128), NEG, np.float32)
    cq = np.arange(64)
    col_start = np.clip(cq - 8, 0, 48)
    col_ok = (cq[None, :] >= col_start[:, None]) & (cq[None, :] < col_start[:, None] + 16)
    coff = np.clip(cq[None, :] - cq[:, None], -15, 15) + 15
    variants = [10, 0, 1, 62, 63]
    for vi, P in enumerate(variants):
        base = min(max(P - 2, 0), 59)
        for rho in range(2):
            r = 2 * P + rho
            rs_ = min(max(r - 4, 0), 120)
            for j in range(5):
                for kap in range(2):
                    kr = 2 * (base + j) + kap
                    if not (rs_ <= kr < rs_ + 8):
                        continue
                    roff = kr - r + 7
                    b = rpb[:, :, roff, :][:, :, coff]
                    b = np.where(col_ok[None, None], b, NEG)
                    out[:, vi, :, j, kap * 64:(kap + 1) * 64, rho * 64:(rho + 1) * 64] = np.transpose(b, (0, 1, 3, 2))
    out = np.transpose(out, (0, 1, 4, 2, 3, 5)).reshape(DEPTH, 5, 128, 6, 640)
    return np.ascontiguousarray(out)


def _amask():
    k = np.arange(128)[:, None]; q = np.arange(128)[None, :]
    mp = np.where(k >= q, 0.0, -30000.0).astype(np.float32); mn = np.where(k <= q, 0.0, -30000.0).astype(np.float32)
    return np.ascontiguousarray(np.stack([np.tile(mp, (1, 3)), np.tile(mn, (1, 3))], axis=1))


def make_in_maps(x, c, ctx, c_ctx, w_mod, b_mod, w_in, conv_w, attn_sink, na_rpb, w_out,
                 ln1_g, ln1_b, w_router, w_gate, w_up, w_down, ln2_g, ln2_b):
    f = lambda a: np.ascontiguousarray(np.asarray(a, dtype=np.float32))
    shared = dict(
        w_mod=f(w_mod), b_mod=f(b_mod), w_in=_win_ext(f(w_in)), rope=_rope_tables(),
        convw=np.ascontiguousarray(np.transpose(f(conv_w).reshape(DEPTH, 3, 2, 128), (0, 3, 2, 1))),
        sink=f(attn_sink), nab=_na_bias(f(na_rpb)), amask=_amask(), w_out=f(w_out),
        ln1_g=f(ln1_g), ln1_b=f(ln1_b), ln2_g=f(ln2_g), ln2_b=f(ln2_b), w_router=f(w_router),
        w_gate=f(w_gate), w_up=f(w_up), w_down=f(w_down))
    x = f(x); ctx = f(ctx); c = f(c); c_ctx = f(c_ctx)
    maps = []
    for b in range(N_CORES):
        m = dict(shared)
        m["xin"] = np.ascontiguousarray(np.concatenate([ctx[b], x[b]], axis=0))
        m["cvec"] = np.ascontiguousarray(np.stack([c[b], c_ctx], axis=0))
        maps.append(m)
    return maps


_NC = {}


def kernel(**inputs):
    if "nc" not in _NC:
        _NC["nc"] = build_nc(reorder=False)
    maps = make_in_maps(**inputs)
    res = run_bass_kernel_spmd(_NC["nc"], maps, core_ids=list(range(N_CORES)))
    return np.stack([np.asarray(r["y"], dtype=np.float32) for r in res.results], axis=0)
```

```python
import numpy as np
import concourse.bass as bass
import concourse.mybir as mybir
from concourse.bass_utils import run_bass_kernel_spmd

F32 = mybir.dt.float32
BF16 = mybir.dt.bfloat16
I32 = mybir.dt.int32
AF = mybir.ActivationFunctionType
ALU = mybir.AluOpType
AX = mybir.AxisListType

D = 1024
NT = 66
TOK = NT * 128
DEPTH = 2
ALPHA = float((2 * DEPTH) ** 0.25)
NE = 16
RW = 1024 + 2 + 32
COMPUTE = ("pe", "dve", "act", "pool")
N_CORES = 4


class Res:
    __slots__ = ("name", "w", "r", "dsem", "wg")

    def __init__(self, name="r"):
        self.name = name
        self.w = None
        self.r = []
        self.dsem = {}
        self.wg = None


class Sched:
    def __init__(self, nc):
        self.nc = nc
        self.ins = {e: [] for e in ("pe", "dve", "act", "pool", "sp")}
        self.dram = {}
        self.ndsem = 0
        self.owners = []
        self.free = {"sp": [], "pool": []}
        self.phase = 0
        self.phase_ev = {0: []}
        self.last_dma = {}
        self.pool_dmas = []
        self.pool_throttle = 0
        self.last_pew = {}

    def dres(self, *key):
        r = self.dram.get(key)
        if r is None:
            r = self.dram[key] = Res(str(key))
        return r

    def _deps(self, eng, reads, writes, pe_acc, group=None):
        deps = []
        for r in reads:
            if r.w is not None:
                if isinstance(r.w, list):
                    deps.extend(r.w)
                else:
                    deps.append(r.w)
        for r in writes:
            if r.w is not None:
                if isinstance(r.w, list):
                    if not (group is not None and r.wg == group):
                        deps.extend(r.w)
                elif not (pe_acc and r.w[0] == "E" and r.w[1] == "pe" and eng == "pe"):
                    deps.append(r.w)
            deps.extend(r.r)
        return deps

    def _post(self, ev, reads, writes, group=None):
        for r in reads:
            r.r.append(ev)
        for r in writes:
            if group is not None and r.wg == group and isinstance(r.w, list):
                r.w.append(ev)
            else:
                r.w = [ev] if group is not None else ev
                r.wg = group
                r.r = []

    def op(self, eng, fn, reads=(), writes=(), pe_acc=False, cost=0.5):
        deps = self._deps(eng, reads, writes, pe_acc)
        idx = len(self.ins[eng])
        order = []
        if eng == "pe":
            for r in writes:
                p = self.last_pew.get(id(r))
                if p is not None:
                    order.append(p)
                self.last_pew[id(r)] = idx
        ev = ("E", eng, idx)
        self.ins[eng].append([fn, deps, None, self.phase, cost, order, cost])
        self._post(ev, reads, writes)
        return ev

    def dma(self, q, fn, reads=(), writes=(), owner=None, group=None, cost=0.1, lat=3.0):
        deps = self._deps(q, reads, writes, False, group)
        sc = owner.dsem.get(q)
        if sc is None:
            if self.free[q]:
                sc = list(self.free[q].pop())
            else:
                sc = [self.ndsem, 0]
                self.ndsem += 1
            owner.dsem[q] = sc
            self.owners.append((owner, q))
        sc[1] += 16
        ev = ("D", sc[0], sc[1])
        if q == "pool" and self.pool_throttle:
            if len(self.pool_dmas) >= self.pool_throttle:
                deps.append(self.pool_dmas[-self.pool_throttle])
            self.pool_dmas.append(ev)
        idx = len(self.ins[q])
        order = []
        p = self.last_dma.get(sc[0])
        if p is not None:
            order.append(p)
        self.last_dma[sc[0]] = idx
        self.ins[q].append([fn, deps, sc[0], self.phase, cost, order, cost + lat, ev])
        self._post(ev, reads, writes, group)
        return ev

    def barrier(self):
        evs = []
        for e in COMPUTE:
            for i in range(len(self.ins[e]) - 1, -1, -1):
                if self.ins[e][i][2] is None:
                    evs.append(("E", e, i))
                    break
        for (o, q) in self.owners:
            sc = o.dsem.pop(q)
            evs.append(("D", sc[0], sc[1]))
            self.free[q].append((sc[0], sc[1]))
        self.owners = []
        self.phase += 1
        self.phase_ev[self.phase] = evs

    def _schedule(self, only=None):
        import heapq
        engs = list(self.ins.keys())
        dprod = {}
        for q in ("sp", "pool"):
            for i, rec in enumerate(self.ins[q]):
                if rec[2] is not None:
                    dprod[(rec[7][1], rec[7][2])] = (q, i)
        fin = {}
        issue = {}
        order = {e: [] for e in engs}
        ptr0 = {e: 0 for e in engs}
        tnow = 0.0
        for ph in range(self.phase + 1):
            nodes = []
            for e in engs:
                lst = self.ins[e]
                i = ptr0[e]
                while i < len(lst) and lst[i][3] == ph:
                    nodes.append((e, i))
                    i += 1
                ptr0[e] = i
            if not nodes:
                continue
            if only is not None and ph not in only:
                for (e, i) in nodes:
                    order[e].append(i)
                continue
            inph = set(nodes)
            ndep = {}
            users = {}
            for (e, i) in nodes:
                rec = self.ins[e][i]
                preds = set()
                for d in rec[1]:
                    p = (d[1], d[2]) if d[0] == "E" else dprod.get((d[1], d[2]))
                    if p is not None and p in inph and p != (e, i):
                        preds.add((p, 0))
                for j in rec[5]:
                    if (e, j) in inph:
                        preds.add(((e, j), 1))
                ndep[(e, i)] = len(preds)
                for pk in preds:
                    users.setdefault(pk[0], []).append(((e, i), pk[1]))
            ready = {}
            heaps = {e: [] for e in engs}
            avail = {e: [] for e in engs}
            efree = {e: tnow for e in engs}
            for n in nodes:
                ready[n] = tnow
                if ndep[n] == 0:
                    heapq.heappush(heaps[n[0]], (tnow, n[1]))
            left = len(nodes)
            tmax = tnow
            while left:
                best = None
                for e in engs:
                    if avail[e]:
                        st_ = efree[e]
                    elif heaps[e]:
                        st_ = max(heaps[e][0][0], efree[e])
                    else:
                        continue
                    if best is None or st_ < best[0]:
                        best = (st_, e)
                st_, e = best
                while heaps[e] and heaps[e][0][0] <= st_:
                    heapq.heappush(avail[e], heapq.heappop(heaps[e])[1])
                i = heapq.heappop(avail[e])
                rec = self.ins[e][i]
                issue[(e, i)] = st_
                efree[e] = st_ + rec[4]
                f_ = st_ + rec[6]
                fin[(e, i)] = f_
                tmax = max(tmax, f_)
                order[e].append(i)
                left -= 1
                for (u, kind) in users.get((e, i), ()):
                    t_ = f_ if kind == 0 else st_
                    if t_ > ready[u]:
                        ready[u] = t_
                    ndep[u] -= 1
                    if ndep[u] == 0:
                        heapq.heappush(heaps[u[0]], (ready[u], u[1]))
            tnow = tmax
        self.sim_time = tnow
        return order

    def _check(self, order, val):
        sems = {}
        pos = {e: 0 for e in self.ins}
        curph = {e: 0 for e in self.ins}
        progress = True
        total = sum(len(v) for v in self.ins.values())
        done = 0
        while progress:
            progress = False
            for e in self.ins:
                while pos[e] < len(order[e]):
                    i = order[e][pos[e]]
                    rec = self.ins[e][i]
                    deps = list(rec[1])
                    if rec[3] != curph[e]:
                        for p in range(curph[e] + 1, rec[3] + 1):
                            deps.extend(self.phase_ev.get(p, ()))
                    ok = True
                    for d in deps:
                        if d[0] == "E":
                            if sems.get(("E", d[1]), 0) < val[d[1]][d[2]]:
                                ok = False
                                break
                        elif sems.get(("D", d[1]), 0) < d[2]:
                            ok = False
                            break
                    if not ok:
                        break
                    curph[e] = rec[3]
                    if rec[2] is not None:
                        sems[("D", rec[2])] = sems.get(("D", rec[2]), 0) + 16
                    elif i in val.get(e, {}):
                        sems[("E", e)] = val[e][i]
                    pos[e] += 1
                    done += 1
                    progress = True
        if done != total:
            msg = []
            for e in self.ins:
                if pos[e] < len(order[e]):
                    i = order[e][pos[e]]
                    msg.append((e, pos[e], i, self.ins[e][i][3], self.ins[e][i][1][:6]))
            raise RuntimeError("schedule deadlock: %s" % msg)

    def emit(self, final_waits=(), reorder=True, only=None):
        import contextlib
        nc = self.nc
        if reorder:
            order = self._schedule(only)
        else:
            order = {e: list(range(len(l))) for e, l in self.ins.items()}
        for e in self.ins:
            assert sorted(order[e]) == list(range(len(self.ins[e]))), e
        lastc = {}
        for e in COMPUTE:
            cur = None
            per = {}
            for i in order[e]:
                if self.ins[e][i][2] is None:
                    per[self.ins[e][i][3]] = i
            lastc[e] = per
        for p in list(self.phase_ev.keys()):
            evs = [d for d in self.phase_ev[p] if d[0] == "D"]
            for e in COMPUTE:
                qs = [q for q in lastc[e] if q < p]
                if qs:
                    evs.append(("E", e, lastc[e][max(qs)]))
            self.phase_ev[p] = evs
        need = {e: set() for e in COMPUTE}
        for e, lst in self.ins.items():
            for rec in lst:
                for d in rec[1]:
                    if d[0] == "E":
                        need[d[1]].add(d[2])
        for evs in self.phase_ev.values():
            for d in evs:
                if d[0] == "E":
                    need[d[1]].add(d[2])
        val = {}
        for e in COMPUTE:
            c = 0
            v = {}
            for i in order[e]:
                if i in need[e]:
                    c += 1
                    v[i] = c
            val[e] = v
        self._check(order, val)
        self.nwaits = {}
        with contextlib.ExitStack() as st:
            esem = {e: st.enter_context(nc.semaphore("s_" + e)) for e in COMPUTE}
            dsem = [st.enter_context(nc.semaphore("d%d" % i)) for i in range(self.ndsem)]
            block = st.enter_context(nc.Block())

            def run(ename, eng):
                waited = {}
                lst = self.ins[ename]
                cur_ph = 0
                for i in order[ename]:
                    rec = lst[i]
                    deps = rec[1]
                    if rec[3] != cur_ph:
                        deps = list(deps)
                        for p in range(cur_ph + 1, rec[3] + 1):
                            deps.extend(self.phase_ev.get(p, ()))
                        cur_ph = rec[3]
                    tg = {}
                    for d in deps:
                        if d[0] == "E":
                            key = ("E", d[1]); v = val[d[1]][d[2]]; sem = esem[d[1]]
                        else:
                            key = ("D", d[1]); v = d[2]; sem = dsem[d[1]]
                        if tg.get(key, (None, 0))[1] < v:
                            tg[key] = (sem, v)
                    for key, (sem, v) in tg.items():
                        if waited.get(key, 0) >= v:
                            continue
                        eng.wait_ge(sem, v)
                        waited[key] = v
                        self.nwaits[ename] = self.nwaits.get(ename, 0) + 1
                    ins = rec[0](eng)
                    if rec[2] is not None:
                        ins.then_inc(dsem[rec[2]], 16)
                    elif i in need[ename]:
                        ins.then_inc(esem[ename], 1)
                if ename == "sp":
                    for d in final_waits:
                        eng.wait_ge(dsem[d[1]], d[2])

            block.tensor(lambda e: run("pe", e))
            block.vector(lambda e: run("dve", e))
            block.scalar(lambda e: run("act", e))
            block.gpsimd(lambda e: run("pool", e))
            block.sync(lambda e: run("sp", e))


def interleave(gens):
    gens = [g for g in gens if g is not None]
    while gens:
        nxt = []
        for g in gens:
            try:
                next(g)
                nxt.append(g)
            except StopIteration:
                pass
            yield
        gens = nxt


class Tl:
    __slots__ = ("t", "r")

    def __init__(self, t, name):
        self.t = t
        self.r = Res(name)

    def __getitem__(self, k):
        return self.t[k]


def build_nc(dbg=False, depth_run=DEPTH, reorder=True, only=None):
    nc = bass.Bass("TRN2", target_bir_lowering=False)
    S = Sched(nc)

    def din(name, shape, dt=F32):
        return nc.dram_tensor(name, list(shape), dt, kind="ExternalInput").ap()

    def dscr(name, shape, dt):
        return nc.dram_tensor(name, list(shape), dt, kind="ExternalOutput" if dbg else "Internal").ap()

    XIN = din("xin", [TOK, D])
    CVEC = din("cvec", [2, D])
    WMOD = din("w_mod", [DEPTH, D, 6 * D])
    BMOD = din("b_mod", [DEPTH, 6 * D])
    WIN = din("w_in", [DEPTH, D, 3072])
    ROPE = din("rope", [4, 128, TOK])
    CONVW = din("convw", [DEPTH, 128, 2, 3])
    SINK = din("sink", [DEPTH, 6])
    NAB = din("nab", [DEPTH, 5, 128, 6, 640])
    AMASK = din("amask", [128, 2, 384])
    WOUT = din("w_out", [DEPTH, D, D])
    LN1G = din("ln1_g", [DEPTH, D]); LN1B = din("ln1_b", [DEPTH, D])
    LN2G = din("ln2_g", [DEPTH, D]); LN2B = din("ln2_b", [DEPTH, D])
    WR = din("w_router", [DEPTH, D, NE])
    WG = din("w_gate", [DEPTH, NE, D, 512]); WU = din("w_up", [DEPTH, NE, D, 512])
    WD = din("w_down", [DEPTH, NE, 512, D])
    Y = nc.dram_tensor("y", [8192, D], F32, kind="ExternalOutput").ap()

    MODROW = dscr("modrow", [2, 6 * D], F32)
    FM = dscr("fm", [128, 14, TOK], BF16)
    VV = dscr("vv", [TOK, 8, 65], BF16)
    XMID = dscr("xmid", [TOK, D], F32)
    H2T = dscr("h2t", [128, 8, TOK], BF16)
    XCUR = dscr("xcur", [TOK, D], F32)
    XH2 = dscr("xh2", [TOK, RW], BF16)
    XE = [dscr("xe%d" % e, [1024, RW], BF16) for e in range(NE)]
    FFN = dscr("ffn", [TOK, D], F32)

    SB_LO = 16512
    SB_HI = 229344
    st = {"pers": SB_LO, "ph": None}

    def _alloc(name, shape, dt, key):
        nb = int(np.prod(shape[1:])) * (2 if dt == BF16 else 4)
        nb = (nb + 31) // 32 * 32
        off = st[key]
        assert off + nb <= SB_HI, (name, off, nb)
        st[key] = off + nb
        return Tl(nc.alloc_sbuf_tensor_at(name, list(shape), dt, offset=off), name)

    def pers(name, shape, dt=F32):
        return _alloc(name, shape, dt, "pers")

    cnt = [0]

    def ph(name, shape, dt=F32):
        cnt[0] += 1
        return _alloc("%s_%d" % (name, cnt[0]), shape, dt, "ph")

    def new_phase():
        S.barrier()
        st["ph"] = st["pers_end"]

    pbig = Tl(nc.alloc_psum_tensor("pbig", [128, 6, 512], F32), "pbig")
    ptr = [Tl(nc.alloc_psum_tensor("ptr%d" % i, [128, 8, 128], BF16), "ptr%d" % i) for i in range(2)]
    pbr = [Res("pb%d" % i) for i in range(6)]

    ident = pers("ident", [128, 128], BF16)
    identf = pers("identf", [128, 128], F32)
    onesf = pers("onesf", [128, 128], F32)
    aff = pers("aff", [128, NT, NE], F32)
    wsel = pers("wsel", [128, NT, NE], F32)
    modT = pers("modT", [128, 48], F32)
    modcT = pers("modcT", [128, 48], F32)
    esink = pers("esink", [128, 6], F32)
    convw = pers("convw", [128, 2, 3], F32)
    amask = pers("amask", [128, 2, 384], BF16)
    eps_t = pers("eps", [128, 1], F32)
    ltri = pers("ltri", [128, 128], F32)
    idxT = pers("idxT", [128, NE, 64], I32)
    acc = pers("acc", [128, 2, D], F32)
    st["pers_end"] = st["pers"]
    st["ph"] = st["pers_end"]

    _breg = {}

    def breg(e):
        if "r" not in _breg:
            _breg["r"] = e.to_reg(1023)
        return _breg["r"]

    def rs(lst):
        return [x.r if isinstance(x, Tl) else x for x in lst]

    def fsz(ap):
        try:
            return float(ap.free_size())
        except Exception:
            return 512.0

    def mm(out, lhsT, rhs, start, stop, reads, writes, tr=False):
        if tr:
            S.op("pe", lambda e: e.matmul(out, lhsT=lhsT, rhs=rhs, is_transpose=True),
                 reads=rs(reads), writes=rs(writes), pe_acc=True, cost=0.08)
        else:
            c = max(fsz(rhs), 64.0) / 2000.0 * (4.0 if lhsT.dtype == F32 else 1.0) + 0.03
            S.op("pe", lambda e: e.matmul(out, lhsT=lhsT, rhs=rhs, start=start, stop=stop),
                 reads=rs(reads), writes=rs(writes), pe_acc=True, cost=c)

    def act(out, in_, func, reads, writes, bias=0.0, scale=1.0, accum=None):
        if accum is None:
            S.op("act", lambda e: e.activation(out=out, in_=in_, func=func, bias=bias, scale=scale),
                 reads=rs(reads), writes=rs(writes), cost=0.2 + fsz(out) / 1300.0)
        else:
            S.op("act", lambda e: e.activation(out=out, in_=in_, func=func, bias=bias, scale=scale,
                                               accum_out=accum), reads=rs(reads), writes=rs(writes))

    def vcost(eng, ap):
        return (0.1 + fsz(ap) / 900.0) if eng == "dve" else (0.2 + fsz(ap) / 450.0)

    def tt(out, in0, in1, op, reads, writes, eng="dve"):
        S.op(eng, lambda e: e.tensor_tensor(out=out, in0=in0, in1=in1, op=op), reads=rs(reads), writes=rs(writes),
             cost=vcost(eng, out))

    def ts(out, in0, s1, s2, op0, op1, reads, writes, eng="dve"):
        if op1 is None:
            S.op(eng, lambda e: e.tensor_scalar(out=out, in0=in0, scalar1=s1, scalar2=None, op0=op0),
                 reads=rs(reads), writes=rs(writes), cost=vcost(eng, out))
        else:
            S.op(eng, lambda e: e.tensor_scalar(out=out, in0=in0, scalar1=s1, scalar2=s2, op0=op0, op1=op1),
                 reads=rs(reads), writes=rs(writes), cost=vcost(eng, out))

    def stt(out, in0, scalar, in1, op0, op1, reads, writes):
        S.op("dve", lambda e: e.scalar_tensor_tensor(out=out, in0=in0, scalar=scalar, in1=in1, op0=op0, op1=op1),
             reads=rs(reads), writes=rs(writes), cost=vcost("dve", out))

    def cp(out, in_, reads, writes, eng="dve"):
        S.op(eng, lambda e: e.tensor_copy(out=out, in_=in_), reads=rs(reads), writes=rs(writes), cost=vcost(eng, out))

    def recip(out, in_, reads, writes):
        S.op("dve", lambda e: e.reciprocal(out=out, in_=in_), reads=rs(reads), writes=rs(writes))

    def reduce(out, in_, op, reads, writes, negate=False):
        S.op("dve", lambda e: e.tensor_reduce(out=out, in_=in_, axis=AX.X, op=op, negate=negate),
             reads=rs(reads), writes=rs(writes), cost=vcost("dve", in_))

    def memset(eng, ap, val, writes):
        S.op(eng, lambda e: e.memset(ap, val), writes=rs(writes), cost=vcost(eng, ap))

    def single(out, in_, scalar, op, reads, writes):
        S.op("dve", lambda e: e.tensor_single_scalar(out=out, in_=in_, scalar=scalar, op=op),
             reads=rs(reads), writes=rs(writes))

    def scan(out, d0, d1, reads, writes):
        S.op("dve", lambda e: e.tensor_tensor_scan(out=out, data0=d0, data1=d1, initial=0.0, op0=ALU.add, op1=ALU.add),
             reads=rs(reads), writes=rs(writes))

    def iota_tail(ap, base, writes):
        S.op("pool", lambda e: e.iota(ap, pattern=[[0, 1]], base=base, channel_multiplier=1), writes=rs(writes))

    def dma(q, out, in_, reads, writes, owner, slow=False, group=None):
        try:
            nbytes = float(out.nbytes())
        except Exception:
            nbytes = 1.0e5
        lat = 2.5 + nbytes / 1.5e5
        cost = 0.1 if q == "sp" else 1.0
        ow = owner.r if isinstance(owner, Tl) else owner
        if group is not None:
            return S.dma(q, lambda e: e.dma_start(out=out, in_=in_), reads=rs(reads), writes=rs(writes),
                         owner=ow, group=group, cost=cost, lat=lat)
        if slow:
            return S.dma(q, lambda e: e.dma_start(out=out, in_=in_, allow_slow_non_contiguous=True),
                         reads=rs(reads), writes=rs(writes), owner=ow, cost=cost, lat=lat + 3.0)
        return S.dma(q, lambda e: e.dma_start(out=out, in_=in_),
                     reads=rs(reads), writes=rs(writes), owner=ow, cost=cost, lat=lat)

    def ln_stats(src_ap, src_res, stats, mv, rstd, nmr=None):
        for h in range(2):
            S.op("dve", lambda e, h=h: e.bn_stats(out=stats[:, h, :], in_=src_ap[:, h * 512:(h + 1) * 512]),
                 reads=rs([src_res]), writes=rs([stats]))
        S.op("dve", lambda e: e.bn_aggr(out=mv[:], in_=stats[:].rearrange("p a b -> p (a b)")),
             reads=rs([stats]), writes=rs([mv]))
        act(rstd[:], mv[:, 1:2], AF.Ln, [mv, eps_t], [rstd], bias=eps_t[:, 0:1])
        act(rstd[:], rstd[:], AF.Exp, [rstd], [rstd], scale=-0.5)
        if nmr is not None:
            stt(nmr[:], mv[:, 0:1], -1.0, rstd[:], ALU.mult, ALU.mult, [mv, rstd], [nmr])

    S.op("pool", lambda e: e.iota(identf[:], pattern=[[1, 128]], base=0, channel_multiplier=-1,
                                  allow_small_or_imprecise_dtypes=True), writes=rs([identf]))
    S.op("dve", lambda e: e.tensor_single_scalar(out=ident[:], in_=identf[:], scalar=0.0, op=ALU.is_equal),
         reads=rs([identf]), writes=rs([ident]))
    S.op("dve", lambda e: e.memset(onesf[:], 1.0), writes=rs([onesf]))
    S.op("dve", lambda e: e.tensor_single_scalar(out=ltri[:], in_=identf[:], scalar=0.0, op=ALU.is_gt),
         reads=rs([identf]), writes=rs([ltri]))
    S.op("dve", lambda e: e.memset(eps_t[:], 1e-6), writes=rs([eps_t]))
    dma("pool", amask[:], AMASK, [S.dres("amask")], [amask], amask)

    fm_res = [S.dres("fm", t) for t in range(NT)]
    vv_res = [S.dres("vv", t) for t in range(NT)]
    xmid_res = [S.dres("xmid", t) for t in range(NT)]
    h2t_res = [S.dres("h2t", t) for t in range(NT)]
    xcur_res = [S.dres("xcur", t) for t in range(NT)]
    xh2_res = [S.dres("xh2", t) for t in range(NT)]
    y_res = [S.dres("y", t) for t in range(64)]
    final = []

    for l in range(depth_run):
        last = l == DEPTH - 1
        T0 = 2 if last else 0
        new_phase()
        cT = ph("cT", [128, 8, 2])
        bm = ph("bm", [2, 6 * D])
        mrow = ph("mrow", [2, 6 * D])
        wm = [ph("wm%d" % i, [128, 8, 512]) for i in range(2)]
        for m_ in range(2):
            dma("sp", cT[:, :, m_], CVEC[m_].rearrange("(k p) -> p k", p=128), [S.dres("cvec")], [cT], cT, slow=True)
        dma("sp", bm[:], BMOD[l].partition_broadcast(2), [S.dres("bmod")], [bm], bm)
        dma("sp", esink[:], SINK[l].partition_broadcast(128), [S.dres("sink")], [esink], esink)
        dma("sp", convw[:], CONVW[l], [S.dres("convw")], [convw], convw)
        act(cT[:], cT[:], AF.Silu, [cT], [cT])
        act(esink[:], esink[:], AF.Exp, [esink], [esink])
        wmv = WMOD[l].rearrange("(k p) n -> p k n", p=128)
        for cc in range(12):
            w_ = wm[cc % 2]
            dma("sp", w_[:], wmv[:, :, cc * 512:(cc + 1) * 512], [S.dres("wmod")], [w_], w_)
            pb = pbig[0:2, cc % 2, :]
            for k in range(8):
                mm(pb, cT[:, k, :], w_[:, k, :], k == 0, k == 7, [cT, w_], [pbr[cc % 2]])
            tt(mrow[:, cc * 512:(cc + 1) * 512], pb, bm[:, cc * 512:(cc + 1) * 512], ALU.add,
               [pbr[cc % 2], bm], [mrow])
        dma("sp", MODROW, mrow[:], [mrow], [S.dres("modrow")], mrow)
        dma("sp", modT[:], MODROW[0].rearrange("(j p) -> p j", p=128), [S.dres("modrow")], [modT], modT, slow=True)
        dma("sp", modcT[:], MODROW[1].rearrange("(j p) -> p j", p=128), [S.dres("modrow")], [modcT], modcT, slow=True)
        for m_ in (modT, modcT):
            ts(m_[:, 8:16], m_[:, 8:16], 1.0, None, ALU.add, None, [m_], [m_])
            ts(m_[:, 32:40], m_[:, 32:40], 1.0, None, ALU.add, None, [m_], [m_])

        new_phase()
        win = ph("win", [128, 8, 3072], BF16)
        wiv = WIN[l].rearrange("(k p) n -> p k n", p=128)
        for k in range(8):
            dma("pool", win[:, k, :], wiv[:, k, :], [S.dres("win")], [win], win)
        xt = [ph("xt%d" % i, [128, D]) for i in range(2)]
        xh = [ph("xh%d" % i, [128, D], BF16) for i in range(2)]
        hT = [ph("hT%d" % i, [128, 8, 512], BF16) for i in range(2)]
        fmo = [ph("fmo%d" % i, [128, 14, 512], BF16) for i in range(2)]
        vo = [ph("vo%d" % i, [128, 4, 8, 65], BF16) for i in range(2)]
        rp = [ph("rp%d" % i, [128, 4, 512]) for i in range(2)]
        tmp = [ph("tmp%d" % i, [128, 512]) for i in range(3)]
        stats = ph("stats", [128, 2, 6]); mv = ph("mv", [128, 2]); rstd = ph("rstd", [128, 1])
        for v_ in vo:
            S.op("pool", lambda e, v_=v_: e.memset(v_[:], 1.0), writes=rs([v_]))
        src = XIN if l == 0 else XCUR
        src_res = (lambda t: S.dres("xin", t)) if l == 0 else (lambda t: xcur_res[t])
        groups = [(0, 2)] + [(2 + 4 * i, 4) for i in range(16)]
        bank = [0]

        def nextbank():
            b = bank[0]
            bank[0] = (b + 1) % 6
            return b

        def gen_L(gi, t0, nt):
            N = nt * 128
            sl = gi % 2
            mT = modcT if gi == 0 else modT
            rpt = rp[sl]
            dma("sp", rpt[:, :, :N], ROPE[:, :, t0 * 128:t0 * 128 + N].rearrange("a p n -> p a n"),
                [S.dres("rope")], [rpt], rpt)
            for s in range(nt):
                tl = t0 + s
                x_ = xt[tl % 2]; xh_ = xh[tl % 2]; pt_ = ptr[tl % 2]
                dma("sp", x_[:], src[tl * 128:(tl + 1) * 128, :], [src_res(tl)], [x_], x_)
                ln_stats(x_, x_, stats, mv, rstd)
                yield
                ts(xh_[:], x_[:], mv[:, 0:1], rstd[:, 0:1], ALU.subtract, ALU.mult, [x_, mv, rstd], [xh_])
                for k in range(8):
                    mm(pt_[:, k, :], xh_[:, k * 128:(k + 1) * 128], ident[:], True, True, [xh_, ident], [pt_], tr=True)
                yield
                for k in range(8):
                    act(hT[sl][:, k, s * 128:(s + 1) * 128], pt_[:, k, :], AF.Identity, [pt_, mT], [hT[sl]],
                        bias=mT[:, k:k + 1], scale=mT[:, 8 + k:9 + k])
                    if k % 4 == 3:
                        yield

        def gen_P(gi, t0, nt):
            N = nt * 128
            sl = gi % 2
            rpt = rp[sl]
            h_ = hT[sl]; fo = fmo[sl]

            def proj(ch):
                b = nextbank()
                for k in range(8):
                    mm(pbig[:, b, :N], win[:, k, ch * 128:(ch + 1) * 128], h_[:, k, :N], k == 0, k == 7,
                       [win, h_], [pbr[b]])
                return b

            def rope(chq, chs, tq, tsn, dst):
                b1 = proj(chq); b2 = proj(chs)
                tt(tmp[0][:, :N], pbig[:, b1, :N], rpt[:, tq, :N], ALU.mult, [pbr[b1], rpt], [tmp[0]])
                tt(tmp[1][:, :N], pbig[:, b2, :N], rpt[:, tsn, :N], ALU.mult, [pbr[b2], rpt], [tmp[1]])
                tt(fo[:, dst, :N], tmp[0][:, :N], tmp[1][:, :N], ALU.add, [tmp[0], tmp[1]], [fo], eng="pool")

            for c_ in range(3):
                rope(c_, 3 + c_, 0, 1, c_)
                yield
            rope(6, 7, 2, 3, 6)
            yield
            for c_ in range(2):
                b1 = proj(8 + c_)
                act(tmp[2][:, :N], pbig[:, b1, :N], AF.Identity, [pbr[b1]], [tmp[2]])
                b2 = proj(12 + c_)
                tt(fo[:, 10 + c_, :N], pbig[:, b2, :N], tmp[2][:, :N], ALU.mult, [pbr[b2], tmp[2]], [fo])
                yield
                b3 = proj(10 + c_)
                act(fo[:, 12 + c_, :N], pbig[:, b3, :N], AF.Identity, [pbr[b3]], [fo])
                yield
            for c_ in range(3):
                b1 = proj(14 + c_)
                act(fo[:, 3 + c_, :N], pbig[:, b1, :N], AF.Identity, [pbr[b1]], [fo], scale=0.125)
                yield
                b2 = proj(17 + c_)
                cp(fo[:, 7 + c_, :N], pbig[:, b2, :N], [pbr[b2]], [fo])
                yield
            for s in range(nt):
                b = nextbank()
                for k in range(8):
                    mm(pbig[:, b, :], h_[:, k, s * 128:(s + 1) * 128], win[:, k, 2560:3072], k == 0, k == 7,
                       [win, h_], [pbr[b]])
                act(vo[sl][:, s, :, 0:64], pbig[:, b, :].rearrange("p (h d) -> p h d", h=8), AF.Identity,
                    [pbr[b]], [vo[sl]])
                yield
            dma("sp", FM[:, :, t0 * 128:t0 * 128 + N], fo[:, :, :N], [fo], fm_res[t0:t0 + nt], fo)
            dma("sp", VV[t0 * 128:t0 * 128 + N].rearrange("(s p) h d -> p s h d", p=128), vo[sl][:, :nt],
                [vo[sl]], vv_res[t0:t0 + nt], vo[sl])
            yield

        prevP = None
        for gi, (t0, nt) in enumerate(groups):
            for _ in interleave([gen_L(gi, t0, nt), prevP]):
                pass
            prevP = gen_P(gi, t0, nt)
        for _ in prevP:
            pass

        new_phase()
        wout = ph("wout", [128, 8, D], BF16)
        wov = WOUT[l].rearrange("(k p) n -> p k n", p=128)
        for k in range(0, 8, 2):
            dma("pool", wout[:, k:k + 2, :], wov[:, k:k + 2, :], [S.dres("wout")], [wout], wout)
        wr = ph("wr", [128, 8, NE], BF16)
        dma("pool", wr[:], WR[l].rearrange("(k p) n -> p k n", p=128), [S.dres("wr")], [wr], wr)
        nabi = ph("nabi", [128, 6, 640], BF16)
        nabe = ph("nabe", [128, 6, 640], BF16)
        dma("pool", nabi[:], NAB[l, 0], [S.dres("nab")], [nabi], nabi)
        kctx = ph("kctx", [128, 4, 256], BF16)
        vctx = ph("vctx", [128, 2, 8, 65], BF16)
        dma("sp", kctx[:], FM[:, 6:10, 0:256], fm_res[0:2], [kctx], kctx)
        dma("sp", vctx[:], VV[0:256].rearrange("(s p) h d -> p s h d", p=128), vv_res[0:2], [vctx], vctx)
        g1b = ph("g1b", [128, D]); cg1b = ph("cg1b", [128, D])
        l1g = ph("l1g", [128, D]); l1b = ph("l1b", [128, D])
        dma("sp", g1b[:], MODROW[0, 2048:3072].partition_broadcast(128), [S.dres("modrow")], [g1b], g1b)
        dma("sp", cg1b[:], MODROW[1, 2048:3072].partition_broadcast(128), [S.dres("modrow")], [cg1b], cg1b)
        dma("sp", l1g[:], LN1G[l].partition_broadcast(128), [S.dres("ln1g")], [l1g], l1g)
        dma("sp", l1b[:], LN1B[l].partition_broadcast(128), [S.dres("ln1b")], [l1b], l1b)
        qw = [ph("qw%d" % i, [128, 6, 128], BF16) for i in range(2)]
        kw = [ph("kw%d" % i, [128, 4, 640], BF16) for i in range(2)]
        vw = [ph("vw%d" % i, [128, 5, 8, 65], BF16) for i in range(2)]
        uw = [ph("uw%d" % i, [128, 2, 130], BF16) for i in range(2)]
        bbw = [ph("bbw%d" % i, [128, 2, 128], BF16) for i in range(2)]
        xa = [ph("xa%d" % i, [128, D]) for i in range(2)]
        pta = [ph("pta%d" % i, [128, 5, 384], BF16) for i in range(2)]
        ptn = [ph("ptn%d" % i, [128, 896], BF16) for i in range(2)]
        sfn = [ph("sfn%d" % i, [128, 640]) for i in range(2)]
        mixc = ph("mixc", [128, 768], BF16)
        mixT = ph("mixT", [128, 8, 128], BF16)
        ctmp = ph("ctmp", [128, 128])
        den = ph("den", [128, 6]);
        t1 = ph("t1", [128, D]); y1 = ph("y1", [128, D]); xm = [ph("xm%d" % i, [128, D]) for i in range(2)]
        xh2 = ph("xh2", [128, RW], BF16)
        h2o = [ph("h2o%d" % i, [128, 8, 128], BF16) for i in range(2)]
        stats = ph("stats", [128, 2, 6]); mv = ph("mv", [128, 2]); rstd = ph("rstd", [128, 1]); nmr = ph("nmr", [128, 1])
        rmx = ph("rmx", [128, 1]); rsum = ph("rsum", [128, 1]); rexp = ph("rexp", [128, NE])

        mixT2 = [mixT, ph("mixTb", [128, 8, 128], BF16)]

        nabe2 = [nabe, ph("nabe_b", [128, 6, 640], BF16)]

        def att_loads(T):
            sl = T % 2
            is_ctx = T < 2
            i = T - 2
            q_ = qw[sl]; k_ = kw[sl]; v_ = vw[sl]; u_ = uw[sl]; bb_ = bbw[sl]
            dma("sp", q_[:], FM[:, 0:6, T * 128:(T + 1) * 128], [fm_res[T]], [q_], q_)
            if not is_ctx:
                base = min(max(i - 2, 0), 59) + 2
                dma("sp", k_[:], FM[:, 6:10, base * 128:(base + 5) * 128], fm_res[base:base + 5], [k_], k_)
                dma("sp", v_[:], VV[base * 128:(base + 5) * 128].rearrange("(s p) h d -> p s h d", p=128),
                    vv_res[base:base + 5], [v_], v_)
                var = 0 if 2 <= i <= 61 else (1 + i if i < 2 else i - 59)
                if var != 0:
                    dma("pool", nabe2[sl][:], NAB[l, var], [S.dres("nab")], [nabe2[sl]], nabe2[sl])
            lo_pad = T in (0, 2); hi_pad = T in (1, NT - 1)
            if lo_pad or hi_pad:
                memset("pool", u_[:], 0.0, [u_])
            c0 = T * 128 - (0 if lo_pad else 1); c1 = (T + 1) * 128 + (0 if hi_pad else 1)
            o0 = 1 if lo_pad else 0
            fr = fm_res[max(T - 1, 0):min(T + 2, NT)]
            dma("sp", u_[:, :, o0:o0 + (c1 - c0)], FM[:, 10:12, c0:c1], fr, [u_], u_)
            dma("sp", bb_[:], FM[:, 12:14, T * 128:(T + 1) * 128], [fm_res[T]], [bb_], bb_)

        def gen_att(T):
            sl = T % 2
            is_ctx = T < 2
            i = T - 2
            q_ = qw[sl]; k_ = kw[sl]; v_ = vw[sl]; u_ = uw[sl]; bb_ = bbw[sl]
            mT_ = mixT2[sl]
            nb = nabi
            base = 0
            if not is_ctx:
                base = min(max(i - 2, 0), 59) + 2
                var = 0 if 2 <= i <= 61 else (1 + i if i < 2 else i - 59)
                if var != 0:
                    nb = nabe2[sl]
            yield
            if is_ctx:
                akeys = [(("c", 0), None), (("c", 1), None)]
                nkeys = [("c", 0), ("c", 1)]
            else:
                akeys = []
                if i > 0:
                    akeys.append((("w", T - 1 - base), 0))
                akeys.append((("w", T - base), None))
                if i < 63:
                    akeys.append((("w", T + 1 - base), 1))
                akeys += [(("c", 0), None), (("c", 1), None)]
                nkeys = [("w", j) for j in range(5)] + [("c", 0), ("c", 1)]

            def kap(kt, ch, p0):
                if kt[0] == "c":
                    return kctx[p0:p0 + 64, ch, kt[1] * 128:(kt[1] + 1) * 128], kctx
                return k_[p0:p0 + 64, ch, kt[1] * 128:(kt[1] + 1) * 128], k_

            def vap(kt, head):
                if kt[0] == "c":
                    return vctx[:, kt[1], head, :], vctx
                return v_[:, kt[1], head, :], v_

            def gen_A():
                for g in range(2):
                    p0 = g * 64
                    pa_ = pta[g]
                    for ki, (kt, mk) in enumerate(akeys):
                        ka_, kr = kap(kt, 0, p0)
                        if mk is not None:
                            mm(pbig[:, 0, 0:384], ident[:], amask[:, mk, :], True, False, [ident, amask], [pbr[0]])
                        mm(pbig[:, 0, 0:384], ka_, q_[p0:p0 + 64, 0:3, :].rearrange("p a b -> p (a b)"), mk is None, True,
                           [kr, q_], [pbr[0]])
                        act(pa_[:, ki, :], pbig[:, 0, 0:384], AF.Exp, [pbr[0]], [pa_])
                        yield
                    po = pbig[:, 2, 0:195].rearrange("p (c d) -> p c d", c=3)
                    for c_ in range(3):
                        for ki, (kt, mk) in enumerate(akeys):
                            va_, vr = vap(kt, g)
                            mm(po[:, c_, :], pa_[:, ki, c_ * 128:(c_ + 1) * 128], va_, ki == 0, ki == len(akeys) - 1,
                               [pa_, vr], [pbr[2]])
                        yield
                    tt(den[:, 0:3], po[:, :, 64], esink[:, 3 * g:3 * g + 3], ALU.add, [pbr[2], esink], [den])
                    recip(den[:, 0:3], den[:, 0:3], [den], [den])
                    tt(mixc[:, g * 192:(g + 1) * 192].rearrange("p (c d) -> p c d", c=3), po[:, :, 0:64],
                       den[:, 0:3].unsqueeze(2).to_broadcast([128, 3, 64]), ALU.mult, [pbr[2], den], [mixcA])
                    yield

            def gen_N():
                po2 = pbig[:, 3, 0:390].rearrange("p (c d) -> p c d", c=6)
                nk = len(nkeys)
                for h in range(6):
                    ch = h // 2; p0 = (h % 2) * 64
                    pn_ = ptn[h % 2]; sf_ = sfn[h % 2]
                    ps2 = pbig[:, 4:6, :].rearrange("p a b -> p (a b)")
                    if is_ctx:
                        for j, kt in enumerate(nkeys):
                            ka_, kr = kap(kt, 1 + ch, p0)
                            mm(ps2[:, j * 128:(j + 1) * 128], ka_, q_[p0:p0 + 64, 3 + ch, :], True, True,
                               [kr, q_], [pbr[4], pbr[5]])
                        act(pn_[:, 0:256], ps2[:, 0:256], AF.Exp, [pbr[4], pbr[5]], [pn_])
                    else:
                        for j in (5, 6):
                            ka_, kr = kap(nkeys[j], 1 + ch, p0)
                            mm(ps2[:, j * 128:(j + 1) * 128], ka_, q_[p0:p0 + 64, 3 + ch, :], True, True,
                               [kr, q_], [pbr[4], pbr[5]])
                        mm(ps2[:, 512:640], ident[:], nb[:, h, 512:640], True, False, [ident, nb], [pbr[4], pbr[5]])
                        mm(ps2[:, 0:512], ident[:], nb[:, h, 0:512], True, False, [ident, nb], [pbr[4], pbr[5]])
                        for j in range(5):
                            ka_, kr = kap(nkeys[j], 1 + ch, p0)
                            mm(ps2[:, j * 128:(j + 1) * 128], ka_, q_[p0:p0 + 64, 3 + ch, :], False, j in (3, 4),
                               [kr, q_], [pbr[4], pbr[5]])
                        act(pn_[:, 0:896], ps2[:, 0:896], AF.Exp, [pbr[4], pbr[5]], [pn_])
                    yield
                    for j, kt in enumerate(nkeys):
                        va_, vr = vap(kt, 2 + h)
                        mm(po2[:, h, :], pn_[:, j * 128:(j + 1) * 128], va_, j == 0, j == nk - 1, [pn_, vr], [pbr[3]])
                    yield
                recip(den2[:], po2[:, :, 64], [pbr[3]], [den2])
                tt(mixc[:, 384:768].rearrange("p (c d) -> p c d", c=6), po2[:, :, 0:64],
                   den2[:].unsqueeze(2).to_broadcast([128, 6, 64]), ALU.mult, [pbr[3], den2], [mixcN])
                yield

            def gen_B():
                for c_ in range(2):
                    ts(ctmp[:], u_[:, c_, 0:128], convw[:, c_, 0:1], None, ALU.mult, None, [u_, convw], [ctmp])
                    stt(ctmp[:], u_[:, c_, 1:129], convw[:, c_, 1:2], ctmp[:], ALU.mult, ALU.add, [u_, convw, ctmp], [ctmp])
                    stt(ctmp[:], u_[:, c_, 2:130], convw[:, c_, 2:3], ctmp[:], ALU.mult, ALU.add, [u_, convw, ctmp], [ctmp])
                    tt(mT_[:, 3 + c_, :], ctmp[:], bb_[:, c_, :], ALU.mult, [ctmp, bb_], [mT_])
                    yield

            for _ in interleave([gen_A(), gen_N(), gen_B()]):
                yield
            pt_ = ptr[0]
            for c_ in range(6):
                mm(pt_[:, c_, :], mixc[:, c_ * 128:(c_ + 1) * 128], ident[:], True, True, [mixcA, mixcN, ident], [pt_], tr=True)
            cp(mT_[:, 0:3, :], pt_[:, 0:3, :], [pt_], [mT_])
            act(mT_[:, 5:8, :], pt_[:, 3:6, :], AF.Identity, [pt_], [mT_])
            yield

        def gen_epi(T):
            sl = T % 2
            is_ctx = T < 2
            x_ = xa[sl]; mT_ = mixT2[sl]
            dma("sp", x_[:], src[T * 128:(T + 1) * 128, :], [src_res(T)], [x_], x_)
            gb = cg1b if is_ctx else g1b
            for hf in range(2):
                for k in range(8):
                    mm(pbig[:, 1, :], mT_[:, k, :], wout[:, k, hf * 512:(hf + 1) * 512], k == 0, k == 7,
                       [mT_, wout], [pbr[1]])
                tt(t1[:, hf * 512:(hf + 1) * 512], pbig[:, 1, :], gb[:, hf * 512:(hf + 1) * 512], ALU.mult,
                   [pbr[1], gb], [t1])
                yield
            stt(y1[:], x_[:], ALPHA, t1[:], ALU.mult, ALU.add, [x_, t1], [y1])
            yield
            ln_stats(y1, y1, stats, mv, rstd, nmr)
            yield
            xm_ = xm[sl]
            act(t1[:], y1[:], AF.Identity, [y1, rstd, nmr], [t1], bias=nmr[:, 0:1], scale=rstd[:, 0:1])
            yield
            tt(t1[:], t1[:], l1g[:], ALU.mult, [t1, l1g], [t1], eng="pool")
            yield
            tt(xm_[:], t1[:], l1b[:], ALU.add, [t1, l1b], [xm_], eng="pool")
            dma("sp", XMID[T * 128:(T + 1) * 128, :], xm_[:], [xm_], [xmid_res[T]], xm_)
            yield
            ln_stats(xm_, xm_, stats, mv, rstd)
            yield
            ts(xh2[:, 0:D], xm_[:], mv[:, 0:1], rstd[:, 0:1], ALU.subtract, ALU.mult, [xm_, mv, rstd], [xh2])
            yield
            pt2 = ptr[1]
            for k in range(8):
                mm(pt2[:, k, :], xh2[:, k * 128:(k + 1) * 128], ident[:], True, True, [xh2, ident], [pt2], tr=True)
            mT = modcT if is_ctx else modT
            h2_ = h2o[sl]
            for k in range(8):
                act(h2_[:, k, :], pt2[:, k, :], AF.Identity, [pt2, mT], [h2_],
                    bias=mT[:, 24 + k:25 + k], scale=mT[:, 32 + k:33 + k])
                if k % 4 == 3:
                    yield
            if is_ctx:
                dma("sp", H2T[:, :, T * 128:(T + 1) * 128], h2_[:], [h2_], [h2t_res[T]], h2_)
            pr = pbig[:, 1, 0:NE]
            for k in range(8):
                mm(pr, h2_[:, k, :], wr[:, k, :], k == 0, k == 7, [h2_, wr], [pbr[1]])
            reduce(rmx[:], pr, ALU.max, [pbr[1]], [rmx], negate=True)
            act(rexp[:], pr, AF.Exp, [pbr[1], rmx], [rexp], bias=rmx[:, 0:1])
            yield
            reduce(rsum[:], rexp[:], ALU.add, [rexp], [rsum])
            recip(rsum[:], rsum[:], [rsum], [rsum])
            ts(aff[:, T, :], rexp[:], rsum[:, 0:1], None, ALU.mult, None, [rexp, rsum], [aff])
            if not is_ctx:
                ts(xh2[:, 1026:1042], rexp[:], rsum[:, 0:1], None, ALU.mult, None, [rexp, rsum], [xh2])
                stt(xh2[:, 1042:1058], rexp[:], rsum[:, 0:1], xh2[:, 1026:1042], ALU.mult, ALU.subtract,
                    [rexp, rsum, xh2], [xh2])
                iota_tail(xh2[:, 1024:1026].bitcast(I32), T * 128, [xh2])
                dma("sp", XH2[T * 128:(T + 1) * 128, :], xh2[:], [xh2], [xh2_res[T]], xh2)
            yield

        mixcA = Res("mixcA"); mixcN = Res("mixcN")
        den2 = ph("den2", [128, 6])
        prev = None
        att_loads(T0)
        for T in range(T0, NT):
            if T + 1 < NT:
                att_loads(T + 1)
            for _ in interleave([gen_att(T), prev]):
                pass
            prev = gen_epi(T)
        for _ in prev:
            pass

        new_phase()
        lo_t = ph("lo", [128, NE]); mid_t = ph("mid", [128, NE]); cntp = ph("cntp", [128, NE]); sel = ph("sel", [128, NE])
        cmp = ph("cmp", [128, NE, 64])
        incl = ph("incl", [128, NE, 64])
        zt = ph("zt", [128, 64])
        offs = ph("offs", [128, NE])
        zbig = ph("zbig", [128, 4096])
        memset("pool", zbig[:], 0.0, [zbig])
        memset("pool", zt[:], 0.0, [zt])
        for j in range(16):
            dma("sp", FFN[256 + j * 512:256 + (j + 1) * 512, :].rearrange("(p a) d -> p (a d)", p=128), zbig[:],
                [zbig], [S.dres("ffn")], zbig, group=("ffnz", l))
        sets = [(2, 64, 1024.0)] if last else [(0, 2, 32.0), (2, 64, 1024.0)]
        for (ta, tn, cap) in sets:
            av = aff[:, ta:ta + tn, :].rearrange("p t e -> p e t")
            memset("dve", lo_t[:], 0.0, [lo_t])
            for it in range(30):
                w_ = 0.5 ** (it + 1)
                ts(mid_t[:], lo_t[:], w_, None, ALU.add, None, [lo_t], [mid_t])
                tt(cmp[:, :, :tn], av, mid_t[:].unsqueeze(2).to_broadcast([128, NE, tn]), ALU.is_ge, [aff, mid_t], [cmp])
                reduce(cntp[:], cmp[:, :, :tn], ALU.add, [cmp], [cntp])
                mm(pbig[:, 0, 0:NE], onesf[:], cntp[:], True, True, [onesf, cntp], [pbr[0]])
                single(sel[:], pbig[:, 0, 0:NE], cap - 0.5, ALU.is_ge, [pbr[0]], [sel])
                stt(lo_t[:], sel[:], w_, lo_t[:], ALU.mult, ALU.add, [sel, lo_t], [lo_t])
            tt(cmp[:, :, :tn], av, lo_t[:].unsqueeze(2).to_broadcast([128, NE, tn]), ALU.is_ge, [aff, lo_t], [cmp])
            if dbg:
                LDBG = nc.dram_tensor("ldbg%d_%d" % (l, tn), [128, NE], F32, kind="ExternalOutput").ap()
                dma("sp", LDBG, lo_t[:], [lo_t], [S.dres("ldbg", tn)], lo_t)
                ADBG = nc.dram_tensor("adbg%d_%d" % (l, tn), [128, NT * NE], F32, kind="ExternalOutput").ap()
                dma("sp", ADBG, aff[:].rearrange("p a b -> p (a b)"), [aff], [S.dres("adbg", tn)], aff)
            if tn == 2:
                wv = wsel[:, ta:ta + tn, :].rearrange("p t e -> p e t")
                tt(wv, av, cmp[:, :, :tn], ALU.mult, [aff, cmp], [wsel])
                continue
            for ex in range(NE):
                scan(incl[:, ex, :], cmp[:, ex, :], zt[:], [cmp, zt], [incl])
            cp(cntp[:], incl[:, :, 63], [incl], [cntp])
            mm(pbig[:, 0, 0:NE], ltri[:], cntp[:], True, True, [ltri, cntp], [pbr[0]])
            cp(offs[:], pbig[:, 0, 0:NE], [pbr[0]], [offs])
            tt(incl[:], incl[:], cmp[:], ALU.subtract, [incl, cmp], [incl])
            tt(incl[:], incl[:], offs[:].unsqueeze(2).to_broadcast([128, NE, 64]), ALU.add, [incl, offs], [incl])
            stt(incl[:].rearrange("p a b -> p (a b)"), incl[:].rearrange("p a b -> p (a b)"), -1.0e6,
                cmp[:].rearrange("p a b -> p (a b)"), ALU.add, ALU.mult, [incl, cmp], [incl])
            ts(idxT[:], incl[:], 1.0e6, None, ALU.add, None, [incl], [idxT])
            if dbg:
                IDBG = nc.dram_tensor("idbg%d" % l, [128, NE * 64], I32, kind="ExternalOutput").ap()
                dma("sp", IDBG, idxT[:].rearrange("p a b -> p (a b)"), [idxT], [S.dres("idbg")], idxT)
                ODBG = nc.dram_tensor("odbg%d" % l, [128, NE], F32, kind="ExternalOutput").ap()
                dma("sp", ODBG, offs[:], [offs], [S.dres("odbg")], offs)
                CDBG = nc.dram_tensor("cdbg%d" % l, [128, NE * 64], F32, kind="ExternalOutput").ap()
                dma("sp", CDBG, cmp[:].rearrange("p a b -> p (a b)"), [cmp], [S.dres("cdbg")], cmp)

        new_phase()
        wgt = [ph("wg%d" % i, [128, 8, 512], BF16) for i in range(2)]
        wut = [ph("wu%d" % i, [128, 8, 512], BF16) for i in range(2)]
        wdt = [ph("wd%d" % i, [128, 4, D], BF16) for i in range(2)]
        tokc = [ph("tokc%d" % i, [128, 8, RW], BF16) for i in range(2)]
        xet = [ph("xet%d" % i, [128, RW], BF16) for i in range(2)]
        h2e = [ph("h2e%d" % i, [128, 8, 512], BF16) for i in range(2)]
        gT = [ph("gT%d" % i, [128, 4, 512], BF16) for i in range(2)]
        sa = [ph("sa%d" % i, [128, 512], BF16) for i in range(2)]
        yo = [ph("yo%d" % i, [128, D]) for i in range(2)]
        idxe = [ph("idxe%d" % i, [128, 1], I32) for i in range(8)]
        gate = ph("gate", [128, 8])
        rmx = ph("rmx", [128, 1]); rsum = ph("rsum", [128, 1]); rexp = ph("rexp", [128, NE])
        wr = ph("wr", [128, 8, NE], BF16)
        dma("pool", wr[:], WR[l].rearrange("(k p) n -> p k n", p=128), [S.dres("wr")], [wr], wr)
        h2g = ph("h2g", [128, 8, 256], BF16)
        accr = [Res("acc%d" % i) for i in range(2)]
        if not last:
            dma("sp", h2g[:], H2T[:, :, 0:256], h2t_res[0:2], [h2g], h2g)
        xe_res = [S.dres("xe", e) for e in range(NE)]
        tcnt = [0]

        def load_w(ex):
            sl = ex % 2
            dma("pool", wgt[sl][:], WG[l, ex].rearrange("(k p) f -> p k f", p=128), [S.dres("wg")], [wgt[sl]], wgt[sl])
            dma("pool", wut[sl][:], WU[l, ex].rearrange("(k p) f -> p k f", p=128), [S.dres("wu")], [wut[sl]], wut[sl])
            dma("pool", wdt[sl][:], WD[l, ex].rearrange("(k p) f -> p k f", p=128), [S.dres("wd")], [wdt[sl]], wdt[sl])

        disp_own = [[Res("disp%d_%d" % (e_, j_)) for j_ in range(2)] for e_ in range(NE)]

        def dispatch_gen(exs):
            for cg in range(8):
                tk = tokc[tcnt[0] % 2]
                tcnt[0] += 1
                dma("sp", tk[:], XH2[(2 + cg * 8) * 128:(2 + cg * 8 + 8) * 128, :].rearrange("(s p) d -> p s d", p=128),
                    xh2_res[2 + cg * 8:2 + cg * 8 + 8], [tk], tk)
                for s in range(8):
                    T = 2 + cg * 8 + s
                    for ex in exs:
                        S.dma("pool", lambda e, tk=tk, s=s, ex=ex, T=T: e.indirect_dma_start(
                            out=XE[ex][:, :], out_offset=bass.IndirectOffsetOnAxis(
                                ap=idxT[:].rearrange("p a b -> p (a b)")[:, ex * 64 + T - 2:ex * 64 + T - 1], axis=0),
                            in_=tk[:, s, :], in_offset=None, bounds_check=breg(e), oob_is_err=False),
                            reads=rs([tk, idxT]), writes=[xe_res[ex]], owner=disp_own[ex][cg % 2], group=("xe", l, ex),
                            cost=1.2, lat=4.0)
                yield

        def take(g, n):
            for _ in range(n):
                try:
                    next(g)
                except StopIteration:
                    return
                yield

        egroups = [[0, 1], [2, 3, 4, 5], [6, 7, 8, 9], [10, 11, 12, 13], [14, 15]]
        gstart = {g[0]: gi for gi, g in enumerate(egroups)}
        gater = [Res("gate%d" % i) for i in range(8)]
        for _ in dispatch_gen(egroups[0]):
            pass
        load_w(0)
        load_w(1)

        def ctx_dense(ex, wg_, wu_, wd_):
                def ffn_chunk(h_src, N, g_):
                    for fc in range(4):
                        ba = 2 * (fc % 2); bu = ba + 1
                        for k in range(8):
                            mm(pbig[:, ba, :N], wg_[:, k, fc * 128:(fc + 1) * 128], h_src[0][:, k, :N], k == 0, k == 7,
                               [wg_, h_src[1]], [pbr[ba]])
                        for k in range(8):
                            mm(pbig[:, bu, :N], wu_[:, k, fc * 128:(fc + 1) * 128], h_src[0][:, k, :N], k == 0, k == 7,
                               [wu_, h_src[1]], [pbr[bu]])
                        s_ = sa[fc % 2]
                        act(s_[:, :N], pbig[:, ba, :N], AF.Silu, [pbr[ba]], [s_])
                        tt(g_[:, fc, :N], pbig[:, bu, :N], s_[:, :N], ALU.mult, [pbr[bu], s_], [g_])

                def down(g_, s):
                    for hf in range(2):
                        for fc in range(4):
                            mm(pbig[:, 4 + hf, :], g_[:, fc, s * 128:(s + 1) * 128], wd_[:, fc, hf * 512:(hf + 1) * 512],
                               fc == 0, fc == 3, [g_, wd_], [pbr[4 + hf]])
                    return pbig[:, 4:6, :]

                if not last:
                    g_ = gT[0]
                    ffn_chunk((h2g, h2g), 256, g_)
                    for s in range(2):
                        py = down(g_, s)
                        av_ = acc[:, s, :].rearrange("p (a b) -> p a b", a=2)
                        if ex == 0:
                            ts(av_, py, wsel[:, s, ex:ex + 1], None, ALU.mult, None, [pbr[4], pbr[5], wsel], [accr[s]])
                        else:
                            stt(av_, py, wsel[:, s, ex:ex + 1], av_, ALU.mult, ALU.add,
                                [pbr[4], pbr[5], wsel, accr[s]], [accr[s]])

        def gen_prep(ex, ci):
            h_ = h2e[ci]
            for s in range(4):
                st_ = ci * 4 + s
                x_ = xet[st_ % 2]
                dma("sp", x_[:], XE[ex][st_ * 128:(st_ + 1) * 128, :], [xe_res[ex]], [x_], x_)
                cp(idxe[st_][:], x_[:, 1024:1026].bitcast(I32), [x_], [idxe[st_]])
                tt(gate[:, st_:st_ + 1], x_[:, 1026 + ex:1027 + ex], x_[:, 1042 + ex:1043 + ex], ALU.add, [x_], [gater[st_]])
                pt_ = ptr[st_ % 2]
                for k in range(8):
                    mm(pt_[:, k, :], x_[:, k * 128:(k + 1) * 128], ident[:], True, True, [x_, ident], [pt_], tr=True)
                yield
                for k in range(8):
                    act(h_[:, k, s * 128:(s + 1) * 128], pt_[:, k, :], AF.Identity, [pt_, modT], [h_],
                        bias=modT[:, 24 + k:25 + k], scale=modT[:, 32 + k:33 + k])
                    if k % 4 == 3:
                        yield

        def gen_ffn(ex, ci, wg_, wu_, wd_):
            h_ = h2e[ci]
            g_ = gT[ci]
            for fc in range(4):
                ba = 2 * (fc % 2); bu = ba + 1
                for k in range(8):
                    mm(pbig[:, ba, :], wg_[:, k, fc * 128:(fc + 1) * 128], h_[:, k, :], k == 0, k == 7, [wg_, h_], [pbr[ba]])
                for k in range(8):
                    mm(pbig[:, bu, :], wu_[:, k, fc * 128:(fc + 1) * 128], h_[:, k, :], k == 0, k == 7, [wu_, h_], [pbr[bu]])
                s_ = sa[fc % 2]
                act(s_[:], pbig[:, ba, :], AF.Silu, [pbr[ba]], [s_])
                tt(g_[:, fc, :], pbig[:, bu, :], s_[:], ALU.mult, [pbr[bu], s_], [g_])
                yield
            for s in range(4):
                st_ = ci * 4 + s
                for hf in range(2):
                    for fc in range(4):
                        mm(pbig[:, 4 + hf, :], g_[:, fc, s * 128:(s + 1) * 128], wd_[:, fc, hf * 512:(hf + 1) * 512],
                           fc == 0, fc == 3, [g_, wd_], [pbr[4 + hf]])
                y_ = yo[st_ % 2]
                act(y_[:].rearrange("p (a b) -> p a b", a=2), pbig[:, 4:6, :], AF.Identity, [pbr[4], pbr[5], gater[st_]], [y_],
                    scale=gate[:, st_:st_ + 1])
                S.dma("pool", lambda e, y_=y_, ie_=idxe[st_]: e.indirect_dma_start(
                    out=FFN[:, :], out_offset=bass.IndirectOffsetOnAxis(ap=ie_[:, :], axis=0),
                    in_=y_[:, :], in_offset=None, compute_op=ALU.add),
                    reads=rs([y_, idxe[st_]]), writes=[S.dres("ffn")], owner=y_.r, group=("ffn", l, ex), cost=1.2, lat=8.0)
                yield

        prev = None
        dg = None
        per = 0
        gend = {g[-1] for g in egroups}
        for ex in range(NE):
            sl = ex % 2
            if ex in gstart and gstart[ex] + 1 < len(egroups):
                dg = dispatch_gen(egroups[gstart[ex] + 1])
                nch = 2 * len(egroups[gstart[ex]])
                per = (8 + nch - 1) // nch
            ctx_dense(ex, wgt[sl], wut[sl], wdt[sl])
            for ci in range(2):
                for _ in interleave([gen_prep(ex, ci), prev, take(dg, per) if dg is not None else None]):
                    pass
                if ci == 0 and ex >= 1 and ex + 1 < NE:
                    load_w(ex + 1)
                prev = gen_ffn(ex, ci, wgt[sl], wut[sl], wdt[sl])
            if ex in gend and dg is not None:
                for _ in dg:
                    pass
                dg = None
        for _ in prev:
            pass

        new_phase()
        g2b = ph("g2b", [128, D]); cg2b = ph("cg2b", [128, D])
        l2g = ph("l2g", [128, D]); l2b = ph("l2b", [128, D])
        dma("sp", g2b[:], MODROW[0, 5120:6144].partition_broadcast(128), [S.dres("modrow")], [g2b], g2b)
        dma("sp", cg2b[:], MODROW[1, 5120:6144].partition_broadcast(128), [S.dres("modrow")], [cg2b], cg2b)
        dma("sp", l2g[:], LN2G[l].partition_broadcast(128), [S.dres("ln2g")], [l2g], l2g)
        dma("sp", l2b[:], LN2B[l].partition_broadcast(128), [S.dres("ln2b")], [l2b], l2b)
        xmt = [ph("xmt%d" % i, [128, D]) for i in range(4)]
        fft = [ph("fft%d" % i, [128, D]) for i in range(4)]
        ot = [ph("ot%d" % i, [128, D]) for i in range(4)]
        y2 = [ph("y2%d" % i, [128, D]) for i in range(4)]
        st2 = [(ph("stats", [128, 2, 6]), ph("mv", [128, 2]), ph("rstd", [128, 1]), ph("nmr", [128, 1])) for _ in range(4)]

        def gen_ln2(T):
            xm_ = xmt[T % 4]; o_ = ot[T % 4]; y2_ = y2[T % 4]
            stats, mv, rstd, nmr = st2[T % 4]
            dma("sp", xm_[:], XMID[T * 128:(T + 1) * 128, :], [xmid_res[T]], [xm_], xm_)
            if T < 2:
                tt(y2_[:], acc[:, T, :], cg2b[:], ALU.mult, [accr[T], cg2b], [y2_])
            else:
                f_ = fft[T % 4]
                dma("sp", f_[:], FFN[T * 128:(T + 1) * 128, :], [S.dres("ffn")], [f_], f_)
                tt(y2_[:], f_[:], g2b[:], ALU.mult, [f_, g2b], [y2_])
            yield
            stt(y2_[:], xm_[:], ALPHA, y2_[:], ALU.mult, ALU.add, [xm_, y2_], [y2_])
            yield
            ln_stats(y2_, y2_, stats, mv, rstd, nmr)
            yield
            act(y2_[:], y2_[:], AF.Identity, [y2_, rstd, nmr], [y2_], bias=nmr[:, 0:1], scale=rstd[:, 0:1])
            yield
            tt(y2_[:], y2_[:], l2g[:], ALU.mult, [y2_, l2g], [y2_], eng="pool")
            yield
            tt(o_[:], y2_[:], l2b[:], ALU.add, [y2_, l2b], [o_], eng="pool")
            if last:
                ev = dma("sp", Y[(T - 2) * 128:(T - 1) * 128, :], o_[:], [o_], [y_res[T - 2]], o_)
                final.append(ev)
            else:
                dma("sp", XCUR[T * 128:(T + 1) * 128, :], o_[:], [o_], [xcur_res[T]], o_)
            yield

        tl_ = list(range(T0, NT))
        for j in range(0, len(tl_), 4):
            for _ in interleave([gen_ln2(T) for T in tl_[j:j + 4]]):
                pass

    S.emit(final_waits=final, reorder=reorder, only=only)
    if dbg:
        print("instr counts", {e: len(v) for e, v in S.ins.items()}, "waits", S.nwaits, flush=True)
    return nc


def _rope_tables():
    t = np.arange(8192)
    row = (t // 64).astype(np.float32); col = (t % 64).astype(np.float32)
    inv = (10000.0 ** (-np.arange(0, 32, 2, dtype=np.float32) / 32)).astype(np.float32)
    cs = np.ones((64, TOK), np.float32); sn = np.zeros((64, TOK), np.float32)
    for a, pos in enumerate((row, col)):
        ang = (pos[:, None] * inv[None, :]).astype(np.float32)
        c = np.cos(ang).T; s = np.sin(ang).T
        cs[a * 32:a * 32 + 16, 256:] = c; cs[a * 32 + 16:a * 32 + 32, 256:] = c
        sn[a * 32:a * 32 + 16, 256:] = -s; sn[a * 32 + 16:a * 32 + 32, 256:] = s
    cs = np.concatenate([cs, cs], 0); sn = np.concatenate([sn, sn], 0)
    return np.stack([cs * 0.125, sn * 0.125, cs, sn]).astype(np.float32)


def _win_ext(w_in):
    qa = w_in[:, :, 0:384]; ka = w_in[:, :, 384:512]; va = w_in[:, :, 512:640]
    bx = w_in[:, :, 640:896]; bb = w_in[:, :, 896:1152]; bc = w_in[:, :, 1152:1408]
    qn = w_in[:, :, 1408:1792]; kn = w_in[:, :, 1792:2176]; vn = w_in[:, :, 2176:2560]
    sw = np.concatenate([np.arange(16, 32), np.arange(0, 16), np.arange(48, 64), np.arange(32, 48)])

    def heads_sw(w, nh):
        idx = np.concatenate([h * 64 + sw for h in range(nh)])
        return w[:, :, idx]

    def qperm(w):
        idx = np.concatenate([np.concatenate([np.arange(c * 64, c * 64 + 64), np.arange((3 + c) * 64, (3 + c) * 64 + 64)])
                              for c in range(3)])
        return w[:, :, idx]

    return np.ascontiguousarray(np.concatenate(
        [qperm(qa), qperm(heads_sw(qa, 6)), ka, heads_sw(ka, 2), bx, bb, bc, qn, kn, va, vn], axis=2))


def _na_bias(rpb):
    NEG = -30000.0
    out = np.full((DEPTH, 5, 6, 5, 128, 128), NEG, np.float32)
    cq = np.arange(64)
    col_start = np.clip(cq - 8, 0, 48)
    col_ok = (cq[None, :] >= col_start[:, None]) & (cq[None, :] < col_start[:, None] + 16)
    coff = np.clip(cq[None, :] - cq[:, None], -15, 15) + 15
    variants = [10, 0, 1, 62, 63]
    for vi, P in enumerate(variants):
        base = min(max(P - 2, 0), 59)
        for rho in range(2):
            r = 2 * P + rho
            rs_ = min(max(r - 4, 0), 120)
            for j in range(5):
                for kap in range(2):
                    kr = 2 * (base + j) + kap
                    if not (rs_ <= kr < rs_ + 8):
                        continue
                    roff = kr - r + 7
                    b = rpb[:, :, roff, :][:, :, coff]
                    b = np.where(col_ok[None, None], b, NEG)
                    out[:, vi, :, j, kap * 64:(kap + 1) * 64, rho * 64:(rho + 1) * 64] = np.transpose(b, (0, 1, 3, 2))
    out = np.transpose(out, (0, 1, 4, 2, 3, 5)).reshape(DEPTH, 5, 128, 6, 640)
    return np.ascontiguousarray(out)


def _amask():
    k = np.arange(128)[:, None]; q = np.arange(128)[None, :]
    mp = np.where(k >= q, 0.0, -30000.0).astype(np.float32); mn = np.where(k <= q, 0.0, -30000.0).astype(np.float32)
    return np.ascontiguousarray(np.stack([np.tile(mp, (1, 3)), np.tile(mn, (1, 3))], axis=1))


def make_in_maps(x, c, ctx, c_ctx, w_mod, b_mod, w_in, conv_w, attn_sink, na_rpb, w_out,
                 ln1_g, ln1_b, w_router, w_gate, w_up, w_down, ln2_g, ln2_b):
    f = lambda a: np.ascontiguousarray(np.asarray(a, dtype=np.float32))
    shared = dict(
        w_mod=f(w_mod), b_mod=f(b_mod), w_in=_win_ext(f(w_in)), rope=_rope_tables(),
        convw=np.ascontiguousarray(np.transpose(f(conv_w).reshape(DEPTH, 3, 2, 128), (0, 3, 2, 1))),
        sink=f(attn_sink), nab=_na_bias(f(na_rpb)), amask=_amask(), w_out=f(w_out),
        ln1_g=f(ln1_g), ln1_b=f(ln1_b), ln2_g=f(ln2_g), ln2_b=f(ln2_b), w_router=f(w_router),
        w_gate=f(w_gate), w_up=f(w_up), w_down=f(w_down))
    x = f(x); ctx = f(ctx); c = f(c); c_ctx = f(c_ctx)
    maps = []
    for b in range(N_CORES):
        m = dict(shared)
        m["xin"] = np.ascontiguousarray(np.concatenate([ctx[b], x[b]], axis=0))
        m["cvec"] = np.ascontiguousarray(np.stack([c[b], c_ctx], axis=0))
        maps.append(m)
    return maps


_NC = {}


def kernel(**inputs):
    if "nc" not in _NC:
        _NC["nc"] = build_nc(reorder=False)
    maps = make_in_maps(**inputs)
    res = run_bass_kernel_spmd(_NC["nc"], maps, core_ids=list(range(N_CORES)))
    return np.stack([np.asarray(r["y"], dtype=np.float32) for r in res.results], axis=0)
```
